# Optimizing a Trainium2 kernel written in Bass

```python
import math
import jax, jax.numpy as jnp
from jax import lax
import numpy as np

D_MODEL = 2048
BATCH = 2
SEQ = 4096
DEPTH = 2

CHUNK = 64
Q_BLOCK = 128
N_BRANCH = 4
D_BRANCH = D_MODEL // 4
D_FF = 4 * D_MODEL
ROPE_THETA = 10000.0
EPS = 1e-6

DIFF_HEADS = 4
DIFF_HD = 64
DIFF_VD = 2 * DIFF_HD
RWKV_HD = 64
RWKV_HEADS = D_BRANCH // RWKV_HD
RWKV_W_RANK = 64
RWKV_A_RANK = 64
RWKV_G_RANK = 128
RWKV_GN_EPS = 64e-5
HGRN_HEADS = 4
HGRN_HD = D_BRANCH // HGRN_HEADS
DSA_HEADS = 8
DSA_HD = 64
IDX_HEADS = 8
IDX_HD = 64
TOPK_MAX = 256

A_WIDTHS = (DIFF_HEADS * 2 * DIFF_HD, DIFF_HEADS * 2 * DIFF_HD, DIFF_HEADS * DIFF_VD)
B_WIDTHS = (D_BRANCH, D_BRANCH, D_BRANCH, RWKV_W_RANK, RWKV_A_RANK, RWKV_G_RANK)
C_WIDTHS = (D_BRANCH, D_BRANCH, D_BRANCH, D_BRANCH)
D_WIDTHS = (DSA_HEADS * DSA_HD, DSA_HD, DSA_HD, IDX_HEADS * IDX_HD, IDX_HD, IDX_HEADS)
GROUP_WIDTHS = (sum(A_WIDTHS), sum(B_WIDTHS), sum(C_WIDTHS), sum(D_WIDTHS), N_BRANCH * D_MODEL)
D_IN = sum(GROUP_WIDTHS)

kernel_name = "chunk_causal_gated_hybrid_trunk"


def _split(t, widths):
    return jnp.split(t, [int(c) for c in np.cumsum(widths)[:-1]], axis=-1)


def rms_norm(x, g, eps=EPS):
    xf = x.astype(jnp.float32)
    y = xf * lax.rsqrt(jnp.mean(xf * xf, axis=-1, keepdims=True) + eps)
    return (y * g.astype(jnp.float32)).astype(x.dtype)


def rope(x, positions):
    d = x.shape[-1]
    inv = ROPE_THETA ** (-jnp.arange(0, d, 2, dtype=jnp.float32) / d)
    ang = positions.astype(jnp.float32)[..., None] * inv
    cos = jnp.cos(ang)[:, :, None, :]
    sin = jnp.sin(ang)[:, :, None, :]
    xf = x.astype(jnp.float32)
    x1, x2 = xf[..., : d // 2], xf[..., d // 2:]
    return jnp.concatenate([x1 * cos - x2 * sin, x2 * cos + x1 * sin], axis=-1).astype(x.dtype)


def chunk_visible(q_idx, k_idx):
    return (k_idx[None, :] // CHUNK) <= (q_idx[:, None] // CHUNK)


def sweep_query_blocks(fn, seq):
    nb = seq // Q_BLOCK
    out = lax.map(fn, jnp.arange(nb))
    out = jnp.moveaxis(out, 0, 1)
    return out.reshape(out.shape[0], nb * Q_BLOCK, *out.shape[3:])


def diff_attention(q, k, v, positions, lam_vecs, subln_g, layer):
    B, S, _ = q.shape
    q = rope(q.reshape(B, S, DIFF_HEADS * 2, DIFF_HD), positions).reshape(B, S, DIFF_HEADS, 2, DIFF_HD)
    k = rope(k.reshape(B, S, DIFF_HEADS * 2, DIFF_HD), positions).reshape(B, S, DIFF_HEADS, 2, DIFF_HD)
    v = v.reshape(B, S, DIFF_HEADS, DIFF_VD)
    lam_init = 0.8 - 0.6 * math.exp(-0.3 * layer)
    lv = lam_vecs.astype(jnp.float32)
    lam = jnp.exp(jnp.sum(lv[0] * lv[1])) - jnp.exp(jnp.sum(lv[2] * lv[3])) + lam_init
    scale = DIFF_HD ** -0.5
    k_idx = jnp.arange(S)

    def block(i):
        start = i * Q_BLOCK
        qb = lax.dynamic_slice_in_dim(q, start, Q_BLOCK, axis=1)
        s = jnp.einsum('bthmd,bshmd->bmhts', qb, k).astype(jnp.float32) * scale
        vis = chunk_visible(start + jnp.arange(Q_BLOCK), k_idx)
        p = jax.nn.softmax(jnp.where(vis, s, -jnp.inf), axis=-1)
        pd = p[:, 0] - lam * p[:, 1]
        return jnp.einsum('bhts,bshe->bthe', pd.astype(v.dtype), v)

    o = sweep_query_blocks(block, S)
    o = rms_norm(o, subln_g) * jnp.asarray(1.0 - lam_init, dtype=o.dtype)
    return o.reshape(B, S, DIFF_HEADS * DIFF_VD)


def token_shift(p, mu):
    prev = jnp.pad(p, ((0, 0), (1, 0), (0, 0)))[:, :-1]
    return p + (prev - p) * mu


def rwkv7(r, k, v, wd, ad, gd, w_up, a_up, g_up, w0, a0, kk_gain, ka_gain, r_k, gn_g, gn_b):
    B, S, _ = r.shape
    H, N = RWKV_HEADS, RWKV_HD
    f32 = lambda t: t.astype(jnp.float32)
    w_log = -jax.nn.softplus(-(f32(w0) + jnp.tanh(f32(wd)) @ f32(w_up))) - 0.5
    decay = jnp.exp(-jnp.exp(w_log))
    a = jax.nn.sigmoid(f32(a0) + f32(ad) @ f32(a_up))
    g = jax.nn.sigmoid(f32(gd)) @ f32(g_up)
    heads = lambda t: f32(t).reshape(B, S, H, N)
    rh, kh, vh, decay, a = heads(r), heads(k), heads(v), heads(decay), heads(a)
    kk = kh * f32(kk_gain).reshape(H, N)
    kk = kk / jnp.maximum(jnp.sqrt(jnp.sum(kk * kk, axis=-1, keepdims=True)), 1e-12)
    kh = kh * (1.0 + (a - 1.0) * f32(ka_gain).reshape(H, N))

    def step(state, inp):
        rt, wt, kt, vt, kkt, at = inp
        sa = jnp.einsum('bhvk,bhk->bhv', state, -kkt)
        state = (state * wt[:, :, None, :] + sa[..., None] * (kkt * at)[:, :, None, :]
                 + vt[..., None] * kt[:, :, None, :])
        return state, jnp.einsum('bhvk,bhk->bhv', state, rt)

    xs = tuple(jnp.moveaxis(t, 1, 0) for t in (rh, decay, kh, vh, kk, a))
    _, y = lax.scan(step, jnp.zeros((B, H, N, N), jnp.float32), xs)
    y = jnp.moveaxis(y, 0, 1)
    mu = jnp.mean(y, axis=-1, keepdims=True)
    var = jnp.mean(jnp.square(y - mu), axis=-1, keepdims=True)
    y = ((y - mu) * lax.rsqrt(var + RWKV_GN_EPS)).reshape(B, S, H * N) * f32(gn_g) + f32(gn_b)
    bonus = jnp.sum(rh * kh * f32(r_k), axis=-1, keepdims=True) * vh
    y = (y + bonus.reshape(B, S, H * N)) * g
    return y.astype(r.dtype)


def hgrn2(q, f_logit, i, g, lb, norm_g):
    B, S, _ = q.shape
    H, dk = HGRN_HEADS, HGRN_HD
    nc = S // CHUNK
    qf = jax.nn.silu(q.astype(jnp.float32))
    fgate = lb.astype(jnp.float32) + (1.0 - lb.astype(jnp.float32)) * jax.nn.sigmoid(f_logit.astype(jnp.float32))
    log_f = jnp.log(fgate)
    kf = 1.0 - fgate

    def chunks(t):
        return t.reshape(B, nc, CHUNK, H, -1).transpose(1, 0, 3, 2, 4)

    causal = jnp.tril(jnp.ones((CHUNK, CHUNK), dtype=bool))

    def step(state, inp):
        qc, kc, ic, lfc = inp
        b = jnp.cumsum(lfc, axis=2)
        diff = b[:, :, :, None, :] - b[:, :, None, :, :]
        dec = jnp.exp(jnp.where(causal[:, :, None], diff, -jnp.inf))
        attn = jnp.einsum('bhtc,bhsc,bhtsc->bhts', qc, kc, dec)
        o = attn @ ic + jnp.einsum('bhtc,bhcv->bhtv', qc * jnp.exp(b), state)
        b_last = b[:, :, -1:, :]
        state = (jnp.exp(b_last)[:, :, 0, :, None] * state
                 + jnp.einsum('bhsc,bhsv->bhcv', kc * jnp.exp(b_last - b), ic))
        return state, o

    xs = (chunks(qf), chunks(kf), chunks(i.astype(jnp.float32)), chunks(log_f))
    _, o = lax.scan(step, jnp.zeros((B, H, dk, HGRN_HD), jnp.float32), xs)
    o = o.transpose(1, 0, 3, 2, 4).reshape(B, S, H, HGRN_HD)
    o = rms_norm(o, norm_g.reshape(H, HGRN_HD)).reshape(B, S, H * HGRN_HD)
    return (o * jax.nn.silu(g.astype(jnp.float32))).astype(q.dtype)


def dsa_attention(q, k, v, iq, ik, iw, positions):
    B, S, _ = q.shape
    top_k = min(TOPK_MAX, S // 4)
    q = rope(q.reshape(B, S, DSA_HEADS, DSA_HD), positions)
    k = rope(k[:, :, None, :], positions)[:, :, 0]
    iq = rope(iq.reshape(B, S, IDX_HEADS, IDX_HD), positions)
    ik = rope(ik[:, :, None, :], positions)[:, :, 0]
    w = iw.astype(jnp.float32) * (IDX_HEADS ** -0.5) * (IDX_HD ** -0.5)
    k_idx = jnp.arange(S)
    gather = jax.vmap(lambda t, idx: t[idx])

    def block(i):
        start = i * Q_BLOCK
        q_idx = start + jnp.arange(Q_BLOCK)
        qb = lax.dynamic_slice_in_dim(q, start, Q_BLOCK, axis=1)
        iqb = lax.dynamic_slice_in_dim(iq, start, Q_BLOCK, axis=1)
        wb = lax.dynamic_slice_in_dim(w, start, Q_BLOCK, axis=1)
        idx_s = jnp.einsum('bthd,bsd->bths', iqb, ik).astype(jnp.float32)
        score = jnp.einsum('bths,bth->bts', jax.nn.relu(idx_s), wb)
        score = jnp.where(chunk_visible(q_idx, k_idx)[None], score, -jnp.inf)
        _, sel = lax.top_k(score, top_k)
        ok = (sel // CHUNK) <= (q_idx[None, :, None] // CHUNK)
        kg, vg = gather(k, sel), gather(v, sel)
        s = jnp.einsum('bthd,btkd->bhtk', qb, kg).astype(jnp.float32) * (DSA_HD ** -0.5)
        p = jax.nn.softmax(jnp.where(ok[:, None], s, -jnp.inf), axis=-1)
        return jnp.einsum('bhtk,btkd->bthd', p.astype(v.dtype), vg)

    o = sweep_query_blocks(block, S)
    return o.reshape(B, S, DSA_HEADS * DSA_HD)


def hybrid_layer(x, positions, layer, norm_g, w_in, diff_lambda, diff_subln_g, rwkv_mu, rwkv_w_up,
                 rwkv_a_up, rwkv_g_up, rwkv_w0, rwkv_a0, rwkv_k_k, rwkv_k_a, rwkv_r_k, rwkv_gn_g,
                 rwkv_gn_b, hgrn_lb, hgrn_norm_g, w_branch, w_out, mlp_w1, mlp_w2):
    B, S, D = x.shape
    h = rms_norm(x, norm_g[0])
    proj = h @ w_in
    pa, pb, pc, pd, gate = _split(proj, GROUP_WIDTHS)
    a_q, a_k, a_v = _split(pa, A_WIDTHS)
    b_r, b_k, b_v, b_wd, b_ad, b_gd = _split(token_shift(pb, rwkv_mu), B_WIDTHS)
    c_q, c_f, c_i, c_g = _split(pc, C_WIDTHS)
    d_q, d_k, d_v, d_iq, d_ik, d_iw = _split(pd, D_WIDTHS)

    y_a = diff_attention(a_q, a_k, a_v, positions, diff_lambda, diff_subln_g, layer)
    y_b = rwkv7(b_r, b_k, b_v, b_wd, b_ad, b_gd, rwkv_w_up, rwkv_a_up, rwkv_g_up, rwkv_w0, rwkv_a0,
                rwkv_k_k, rwkv_k_a, rwkv_r_k, rwkv_gn_g, rwkv_gn_b)
    y_c = hgrn2(c_q, c_f, c_i, c_g, hgrn_lb, hgrn_norm_g)
    y_d = dsa_attention(d_q, d_k, d_v, d_iq, d_ik, d_iw, positions)

    ys = (y_a, y_b, y_c, y_d)
    gates = _split(gate, (D_MODEL,) * N_BRANCH)
    merged = jnp.zeros_like(x)
    for n in range(N_BRANCH):
        gn = jax.nn.sigmoid(gates[n].astype(jnp.float32)).astype(x.dtype)
        merged = merged + gn * (ys[n] @ w_branch[n])
    x = x + rms_norm(merged @ w_out, norm_g[1])

    h2 = rms_norm(x, norm_g[2])
    ff = jnp.square(jax.nn.relu(h2 @ mlp_w1)) @ mlp_w2
    return x + rms_norm(ff, norm_g[3])


def setup_inputs(seed: int = 0) -> dict:
    key = jax.random.key(seed)
    ks = jax.random.split(key, 24)
    nrm = lambda k, shape, s: jax.random.normal(k, shape, jnp.float32) * s
    offset = jax.random.randint(ks[1], (BATCH,), 0, 64, dtype=jnp.int32) * CHUNK
    positions = offset[:, None] + jnp.arange(SEQ, dtype=jnp.int32)[None, :]
    return {
        "x": nrm(ks[0], (BATCH, SEQ, D_MODEL), 1.0),
        "positions": positions,
        "norm_g": 1.0 + nrm(ks[2], (DEPTH, 4, D_MODEL), 0.02),
        "w_in": nrm(ks[3], (DEPTH, D_MODEL, D_IN), D_MODEL ** -0.5),
        "diff_lambda": nrm(ks[4], (DEPTH, 4, DIFF_HD), 0.1),
        "diff_subln_g": 1.0 + nrm(ks[5], (DEPTH, DIFF_VD), 0.02),
        "rwkv_mu": jax.random.uniform(ks[6], (DEPTH, sum(B_WIDTHS)), jnp.float32),
        "rwkv_w_up": nrm(ks[7], (DEPTH, RWKV_W_RANK, D_BRANCH), 0.5 * RWKV_W_RANK ** -0.5),
        "rwkv_a_up": nrm(ks[8], (DEPTH, RWKV_A_RANK, D_BRANCH), 0.5 * RWKV_A_RANK ** -0.5),
        "rwkv_g_up": nrm(ks[9], (DEPTH, RWKV_G_RANK, D_BRANCH), RWKV_G_RANK ** -0.5),
        "rwkv_w0": jax.random.uniform(ks[10], (DEPTH, D_BRANCH), jnp.float32, -6.0, -1.0),
        "rwkv_a0": nrm(ks[11], (DEPTH, D_BRANCH), 0.1),
        "rwkv_k_k": 0.85 + nrm(ks[12], (DEPTH, D_BRANCH), 0.02),
        "rwkv_k_a": 1.0 + nrm(ks[13], (DEPTH, D_BRANCH), 0.02),
        "rwkv_r_k": nrm(ks[14], (DEPTH, RWKV_HEADS, RWKV_HD), 0.1),
        "rwkv_gn_g": 1.0 + nrm(ks[15], (DEPTH, D_BRANCH), 0.02),
        "rwkv_gn_b": nrm(ks[16], (DEPTH, D_BRANCH), 0.01),
        "hgrn_lb_logits": nrm(ks[17], (DEPTH, D_BRANCH), 0.1),
        "hgrn_norm_g": 1.0 + nrm(ks[18], (DEPTH, D_BRANCH), 0.02),
        "w_branch": nrm(ks[19], (DEPTH, N_BRANCH, D_BRANCH, D_MODEL), D_BRANCH ** -0.5),
        "w_out": nrm(ks[20], (DEPTH, D_MODEL, D_MODEL), D_MODEL ** -0.5),
        "mlp_w1": nrm(ks[21], (DEPTH, D_MODEL, D_FF), D_MODEL ** -0.5),
        "mlp_w2": nrm(ks[22], (DEPTH, D_FF, D_MODEL), D_FF ** -0.5),
    }


def reference(x, positions, norm_g, w_in, diff_lambda, diff_subln_g, rwkv_mu, rwkv_w_up, rwkv_a_up,
              rwkv_g_up, rwkv_w0, rwkv_a0, rwkv_k_k, rwkv_k_a, rwkv_r_k, rwkv_gn_g, rwkv_gn_b,
              hgrn_lb_logits, hgrn_norm_g, w_branch, w_out, mlp_w1, mlp_w2):
    lb = jax.nn.softmax(hgrn_lb_logits.astype(jnp.float32), axis=0)
    lb = jnp.cumsum(lb, axis=0) - lb[0]
    for l in range(DEPTH):
        x = hybrid_layer(x, positions, l, norm_g[l], w_in[l], diff_lambda[l], diff_subln_g[l], rwkv_mu[l],
                         rwkv_w_up[l], rwkv_a_up[l], rwkv_g_up[l], rwkv_w0[l], rwkv_a0[l], rwkv_k_k[l],
                         rwkv_k_a[l], rwkv_r_k[l], rwkv_gn_g[l], rwkv_gn_b[l], lb[l], hgrn_norm_g[l],
                         w_branch[l], w_out[l], mlp_w1[l], mlp_w2[l])
    return x
```

```python
import math
import numpy as np
import ml_dtypes
from contextlib import ExitStack
import concourse.bass as bass
import concourse.mybir as mybir
from concourse.bass_utils import run_bass_kernel_spmd

BF = ml_dtypes.bfloat16


F32 = mybir.dt.float32
BF16 = mybir.dt.bfloat16
I32 = mybir.dt.int32
AF = mybir.ActivationFunctionType
ALU = mybir.AluOpType
AX = mybir.AxisListType

SAME_ENG_SYNC = True
NDMA = 24


class Dep:
    __slots__ = ("w", "r")

    def __init__(self):
        self.w = None
        self.r = []


class Buf:
    def __init__(self, t, nreg=1):
        self.t = t
        self.d = Dep()
        self.regs = {}

    def reg(self, key):
        if key not in self.regs:
            self.regs[key] = Dep()
        return self.regs[key]

    def __getitem__(self, idx):
        return self.t[idx]


class Prog:
    def __init__(self):
        self.nc = bass.Bass("TRN2", target_bir_lowering=False)
        nc = self.nc
        self.es = ExitStack()
        self.eng = {"pe": nc.tensor, "act": nc.scalar, "dve": nc.vector, "pool": nc.gpsimd, "sp": nc.sync}
        self.sem = {}
        self.cnt = {}
        self.clock = {}
        self.hist = {}
        for e in self.eng:
            self.sem[e] = self.es.enter_context(nc.semaphore("s_" + e))
            self.cnt[e] = 0
            self.clock[e] = {}
            self.hist[e] = {}
        for j in range(NDMA):
            e = "d%d" % j
            self.sem[e] = self.es.enter_context(nc.semaphore("s_" + e))
            self.cnt[e] = 0
            self.hist[e] = {}
        self.dma_k = 0
        self.nwaits = 0
        self.ninst = 0
        self.q = {e: [] for e in self.eng}

    def dram(self, name, shape, dtype, kind):
        return self.nc.dram_tensor(name, list(shape), dtype, kind=kind).ap()

    def sb(self, name, shape, dtype=F32):
        return Buf(self.es.enter_context(self.nc.sbuf_tensor(name, list(shape), dtype)))

    def ps(self, name, shape, dtype=F32):
        return Buf(self.es.enter_context(self.nc.psum_tensor(name, list(shape), dtype)))

    def _semval(self, e, n):
        return n * 16 if (e[0] == "d" and e[1:].isdigit()) else n

    def _wait(self, e, deps):
        need = {}
        for (e2, n) in deps:
            if e2 == e and (e == "pe" or not SAME_ENG_SYNC):
                continue
            if self.clock[e].get(e2, 0) >= n:
                continue
            if need.get(e2, 0) < n:
                need[e2] = n
        for e2, n in need.items():
            if self.clock[e].get(e2, 0) >= n:
                continue
            self.q[e].append(("w", self.sem[e2], self._semval(e2, n)))
            self.nwaits += 1
            h = self.hist[e2].get(n)
            if h:
                for k, v in h.items():
                    if self.clock[e].get(k, 0) < v:
                        self.clock[e][k] = v
            self.clock[e][e2] = max(self.clock[e].get(e2, 0), n)

    def _deps(self, r, w):
        deps = []
        for d in r:
            d = d.d if isinstance(d, Buf) else d
            if d.w is not None:
                deps.append(d.w)
        for d in w:
            d = d.d if isinstance(d, Buf) else d
            if d.w is not None:
                deps.append(d.w)
            deps.extend(d.r)
        return deps

    def _commit(self, tag, r, w):
        for d in r:
            d = d.d if isinstance(d, Buf) else d
            d.r.append(tag)
            if len(d.r) > 64:
                best = {}
                for (e2, n) in d.r:
                    if best.get(e2, 0) < n:
                        best[e2] = n
                d.r = list(best.items())
        for d in w:
            d = d.d if isinstance(d, Buf) else d
            d.w = tag
            d.r = []

    def I(self, e, method, r=(), w=(), **kw):
        self._wait(e, self._deps(r, w))
        self.cnt[e] += 1
        n = self.cnt[e]
        self.q[e].append(("i", (method, kw), self.sem[e], 1))
        self.hist[e][n] = dict(self.clock[e])
        if e == "pe" or not SAME_ENG_SYNC:
            self.clock[e][e] = n
        self._commit((e, n), r, w)
        self.ninst += 1

    def dma(self, out, in_, r=(), w=(), q="sp", **kw):
        j = self.dma_k % NDMA
        self.dma_k += 1
        de = "d%d" % j
        prev = self.cnt[de]
        deps = self._deps(r, w)
        if prev > 0:
            deps.append((de, prev))
        self._wait(q, deps)
        self.q[q].append(("d", (out, in_, kw), self.sem[de], 16))
        self.cnt[de] = prev + 1
        self.hist[de][prev + 1] = dict(self.clock[q])
        self._commit((de, prev + 1), r, w)
        self.ninst += 1

    def coll(self, kind, ins, outs, r=(), w=()):
        q = "pool"
        j = self.dma_k % NDMA
        self.dma_k += 1
        de = "d%d" % j
        prev = self.cnt[de]
        deps = self._deps(r, w)
        if prev > 0:
            deps.append((de, prev))
        self._wait(q, deps)
        self.q[q].append(("c", (kind, ins, outs), self.sem[de], 16))
        self.cnt[de] = prev + 1
        self.hist[de][prev + 1] = dict(self.clock[q])
        self._commit((de, prev + 1), r, w)
        self.ninst += 1

    def finish(self, q="sp"):
        deps = []
        for j in range(NDMA):
            de = "d%d" % j
            if self.cnt[de] > 0:
                deps.append((de, self.cnt[de]))
        self._wait(q, deps)

        nc = self.nc
        prog = self
        with nc.Block() as block:
            def mk(e):
                def body(engh):
                    for it in prog.q[e]:
                        if it[0] == "w":
                            engh.wait_ge(it[1], it[2])
                        elif it[0] == "i":
                            getattr(engh, it[1][0])(**it[1][1]).then_inc(it[2], it[3])
                        elif it[0] == "c":
                            kind, ins, outs = it[1]
                            engh.collective_compute(kind, ALU.bypass, [[0,1,2,3],[4,5,6,7]], ins, outs).then_inc(it[2], it[3])
                        else:
                            o, i, kw = it[1]
                            engh.dma_start(out=o, in_=i, **kw).then_inc(it[2], it[3])
                return body
            block.tensor(mk("pe"))
            block.scalar(mk("act"))
            block.vector(mk("dve"))
            block.gpsimd(mk("pool"))
            block.sync(mk("sp"))
        self.es.close()


D = 2048
DIN = 14792
NCB = 13
NMIX = 6600
EPS = 1e-6
ROPE = {0: [(0, 8)], 1: [(0, 8)], 10: [(256, 4)], 11: [(0, 5), (384, 2)], 12: [(0, 7)]}


def build_L0(n):
    p = Prog()
    src = p.dram("src", [128, n], F32, "ExternalInput")
    dst = p.dram("dst", [128, n], BF16, "ExternalOutput")
    CH = 4096
    st = [p.sb("st%d" % i, [128, CH], F32) for i in range(3)]
    ob = [p.sb("ob%d" % i, [128, CH], BF16) for i in range(3)]
    k = 0
    for c0 in range(0, n, CH):
        c1 = min(n, c0 + CH)
        s, o = st[k % 3], ob[k % 3]
        p.dma(s[:, 0:c1 - c0], src[:, c0:c1], w=[s], q="sp")
        e = ("dve", "pool")[k % 2]
        p.I(e, "tensor_copy", r=[s], w=[o], out=o[:, 0:c1 - c0], in_=s[:, 0:c1 - c0])
        p.dma(dst[:, c0:c1], o[:, 0:c1 - c0], r=[o], q="act")
        k += 1
    p.finish()
    return p


def fm_rmsnorm(p, src, gt, gcol, dst, KC, T, ones, psl, sq, rstd, dim):
    nh = T // 512
    for kc in range(KC):
        s = sq[kc % 2]
        p.I("act", "activation", r=[src], w=[s], out=s[:, 0:T], in_=src[:, kc, :], func=AF.Square)
        for h in range(nh):
            p.I("pe", "matmul", r=[s, ones], w=[psl[h]], out=psl[h][:], lhsT=ones[:], rhs=s[:, h * 512:(h + 1) * 512],
                start=(kc == 0), stop=(kc == KC - 1))
    for h in range(nh):
        p.I("act", "activation", r=[psl[h], EPSB[0]], w=[rstd], out=rstd[:, h * 512:(h + 1) * 512], in_=psl[h][:],
            func=AF.Sqrt, scale=1.0 / dim, bias=EPSB[0][:, 0:1])
    p.I("dve", "reciprocal", r=[rstd], w=[rstd], out=rstd[:, 0:T], in_=rstd[:, 0:T])
    for kc in range(KC):
        p.I("dve", "scalar_tensor_tensor", r=[src, gt, rstd], w=[dst], out=dst[:, kc, :], in0=src[:, kc, :],
            scalar=gt[:, gcol + kc:gcol + kc + 1], in1=rstd[:, 0:T], op0=ALU.mult, op1=ALU.mult)


EPSB = [None]


def consts(p):
    ones = p.sb("ones", [128, 128], BF16)
    p.I("dve", "memset", w=[ones], ap=ones[:], constant=1.0)
    eb = p.sb("epsb", [128, 1], F32)
    p.I("dve", "memset", w=[eb], ap=eb[:], constant=EPS)
    EPSB[0] = eb
    return ones


def build_L1():
    p = Prog()
    T = 1024
    xT = p.dram("xT", [D, T], F32, "ExternalInput")
    g = p.dram("g", [128, 16], F32, "ExternalInput")
    wb = p.dram("wb", [NCB, 128, 8192], BF16, "ExternalInput")
    pos = p.dram("pos", [128, 8], I32, "ExternalInput")
    cf = p.dram("cf", [128, 32], F32, "ExternalInput")
    proj = p.dram("proj", [T, NCB * 512], F32, "ExternalOutput")
    ones = consts(p)
    xs = p.sb("xs", [128, 16, T], F32)
    hT = p.sb("hT", [128, 16, T], BF16)
    gt = p.sb("gt", [128, 16], F32)
    sq = [p.sb("sq%d" % i, [128, T], BF16) for i in range(2)]
    rstd = p.sb("rstd", [128, T], F32)
    psn = [p.ps("psn%d" % i, [128, 512], F32) for i in range(2)]
    pst = [p.ps("ps%d" % i, [128, 512], F32) for i in range(4)]
    xv = xT.rearrange("(kc p) t -> p kc t", p=128)
    for kc in range(0, 16, 4):
        p.dma(xs[:, kc:kc + 4, :], xv[:, kc:kc + 4, :], w=[xs], q=("sp", "act")[(kc // 4) % 2])
    p.dma(gt[:], g, w=[gt])
    posi = p.sb("posi", [128, 8], I32)
    posf = p.sb("posf", [128, 8], F32)
    cft = p.sb("cft", [128, 32], F32)
    p.dma(posi[:], pos, w=[posi])
    p.dma(cft[:], cf, w=[cft])
    p.I("dve", "tensor_copy", r=[posi], w=[posf], out=posf[:], in_=posi[:])
    qq = p.sb("qq", [128, 2, 8, 32], F32)
    qi = p.sb("qi", [128, 2, 8, 32], I32)
    qf = p.sb("qf", [128, 2, 8, 32], F32)
    msk = p.sb("msk", [128, 2, 8, 32], F32)
    sc = p.sb("sc", [128, 2, 8, 32], F32)
    for tt in range(8):
        p.I("dve", "tensor_scalar", r=[cft, posf], w=[qq], out=qq[:, 0, tt, :], in0=cft[:], scalar1=posf[:, tt:tt + 1],
            scalar2=None, op0=ALU.mult)
    p.I("dve", "tensor_scalar", r=[qq], w=[qq], out=qq[:, 1, :, :], in0=qq[:, 0, :, :], scalar1=0.25, scalar2=None, op0=ALU.add)
    p.I("dve", "tensor_copy", r=[qq], w=[qi], out=qi[:], in_=qq[:])
    p.I("dve", "tensor_copy", r=[qi], w=[qf], out=qf[:], in_=qi[:])
    p.I("dve", "tensor_tensor", r=[qq, qf], w=[qq], out=qq[:], in0=qq[:], in1=qf[:], op=ALU.subtract)
    p.I("dve", "tensor_scalar", r=[qq], w=[msk], out=msk[:], in0=qq[:], scalar1=0.5, scalar2=None, op0=ALU.is_gt)
    p.I("dve", "tensor_tensor", r=[qq, msk], w=[qq], out=qq[:], in0=qq[:], in1=msk[:], op=ALU.subtract)
    p.I("dve", "tensor_scalar", r=[qq], w=[msk], out=msk[:], in0=qq[:], scalar1=-0.5, scalar2=None, op0=ALU.is_lt)
    p.I("dve", "tensor_tensor", r=[qq, msk], w=[qq], out=qq[:], in0=qq[:], in1=msk[:], op=ALU.add)
    p.I("act", "activation", r=[qq], w=[sc], out=sc[:], in_=qq[:], func=AF.Sin, scale=6.28318)
    fm_rmsnorm(p, xs, gt, 0, hT, 16, T, ones, psn, sq, rstd, D)
    wt = [p.sb("w%d" % i, [128, 8192], BF16) for i in range(3)]
    ot = [p.sb("o%d" % i, [128, 512], F32) for i in range(4)]
    tmp = [p.sb("rt%d" % i, [128, 8, 32], F32) for i in range(4)]
    k = 0
    for cb in range(NCB):
        w = wt[cb % 3]
        p.dma(w[:, 0:4096], wb[cb, :, 0:4096], w=[w], q="sp")
        p.dma(w[:, 4096:8192], wb[cb, :, 4096:8192], w=[w], q="act")
        for tt in range(8):
            ps = pst[k % 4]
            o = ot[k % 4]
            k += 1
            for kc in range(16):
                p.I("pe", "matmul", r=[hT, w], w=[ps], out=ps[:], lhsT=hT[:, kc, tt * 128:(tt + 1) * 128],
                    rhs=w[:, kc * 512:(kc + 1) * 512], start=(kc == 0), stop=(kc == 15))
            p.I("act", "activation", r=[ps], w=[o], out=o[:], in_=ps[:], func=AF.Copy)
            for (s0, nh) in ROPE.get(cb, []):
                ov = o[:, s0:s0 + nh * 64].rearrange("p (h two d) -> p h two d", two=2, d=32)
                x1, x2 = ov[:, :, 0, :], ov[:, :, 1, :]
                sn = sc[:, 0, tt:tt + 1, :].to_broadcast([128, nh, 32])
                cs = sc[:, 1, tt:tt + 1, :].to_broadcast([128, nh, 32])
                t1, t2, t3, t4 = [t[:, 0:nh, :] for t in tmp]
                p.I("dve", "tensor_tensor", r=[o, sc], w=[tmp[0]], out=t1, in0=x1, in1=cs, op=ALU.mult)
                p.I("dve", "tensor_tensor", r=[o, sc], w=[tmp[1]], out=t2, in0=x2, in1=sn, op=ALU.mult)
                p.I("dve", "tensor_tensor", r=[o, sc], w=[tmp[2]], out=t3, in0=x2, in1=cs, op=ALU.mult)
                p.I("dve", "tensor_tensor", r=[o, sc], w=[tmp[3]], out=t4, in0=x1, in1=sn, op=ALU.mult)
                p.I("dve", "tensor_tensor", r=[tmp[0], tmp[1]], w=[o], out=x1, in0=t1, in1=t2, op=ALU.subtract)
                p.I("dve", "tensor_tensor", r=[tmp[2], tmp[3]], w=[o], out=x2, in0=t3, in1=t4, op=ALU.add)
            p.dma(proj[tt * 128:(tt + 1) * 128, cb * 512:(cb + 1) * 512], o[:], r=[o], q="sp")
    p.finish()
    return p


def prep_w_in(wbf):
    w = np.zeros((D, NCB * 512), dtype=wbf.dtype)
    w[:, :NMIX] = wbf[:, :NMIX]
    w = w.reshape(16, 128, NCB, 512).transpose(2, 1, 0, 3).reshape(NCB, 128, 8192)
    return np.ascontiguousarray(w)


def cf_const():
    inv = 10000.0 ** (-np.arange(0, 64, 2, dtype=np.float64) / 64.0)
    c = (inv / (2 * math.pi)).astype(np.float32)
    return np.ascontiguousarray(np.broadcast_to(c[None, :], (128, 32)))


def build_L3(T=2048):
    p = Prog()
    H = 512
    xT = p.dram("xT", [D, T], F32, "ExternalInput")
    yT = p.dram("yT", [4, 512, T], F32, "ExternalInput")
    wg = p.dram("wg", [64, 128, 2048], BF16, "ExternalInput")
    g3 = p.dram("g3", [128, 64], F32, "ExternalInput")
    wbr = p.dram("wbr", [16, 128, 2048], BF16, "ExternalInput")
    wout = p.dram("wout", [16, 128, 2048], BF16, "ExternalInput")
    w1 = p.dram("w1", [64, 128, 2048], BF16, "ExternalInput")
    w2 = p.dram("w2", [16, 4, 128, 2048], BF16, "ExternalInput")
    xo = p.dram("xo", [D, T], F32, "ExternalOutput")
    ones = consts(p)
    xh = p.sb("xh", [128, 16, H], F32)
    zT = p.sb("zT", [128, 16, H], F32)
    mb = p.sb("mb", [128, 16, H], BF16)
    uT = p.sb("uT", [128, 64, H], BF16)
    gt = p.sb("gt", [128, 64], F32)
    hT = p.sb("hT", [128, 16, H], BF16)
    sq = [p.sb("sq%d" % i, [128, H], BF16) for i in range(2)]
    rstd = p.sb("rstd", [128, H], F32)
    psn = [p.ps("psn0", [128, 512], F32)]
    pst = [p.ps("ps%d" % i, [128, 512], F32) for i in range(4)]
    wt = [p.sb("wt%d" % i, [128, 2048], BF16) for i in range(4)]
    psg = [p.ps("psg%d" % i, [128, 512], F32) for i in range(2)]
    sg = [p.sb("sg%d" % i, [128, H], F32) for i in range(2)]
    tm = [p.sb("tm%d" % i, [128, H], F32) for i in range(2)]
    ys = [p.sb("ys%d" % i, [128, 4, H], F32) for i in range(1)]
    wgp = [p.sb("wgp%d" % i, [128, 2048], BF16) for i in range(2)]
    gk = 0
    p.dma(gt[:], g3, w=[gt])
    xv = xT.rearrange("(kc p) t -> p kc t", p=128)
    xov = xo.rearrange("(kc p) t -> p kc t", p=128)
    yv = yT.rearrange("n (kc p) t -> n p kc t", p=128)
    wk = 0
    pk = 0
    for hf in range(T // H):
        tsl = slice(hf * H, (hf + 1) * H)
        for kc in range(0, 16, 8):
            p.dma(xh[:, kc:kc + 8, :], xv[:, kc:kc + 8, tsl], w=[xh], q="act")
        fm_rmsnorm(p, xh, gt, 0, hT, 16, H, ones, psn, sq, rstd, D)
        for n in range(4):
            y = ys[0]
            p.dma(y[:], yv[n, :, :, tsl], w=[y], q="act")
            p.I("dve", "tensor_copy", r=[y], w=[uT], out=uT[:, n * 4:(n + 1) * 4, :], in_=y[:])
        for oc in range(16):
            w = wt[wk % 4]
            wk += 1
            p.dma(w[:], wbr[oc], w=[w], q="sp")
            for n in range(4):
                wgt = wgp[gk % 2]
                gk += 1
                p.dma(wgt[:], wg[oc * 4 + n], w=[wgt], q="act")
                pg = psg[n % 2]
                for kc in range(16):
                    p.I("pe", "matmul", r=[hT, wgt], w=[pg], out=pg[:], lhsT=wgt[:, kc * 128:(kc + 1) * 128], rhs=hT[:, kc, :],
                        start=(kc == 0), stop=(kc == 15))
                ps = pst[pk % 4]
                pk += 1
                for kc in range(4):
                    p.I("pe", "matmul", r=[uT, w], w=[ps], out=ps[:], lhsT=w[:, (n * 4 + kc) * 128:(n * 4 + kc + 1) * 128],
                        rhs=uT[:, n * 4 + kc, :], start=(kc == 0), stop=(kc == 3))
                s = sg[n % 2]
                p.I("act", "activation", r=[pg], w=[s], out=s[:], in_=pg[:], func=AF.Sigmoid)
                if n == 0:
                    p.I("dve", "tensor_tensor", r=[ps, s], w=[zT], out=zT[:, oc, :], in0=ps[:], in1=s[:], op=ALU.mult)
                else:
                    t = tm[n % 2]
                    p.I("dve", "tensor_tensor", r=[ps, s], w=[t], out=t[:], in0=ps[:], in1=s[:], op=ALU.mult)
                    p.I("pool", "tensor_tensor", r=[t, zT], w=[zT], out=zT[:, oc, :], in0=zT[:, oc, :], in1=t[:], op=ALU.add)
            p.I("pool", "tensor_copy", r=[zT], w=[mb], out=mb[:, oc, :], in_=zT[:, oc, :])
        for oc in range(16):
            w = wt[wk % 4]
            wk += 1
            p.dma(w[:], wout[oc], w=[w], q="sp")
            ps = pst[pk % 4]
            pk += 1
            for kc in range(16):
                p.I("pe", "matmul", r=[mb, w], w=[ps], out=ps[:], lhsT=w[:, kc * 128:(kc + 1) * 128], rhs=mb[:, kc, :],
                    start=(kc == 0), stop=(kc == 15))
            p.I("act", "activation", r=[ps], w=[zT], out=zT[:, oc, :], in_=ps[:], func=AF.Copy)
        fm_rmsnorm(p, zT, gt, 16, zT, 16, H, ones, psn, sq, rstd, D)
        for kc in range(0, 16, 4):
            p.I("pool", "tensor_tensor", r=[xh, zT], w=[xh], out=xh[:, kc:kc + 4, :], in0=xh[:, kc:kc + 4, :], in1=zT[:, kc:kc + 4, :], op=ALU.add)
        fm_rmsnorm(p, xh, gt, 32, mb, 16, H, ones, psn, sq, rstd, D)
        for oc in range(64):
            w = wt[wk % 4]
            wk += 1
            p.dma(w[:], w1[oc], w=[w], q=("sp", "act")[oc % 2])
            ps = pst[pk % 4]
            pk += 1
            for kc in range(16):
                p.I("pe", "matmul", r=[mb, w], w=[ps], out=ps[:], lhsT=w[:, kc * 128:(kc + 1) * 128], rhs=mb[:, kc, :],
                    start=(kc == 0), stop=(kc == 15))
            t = tm[oc % 2]
            p.I("act", "activation", r=[ps], w=[t], out=t[:], in_=ps[:], func=AF.Relu)
            p.I(("dve", "pool")[oc % 2], "tensor_tensor", r=[t], w=[uT], out=uT[:, oc, :], in0=t[:], in1=t[:], op=ALU.mult)
        for oc in range(16):
            ps = pst[pk % 4]
            pk += 1
            for q in range(4):
                w = wt[wk % 4]
                wk += 1
                p.dma(w[:], w2[oc, q], w=[w], q=("sp", "act")[q % 2])
                for kc in range(16):
                    p.I("pe", "matmul", r=[uT, w], w=[ps], out=ps[:], lhsT=w[:, kc * 128:(kc + 1) * 128], rhs=uT[:, q * 16 + kc, :],
                        start=(q == 0 and kc == 0), stop=(q == 3 and kc == 15))
            p.I("act", "activation", r=[ps], w=[zT], out=zT[:, oc, :], in_=ps[:], func=AF.Copy)
        fm_rmsnorm(p, zT, gt, 48, zT, 16, H, ones, psn, sq, rstd, D)
        for kc in range(0, 16, 4):
            p.I("pool", "tensor_tensor", r=[xh, zT], w=[xh], out=xh[:, kc:kc + 4, :], in0=xh[:, kc:kc + 4, :], in1=zT[:, kc:kc + 4, :], op=ALU.add)
        for kc in range(0, 16, 8):
            p.dma(xov[:, kc:kc + 8, tsl], xh[:, kc:kc + 8, :], r=[xh], q="sp")
    p.finish()
    return p


def prep_wg(wgate):
    g = wgate.reshape(16, 128, 4, 16, 128).transpose(3, 2, 1, 0, 4).reshape(64, 128, 2048)
    return np.ascontiguousarray(g)


def prep_L3_weights(wbr, wout, w1, w2):
    a = wbr.reshape(4, 4, 128, 16, 128).transpose(3, 2, 0, 1, 4).reshape(16, 128, 2048)
    b = wout.reshape(16, 128, 16, 128).transpose(2, 1, 0, 3).reshape(16, 128, 2048)
    c = w1.reshape(16, 128, 64, 128).transpose(2, 1, 0, 3).reshape(64, 128, 2048)
    d = w2.reshape(4, 16, 128, 16, 128).transpose(3, 0, 2, 1, 4).reshape(16, 4, 128, 2048)
    return [np.ascontiguousarray(t) for t in (a, b, c, d)]


S = 4096


def build_A(layer):
    p = Prog()
    lam_init = 0.8 - 0.6 * math.exp(-0.3 * layer)
    qT = p.dram("qT", [2, 64, S], F32, "ExternalInput")
    kT = p.dram("kT", [2, 64, S], F32, "ExternalInput")
    v = p.dram("v", [S, 128], F32, "ExternalInput")
    lam4 = p.dram("lam4", [128, 256], F32, "ExternalInput")
    sg = p.dram("sg", [128, 128], F32, "ExternalInput")
    masks = p.dram("masks", [4, 128, 512], BF16, "ExternalInput")
    ya = p.dram("ya", [S, 128], F32, "ExternalOutput")
    qb = [p.sb("qb%d" % m, [64, S], BF16) for m in range(2)]
    kb = [p.sb("kb%d" % m, [64, S], BF16) for m in range(2)]
    vb = p.sb("vb", [128, 32, 129], BF16)
    st = [p.sb("st%d" % i, [128, 4096], F32) for i in range(2)]
    mk_ = p.sb("mk", [128, 4, 512], BF16)
    lt = p.sb("lt", [128, 256], F32)
    sgt = p.sb("sgt", [128, 128], F32)
    epsb = p.sb("epsb", [128, 1], F32)
    p.I("dve", "memset", w=[epsb], ap=epsb[:], constant=1e-6)
    k = 0
    for m in range(2):
        for (src, dst) in ((qT, qb[m]), (kT, kb[m])):
            s = st[k % 2]
            k += 1
            p.dma(s[0:64, :], src[m], w=[s])
            p.I(("dve", "pool")[k % 2], "tensor_copy", r=[s], w=[dst], out=dst[:], in_=s[0:64, :])
    s = st[k % 2]
    k += 1
    p.dma(s[:].rearrange("p (t e) -> p t e", e=128), v.rearrange("(t p) e -> p t e", p=128), w=[s])
    p.I("pool", "memset", w=[vb], ap=vb[:], constant=1.0)
    p.I("dve", "tensor_copy", r=[s], w=[vb], out=vb[:, :, 0:128], in_=s[:].rearrange("p (t e) -> p t e", e=128))
    p.dma(mk_[:], masks.rearrange("j p f -> p j f"), w=[mk_])
    p.dma(lt[:], lam4, w=[lt])
    p.dma(sgt[:], sg, w=[sgt])
    pr = p.sb("pr", [128, 2, 64], F32)
    s12 = p.sb("s12", [128, 2], F32)
    e12 = p.sb("e12", [128, 2], F32)
    nlam = p.sb("nlam", [128, 1], F32)
    ltv = lt[:].rearrange("p (a d) -> p a d", d=64)
    p.I("dve", "tensor_tensor", r=[lt], w=[pr], out=pr[:, 0, :], in0=ltv[:, 0, :], in1=ltv[:, 1, :], op=ALU.mult)
    p.I("dve", "tensor_tensor", r=[lt], w=[pr], out=pr[:, 1, :], in0=ltv[:, 2, :], in1=ltv[:, 3, :], op=ALU.mult)
    p.I("dve", "tensor_reduce", r=[pr], w=[s12], out=s12[:], in_=pr[:], axis=AX.X, op=ALU.add)
    p.I("act", "activation", r=[s12], w=[e12], out=e12[:], in_=s12[:], func=AF.Exp)
    p.I("dve", "tensor_tensor", r=[e12], w=[nlam], out=nlam[:], in0=e12[:, 1:2], in1=e12[:, 0:1], op=ALU.subtract)
    p.I("dve", "tensor_scalar", r=[nlam], w=[nlam], out=nlam[:], in0=nlam[:], scalar1=-lam_init, scalar2=None, op0=ALU.add)
    p.I("dve", "tensor_scalar", r=[sgt], w=[sgt], out=sgt[:], in0=sgt[:], scalar1=1.0 - lam_init, scalar2=None, op0=ALU.mult)
    pss = [p.ps("pss%d" % i, [128, 512], F32) for i in range(3)]
    psa = [p.ps("psa%d" % i, [128, 512], F32) for i in range(3)]
    pts = p.sb("pts", [128, 32, 512], BF16)
    ob = [p.sb("ob%d" % i, [128, 4, 128], F32) for i in range(2)]
    sm = [p.sb("sm%d" % i, [128, 4], F32) for i in range(3)]
    junk = p.sb("junk", [128, 128], F32)
    ks = 0
    ka = 0
    for qblk in range(8):
        Q0 = qblk * 512
        nkt = 4 * qblk + 4
        o = ob[qblk % 2]
        for m in range(2):
            for kt in range(nkt):
                ps = pss[ks % 3]
                ks += 1
                p.I("pe", "matmul", r=[kb[m], qb[m]], w=[ps], out=ps[:], lhsT=kb[m][:, kt * 128:(kt + 1) * 128],
                    rhs=qb[m][:, Q0:Q0 + 512], start=True, stop=True)
                dpt = pts.reg(kt)
                p.I("act", "activation", r=[ps], w=[dpt], out=pts[:, kt, :], in_=ps[:], func=AF.Exp, scale=0.125)
                if kt >= 4 * qblk:
                    p.I("pool", "tensor_tensor", r=[dpt, mk_], w=[dpt], out=pts[:, kt, :], in0=pts[:, kt, :],
                        in1=mk_[:, kt - 4 * qblk, :], op=ALU.mult)
            for j in range(4):
                pa = psa[ka % 3]
                ka += 1
                for kt in range(nkt):
                    p.I("pe", "matmul", r=[pts.reg(kt), vb], w=[pa], out=pa[:, 0:129], lhsT=pts[:, kt, j * 128:(j + 1) * 128],
                        rhs=vb[:, kt, :], start=(kt == 0), stop=(kt == nkt - 1))
                r = sm[ka % 3]
                p.I("dve", "reciprocal", r=[pa], w=[r], out=r[:, 0:1], in_=pa[:, 128:129])
                if m == 0:
                    p.I("dve", "tensor_scalar", r=[pa, r], w=[o], out=o[:, j, :], in0=pa[:, 0:128], scalar1=r[:, 0:1],
                        scalar2=None, op0=ALU.mult)
                else:
                    p.I("dve", "tensor_tensor", r=[r, nlam], w=[r], out=r[:, 1:2], in0=r[:, 0:1], in1=nlam[:], op=ALU.mult)
                    p.I("dve", "scalar_tensor_tensor", r=[pa, r, o], w=[o], out=o[:, j, :], in0=pa[:, 0:128],
                        scalar=r[:, 1:2], in1=o[:, j, :], op0=ALU.mult, op1=ALU.add)
                    p.I("act", "activation", r=[o], w=[junk, r], out=junk[:], in_=o[:, j, :], func=AF.Square,
                        accum_out=r[:, 2:3])
                    p.I("act", "activation", r=[r, epsb], w=[r], out=r[:, 3:4], in_=r[:, 2:3], func=AF.Sqrt, scale=1.0 / 128,
                        bias=epsb[:, 0:1])
                    p.I("dve", "reciprocal", r=[r], w=[r], out=r[:, 3:4], in_=r[:, 3:4])
                    p.I("dve", "scalar_tensor_tensor", r=[o, r, sgt], w=[o], out=o[:, j, :], in0=o[:, j, :],
                        scalar=r[:, 3:4], in1=sgt[:], op0=ALU.mult, op1=ALU.mult)
        p.dma(ya[Q0:Q0 + 512, :].rearrange("(j p) e -> p j e", p=128), o[:], r=[o])
    p.finish()
    return p


def mask_const():
    m = np.zeros((4, 128, 512), np.float32)
    pp = np.arange(128)[:, None] // 64
    ff = np.arange(512)[None, :] // 64
    for j in range(4):
        m[j] = ((2 * j + pp) <= ff)
    return m.astype(BF)


S = 4096
NCH = 64
EM05 = math.exp(-0.5)
GN_EPS = 64e-5


def build_B(stop=99):
    p = Prog()
    rkvT = p.dram("rkvT", [3, 128, S], F32, "ExternalInput")
    lowT = p.dram("lowT", [256, S], F32, "ExternalInput")
    chp = p.dram("chp", [128, 16], F32, "ExternalInput")
    wup = p.dram("wup", [128, 128], F32, "ExternalInput")
    gup = p.dram("gup", [128, 128], F32, "ExternalInput")
    cst = p.dram("cst", [128, 1152], F32, "ExternalInput")
    ybT = p.dram("ybT", [128, S], F32, "ExternalOutput")

    ch = p.sb("ch", [128, 16], F32)
    cs = p.sb("cs", [128, 1152], F32)
    wupf = p.sb("wupf", [128, 128], F32)
    gupf = p.sb("gupf", [128, 128], F32)
    wupb = p.sb("wupb", [128, 128], BF16)
    gupb = p.sb("gupb", [128, 128], BF16)
    bones = p.sb("bones", [128, 128], BF16)
    p.dma(ch[:], chp, w=[ch])
    p.dma(cs[:], cst, w=[cs])
    p.dma(wupf[:], wup, w=[wupf])
    p.dma(gupf[:], gup, w=[gupf])
    p.I("dve", "tensor_copy", r=[wupf], w=[wupb], out=wupb[:], in_=wupf[:])
    p.I("dve", "tensor_copy", r=[gupf], w=[gupb], out=gupb[:], in_=gupf[:])
    p.I("dve", "tensor_copy", r=[cs], w=[bones], out=bones[:], in_=cs[:, 0:128])
    ident = cs[:, 128:256]
    rmask = cs[:, 256:768]
    epsb = p.sb("epsb", [128, 2], F32)
    p.I("dve", "memset", w=[epsb], ap=epsb[:, 0:1], constant=GN_EPS)
    p.I("dve", "memset", w=[epsb], ap=epsb[:, 1:2], constant=0.0)

    ARd = p.nc.dram_tensor("ARd", [128, NCH * 128], BF16, kind="Internal").ap()
    BKd = p.nc.dram_tensor("BKd", [128, NCH * 128], BF16, kind="Internal").ap()
    PCd = p.nc.dram_tensor("PCd", [128, NCH], F32, kind="Internal").ap()
    yd2 = p.nc.dram_tensor("yd2", [128, S], F32, kind="Internal").ap()
    dARd, dBKd, dPCd, dyd2 = Dep(), Dep(), Dep(), Dep()
    ARs = [p.sb("ARs%d" % i, [128, 8, 2, 64], BF16) for i in range(2)]
    BKs = [p.sb("BKs%d" % i, [128, 8, 2, 64], BF16) for i in range(2)]
    Bh = p.sb("Bh", [64, NCH, 2, 64], BF16)
    Kh = p.sb("Kh", [64, NCH, 2, 64], BF16)
    Vh = p.sb("Vh", [64, NCH, 2, 64], BF16)
    PC = p.sb("PC", [128, NCH], F32)
    bonus = p.sb("bonus", [128, S], BF16)
    gT = p.sb("gT", [128, S], BF16)

    NT = 12
    tf = [p.sb("tf%d" % i, [128, 512], F32) for i in range(NT)]
    tb = [p.sb("tb%d" % i, [128, 512], BF16) for i in range(4)]
    xin = [p.sb("xin%d" % i, [128, 513], F32) for i in range(5)]
    ps = [p.ps("ps%d" % i, [128, 512], F32) for i in range(8)]

    MU_R, MU_K, MU_V, MU_WA, MU_G, W0, A0, KKG, KAG, RK, GNG, GNB = range(12)

    def col(i):
        return ch[:, i:i + 1]

    for blk in range(8):
        t0 = blk * 512
        srcs = [rkvT[0], rkvT[1], rkvT[2], lowT[0:128], lowT[128:256]]
        for i in range(5):
            if blk == 0:
                p.I("pool", "memset", w=[xin[i]], ap=xin[i][:, 0:1], constant=0.0)
                p.dma(xin[i][:, 1:513], srcs[i][:, 0:512], w=[xin[i]], q=("sp", "act")[i % 2])
            else:
                p.dma(xin[i][:], srcs[i][:, t0 - 1:t0 + 512], w=[xin[i]], q=("sp", "act")[i % 2])
        for i in range(5):
            d = tf[5]
            p.I("dve", "tensor_tensor", r=[xin[i]], w=[d], out=d[:], in0=xin[i][:, 0:512], in1=xin[i][:, 1:513], op=ALU.subtract)
            p.I("dve", "scalar_tensor_tensor", r=[d, ch, xin[i]], w=[tf[i]], out=tf[i][:], in0=d[:], scalar=col(MU_R + i),
                in1=xin[i][:, 1:513], op0=ALU.mult, op1=ALU.add)
        r_, k_, v_, wa_, gd_ = tf[0], tf[1], tf[2], tf[3], tf[4]
        p.I("act", "activation", r=[wa_], w=[tb[0]], out=tb[0][0:64, :], in_=wa_[0:64, :], func=AF.Tanh)
        p.I("act", "activation", r=[wa_], w=[tb[0]], out=tb[0][64:128, :], in_=wa_[64:128, :], func=AF.Copy)
        p.I("act", "activation", r=[gd_], w=[tb[1]], out=tb[1][:], in_=gd_[:], func=AF.Sigmoid)
        p.I("pe", "matmul", r=[wupb, tb[0]], w=[ps[0]], out=ps[0][:], lhsT=wupb[0:64, :], rhs=tb[0][0:64, :], start=True, stop=True)
        p.I("pe", "matmul", r=[wupb, tb[0]], w=[ps[1]], out=ps[1][:], lhsT=wupb[64:128, :], rhs=tb[0][64:128, :], start=True, stop=True)
        p.I("pe", "matmul", r=[gupb, tb[1]], w=[ps[2]], out=ps[2][:], lhsT=gupb[:], rhs=tb[1][:], start=True, stop=True)
        dl, a_ = tf[5], tf[6]
        p.I("act", "activation", r=[ps[0], ch], w=[dl], out=dl[:], in_=ps[0][:], func=AF.Sigmoid, bias=col(W0))
        p.I("dve", "tensor_scalar", r=[dl], w=[dl], out=dl[:], in0=dl[:], scalar1=-EM05, scalar2=None, op0=ALU.mult)
        p.I("act", "activation", r=[ps[1], ch], w=[a_], out=a_[:], in_=ps[1][:], func=AF.Sigmoid, bias=col(A0))
        p.I("act", "activation", r=[ps[2]], w=[gT], out=gT[:, t0:t0 + 512], in_=ps[2][:], func=AF.Copy)
        kk, kap = tf[7], tf[8]
        p.I("dve", "tensor_scalar", r=[k_, ch], w=[kk], out=kk[:], in0=k_[:], scalar1=col(KKG), scalar2=None, op0=ALU.mult)
        p.I("pool", "tensor_tensor", r=[kk], w=[tb[2]], out=tb[2][:], in0=kk[:], in1=kk[:], op=ALU.mult)
        p.I("pe", "matmul", r=[bones, tb[2]], w=[ps[3]], out=ps[3][:], lhsT=bones[:], rhs=tb[2][:], start=True, stop=True)
        rn = tf[9]
        p.I("act", "activation", r=[ps[3], epsb], w=[rn], out=rn[:], in_=ps[3][:], func=AF.Sqrt, bias=epsb[:, 1:2])
        p.I("dve", "tensor_scalar", r=[rn], w=[rn], out=rn[:], in0=rn[:], scalar1=1e-12, scalar2=None, op0=ALU.max)
        p.I("dve", "reciprocal", r=[rn], w=[rn], out=rn[:], in_=rn[:])
        p.I("dve", "tensor_tensor", r=[kk, rn], w=[kap], out=kap[:], in0=kk[:], in1=rn[:], op=ALU.mult)
        km = tf[7]
        p.I("dve", "tensor_scalar", r=[a_, ch], w=[tf[9]], out=tf[9][:], in0=a_[:], scalar1=-1.0, scalar2=col(KAG), op0=ALU.add, op1=ALU.mult)
        p.I("dve", "scalar_tensor_tensor", r=[tf[9], k_], w=[km], out=km[:], in0=tf[9][:], scalar=1.0, in1=k_[:], op0=ALU.add, op1=ALU.mult)
        p.I("dve", "scalar_tensor_tensor", r=[r_, ch, km], w=[tb[3]], out=tb[3][:], in0=r_[:], scalar=col(RK), in1=km[:], op0=ALU.mult, op1=ALU.mult)
        p.I("pe", "matmul", r=[bones, tb[3]], w=[ps[4]], out=ps[4][:], lhsT=bones[:], rhs=tb[3][:], start=True, stop=True)
        p.I("dve", "tensor_tensor", r=[ps[4], v_], w=[bonus], out=bonus[:, t0:t0 + 512], in0=ps[4][:], in1=v_[:], op=ALU.mult)
        L = tf[9]
        p.I("dve", "tensor_tensor_scan", r=[cs, dl], w=[L], out=L[:], data0=rmask, data1=dl[:], initial=0.0, op0=ALU.mult, op1=ALU.add)
        P_, Pp, Pi, E_ = tf[10], tf[11], tf[1], tf[4]
        p.I("act", "activation", r=[L], w=[P_], out=P_[:], in_=L[:], func=AF.Exp)
        p.I("dve", "tensor_tensor", r=[L, dl], w=[Pp], out=Pp[:], in0=L[:], in1=dl[:], op=ALU.subtract)
        p.I("act", "activation", r=[Pp], w=[Pp], out=Pp[:], in_=Pp[:], func=AF.Exp)
        p.I("act", "activation", r=[L], w=[Pi], out=Pi[:], in_=L[:], func=AF.Exp, scale=-1.0)
        Lv = L[:].rearrange("p (c t) -> p c t", t=64)
        p.I("dve", "tensor_tensor", r=[L], w=[E_], out=E_[:].rearrange("p (c t) -> p c t", t=64), in0=Lv,
            in1=Lv[:, :, 63:64].to_broadcast([128, 8, 64]), op=ALU.subtract)
        p.I("act", "activation", r=[E_], w=[E_], out=E_[:], in_=E_[:], func=AF.Exp, scale=-1.0)
        p.I("pool", "tensor_copy", r=[P_], w=[PC], out=PC[:, blk * 8:(blk + 1) * 8],
            in_=P_[:].rearrange("p (c t) -> p c t", t=64)[:, :, 63])
        csl = slice(0, 8)
        AR, BK = ARs[blk % 2], BKs[blk % 2]
        v3 = lambda t: t[:].rearrange("p (c t) -> p c t", t=64)
        p.I("dve", "scalar_tensor_tensor", r=[kap, Pp], w=[AR], out=AR[:, csl, 0, :], in0=v3(kap), scalar=-1.0, in1=v3(Pp), op0=ALU.mult, op1=ALU.mult)
        p.I("pool", "tensor_tensor", r=[r_, P_], w=[AR], out=AR[:, csl, 1, :], in0=v3(r_), in1=v3(P_), op=ALU.mult)
        ka = tf[5]
        p.I("dve", "tensor_tensor", r=[kap, a_], w=[ka], out=ka[:], in0=kap[:], in1=a_[:], op=ALU.mult)
        p.I("dve", "tensor_tensor", r=[ka, Pi], w=[BK], out=BK[:, csl, 0, :], in0=v3(ka), in1=v3(Pi), op=ALU.mult)
        p.I("pool", "tensor_tensor", r=[km, Pi], w=[BK], out=BK[:, csl, 1, :], in0=v3(km), in1=v3(Pi), op=ALU.mult)
        p.dma(ARd[:, blk * 1024:(blk + 1) * 1024], AR[:].rearrange("p c a k -> p (c a k)"), r=[AR], w=[dARd])
        p.dma(BKd[:, blk * 1024:(blk + 1) * 1024], BK[:].rearrange("p c a k -> p (c a k)"), r=[BK], w=[dBKd], q="act")
        Bf, Kf = tf[6], tf[8]
        p.I("dve", "tensor_tensor", r=[ka, E_], w=[Bf], out=Bf[:], in0=ka[:], in1=E_[:], op=ALU.mult)
        p.I("pool", "tensor_tensor", r=[km, E_], w=[Kf], out=Kf[:], in0=km[:], in1=E_[:], op=ALU.mult)
        for (src, dst, pi) in ((Bf, Bh, 5), (Kf, Kh, 6), (v_, Vh, 7)):
            for half in range(2):
                pt = ps[pi] if half == 0 else ps[(pi + 3) % 8 if pi != 7 else 0]
                for c4 in range(4):
                    c = half * 4 + c4
                    p.I("pe", "transpose", r=[src, cs], w=[pt], out=pt[0:64, c4 * 128:(c4 + 1) * 128], in_=src[:, c * 64:(c + 1) * 64], identity=ident)
                p.I("act", "activation", r=[pt], w=[dst], out=dst[:, blk * 8 + half * 4:blk * 8 + half * 4 + 4, :, :],
                    in_=pt[0:64, :].rearrange("p (c h k) -> p c h k", c=4, h=2), func=AF.Copy)

    p.dma(PCd, PC[:], r=[PC], w=[dPCd])
    if stop == 1:
        p.finish()
        return p
    m5 = cs[0:64, 768:1088]
    eye = cs[0:64, 1088:1152]
    mstrict = cs[0:64, 768:832]
    mlower = cs[0:64, 1024:1088]
    m4 = cs[0:64, 768:1024]
    ARx = p.sb("ARx", [64, NCH, 2, 64], BF16)
    BKx = p.sb("BKx", [64, NCH, 2, 64], BF16)
    PCx = p.sb("PCx", [64, NCH], F32)
    TT = p.sb("TT", [64, NCH, 64], BF16)
    yTh = p.sb("yTh", [64, S], F32)
    LM = [[p.sb("LM%d_%d" % (s_, i), [64, 2, 64], F32) for i in range(2)] for s_ in range(4)]
    XX = [[p.sb("XX%d_%d" % (s_, i), [64, 64], F32) for i in range(2)] for s_ in range(4)]
    S32 = p.sb("S32", [64, 64], F32)
    Sb = p.sb("Sb", [64, 64], BF16)
    Am = [p.sb("Am%d" % i, [64, 4, 64], BF16) for i in range(2)]
    Zb = [p.sb("Zb%d" % i, [64, 64], BF16) for i in range(2)]
    Ub = [p.sb("Ub%d" % i, [64, 64], BF16) for i in range(2)]
    for hd in range(2):
        hs = slice(hd * 64, (hd + 1) * 64)
        p.dma(ARx[:].rearrange("p n a k -> p (n a k)"), ARd[hs, :], r=[dARd], w=[ARx])
        p.dma(BKx[:].rearrange("p n a k -> p (n a k)"), BKd[hs, :], r=[dBKd], w=[BKx], q="act")
        p.dma(PCx[:], PCd[hs, :], r=[dPCd], w=[PCx])
        for n in range(NCH):
            s_ = n % 4
            pa, pb = ps[2 * s_], ps[2 * s_ + 1]
            lm0, x0 = LM[s_][0], XX[s_][0]
            p.I("pe", "matmul", r=[BKx, ARx], w=[pa], out=pa[0:64, 64:128], lhsT=BKx[:, n, 0, :], rhs=ARx[:, n, 0, :], start=True, stop=True)
            p.I("pe", "matmul", r=[BKx, ARx], w=[pa], out=pa[0:64, 0:64], lhsT=ARx[:, n, 0, :], rhs=BKx[:, n, 0, :], start=True, stop=True)
            p.I("dve", "tensor_tensor", r=[pa, cs], w=[lm0], out=lm0[:, 0, :], in0=pa[0:64, 0:64], in1=mlower, op=ALU.mult)
            p.I("dve", "tensor_tensor", r=[pa, cs], w=[lm0], out=lm0[:, 1, :], in0=pa[0:64, 64:128], in1=mstrict, op=ALU.mult)
            p.I("dve", "tensor_tensor", r=[lm0, cs], w=[x0], out=x0[:], in0=lm0[:, 1, :], in1=eye, op=ALU.add)
            cur = 0
            for j in range(1, 6):
                lmp, lmn = LM[s_][cur], LM[s_][1 - cur]
                xp, xn = XX[s_][cur], XX[s_][1 - cur]
                p.I("pe", "matmul", r=[lmp], w=[pa], out=pa[0:64, 0:64], lhsT=lmp[:, 1, :], rhs=lmp[:, 0, :], start=True, stop=True)
                if j < 5:
                    p.I("pe", "matmul", r=[lmp], w=[pa], out=pa[0:64, 64:128], lhsT=lmp[:, 0, :], rhs=lmp[:, 1, :], start=True, stop=True)
                p.I("act", "activation", r=[pa], w=[lmn], out=lmn[:].rearrange("p two k -> p (two k)"), in_=pa[0:64, 0:128], func=AF.Copy)
                p.I("pe", "matmul", r=[lmn, xp], w=[pb], out=pb[0:64, 0:64], lhsT=lmn[:, 0, :], rhs=xp[:], start=True, stop=True)
                p.I("dve", "tensor_tensor", r=[pb, xp], w=[xn], out=xn[:], in0=pb[0:64, 0:64], in1=xp[:], op=ALU.add)
                cur = 1 - cur
            p.I("pool", "tensor_copy", r=[XX[s_][cur]], w=[TT.reg(n)], out=TT[:, n, :], in_=XX[s_][cur][:])
        if stop == 2 + 2 * hd:
            p.finish()
            return p
        p.I("dve", "memset", w=[S32], ap=S32[:], constant=0.0)
        p.I("pool", "memset", w=[Sb], ap=Sb[:], constant=0.0)
        for n in range(NCH):
            am, zb, ub = Am[n % 2], Zb[n % 2], Ub[n % 2]
            o4 = 5 * (n % 2)
            pg, pz, pu, py, pS = ps[o4], ps[o4 + 1], ps[o4 + 2], ps[3], ps[4]
            p.I("pe", "matmul", r=[BKx, ARx], w=[pg], out=pg[0:64, 0:128], lhsT=BKx[:, n, 0, :], rhs=ARx[:, n, :, :], start=True, stop=True)
            p.I("pe", "matmul", r=[BKx, ARx], w=[pg], out=pg[0:64, 128:256], lhsT=BKx[:, n, 1, :], rhs=ARx[:, n, :, :], start=True, stop=True)
            p.I("dve", "tensor_tensor", r=[pg, cs], w=[am], out=am[:].rearrange("p q t -> p (q t)"), in0=pg[0:64, 0:256], in1=m4, op=ALU.mult)
            p.I("pe", "matmul", r=[am, Vh], w=[pz], out=pz[0:64, 0:64], lhsT=am[:, 2, :], rhs=Vh[:, n, hd, :], start=True, stop=False)
            p.I("pe", "matmul", r=[ARx, Sb], w=[pz], out=pz[0:64, 0:64], lhsT=ARx[:, n, 0, :], rhs=Sb[:], start=False, stop=True)
            p.I("act", "activation", r=[pz], w=[zb], out=zb[:], in_=pz[0:64, 0:64], func=AF.Copy)
            p.I("pe", "matmul", r=[TT.reg(n), zb], w=[pu], out=pu[0:64, 0:64], lhsT=TT[:, n, :], rhs=zb[:], start=True, stop=True)
            p.I("act", "activation", r=[pu], w=[ub], out=ub[:], in_=pu[0:64, 0:64], func=AF.Copy)
            p.I("pe", "matmul", r=[Sb, ARx], w=[py], out=py[0:64, 0:64], lhsT=Sb[:], rhs=ARx[:, n, 1, :], start=True, stop=False)
            p.I("pe", "matmul", r=[ub, am], w=[py], out=py[0:64, 0:64], lhsT=ub[:], rhs=am[:, 1, :], start=False, stop=False)
            p.I("pe", "matmul", r=[Vh, am], w=[py], out=py[0:64, 0:64], lhsT=Vh[:, n, hd, :], rhs=am[:, 3, :], start=False, stop=True)
            p.I("pe", "matmul", r=[Bh, ub], w=[pS], out=pS[0:64, 64:128], lhsT=Bh[:, n, hd, :], rhs=ub[:], start=True, stop=False)
            p.I("pe", "matmul", r=[Kh, Vh], w=[pS], out=pS[0:64, 64:128], lhsT=Kh[:, n, hd, :], rhs=Vh[:, n, hd, :], start=False, stop=True)
            p.I("act", "activation", r=[py], w=[yTh], out=yTh[:, n * 64:(n + 1) * 64], in_=py[0:64, 0:64], func=AF.Copy)
            p.I("dve", "scalar_tensor_tensor", r=[S32, PCx, pS], w=[S32], out=S32[:], in0=S32[:], scalar=PCx[:, n:n + 1], in1=pS[0:64, 64:128],
                op0=ALU.mult, op1=ALU.add)
            p.I("dve", "tensor_copy", r=[S32], w=[Sb], out=Sb[:], in_=S32[:])
        p.dma(yd2[hs, :], yTh[:], r=[yTh], w=[dyd2])
        if stop == 3 + 2 * hd:
            p.finish()
            return p

    bavg = p.sb("bavg", [128, 128], F32)
    p.I("dve", "tensor_scalar", r=[cs], w=[bavg], out=bavg[:], in0=cs[:, 0:128], scalar1=1.0 / 64, scalar2=None, op0=ALU.mult)
    for blk in range(8):
        sl = slice(blk * 512, (blk + 1) * 512)
        pm, pv = ps[(2 * blk) % 8], ps[(2 * blk + 1) % 8]
        yc, y2, o, yl = tf[0], tf[1], tf[2], tf[3 + blk % 2]
        p.dma(yl[:], yd2[:, sl], r=[dyd2], w=[yl])
        p.I("pool", "tensor_copy", r=[yl], w=[tb[0]], out=tb[0][:], in_=yl[:])
        p.I("pe", "matmul", r=[bones, tb[0]], w=[pm], out=pm[:], lhsT=bones[:], rhs=tb[0][:], start=True, stop=True)
        p.I("dve", "scalar_tensor_tensor", r=[yl, pm], w=[yc], out=yc[:], in0=pm[:], scalar=-1.0 / 64, in1=yl[:], op0=ALU.mult, op1=ALU.add)
        p.I("pool", "tensor_tensor", r=[yc], w=[tb[1]], out=tb[1][:], in0=yc[:], in1=yc[:], op=ALU.mult)
        p.I("pe", "matmul", r=[bones, tb[1]], w=[pv], out=pv[:], lhsT=bones[:], rhs=tb[1][:], start=True, stop=True)
        p.I("act", "activation", r=[pv, epsb], w=[y2], out=y2[:], in_=pv[:], func=AF.Sqrt, bias=epsb[:, 0:1], scale=1.0 / 64)
        p.I("dve", "reciprocal", r=[y2], w=[y2], out=y2[:], in_=y2[:])
        p.I("dve", "tensor_tensor", r=[yc, y2], w=[yc], out=yc[:], in0=yc[:], in1=y2[:], op=ALU.mult)
        p.I("dve", "tensor_scalar", r=[yc, ch], w=[yc], out=yc[:], in0=yc[:], scalar1=col(GNG), scalar2=col(GNB), op0=ALU.mult, op1=ALU.add)
        p.I("dve", "tensor_tensor", r=[yc, bonus], w=[yc], out=yc[:], in0=yc[:], in1=bonus[:, sl], op=ALU.add)
        p.I("dve", "tensor_tensor", r=[yc, gT], w=[o], out=o[:], in0=yc[:], in1=gT[:, sl], op=ALU.mult)
        p.dma(ybT[:, sl], o[:], r=[o])
    p.finish()
    return p


def cst_B():
    c = np.zeros((128, 1152), np.float32)
    c[0:64, 0:64] = 1.0
    c[64:128, 64:128] = 1.0
    c[:, 128:256] = np.eye(128)
    rm = np.ones(512, np.float32)
    rm[0::64] = 0.0
    c[:, 256:768] = rm[None, :]
    i = np.arange(64)[:, None]
    t = np.arange(64)[None, :]
    strict = (i < t).astype(np.float32)
    incl = (i <= t).astype(np.float32)
    lower = (t < i).astype(np.float32)
    c[0:64, 768:832] = strict
    c[0:64, 832:896] = incl
    c[0:64, 896:960] = strict
    c[0:64, 960:1024] = incl
    c[0:64, 1024:1088] = lower
    c[0:64, 1088:1152] = np.eye(64)
    return c


S = 4096
NCH = 64


def build_C(layer):
    p = Prog()
    qfT = p.dram("qfT", [2, 128, S], F32, "ExternalInput")
    ig = p.dram("ig", [2, S, 128], F32, "ExternalInput")
    lbl = p.dram("lbl", [128, 2], F32, "ExternalInput")
    ng = p.dram("ng", [64, 128], F32, "ExternalInput")
    cst = p.dram("cst", [128, 768], F32, "ExternalInput")
    yc = p.dram("yc", [S, 128], F32, "ExternalOutput")
    cs = p.sb("cs", [128, 768], F32)
    lb = p.sb("lb", [128, 4], F32)
    ngt = p.sb("ngt", [64, 128], F32)
    epsb = p.sb("epsb", [128, 1], F32)
    p.I("dve", "memset", w=[epsb], ap=epsb[:], constant=1e-6)
    p.dma(cs[:], cst, w=[cs])
    p.dma(lb[:, 0:2], lbl, w=[lb])
    p.dma(ngt[:], ng, w=[ngt])
    rmask = cs[:, 0:512]
    ident = cs[:, 512:640]
    incl = cs[0:64, 640:704]
    if layer == 0:
        p.I("dve", "memset", w=[lb], ap=lb[:, 2:3], constant=0.0)
    else:
        p.I("dve", "tensor_tensor", r=[lb], w=[lb], out=lb[:, 2:3], in0=lb[:, 1:2], in1=lb[:, 0:1], op=ALU.subtract)
        p.I("act", "activation", r=[lb], w=[lb], out=lb[:, 2:3], in_=lb[:, 2:3], func=AF.Sigmoid)
    p.I("dve", "tensor_scalar", r=[lb], w=[lb], out=lb[:, 3:4], in0=lb[:, 2:3], scalar1=-1.0, scalar2=1.0, op0=ALU.mult, op1=ALU.add)

    Qt = p.sb("Qt", [128, S], BF16)
    Kt = p.sb("Kt", [128, S], BF16)
    Qb = p.sb("Qb", [128, S], BF16)
    Kbh = p.sb("Kbh", [64, NCH, 128], BF16)
    Ih = p.sb("Ih", [64, NCH, 128], BF16)
    Sall = p.sb("Sall", [128, NCH, 128], BF16)
    dec = p.sb("dec", [128, NCH], F32)
    tf = [p.sb("tf%d" % i, [128, 512], F32) for i in range(8)]
    xin = [p.sb("xin%d" % i, [128, 512], F32) for i in range(2)]
    ist = [p.sb("ist%d" % i, [64, 8, 128], F32) for i in range(2)]
    ps = [p.ps("ps%d" % i, [128, 512], F32) for i in range(8)]
    v3 = lambda t: t[:].rearrange("p (c t) -> p c t", t=64)
    igv = ig.rearrange("w (n s) v -> w s n v", s=64)
    for blk in range(8):
        sl = slice(blk * 512, (blk + 1) * 512)
        p.dma(xin[0][:], qfT[0][:, sl], w=[xin[0]])
        p.dma(xin[1][:], qfT[1][:, sl], w=[xin[1]], q="act")
        it = ist[blk % 2]
        p.dma(it[:], igv[0][:, blk * 8:(blk + 1) * 8, :], w=[it])
        p.I("pool", "tensor_copy", r=[it], w=[Ih], out=Ih[:, blk * 8:(blk + 1) * 8, :], in_=it[:])
        qf, fg, lf, kf, b, t1, t2, t3 = tf
        p.I("act", "activation", r=[xin[0]], w=[qf], out=qf[:], in_=xin[0][:], func=AF.Silu)
        p.I("act", "activation", r=[xin[1]], w=[fg], out=fg[:], in_=xin[1][:], func=AF.Sigmoid)
        p.I("dve", "tensor_scalar", r=[fg, lb], w=[fg], out=fg[:], in0=fg[:], scalar1=lb[:, 3:4], scalar2=lb[:, 2:3], op0=ALU.mult, op1=ALU.add)
        p.I("act", "activation", r=[fg], w=[lf], out=lf[:], in_=fg[:], func=AF.Ln)
        p.I("dve", "tensor_scalar", r=[fg], w=[kf], out=kf[:], in0=fg[:], scalar1=-1.0, scalar2=1.0, op0=ALU.mult, op1=ALU.add)
        p.I("dve", "tensor_tensor_scan", r=[cs, lf], w=[b], out=b[:], data0=rmask, data1=lf[:], initial=0.0, op0=ALU.mult, op1=ALU.add)
        bv = v3(b)
        p.I("dve", "tensor_tensor", r=[b], w=[t1], out=v3(t1), in0=bv, in1=bv[:, :, 31:32].to_broadcast([128, 8, 64]), op=ALU.subtract)
        p.I("act", "activation", r=[t1], w=[t2], out=t2[:], in_=t1[:], func=AF.Exp)
        p.I("dve", "tensor_tensor", r=[qf, t2], w=[Qt], out=Qt[:, sl], in0=qf[:], in1=t2[:], op=ALU.mult)
        p.I("act", "activation", r=[t1], w=[t2], out=t2[:], in_=t1[:], func=AF.Exp, scale=-1.0)
        p.I("dve", "tensor_tensor", r=[kf, t2], w=[Kt], out=Kt[:, sl], in0=kf[:], in1=t2[:], op=ALU.mult)
        p.I("act", "activation", r=[b], w=[t2], out=t2[:], in_=b[:], func=AF.Exp)
        p.I("pool", "tensor_tensor", r=[qf, t2], w=[Qb], out=Qb[:, sl], in0=qf[:], in1=t2[:], op=ALU.mult)
        p.I("pool", "tensor_copy", r=[t2], w=[dec], out=dec[:, blk * 8:(blk + 1) * 8], in_=v3(t2)[:, :, 63])
        p.I("dve", "tensor_tensor", r=[b], w=[t1], out=v3(t1), in0=bv, in1=bv[:, :, 63:64].to_broadcast([128, 8, 64]), op=ALU.subtract)
        p.I("act", "activation", r=[t1], w=[t3], out=t3[:], in_=t1[:], func=AF.Exp, scale=-1.0)
        p.I("dve", "tensor_tensor", r=[kf, t3], w=[t3], out=t3[:], in0=kf[:], in1=t3[:], op=ALU.mult)
        for half in range(2):
            pt = ps[half]
            for c4 in range(4):
                c = half * 4 + c4
                p.I("pe", "transpose", r=[t3, cs], w=[pt], out=pt[0:64, c4 * 128:(c4 + 1) * 128], in_=t3[:, c * 64:(c + 1) * 64], identity=ident)
            p.I("act", "activation", r=[pt], w=[Kbh], out=Kbh[:, blk * 8 + half * 4:blk * 8 + half * 4 + 4, :],
                in_=pt[0:64, :].rearrange("p (c k) -> p c k", c=4), func=AF.Copy)
    St = p.sb("St", [128, 128], F32)
    p.I("dve", "memset", w=[St], ap=St[:], constant=0.0)
    p.I("pool", "memset", w=[Sall.reg(0)], ap=Sall[:, 0, :], constant=0.0)
    for n in range(NCH - 1):
        pk = ps[2 + n % 3]
        p.I("pe", "matmul", r=[Kbh, Ih], w=[pk], out=pk[:, 0:128], lhsT=Kbh[:, n, :], rhs=Ih[:, n, :], start=True, stop=True)
        p.I("dve", "scalar_tensor_tensor", r=[St, dec, pk], w=[St], out=St[:], in0=St[:], scalar=dec[:, n:n + 1], in1=pk[:, 0:128],
            op0=ALU.mult, op1=ALU.add)
        p.I("pool", "tensor_copy", r=[St], w=[Sall.reg(n + 1)], out=Sall[:, n + 1, :], in_=St[:])
    at = [p.sb("at%d" % i, [64, 64], BF16) for i in range(2)]
    o1 = [p.sb("o1_%d" % i, [64, 128], F32) for i in range(2)]
    ot = [p.sb("ot%d" % i, [64, 8, 128], F32) for i in range(2)]
    gs = [p.sb("gs%d" % i, [64, 8, 128], F32) for i in range(2)]
    sm = [p.sb("sm%d" % i, [64, 2], F32) for i in range(2)]
    junk = p.sb("junk", [64, 128], F32)
    for n in range(NCH):
        g8 = n // 8
        if n % 8 == 0:
            gt = gs[g8 % 2]
            p.dma(gt[:], igv[1][:, n:n + 8, :], w=[gt])
            p.I("act", "activation", r=[gt], w=[gt], out=gt[:], in_=gt[:], func=AF.Silu)
            p.I("dve", "tensor_tensor", r=[gt, ngt], w=[gt], out=gt[:], in0=gt[:],
                in1=ngt[:].rearrange("p (o v) -> p o v", o=1).to_broadcast([64, 8, 128]), op=ALU.mult)
        gt = gs[g8 % 2]
        o8 = ot[g8 % 2]
        csl = slice(n * 64, (n + 1) * 64)
        pa, po = ps[5 + n % 2], ps[7 if n % 2 else 0]
        p.I("pe", "matmul", r=[Kt, Qt], w=[pa], out=pa[0:64, 0:64], lhsT=Kt[:, csl], rhs=Qt[:, csl], start=True, stop=True)
        a = at[n % 2]
        p.I("dve", "tensor_tensor", r=[pa, cs], w=[a], out=a[:], in0=pa[0:64, 0:64], in1=incl, op=ALU.mult)
        p.I("pe", "matmul", r=[a, Ih], w=[po], out=po[0:64, 0:128], lhsT=a[:], rhs=Ih[:, n, :], start=True, stop=True)
        p.I("pe", "matmul", r=[Qb, Sall.reg(n)], w=[po], out=po[0:64, 128:256], lhsT=Qb[:, csl], rhs=Sall[:, n, :], start=True, stop=True)
        oo = o1[n % 2]
        p.I("act", "activation", r=[po], w=[oo], out=oo[:], in_=po[0:64, 0:128], func=AF.Copy)
        p.I("dve", "tensor_tensor", r=[oo, po], w=[oo], out=oo[:], in0=oo[:], in1=po[0:64, 128:256], op=ALU.add)
        r = sm[n % 2]
        p.I("act", "activation", r=[oo], w=[junk, r], out=junk[:], in_=oo[:], func=AF.Square, accum_out=r[:, 0:1])
        p.I("act", "activation", r=[r, epsb], w=[r], out=r[:, 1:2], in_=r[:, 0:1], func=AF.Sqrt, scale=1.0 / 128, bias=epsb[0:64, 0:1])
        p.I("dve", "reciprocal", r=[r], w=[r], out=r[:, 1:2], in_=r[:, 1:2])
        p.I("dve", "scalar_tensor_tensor", r=[oo, r, gt], w=[o8], out=o8[:, n % 8, :], in0=oo[:], scalar=r[:, 1:2], in1=gt[:, n % 8, :],
            op0=ALU.mult, op1=ALU.mult)
        if n % 8 == 7:
            p.dma(yc.rearrange("(n s) v -> s n v", s=64)[:, n - 7:n + 1, :], o8[:], r=[o8])
    p.finish()
    return p


def cst_C():
    c = np.zeros((128, 768), np.float32)
    rm = np.ones(512, np.float32)
    rm[0::64] = 0.0
    c[:, 0:512] = rm[None, :]
    c[:, 512:640] = np.eye(128)
    i = np.arange(64)[:, None]
    t = np.arange(64)[None, :]
    c[0:64, 640:704] = (i <= t)
    return c


S = 4096
NEG = -30000.0


def build_D():
    p = Prog()
    qT = p.dram("qT", [64, 8192], F32, "ExternalInput")
    iqT = p.dram("iqT", [64, 8192], F32, "ExternalInput")
    iw = p.dram("iw", [128, 64], F32, "ExternalInput")
    kT = p.dram("kT", [64, S], F32, "ExternalInput")
    ikT = p.dram("ikT", [64, S], F32, "ExternalInput")
    v = p.dram("v", [S, 64], F32, "ExternalInput")
    vmask = p.dram("vmask", [128, 512], F32, "ExternalInput")
    id4 = p.dram("id4", [128, 512], BF16, "ExternalInput")
    yd = p.dram("yd", [1024, 512], F32, "ExternalOutput")
    qb = p.sb("qb", [64, 8192], BF16)
    iqb = p.sb("iqb", [64, 8192], BF16)
    kb = p.sb("kb", [64, S], BF16)
    ikb = p.sb("ikb", [64, S], BF16)
    vb = p.sb("vb", [128, 32, 65], BF16)
    iwt = p.sb("iwt", [128, 64], F32)
    vm = p.sb("vm", [128, 512], F32)
    i4 = p.sb("i4", [128, 512], BF16)
    st = [p.sb("st%d" % i, [128, 2048], F32) for i in range(2)]
    k = 0
    for (src, dst, n) in ((qT, qb, 8192), (iqT, iqb, 8192), (kT, kb, S), (ikT, ikb, S)):
        for c0 in range(0, n, 2048):
            s = st[k % 2]
            k += 1
            p.dma(s[0:64, :], src[:, c0:c0 + 2048], w=[s])
            p.I(("dve", "pool")[k % 2], "tensor_copy", r=[s], w=[dst], out=dst[:, c0:c0 + 2048], in_=s[0:64, :])
    s = st[k % 2]
    k += 1
    p.dma(s[:].rearrange("p (t e) -> p t e", e=64), v.rearrange("(t p) e -> p t e", p=128), w=[s])
    p.I("pool", "memset", w=[vb], ap=vb[:], constant=1.0)
    p.I("dve", "tensor_copy", r=[s], w=[vb], out=vb[:, :, 0:64], in_=s[:].rearrange("p (t e) -> p t e", e=64))
    p.dma(iwt[:], iw, w=[iwt])
    p.dma(vm[:], vmask, w=[vm])
    p.dma(i4[:], id4, w=[i4])
    p.I("dve", "tensor_scalar", r=[iwt], w=[iwt], out=iwt[:], in0=iwt[:], scalar1=(8 ** -0.5) * (64 ** -0.5), scalar2=None, op0=ALU.mult)

    sc = p.sb("sc", [128, S], F32)
    wk = p.sb("wk", [128, S], F32)
    bias = p.sb("bias", [128, S], BF16)
    pts = p.sb("pts", [128, 32, 512], BF16)
    rl = [p.sb("rl%d" % i, [128, 512], F32) for i in range(2)]
    mx = p.sb("mx", [128, 8], F32)
    yo = [p.sb("yo%d" % i, [128, 8, 64], F32) for i in range(2)]
    sm = [p.sb("sm%d" % i, [128, 1], F32) for i in range(3)]
    pss = [p.ps("pss%d" % i, [128, 512], F32) for i in range(4)]
    psa = [p.ps("psa%d" % i, [128, 512], F32) for i in range(3)]
    ks = 0
    ka = 0
    kr = 0
    for m in range(8):
        N = (m + 1) * 512
        nkt = 4 * m + 4
        qsl = slice(m * 1024, (m + 1) * 1024)
        for g in range(m + 1):
            gs = slice(g * 512, (g + 1) * 512)
            for h in range(8):
                ps = pss[ks % 4]
                ks += 1
                p.I("pe", "matmul", r=[iqb, ikb], w=[ps], out=ps[:], lhsT=iqb[:, m * 1024 + h * 128:m * 1024 + (h + 1) * 128],
                    rhs=ikb[:, gs], start=True, stop=True)
                r = rl[kr % 2]
                kr += 1
                p.I("act", "activation", r=[ps], w=[r], out=r[:], in_=ps[:], func=AF.Relu)
                ws = iwt[:, m * 8 + h:m * 8 + h + 1]
                if h == 0:
                    p.I("dve", "tensor_scalar", r=[r, iwt], w=[sc], out=sc[:, gs], in0=r[:], scalar1=ws, scalar2=None, op0=ALU.mult)
                else:
                    p.I("dve", "scalar_tensor_tensor", r=[r, iwt, sc], w=[sc], out=sc[:, gs], in0=r[:], scalar=ws, in1=sc[:, gs],
                        op0=ALU.mult, op1=ALU.add)
            if g == m:
                p.I("dve", "tensor_tensor", r=[sc, vm], w=[sc], out=sc[:, gs], in0=sc[:, gs], in1=vm[:], op=ALU.add)
        p.I("pool", "tensor_copy", r=[sc], w=[wk], out=wk[:, 0:N], in_=sc[:, 0:N])
        for rnd in range(32):
            p.I("dve", "max", r=[wk], w=[mx], out=mx[:], in_=wk[:, 0:N])
            if rnd < 31:
                p.I("dve", "match_replace", r=[wk, mx], w=[wk], out=wk[:, 0:N], in_to_replace=mx[:], in_values=wk[:, 0:N], imm_value=-1e30)
        p.I("dve", "tensor_scalar", r=[sc, mx], w=[bias], out=bias[:, 0:N], in0=sc[:, 0:N], scalar1=mx[:, 7:8], scalar2=NEG,
            op0=ALU.is_lt, op1=ALU.mult)
        p.I("dve", "tensor_tensor", r=[bias, vm], w=[bias], out=bias[:, m * 512:N], in0=bias[:, m * 512:N], in1=vm[:], op=ALU.add)
        o = yo[m % 2]
        for hg in range(2):
            for kt in range(nkt):
                ps = pss[ks % 4]
                ks += 1
                p.I("pe", "matmul", r=[kb, qb], w=[ps], out=ps[:], lhsT=kb[:, kt * 128:(kt + 1) * 128],
                    rhs=qb[:, m * 1024 + hg * 512:m * 1024 + (hg + 1) * 512], start=True, stop=False)
                p.I("pe", "matmul", r=[bias, i4], w=[ps], out=ps[:], lhsT=bias[:, kt * 128:(kt + 1) * 128], rhs=i4[:],
                    start=False, stop=True)
                p.I("act", "activation", r=[ps], w=[pts.reg(kt)], out=pts[:, kt, :], in_=ps[:], func=AF.Exp, scale=0.125)
            for h4 in range(4):
                pa = psa[ka % 3]
                r = sm[ka % 3]
                ka += 1
                for kt in range(nkt):
                    p.I("pe", "matmul", r=[pts.reg(kt), vb], w=[pa], out=pa[:, 0:65], lhsT=pts[:, kt, h4 * 128:(h4 + 1) * 128],
                        rhs=vb[:, kt, :], start=(kt == 0), stop=(kt == nkt - 1))
                p.I("dve", "reciprocal", r=[pa], w=[r], out=r[:], in_=pa[:, 64:65])
                p.I("dve", "tensor_scalar", r=[pa, r], w=[o], out=o[:, hg * 4 + h4, :], in0=pa[:, 0:64], scalar1=r[:, 0:1],
                    scalar2=None, op0=ALU.mult)
        p.dma(yd[m * 128:(m + 1) * 128, :], o[:].rearrange("p h d -> p (h d)"), r=[o])
    p.finish()
    return p


def vmask_const(j):
    pp = 2 * j + np.arange(128)[:, None] // 64
    ff = np.arange(512)[None, :] // 64
    return np.where(ff <= pp, 0.0, NEG).astype(np.float32)


def id4_const():
    return np.ascontiguousarray(np.tile(np.eye(128, dtype=np.float32), (1, 4))).astype(BF)


_PROGS = {}


def _prog(key, fn):
    if key not in _PROGS:
        _PROGS[key] = fn()
    return _PROGS[key]


def _run(p, ims):
    n = len(ims)
    return run_bass_kernel_spmd(p.nc, ims, core_ids=list(range(n))).results


def _cast_weights(arrs):
    sizes = [a.size for a in arrs]
    tot = sum(sizes)
    per = -(-tot // (8 * 128 * 4096)) * 4096
    flat = np.zeros(8 * 128 * per, np.float32)
    o = 0
    for a in arrs:
        flat[o:o + a.size] = a.reshape(-1)
        o += a.size
    flat = flat.reshape(8, 128, per)
    p = _prog(("L0", per), lambda: build_L0(per))
    res = _run(p, [{"src": flat[c]} for c in range(8)])
    out = np.concatenate([r["dst"].reshape(-1) for r in res])
    outs = []
    o = 0
    for a in arrs:
        outs.append(out[o:o + a.size].reshape(a.shape))
        o += a.size
    return outs


def _layer(l, x, positions, inp, W):
    f32 = np.float32
    g0 = np.ascontiguousarray(inp["norm_g"][l, 0].reshape(16, 128).T)
    cf = cf_const()
    ims = []
    for c in range(8):
        sl = slice(c * 1024, (c + 1) * 1024)
        ims.append({"xT": np.ascontiguousarray(x[sl].T), "g": g0, "wb": W["wb"],
                    "pos": np.ascontiguousarray(positions[sl].reshape(8, 128).T.astype(np.int32)), "cf": cf})
    res = _run(_prog("L1", build_L1), ims)
    proj = np.concatenate([r["proj"] for r in res], 0)
    y = np.zeros((4, 8192, 512), f32)
    lam_v = inp["diff_lambda"][l]
    sgv = inp["diff_subln_g"][l]
    ims = []
    for c in range(8):
        b, h = c // 4, c % 4
        pb = proj[b * 4096:(b + 1) * 4096]
        q = pb[:, h * 128:(h + 1) * 128].reshape(4096, 2, 64)
        k = pb[:, 512 + h * 128:512 + (h + 1) * 128].reshape(4096, 2, 64)
        ims.append({"qT": np.ascontiguousarray(q.transpose(1, 2, 0)), "kT": np.ascontiguousarray(k.transpose(1, 2, 0)),
                    "v": np.ascontiguousarray(pb[:, 1024 + h * 128:1024 + (h + 1) * 128]),
                    "lam4": np.ascontiguousarray(np.broadcast_to(lam_v.reshape(1, 256), (128, 256))),
                    "sg": np.ascontiguousarray(np.broadcast_to(sgv[None, :], (128, 128))), "masks": mask_const()})
    res = _run(_prog(("A", l), lambda: build_A(l)), ims)
    for c in range(8):
        b, h = c // 4, c % 4
        y[0, b * 4096:(b + 1) * 4096, h * 128:(h + 1) * 128] = res[c]["ya"]
    mu = inp["rwkv_mu"][l]
    ims = []
    cstb = cst_B()
    for c in range(8):
        b, hp = c // 4, c % 4
        pb = proj[b * 4096:(b + 1) * 4096, 1536:3328]
        cols = slice(hp * 128, (hp + 1) * 128)
        rkv = np.stack([pb[:, i * 512 + hp * 128:i * 512 + (hp + 1) * 128].T for i in range(3)])
        chp = np.zeros((128, 16), f32)
        for i in range(3):
            chp[:, i] = mu[i * 512 + hp * 128:i * 512 + (hp + 1) * 128]
        chp[:, 3] = mu[1536:1664]
        chp[:, 4] = mu[1664:1792]
        chp[:, 5] = inp["rwkv_w0"][l][cols]
        chp[:, 6] = inp["rwkv_a0"][l][cols]
        chp[:, 7] = inp["rwkv_k_k"][l][cols]
        chp[:, 8] = inp["rwkv_k_a"][l][cols]
        chp[:, 9] = inp["rwkv_r_k"][l].reshape(512)[cols]
        chp[:, 10] = inp["rwkv_gn_g"][l][cols]
        chp[:, 11] = inp["rwkv_gn_b"][l][cols]
        wup = np.concatenate([inp["rwkv_w_up"][l][:, cols], inp["rwkv_a_up"][l][:, cols]], 0)
        ims.append({"rkvT": np.ascontiguousarray(rkv), "lowT": np.ascontiguousarray(pb[:, 1536:1792].T), "chp": chp,
                    "wup": np.ascontiguousarray(wup), "gup": np.ascontiguousarray(inp["rwkv_g_up"][l][:, cols]), "cst": cstb})
    res = _run(_prog("B", build_B), ims)
    for c in range(8):
        b, hp = c // 4, c % 4
        y[1, b * 4096:(b + 1) * 4096, hp * 128:(hp + 1) * 128] = res[c]["ybT"].T
    ims = []
    cstc = cst_C()
    for c in range(8):
        b, h = c // 4, c % 4
        pc = proj[b * 4096:(b + 1) * 4096, 3328:5376]
        hc = slice(h * 128, (h + 1) * 128)
        qf = np.stack([pc[:, 0:512][:, hc].T, pc[:, 512:1024][:, hc].T])
        ig = np.stack([pc[:, 1024:1536][:, hc], pc[:, 1536:2048][:, hc]])
        ims.append({"qfT": np.ascontiguousarray(qf), "ig": np.ascontiguousarray(ig),
                    "lbl": np.ascontiguousarray(inp["hgrn_lb_logits"][:, hc].T),
                    "ng": np.ascontiguousarray(np.broadcast_to(inp["hgrn_norm_g"][l][hc][None, :], (64, 128))), "cst": cstc})
    res = _run(_prog(("C", l), lambda: build_C(l)), ims)
    for c in range(8):
        b, h = c // 4, c % 4
        y[2, b * 4096:(b + 1) * 4096, h * 128:(h + 1) * 128] = res[c]["yc"]
    O = 5376
    ims = []
    rows_all = []
    for c in range(8):
        b, j = c // 4, c % 4
        pb = proj[b * 4096:(b + 1) * 4096]
        rows = np.concatenate([np.arange(i * 128, (i + 1) * 128) for i in [4 * m + j for m in range(8)]])
        rows_all.append(rows)
        q = pb[rows, O:O + 512].reshape(8, 128, 8, 64)
        iq = pb[rows, O + 640:O + 1152].reshape(8, 128, 8, 64)
        iw = pb[rows, O + 1216:O + 1224].reshape(8, 128, 8)
        ims.append({"qT": np.ascontiguousarray(q.transpose(3, 0, 2, 1).reshape(64, 8192)),
                    "iqT": np.ascontiguousarray(iq.transpose(3, 0, 2, 1).reshape(64, 8192)),
                    "iw": np.ascontiguousarray(iw.transpose(1, 0, 2).reshape(128, 64)),
                    "kT": np.ascontiguousarray(pb[:, O + 512:O + 576].T), "ikT": np.ascontiguousarray(pb[:, O + 1152:O + 1216].T),
                    "v": np.ascontiguousarray(pb[:, O + 576:O + 640]), "vmask": vmask_const(j), "id4": id4_const()})
    res = _run(_prog("D", build_D), ims)
    for c in range(8):
        b = c // 4
        y[3, b * 4096 + rows_all[c]] = res[c]["yd"]
    del proj
    g3 = np.ascontiguousarray(np.concatenate([inp["norm_g"][l, i].reshape(16, 128).T for i in range(4)], axis=1))
    ims = []
    for c in range(4):
        sl = slice(c * 2048, (c + 1) * 2048)
        ims.append({"xT": np.ascontiguousarray(x[sl].T), "yT": np.ascontiguousarray(y[:, sl].transpose(0, 2, 1)), "wg": W["wg"], "g3": g3,
                    "wbr": W["wbr"], "wout": W["wout"], "w1": W["w1"], "w2": W["w2"]})
    res = _run(_prog("L3", lambda: build_L3(2048)), ims)
    return np.ascontiguousarray(np.concatenate([r["xo"].T for r in res], 0))


def kernel(**inputs):
    inp = {k: np.asarray(v) for k, v in inputs.items()}
    x = np.ascontiguousarray(inp["x"].reshape(8192, D)).astype(np.float32, copy=False)
    positions = inp["positions"].reshape(8192)
    tiled = []
    for l in range(2):
        w_in = inp["w_in"][l]
        tiled.append(prep_w_in(w_in))
        tiled.append(prep_wg(np.ascontiguousarray(w_in[:, NMIX:])))
        tiled.extend(prep_L3_weights(inp["w_branch"][l], inp["w_out"][l], inp["mlp_w1"][l], inp["mlp_w2"][l]))
    cast = _cast_weights(tiled)
    del tiled
    for l in range(2):
        W = dict(zip(("wb", "wg", "wbr", "wout", "w1", "w2"), cast[l * 6:(l + 1) * 6]))
        x = _layer(l, x, positions, inp, W)
    return x.reshape(2, 4096, D).astype(np.float32)
```

```python
import math
import numpy as np
import ml_dtypes
from contextlib import ExitStack
import concourse.bass as bass
import concourse.mybir as mybir
from concourse.bass_utils import run_bass_kernel_spmd

BF = ml_dtypes.bfloat16


F32 = mybir.dt.float32
BF16 = mybir.dt.bfloat16
I32 = mybir.dt.int32
AF = mybir.ActivationFunctionType
ALU = mybir.AluOpType
AX = mybir.AxisListType

SAME_ENG_SYNC = True
NDMA = 24


class Dep:
    __slots__ = ("w", "r")

    def __init__(self):
        self.w = None
        self.r = []


class Buf:
    def __init__(self, t, nreg=1):
        self.t = t
        self.d = Dep()
        self.regs = {}

    def reg(self, key):
        if key not in self.regs:
            self.regs[key] = Dep()
        return self.regs[key]

    def __getitem__(self, idx):
        return self.t[idx]


class Prog:
    def __init__(self):
        self.nc = bass.Bass("TRN2", target_bir_lowering=False)
        nc = self.nc
        self.es = ExitStack()
        self.eng = {"pe": nc.tensor, "act": nc.scalar, "dve": nc.vector, "pool": nc.gpsimd, "sp": nc.sync}
        self.sem = {}
        self.cnt = {}
        self.clock = {}
        self.hist = {}
        for e in self.eng:
            self.sem[e] = self.es.enter_context(nc.semaphore("s_" + e))
            self.cnt[e] = 0
            self.clock[e] = {}
            self.hist[e] = {}
        for j in range(NDMA):
            e = "d%d" % j
            self.sem[e] = self.es.enter_context(nc.semaphore("s_" + e))
            self.cnt[e] = 0
            self.hist[e] = {}
        self.dma_k = 0
        self.nwaits = 0
        self.ninst = 0
        self.q = {e: [] for e in self.eng}

    def dram(self, name, shape, dtype, kind):
        return self.nc.dram_tensor(name, list(shape), dtype, kind=kind).ap()

    def sb(self, name, shape, dtype=F32):
        return Buf(self.es.enter_context(self.nc.sbuf_tensor(name, list(shape), dtype)))

    def ps(self, name, shape, dtype=F32):
        return Buf(self.es.enter_context(self.nc.psum_tensor(name, list(shape), dtype)))

    def _semval(self, e, n):
        return n * 16 if (e[0] == "d" and e[1:].isdigit()) else n

    def _wait(self, e, deps):
        need = {}
        for (e2, n) in deps:
            if e2 == e and (e == "pe" or not SAME_ENG_SYNC):
                continue
            if self.clock[e].get(e2, 0) >= n:
                continue
            if need.get(e2, 0) < n:
                need[e2] = n
        for e2, n in need.items():
            if self.clock[e].get(e2, 0) >= n:
                continue
            self.q[e].append(("w", self.sem[e2], self._semval(e2, n)))
            self.nwaits += 1
            h = self.hist[e2].get(n)
            if h:
                for k, v in h.items():
                    if self.clock[e].get(k, 0) < v:
                        self.clock[e][k] = v
            self.clock[e][e2] = max(self.clock[e].get(e2, 0), n)

    def _deps(self, r, w):
        deps = []
        for d in r:
            d = d.d if isinstance(d, Buf) else d
            if d.w is not None:
                deps.append(d.w)
        for d in w:
            d = d.d if isinstance(d, Buf) else d
            if d.w is not None:
                deps.append(d.w)
            deps.extend(d.r)
        return deps

    def _commit(self, tag, r, w):
        for d in r:
            d = d.d if isinstance(d, Buf) else d
            d.r.append(tag)
            if len(d.r) > 64:
                best = {}
                for (e2, n) in d.r:
                    if best.get(e2, 0) < n:
                        best[e2] = n
                d.r = list(best.items())
        for d in w:
            d = d.d if isinstance(d, Buf) else d
            d.w = tag
            d.r = []

    def I(self, e, method, r=(), w=(), **kw):
        self._wait(e, self._deps(r, w))
        self.cnt[e] += 1
        n = self.cnt[e]
        self.q[e].append(("i", (method, kw), self.sem[e], 1))
        self.hist[e][n] = dict(self.clock[e])
        if e == "pe" or not SAME_ENG_SYNC:
            self.clock[e][e] = n
        self._commit((e, n), r, w)
        self.ninst += 1

    def dma(self, out, in_, r=(), w=(), q="sp", **kw):
        j = self.dma_k % NDMA
        self.dma_k += 1
        de = "d%d" % j
        prev = self.cnt[de]
        deps = self._deps(r, w)
        if prev > 0:
            deps.append((de, prev))
        self._wait(q, deps)
        self.q[q].append(("d", (out, in_, kw), self.sem[de], 16))
        self.cnt[de] = prev + 1
        self.hist[de][prev + 1] = dict(self.clock[q])
        self._commit((de, prev + 1), r, w)
        self.ninst += 1

    def coll(self, kind, ins, outs, r=(), w=()):
        q = "pool"
        j = self.dma_k % NDMA
        self.dma_k += 1
        de = "d%d" % j
        prev = self.cnt[de]
        deps = self._deps(r, w)
        if prev > 0:
            deps.append((de, prev))
        self._wait(q, deps)
        self.q[q].append(("c", (kind, ins, outs), self.sem[de], 16))
        self.cnt[de] = prev + 1
        self.hist[de][prev + 1] = dict(self.clock[q])
        self._commit((de, prev + 1), r, w)
        self.ninst += 1

    def finish(self, q="sp"):
        deps = []
        for j in range(NDMA):
            de = "d%d" % j
            if self.cnt[de] > 0:
                deps.append((de, self.cnt[de]))
        self._wait(q, deps)

        nc = self.nc
        prog = self
        with nc.Block() as block:
            def mk(e):
                def body(engh):
                    for it in prog.q[e]:
                        if it[0] == "w":
                            engh.wait_ge(it[1], it[2])
                        elif it[0] == "i":
                            getattr(engh, it[1][0])(**it[1][1]).then_inc(it[2], it[3])
                        elif it[0] == "c":
                            kind, ins, outs = it[1]
                            engh.collective_compute(kind, ALU.bypass, [[0,1,2,3],[4,5,6,7]], ins, outs).then_inc(it[2], it[3])
                        else:
                            o, i, kw = it[1]
                            engh.dma_start(out=o, in_=i, **kw).then_inc(it[2], it[3])
                return body
            block.tensor(mk("pe"))
            block.scalar(mk("act"))
            block.vector(mk("dve"))
            block.gpsimd(mk("pool"))
            block.sync(mk("sp"))
        self.es.close()


D = 2048
DIN = 14792
NCB = 13
NMIX = 6600
EPS = 1e-6
ROPE = {0: [(0, 8)], 1: [(0, 8)], 10: [(256, 4)], 11: [(0, 5), (384, 2)], 12: [(0, 7)]}


def build_L0(n):
    p = Prog()
    src = p.dram("src", [128, n], F32, "ExternalInput")
    dst = p.dram("dst", [128, n], BF16, "ExternalOutput")
    CH = 4096
    st = [p.sb("st%d" % i, [128, CH], F32) for i in range(3)]
    ob = [p.sb("ob%d" % i, [128, CH], BF16) for i in range(3)]
    k = 0
    for c0 in range(0, n, CH):
        c1 = min(n, c0 + CH)
        s, o = st[k % 3], ob[k % 3]
        p.dma(s[:, 0:c1 - c0], src[:, c0:c1], w=[s], q="sp")
        e = ("dve", "pool")[k % 2]
        p.I(e, "tensor_copy", r=[s], w=[o], out=o[:, 0:c1 - c0], in_=s[:, 0:c1 - c0])
        p.dma(dst[:, c0:c1], o[:, 0:c1 - c0], r=[o], q="act")
        k += 1
    p.finish()
    return p


def fm_rmsnorm(p, src, gt, gcol, dst, KC, T, ones, psl, sq, rstd, dim):
    nh = T // 512
    for kc in range(KC):
        s = sq[kc % 2]
        p.I("act", "activation", r=[src], w=[s], out=s[:, 0:T], in_=src[:, kc, :], func=AF.Square)
        for h in range(nh):
            p.I("pe", "matmul", r=[s, ones], w=[psl[h]], out=psl[h][:], lhsT=ones[:], rhs=s[:, h * 512:(h + 1) * 512],
                start=(kc == 0), stop=(kc == KC - 1))
    for h in range(nh):
        p.I("act", "activation", r=[psl[h], EPSB[0]], w=[rstd], out=rstd[:, h * 512:(h + 1) * 512], in_=psl[h][:],
            func=AF.Sqrt, scale=1.0 / dim, bias=EPSB[0][:, 0:1])
    p.I("dve", "reciprocal", r=[rstd], w=[rstd], out=rstd[:, 0:T], in_=rstd[:, 0:T])
    for kc in range(KC):
        p.I("dve", "scalar_tensor_tensor", r=[src, gt, rstd], w=[dst], out=dst[:, kc, :], in0=src[:, kc, :],
            scalar=gt[:, gcol + kc:gcol + kc + 1], in1=rstd[:, 0:T], op0=ALU.mult, op1=ALU.mult)


EPSB = [None]


def consts(p):
    ones = p.sb("ones", [128, 128], BF16)
    p.I("dve", "memset", w=[ones], ap=ones[:], constant=1.0)
    eb = p.sb("epsb", [128, 1], F32)
    p.I("dve", "memset", w=[eb], ap=eb[:], constant=EPS)
    EPSB[0] = eb
    return ones


def build_L1():
    p = Prog()
    T = 1024
    xT = p.dram("xT", [D, T], F32, "ExternalInput")
    g = p.dram("g", [128, 16], F32, "ExternalInput")
    wb = p.dram("wb", [NCB, 128, 8192], BF16, "ExternalInput")
    pos = p.dram("pos", [128, 8], I32, "ExternalInput")
    cf = p.dram("cf", [128, 32], F32, "ExternalInput")
    proj = p.dram("proj", [T, NCB * 512], F32, "ExternalOutput")
    ones = consts(p)
    xs = p.sb("xs", [128, 16, T], F32)
    hT = p.sb("hT", [128, 16, T], BF16)
    gt = p.sb("gt", [128, 16], F32)
    sq = [p.sb("sq%d" % i, [128, T], BF16) for i in range(2)]
    rstd = p.sb("rstd", [128, T], F32)
    psn = [p.ps("psn%d" % i, [128, 512], F32) for i in range(2)]
    pst = [p.ps("ps%d" % i, [128, 512], F32) for i in range(4)]
    xv = xT.rearrange("(kc p) t -> p kc t", p=128)
    for kc in range(0, 16, 4):
        p.dma(xs[:, kc:kc + 4, :], xv[:, kc:kc + 4, :], w=[xs], q=("sp", "act")[(kc // 4) % 2])
    p.dma(gt[:], g, w=[gt])
    posi = p.sb("posi", [128, 8], I32)
    posf = p.sb("posf", [128, 8], F32)
    cft = p.sb("cft", [128, 32], F32)
    p.dma(posi[:], pos, w=[posi])
    p.dma(cft[:], cf, w=[cft])
    p.I("dve", "tensor_copy", r=[posi], w=[posf], out=posf[:], in_=posi[:])
    qq = p.sb("qq", [128, 2, 8, 32], F32)
    qi = p.sb("qi", [128, 2, 8, 32], I32)
    qf = p.sb("qf", [128, 2, 8, 32], F32)
    msk = p.sb("msk", [128, 2, 8, 32], F32)
    sc = p.sb("sc", [128, 2, 8, 32], F32)
    for tt in range(8):
        p.I("dve", "tensor_scalar", r=[cft, posf], w=[qq], out=qq[:, 0, tt, :], in0=cft[:], scalar1=posf[:, tt:tt + 1],
            scalar2=None, op0=ALU.mult)
    p.I("dve", "tensor_scalar", r=[qq], w=[qq], out=qq[:, 1, :, :], in0=qq[:, 0, :, :], scalar1=0.25, scalar2=None, op0=ALU.add)
    p.I("dve", "tensor_copy", r=[qq], w=[qi], out=qi[:], in_=qq[:])
    p.I("dve", "tensor_copy", r=[qi], w=[qf], out=qf[:], in_=qi[:])
    p.I("dve", "tensor_tensor", r=[qq, qf], w=[qq], out=qq[:], in0=qq[:], in1=qf[:], op=ALU.subtract)
    p.I("dve", "tensor_scalar", r=[qq], w=[msk], out=msk[:], in0=qq[:], scalar1=0.5, scalar2=None, op0=ALU.is_gt)
    p.I("dve", "tensor_tensor", r=[qq, msk], w=[qq], out=qq[:], in0=qq[:], in1=msk[:], op=ALU.subtract)
    p.I("dve", "tensor_scalar", r=[qq], w=[msk], out=msk[:], in0=qq[:], scalar1=-0.5, scalar2=None, op0=ALU.is_lt)
    p.I("dve", "tensor_tensor", r=[qq, msk], w=[qq], out=qq[:], in0=qq[:], in1=msk[:], op=ALU.add)
    p.I("act", "activation", r=[qq], w=[sc], out=sc[:], in_=qq[:], func=AF.Sin, scale=6.28318)
    fm_rmsnorm(p, xs, gt, 0, hT, 16, T, ones, psn, sq, rstd, D)
    wt = [p.sb("w%d" % i, [128, 8192], BF16) for i in range(3)]
    ot = [p.sb("o%d" % i, [128, 512], F32) for i in range(4)]
    tmp = [p.sb("rt%d" % i, [128, 8, 32], F32) for i in range(4)]
    k = 0
    for cb in range(NCB):
        w = wt[cb % 3]
        p.dma(w[:, 0:4096], wb[cb, :, 0:4096], w=[w], q="sp")
        p.dma(w[:, 4096:8192], wb[cb, :, 4096:8192], w=[w], q="act")
        for tt in range(8):
            ps = pst[k % 4]
            o = ot[k % 4]
            k += 1
            for kc in range(16):
                p.I("pe", "matmul", r=[hT, w], w=[ps], out=ps[:], lhsT=hT[:, kc, tt * 128:(tt + 1) * 128],
                    rhs=w[:, kc * 512:(kc + 1) * 512], start=(kc == 0), stop=(kc == 15))
            p.I("act", "activation", r=[ps], w=[o], out=o[:], in_=ps[:], func=AF.Copy)
            for (s0, nh) in ROPE.get(cb, []):
                ov = o[:, s0:s0 + nh * 64].rearrange("p (h two d) -> p h two d", two=2, d=32)
                x1, x2 = ov[:, :, 0, :], ov[:, :, 1, :]
                sn = sc[:, 0, tt:tt + 1, :].to_broadcast([128, nh, 32])
                cs = sc[:, 1, tt:tt + 1, :].to_broadcast([128, nh, 32])
                t1, t2, t3, t4 = [t[:, 0:nh, :] for t in tmp]
                p.I("dve", "tensor_tensor", r=[o, sc], w=[tmp[0]], out=t1, in0=x1, in1=cs, op=ALU.mult)
                p.I("dve", "tensor_tensor", r=[o, sc], w=[tmp[1]], out=t2, in0=x2, in1=sn, op=ALU.mult)
                p.I("dve", "tensor_tensor", r=[o, sc], w=[tmp[2]], out=t3, in0=x2, in1=cs, op=ALU.mult)
                p.I("dve", "tensor_tensor", r=[o, sc], w=[tmp[3]], out=t4, in0=x1, in1=sn, op=ALU.mult)
                p.I("dve", "tensor_tensor", r=[tmp[0], tmp[1]], w=[o], out=x1, in0=t1, in1=t2, op=ALU.subtract)
                p.I("dve", "tensor_tensor", r=[tmp[2], tmp[3]], w=[o], out=x2, in0=t3, in1=t4, op=ALU.add)
            p.dma(proj[tt * 128:(tt + 1) * 128, cb * 512:(cb + 1) * 512], o[:], r=[o], q="sp")
    p.finish()
    return p


def prep_w_in(wbf):
    w = np.zeros((D, NCB * 512), dtype=wbf.dtype)
    w[:, :NMIX] = wbf[:, :NMIX]
    w = w.reshape(16, 128, NCB, 512).transpose(2, 1, 0, 3).reshape(NCB, 128, 8192)
    return np.ascontiguousarray(w)


def cf_const():
    inv = 10000.0 ** (-np.arange(0, 64, 2, dtype=np.float64) / 64.0)
    c = (inv / (2 * math.pi)).astype(np.float32)
    return np.ascontiguousarray(np.broadcast_to(c[None, :], (128, 32)))


def build_L3(T=2048):
    p = Prog()
    H = 512
    xT = p.dram("xT", [D, T], F32, "ExternalInput")
    yT = p.dram("yT", [4, 512, T], F32, "ExternalInput")
    wg = p.dram("wg", [64, 128, 2048], BF16, "ExternalInput")
    g3 = p.dram("g3", [128, 64], F32, "ExternalInput")
    wbr = p.dram("wbr", [16, 128, 2048], BF16, "ExternalInput")
    wout = p.dram("wout", [16, 128, 2048], BF16, "ExternalInput")
    w1 = p.dram("w1", [64, 128, 2048], BF16, "ExternalInput")
    w2 = p.dram("w2", [16, 4, 128, 2048], BF16, "ExternalInput")
    xo = p.dram("xo", [D, T], F32, "ExternalOutput")
    ones = consts(p)
    xh = p.sb("xh", [128, 16, H], F32)
    zT = p.sb("zT", [128, 16, H], F32)
    mb = p.sb("mb", [128, 16, H], BF16)
    uT = p.sb("uT", [128, 64, H], BF16)
    gt = p.sb("gt", [128, 64], F32)
    hT = p.sb("hT", [128, 16, H], BF16)
    sq = [p.sb("sq%d" % i, [128, H], BF16) for i in range(2)]
    rstd = p.sb("rstd", [128, H], F32)
    psn = [p.ps("psn0", [128, 512], F32)]
    pst = [p.ps("ps%d" % i, [128, 512], F32) for i in range(4)]
    wt = [p.sb("wt%d" % i, [128, 2048], BF16) for i in range(4)]
    psg = [p.ps("psg%d" % i, [128, 512], F32) for i in range(2)]
    sg = [p.sb("sg%d" % i, [128, H], F32) for i in range(2)]
    tm = [p.sb("tm%d" % i, [128, H], F32) for i in range(2)]
    ys = [p.sb("ys%d" % i, [128, 4, H], F32) for i in range(1)]
    wgp = [p.sb("wgp%d" % i, [128, 2048], BF16) for i in range(2)]
    gk = 0
    p.dma(gt[:], g3, w=[gt])
    xv = xT.rearrange("(kc p) t -> p kc t", p=128)
    xov = xo.rearrange("(kc p) t -> p kc t", p=128)
    yv = yT.rearrange("n (kc p) t -> n p kc t", p=128)
    wk = 0
    pk = 0
    for hf in range(T // H):
        tsl = slice(hf * H, (hf + 1) * H)
        for kc in range(0, 16, 8):
            p.dma(xh[:, kc:kc + 8, :], xv[:, kc:kc + 8, tsl], w=[xh], q="act")
        fm_rmsnorm(p, xh, gt, 0, hT, 16, H, ones, psn, sq, rstd, D)
        for n in range(4):
            y = ys[0]
            p.dma(y[:], yv[n, :, :, tsl], w=[y], q="act")
            p.I("dve", "tensor_copy", r=[y], w=[uT], out=uT[:, n * 4:(n + 1) * 4, :], in_=y[:])
        for oc in range(16):
            w = wt[wk % 4]
            wk += 1
            p.dma(w[:], wbr[oc], w=[w], q="sp")
            for n in range(4):
                wgt = wgp[gk % 2]
                gk += 1
                p.dma(wgt[:], wg[oc * 4 + n], w=[wgt], q="act")
                pg = psg[n % 2]
                for kc in range(16):
                    p.I("pe", "matmul", r=[hT, wgt], w=[pg], out=pg[:], lhsT=wgt[:, kc * 128:(kc + 1) * 128], rhs=hT[:, kc, :],
                        start=(kc == 0), stop=(kc == 15))
                ps = pst[pk % 4]
                pk += 1
                for kc in range(4):
                    p.I("pe", "matmul", r=[uT, w], w=[ps], out=ps[:], lhsT=w[:, (n * 4 + kc) * 128:(n * 4 + kc + 1) * 128],
                        rhs=uT[:, n * 4 + kc, :], start=(kc == 0), stop=(kc == 3))
                s = sg[n % 2]
                p.I("act", "activation", r=[pg], w=[s], out=s[:], in_=pg[:], func=AF.Sigmoid)
                if n == 0:
                    p.I("dve", "tensor_tensor", r=[ps, s], w=[zT], out=zT[:, oc, :], in0=ps[:], in1=s[:], op=ALU.mult)
                else:
                    t = tm[n % 2]
                    p.I("dve", "tensor_tensor", r=[ps, s], w=[t], out=t[:], in0=ps[:], in1=s[:], op=ALU.mult)
                    p.I("pool", "tensor_tensor", r=[t, zT], w=[zT], out=zT[:, oc, :], in0=zT[:, oc, :], in1=t[:], op=ALU.add)
            p.I("pool", "tensor_copy", r=[zT], w=[mb], out=mb[:, oc, :], in_=zT[:, oc, :])
        for oc in range(16):
            w = wt[wk % 4]
            wk += 1
            p.dma(w[:], wout[oc], w=[w], q="sp")
            ps = pst[pk % 4]
            pk += 1
            for kc in range(16):
                p.I("pe", "matmul", r=[mb, w], w=[ps], out=ps[:], lhsT=w[:, kc * 128:(kc + 1) * 128], rhs=mb[:, kc, :],
                    start=(kc == 0), stop=(kc == 15))
            p.I("act", "activation", r=[ps], w=[zT], out=zT[:, oc, :], in_=ps[:], func=AF.Copy)
        fm_rmsnorm(p, zT, gt, 16, zT, 16, H, ones, psn, sq, rstd, D)
        for kc in range(0, 16, 4):
            p.I("pool", "tensor_tensor", r=[xh, zT], w=[xh], out=xh[:, kc:kc + 4, :], in0=xh[:, kc:kc + 4, :], in1=zT[:, kc:kc + 4, :], op=ALU.add)
        fm_rmsnorm(p, xh, gt, 32, mb, 16, H, ones, psn, sq, rstd, D)
        for oc in range(64):
            w = wt[wk % 4]
            wk += 1
            p.dma(w[:], w1[oc], w=[w], q=("sp", "act")[oc % 2])
            ps = pst[pk % 4]
            pk += 1
            for kc in range(16):
                p.I("pe", "matmul", r=[mb, w], w=[ps], out=ps[:], lhsT=w[:, kc * 128:(kc + 1) * 128], rhs=mb[:, kc, :],
                    start=(kc == 0), stop=(kc == 15))
            t = tm[oc % 2]
            p.I("act", "activation", r=[ps], w=[t], out=t[:], in_=ps[:], func=AF.Relu)
            p.I(("dve", "pool")[oc % 2], "tensor_tensor", r=[t], w=[uT], out=uT[:, oc, :], in0=t[:], in1=t[:], op=ALU.mult)
        for oc in range(16):
            ps = pst[pk % 4]
            pk += 1
            for q in range(4):
                w = wt[wk % 4]
                wk += 1
                p.dma(w[:], w2[oc, q], w=[w], q=("sp", "act")[q % 2])
                for kc in range(16):
                    p.I("pe", "matmul", r=[uT, w], w=[ps], out=ps[:], lhsT=w[:, kc * 128:(kc + 1) * 128], rhs=uT[:, q * 16 + kc, :],
                        start=(q == 0 and kc == 0), stop=(q == 3 and kc == 15))
            p.I("act", "activation", r=[ps], w=[zT], out=zT[:, oc, :], in_=ps[:], func=AF.Copy)
        fm_rmsnorm(p, zT, gt, 48, zT, 16, H, ones, psn, sq, rstd, D)
        for kc in range(0, 16, 4):
            p.I("pool", "tensor_tensor", r=[xh, zT], w=[xh], out=xh[:, kc:kc + 4, :], in0=xh[:, kc:kc + 4, :], in1=zT[:, kc:kc + 4, :], op=ALU.add)
        for kc in range(0, 16, 8):
            p.dma(xov[:, kc:kc + 8, tsl], xh[:, kc:kc + 8, :], r=[xh], q="sp")
    p.finish()
    return p


def prep_wg(wgate):
    g = wgate.reshape(16, 128, 4, 16, 128).transpose(3, 2, 1, 0, 4).reshape(64, 128, 2048)
    return np.ascontiguousarray(g)


def prep_L3_weights(wbr, wout, w1, w2):
    a = wbr.reshape(4, 4, 128, 16, 128).transpose(3, 2, 0, 1, 4).reshape(16, 128, 2048)
    b = wout.reshape(16, 128, 16, 128).transpose(2, 1, 0, 3).reshape(16, 128, 2048)
    c = w1.reshape(16, 128, 64, 128).transpose(2, 1, 0, 3).reshape(64, 128, 2048)
    d = w2.reshape(4, 16, 128, 16, 128).transpose(3, 0, 2, 1, 4).reshape(16, 4, 128, 2048)
    return [np.ascontiguousarray(t) for t in (a, b, c, d)]


S = 4096


def build_A(layer):
    p = Prog()
    lam_init = 0.8 - 0.6 * math.exp(-0.3 * layer)
    qT = p.dram("qT", [2, 64, S], F32, "ExternalInput")
    kT = p.dram("kT", [2, 64, S], F32, "ExternalInput")
    v = p.dram("v", [S, 128], F32, "ExternalInput")
    lam4 = p.dram("lam4", [128, 256], F32, "ExternalInput")
    sg = p.dram("sg", [128, 128], F32, "ExternalInput")
    masks = p.dram("masks", [4, 128, 512], BF16, "ExternalInput")
    ya = p.dram("ya", [S, 128], F32, "ExternalOutput")
    qb = [p.sb("qb%d" % m, [64, S], BF16) for m in range(2)]
    kb = [p.sb("kb%d" % m, [64, S], BF16) for m in range(2)]
    vb = p.sb("vb", [128, 32, 129], BF16)
    st = [p.sb("st%d" % i, [128, 4096], F32) for i in range(2)]
    mk_ = p.sb("mk", [128, 4, 512], BF16)
    lt = p.sb("lt", [128, 256], F32)
    sgt = p.sb("sgt", [128, 128], F32)
    epsb = p.sb("epsb", [128, 1], F32)
    p.I("dve", "memset", w=[epsb], ap=epsb[:], constant=1e-6)
    k = 0
    for m in range(2):
        for (src, dst) in ((qT, qb[m]), (kT, kb[m])):
            s = st[k % 2]
            k += 1
            p.dma(s[0:64, :], src[m], w=[s])
            p.I(("dve", "pool")[k % 2], "tensor_copy", r=[s], w=[dst], out=dst[:], in_=s[0:64, :])
    s = st[k % 2]
    k += 1
    p.dma(s[:].rearrange("p (t e) -> p t e", e=128), v.rearrange("(t p) e -> p t e", p=128), w=[s])
    p.I("pool", "memset", w=[vb], ap=vb[:], constant=1.0)
    p.I("dve", "tensor_copy", r=[s], w=[vb], out=vb[:, :, 0:128], in_=s[:].rearrange("p (t e) -> p t e", e=128))
    p.dma(mk_[:], masks.rearrange("j p f -> p j f"), w=[mk_])
    p.dma(lt[:], lam4, w=[lt])
    p.dma(sgt[:], sg, w=[sgt])
    pr = p.sb("pr", [128, 2, 64], F32)
    s12 = p.sb("s12", [128, 2], F32)
    e12 = p.sb("e12", [128, 2], F32)
    nlam = p.sb("nlam", [128, 1], F32)
    ltv = lt[:].rearrange("p (a d) -> p a d", d=64)
    p.I("dve", "tensor_tensor", r=[lt], w=[pr], out=pr[:, 0, :], in0=ltv[:, 0, :], in1=ltv[:, 1, :], op=ALU.mult)
    p.I("dve", "tensor_tensor", r=[lt], w=[pr], out=pr[:, 1, :], in0=ltv[:, 2, :], in1=ltv[:, 3, :], op=ALU.mult)
    p.I("dve", "tensor_reduce", r=[pr], w=[s12], out=s12[:], in_=pr[:], axis=AX.X, op=ALU.add)
    p.I("act", "activation", r=[s12], w=[e12], out=e12[:], in_=s12[:], func=AF.Exp)
    p.I("dve", "tensor_tensor", r=[e12], w=[nlam], out=nlam[:], in0=e12[:, 1:2], in1=e12[:, 0:1], op=ALU.subtract)
    p.I("dve", "tensor_scalar", r=[nlam], w=[nlam], out=nlam[:], in0=nlam[:], scalar1=-lam_init, scalar2=None, op0=ALU.add)
    p.I("dve", "tensor_scalar", r=[sgt], w=[sgt], out=sgt[:], in0=sgt[:], scalar1=1.0 - lam_init, scalar2=None, op0=ALU.mult)
    pss = [p.ps("pss%d" % i, [128, 512], F32) for i in range(3)]
    psa = [p.ps("psa%d" % i, [128, 512], F32) for i in range(3)]
    pts = p.sb("pts", [128, 32, 512], BF16)
    ob = [p.sb("ob%d" % i, [128, 4, 128], F32) for i in range(2)]
    sm = [p.sb("sm%d" % i, [128, 4], F32) for i in range(3)]
    junk = p.sb("junk", [128, 128], F32)
    ks = 0
    ka = 0
    for qblk in range(8):
        Q0 = qblk * 512
        nkt = 4 * qblk + 4
        o = ob[qblk % 2]
        for m in range(2):
            for kt in range(nkt):
                ps = pss[ks % 3]
                ks += 1
                p.I("pe", "matmul", r=[kb[m], qb[m]], w=[ps], out=ps[:], lhsT=kb[m][:, kt * 128:(kt + 1) * 128],
                    rhs=qb[m][:, Q0:Q0 + 512], start=True, stop=True)
                dpt = pts.reg(kt)
                p.I("act", "activation", r=[ps], w=[dpt], out=pts[:, kt, :], in_=ps[:], func=AF.Exp, scale=0.125)
                if kt >= 4 * qblk:
                    p.I("pool", "tensor_tensor", r=[dpt, mk_], w=[dpt], out=pts[:, kt, :], in0=pts[:, kt, :],
                        in1=mk_[:, kt - 4 * qblk, :], op=ALU.mult)
            for j in range(4):
                pa = psa[ka % 3]
                ka += 1
                for kt in range(nkt):
                    p.I("pe", "matmul", r=[pts.reg(kt), vb], w=[pa], out=pa[:, 0:129], lhsT=pts[:, kt, j * 128:(j + 1) * 128],
                        rhs=vb[:, kt, :], start=(kt == 0), stop=(kt == nkt - 1))
                r = sm[ka % 3]
                p.I("dve", "reciprocal", r=[pa], w=[r], out=r[:, 0:1], in_=pa[:, 128:129])
                if m == 0:
                    p.I("dve", "tensor_scalar", r=[pa, r], w=[o], out=o[:, j, :], in0=pa[:, 0:128], scalar1=r[:, 0:1],
                        scalar2=None, op0=ALU.mult)
                else:
                    p.I("dve", "tensor_tensor", r=[r, nlam], w=[r], out=r[:, 1:2], in0=r[:, 0:1], in1=nlam[:], op=ALU.mult)
                    p.I("dve", "scalar_tensor_tensor", r=[pa, r, o], w=[o], out=o[:, j, :], in0=pa[:, 0:128],
                        scalar=r[:, 1:2], in1=o[:, j, :], op0=ALU.mult, op1=ALU.add)
                    p.I("act", "activation", r=[o], w=[junk, r], out=junk[:], in_=o[:, j, :], func=AF.Square,
                        accum_out=r[:, 2:3])
                    p.I("act", "activation", r=[r, epsb], w=[r], out=r[:, 3:4], in_=r[:, 2:3], func=AF.Sqrt, scale=1.0 / 128,
                        bias=epsb[:, 0:1])
                    p.I("dve", "reciprocal", r=[r], w=[r], out=r[:, 3:4], in_=r[:, 3:4])
                    p.I("dve", "scalar_tensor_tensor", r=[o, r, sgt], w=[o], out=o[:, j, :], in0=o[:, j, :],
                        scalar=r[:, 3:4], in1=sgt[:], op0=ALU.mult, op1=ALU.mult)
        p.dma(ya[Q0:Q0 + 512, :].rearrange("(j p) e -> p j e", p=128), o[:], r=[o])
    p.finish()
    return p


def mask_const():
    m = np.zeros((4, 128, 512), np.float32)
    pp = np.arange(128)[:, None] // 64
    ff = np.arange(512)[None, :] // 64
    for j in range(4):
        m[j] = ((2 * j + pp) <= ff)
    return m.astype(BF)


S = 4096
NCH = 64
EM05 = math.exp(-0.5)
GN_EPS = 64e-5


def build_B(stop=99):
    p = Prog()
    rkvT = p.dram("rkvT", [3, 128, S], F32, "ExternalInput")
    lowT = p.dram("lowT", [256, S], F32, "ExternalInput")
    chp = p.dram("chp", [128, 16], F32, "ExternalInput")
    wup = p.dram("wup", [128, 128], F32, "ExternalInput")
    gup = p.dram("gup", [128, 128], F32, "ExternalInput")
    cst = p.dram("cst", [128, 1152], F32, "ExternalInput")
    ybT = p.dram("ybT", [128, S], F32, "ExternalOutput")

    ch = p.sb("ch", [128, 16], F32)
    cs = p.sb("cs", [128, 1152], F32)
    wupf = p.sb("wupf", [128, 128], F32)
    gupf = p.sb("gupf", [128, 128], F32)
    wupb = p.sb("wupb", [128, 128], BF16)
    gupb = p.sb("gupb", [128, 128], BF16)
    bones = p.sb("bones", [128, 128], BF16)
    p.dma(ch[:], chp, w=[ch])
    p.dma(cs[:], cst, w=[cs])
    p.dma(wupf[:], wup, w=[wupf])
    p.dma(gupf[:], gup, w=[gupf])
    p.I("dve", "tensor_copy", r=[wupf], w=[wupb], out=wupb[:], in_=wupf[:])
    p.I("dve", "tensor_copy", r=[gupf], w=[gupb], out=gupb[:], in_=gupf[:])
    p.I("dve", "tensor_copy", r=[cs], w=[bones], out=bones[:], in_=cs[:, 0:128])
    ident = cs[:, 128:256]
    rmask = cs[:, 256:768]
    epsb = p.sb("epsb", [128, 2], F32)
    p.I("dve", "memset", w=[epsb], ap=epsb[:, 0:1], constant=GN_EPS)
    p.I("dve", "memset", w=[epsb], ap=epsb[:, 1:2], constant=0.0)

    ARd = p.nc.dram_tensor("ARd", [128, NCH * 128], BF16, kind="Internal").ap()
    BKd = p.nc.dram_tensor("BKd", [128, NCH * 128], BF16, kind="Internal").ap()
    PCd = p.nc.dram_tensor("PCd", [128, NCH], F32, kind="Internal").ap()
    yd2 = p.nc.dram_tensor("yd2", [128, S], F32, kind="Internal").ap()
    dARd, dBKd, dPCd, dyd2 = Dep(), Dep(), Dep(), Dep()
    ARs = [p.sb("ARs%d" % i, [128, 8, 2, 64], BF16) for i in range(2)]
    BKs = [p.sb("BKs%d" % i, [128, 8, 2, 64], BF16) for i in range(2)]
    Bh = p.sb("Bh", [64, NCH, 2, 64], BF16)
    Kh = p.sb("Kh", [64, NCH, 2, 64], BF16)
    Vh = p.sb("Vh", [64, NCH, 2, 64], BF16)
    PC = p.sb("PC", [128, NCH], F32)
    bonus = p.sb("bonus", [128, S], BF16)
    gT = p.sb("gT", [128, S], BF16)

    NT = 12
    tf = [p.sb("tf%d" % i, [128, 512], F32) for i in range(NT)]
    tb = [p.sb("tb%d" % i, [128, 512], BF16) for i in range(4)]
    xin = [p.sb("xin%d" % i, [128, 513], F32) for i in range(5)]
    ps = [p.ps("ps%d" % i, [128, 512], F32) for i in range(8)]

    MU_R, MU_K, MU_V, MU_WA, MU_G, W0, A0, KKG, KAG, RK, GNG, GNB = range(12)

    def col(i):
        return ch[:, i:i + 1]

    for blk in range(8):
        t0 = blk * 512
        srcs = [rkvT[0], rkvT[1], rkvT[2], lowT[0:128], lowT[128:256]]
        for i in range(5):
            if blk == 0:
                p.I("pool", "memset", w=[xin[i]], ap=xin[i][:, 0:1], constant=0.0)
                p.dma(xin[i][:, 1:513], srcs[i][:, 0:512], w=[xin[i]], q=("sp", "act")[i % 2])
            else:
                p.dma(xin[i][:], srcs[i][:, t0 - 1:t0 + 512], w=[xin[i]], q=("sp", "act")[i % 2])
        for i in range(5):
            d = tf[5]
            p.I("dve", "tensor_tensor", r=[xin[i]], w=[d], out=d[:], in0=xin[i][:, 0:512], in1=xin[i][:, 1:513], op=ALU.subtract)
            p.I("dve", "scalar_tensor_tensor", r=[d, ch, xin[i]], w=[tf[i]], out=tf[i][:], in0=d[:], scalar=col(MU_R + i),
                in1=xin[i][:, 1:513], op0=ALU.mult, op1=ALU.add)
        r_, k_, v_, wa_, gd_ = tf[0], tf[1], tf[2], tf[3], tf[4]
        p.I("act", "activation", r=[wa_], w=[tb[0]], out=tb[0][0:64, :], in_=wa_[0:64, :], func=AF.Tanh)
        p.I("act", "activation", r=[wa_], w=[tb[0]], out=tb[0][64:128, :], in_=wa_[64:128, :], func=AF.Copy)
        p.I("act", "activation", r=[gd_], w=[tb[1]], out=tb[1][:], in_=gd_[:], func=AF.Sigmoid)
        p.I("pe", "matmul", r=[wupb, tb[0]], w=[ps[0]], out=ps[0][:], lhsT=wupb[0:64, :], rhs=tb[0][0:64, :], start=True, stop=True)
        p.I("pe", "matmul", r=[wupb, tb[0]], w=[ps[1]], out=ps[1][:], lhsT=wupb[64:128, :], rhs=tb[0][64:128, :], start=True, stop=True)
        p.I("pe", "matmul", r=[gupb, tb[1]], w=[ps[2]], out=ps[2][:], lhsT=gupb[:], rhs=tb[1][:], start=True, stop=True)
        dl, a_ = tf[5], tf[6]
        p.I("act", "activation", r=[ps[0], ch], w=[dl], out=dl[:], in_=ps[0][:], func=AF.Sigmoid, bias=col(W0))
        p.I("dve", "tensor_scalar", r=[dl], w=[dl], out=dl[:], in0=dl[:], scalar1=-EM05, scalar2=None, op0=ALU.mult)
        p.I("act", "activation", r=[ps[1], ch], w=[a_], out=a_[:], in_=ps[1][:], func=AF.Sigmoid, bias=col(A0))
        p.I("act", "activation", r=[ps[2]], w=[gT], out=gT[:, t0:t0 + 512], in_=ps[2][:], func=AF.Copy)
        kk, kap = tf[7], tf[8]
        p.I("dve", "tensor_scalar", r=[k_, ch], w=[kk], out=kk[:], in0=k_[:], scalar1=col(KKG), scalar2=None, op0=ALU.mult)
        p.I("pool", "tensor_tensor", r=[kk], w=[tb[2]], out=tb[2][:], in0=kk[:], in1=kk[:], op=ALU.mult)
        p.I("pe", "matmul", r=[bones, tb[2]], w=[ps[3]], out=ps[3][:], lhsT=bones[:], rhs=tb[2][:], start=True, stop=True)
        rn = tf[9]
        p.I("act", "activation", r=[ps[3], epsb], w=[rn], out=rn[:], in_=ps[3][:], func=AF.Sqrt, bias=epsb[:, 1:2])
        p.I("dve", "tensor_scalar", r=[rn], w=[rn], out=rn[:], in0=rn[:], scalar1=1e-12, scalar2=None, op0=ALU.max)
        p.I("dve", "reciprocal", r=[rn], w=[rn], out=rn[:], in_=rn[:])
        p.I("dve", "tensor_tensor", r=[kk, rn], w=[kap], out=kap[:], in0=kk[:], in1=rn[:], op=ALU.mult)
        km = tf[7]
        p.I("dve", "tensor_scalar", r=[a_, ch], w=[tf[9]], out=tf[9][:], in0=a_[:], scalar1=-1.0, scalar2=col(KAG), op0=ALU.add, op1=ALU.mult)
        p.I("dve", "scalar_tensor_tensor", r=[tf[9], k_], w=[km], out=km[:], in0=tf[9][:], scalar=1.0, in1=k_[:], op0=ALU.add, op1=ALU.mult)
        p.I("dve", "scalar_tensor_tensor", r=[r_, ch, km], w=[tb[3]], out=tb[3][:], in0=r_[:], scalar=col(RK), in1=km[:], op0=ALU.mult, op1=ALU.mult)
        p.I("pe", "matmul", r=[bones, tb[3]], w=[ps[4]], out=ps[4][:], lhsT=bones[:], rhs=tb[3][:], start=True, stop=True)
        p.I("dve", "tensor_tensor", r=[ps[4], v_], w=[bonus], out=bonus[:, t0:t0 + 512], in0=ps[4][:], in1=v_[:], op=ALU.mult)
        L = tf[9]
        p.I("dve", "tensor_tensor_scan", r=[cs, dl], w=[L], out=L[:], data0=rmask, data1=dl[:], initial=0.0, op0=ALU.mult, op1=ALU.add)
        P_, Pp, Pi, E_ = tf[10], tf[11], tf[1], tf[4]
        p.I("act", "activation", r=[L], w=[P_], out=P_[:], in_=L[:], func=AF.Exp)
        p.I("dve", "tensor_tensor", r=[L, dl], w=[Pp], out=Pp[:], in0=L[:], in1=dl[:], op=ALU.subtract)
        p.I("act", "activation", r=[Pp], w=[Pp], out=Pp[:], in_=Pp[:], func=AF.Exp)
        p.I("act", "activation", r=[L], w=[Pi], out=Pi[:], in_=L[:], func=AF.Exp, scale=-1.0)
        Lv = L[:].rearrange("p (c t) -> p c t", t=64)
        p.I("dve", "tensor_tensor", r=[L], w=[E_], out=E_[:].rearrange("p (c t) -> p c t", t=64), in0=Lv,
            in1=Lv[:, :, 63:64].to_broadcast([128, 8, 64]), op=ALU.subtract)
        p.I("act", "activation", r=[E_], w=[E_], out=E_[:], in_=E_[:], func=AF.Exp, scale=-1.0)
        p.I("pool", "tensor_copy", r=[P_], w=[PC], out=PC[:, blk * 8:(blk + 1) * 8],
            in_=P_[:].rearrange("p (c t) -> p c t", t=64)[:, :, 63])
        csl = slice(0, 8)
        AR, BK = ARs[blk % 2], BKs[blk % 2]
        v3 = lambda t: t[:].rearrange("p (c t) -> p c t", t=64)
        p.I("dve", "scalar_tensor_tensor", r=[kap, Pp], w=[AR], out=AR[:, csl, 0, :], in0=v3(kap), scalar=-1.0, in1=v3(Pp), op0=ALU.mult, op1=ALU.mult)
        p.I("pool", "tensor_tensor", r=[r_, P_], w=[AR], out=AR[:, csl, 1, :], in0=v3(r_), in1=v3(P_), op=ALU.mult)
        ka = tf[5]
        p.I("dve", "tensor_tensor", r=[kap, a_], w=[ka], out=ka[:], in0=kap[:], in1=a_[:], op=ALU.mult)
        p.I("dve", "tensor_tensor", r=[ka, Pi], w=[BK], out=BK[:, csl, 0, :], in0=v3(ka), in1=v3(Pi), op=ALU.mult)
        p.I("pool", "tensor_tensor", r=[km, Pi], w=[BK], out=BK[:, csl, 1, :], in0=v3(km), in1=v3(Pi), op=ALU.mult)
        p.dma(ARd[:, blk * 1024:(blk + 1) * 1024], AR[:].rearrange("p c a k -> p (c a k)"), r=[AR], w=[dARd])
        p.dma(BKd[:, blk * 1024:(blk + 1) * 1024], BK[:].rearrange("p c a k -> p (c a k)"), r=[BK], w=[dBKd], q="act")
        Bf, Kf = tf[6], tf[8]
        p.I("dve", "tensor_tensor", r=[ka, E_], w=[Bf], out=Bf[:], in0=ka[:], in1=E_[:], op=ALU.mult)
        p.I("pool", "tensor_tensor", r=[km, E_], w=[Kf], out=Kf[:], in0=km[:], in1=E_[:], op=ALU.mult)
        for (src, dst, pi) in ((Bf, Bh, 5), (Kf, Kh, 6), (v_, Vh, 7)):
            for half in range(2):
                pt = ps[pi] if half == 0 else ps[(pi + 3) % 8 if pi != 7 else 0]
                for c4 in range(4):
                    c = half * 4 + c4
                    p.I("pe", "transpose", r=[src, cs], w=[pt], out=pt[0:64, c4 * 128:(c4 + 1) * 128], in_=src[:, c * 64:(c + 1) * 64], identity=ident)
                p.I("act", "activation", r=[pt], w=[dst], out=dst[:, blk * 8 + half * 4:blk * 8 + half * 4 + 4, :, :],
                    in_=pt[0:64, :].rearrange("p (c h k) -> p c h k", c=4, h=2), func=AF.Copy)

    p.dma(PCd, PC[:], r=[PC], w=[dPCd])
    if stop == 1:
        p.finish()
        return p
    m5 = cs[0:64, 768:1088]
    eye = cs[0:64, 1088:1152]
    mstrict = cs[0:64, 768:832]
    mlower = cs[0:64, 1024:1088]
    m4 = cs[0:64, 768:1024]
    ARx = p.sb("ARx", [64, NCH, 2, 64], BF16)
    BKx = p.sb("BKx", [64, NCH, 2, 64], BF16)
    PCx = p.sb("PCx", [64, NCH], F32)
    TT = p.sb("TT", [64, NCH, 64], BF16)
    yTh = p.sb("yTh", [64, S], F32)
    LM = [[p.sb("LM%d_%d" % (s_, i), [64, 2, 64], F32) for i in range(2)] for s_ in range(8)]
    XX = [[p.sb("XX%d_%d" % (s_, i), [64, 64], F32) for i in range(2)] for s_ in range(8)]
    S32 = p.sb("S32", [64, 64], F32)
    Sb = p.sb("Sb", [64, 64], BF16)
    Am = [p.sb("Am%d" % i, [64, 4, 64], BF16) for i in range(2)]
    Zb = [p.sb("Zb%d" % i, [64, 64], BF16) for i in range(2)]
    Ub = [p.sb("Ub%d" % i, [64, 64], BF16) for i in range(2)]
    for hd in range(2):
        hs = slice(hd * 64, (hd + 1) * 64)
        p.dma(ARx[:].rearrange("p n a k -> p (n a k)"), ARd[hs, :], r=[dARd], w=[ARx])
        p.dma(BKx[:].rearrange("p n a k -> p (n a k)"), BKd[hs, :], r=[dBKd], w=[BKx], q="act")
        p.dma(PCx[:], PCd[hs, :], r=[dPCd], w=[PCx])
        for n in range(NCH):
            s_ = n % 8
            pa, pb = ps[s_], ps[s_]
            lm0, x0 = LM[s_][0], XX[s_][0]
            p.I("pe", "matmul", r=[BKx, ARx], w=[pa], out=pa[0:64, 64:128], lhsT=BKx[:, n, 0, :], rhs=ARx[:, n, 0, :], start=True, stop=True)
            p.I("pe", "matmul", r=[BKx, ARx], w=[pa], out=pa[0:64, 0:64], lhsT=ARx[:, n, 0, :], rhs=BKx[:, n, 0, :], start=True, stop=True)
            p.I("dve", "tensor_tensor", r=[pa, cs], w=[lm0], out=lm0[:, 0, :], in0=pa[0:64, 0:64], in1=mlower, op=ALU.mult)
            p.I("dve", "tensor_tensor", r=[pa, cs], w=[lm0], out=lm0[:, 1, :], in0=pa[0:64, 64:128], in1=mstrict, op=ALU.mult)
            p.I("dve", "tensor_tensor", r=[lm0, cs], w=[x0], out=x0[:], in0=lm0[:, 1, :], in1=eye, op=ALU.add)
            cur = 0
            for j in range(1, 6):
                lmp, lmn = LM[s_][cur], LM[s_][1 - cur]
                xp, xn = XX[s_][cur], XX[s_][1 - cur]
                p.I("pe", "matmul", r=[lmp], w=[pa], out=pa[0:64, 0:64], lhsT=lmp[:, 1, :], rhs=lmp[:, 0, :], start=True, stop=True)
                if j < 5:
                    p.I("pe", "matmul", r=[lmp], w=[pa], out=pa[0:64, 64:128], lhsT=lmp[:, 0, :], rhs=lmp[:, 1, :], start=True, stop=True)
                p.I("act", "activation", r=[pa], w=[lmn], out=lmn[:].rearrange("p two k -> p (two k)"), in_=pa[0:64, 0:128], func=AF.Copy)
                p.I("pe", "matmul", r=[lmn, xp], w=[pb], out=pb[0:64, 128:192], lhsT=lmn[:, 0, :], rhs=xp[:], start=True, stop=True)
                p.I("dve", "tensor_tensor", r=[pb, xp], w=[xn], out=xn[:], in0=pb[0:64, 128:192], in1=xp[:], op=ALU.add)
                cur = 1 - cur
            p.I("pool", "tensor_copy", r=[XX[s_][cur]], w=[TT.reg(n)], out=TT[:, n, :], in_=XX[s_][cur][:])
        if stop == 2 + 2 * hd:
            p.finish()
            return p
        p.I("dve", "memset", w=[S32], ap=S32[:], constant=0.0)
        p.I("pool", "memset", w=[Sb], ap=Sb[:], constant=0.0)
        for n in range(NCH):
            am, zb, ub = Am[n % 2], Zb[n % 2], Ub[n % 2]
            o4 = 5 * (n % 2)
            pg, pz, pu, py, pS = ps[o4], ps[o4 + 1], ps[o4 + 2], ps[3], ps[4]
            p.I("pe", "matmul", r=[BKx, ARx], w=[pg], out=pg[0:64, 0:128], lhsT=BKx[:, n, 0, :], rhs=ARx[:, n, :, :], start=True, stop=True)
            p.I("pe", "matmul", r=[BKx, ARx], w=[pg], out=pg[0:64, 128:256], lhsT=BKx[:, n, 1, :], rhs=ARx[:, n, :, :], start=True, stop=True)
            p.I("dve", "tensor_tensor", r=[pg, cs], w=[am], out=am[:].rearrange("p q t -> p (q t)"), in0=pg[0:64, 0:256], in1=m4, op=ALU.mult)
            p.I("pe", "matmul", r=[am, Vh], w=[pz], out=pz[0:64, 0:64], lhsT=am[:, 2, :], rhs=Vh[:, n, hd, :], start=True, stop=False)
            p.I("pe", "matmul", r=[ARx, Sb], w=[pz], out=pz[0:64, 0:64], lhsT=ARx[:, n, 0, :], rhs=Sb[:], start=False, stop=True)
            p.I("act", "activation", r=[pz], w=[zb], out=zb[:], in_=pz[0:64, 0:64], func=AF.Copy)
            p.I("pe", "matmul", r=[TT.reg(n), zb], w=[pu], out=pu[0:64, 0:64], lhsT=TT[:, n, :], rhs=zb[:], start=True, stop=True)
            p.I("act", "activation", r=[pu], w=[ub], out=ub[:], in_=pu[0:64, 0:64], func=AF.Copy)
            p.I("pe", "matmul", r=[Sb, ARx], w=[py], out=py[0:64, 0:64], lhsT=Sb[:], rhs=ARx[:, n, 1, :], start=True, stop=False)
            p.I("pe", "matmul", r=[ub, am], w=[py], out=py[0:64, 0:64], lhsT=ub[:], rhs=am[:, 1, :], start=False, stop=False)
            p.I("pe", "matmul", r=[Vh, am], w=[py], out=py[0:64, 0:64], lhsT=Vh[:, n, hd, :], rhs=am[:, 3, :], start=False, stop=True)
            p.I("pe", "matmul", r=[Bh, ub], w=[pS], out=pS[0:64, 64:128], lhsT=Bh[:, n, hd, :], rhs=ub[:], start=True, stop=False)
            p.I("pe", "matmul", r=[Kh, Vh], w=[pS], out=pS[0:64, 64:128], lhsT=Kh[:, n, hd, :], rhs=Vh[:, n, hd, :], start=False, stop=True)
            p.I("act", "activation", r=[py], w=[yTh], out=yTh[:, n * 64:(n + 1) * 64], in_=py[0:64, 0:64], func=AF.Copy)
            p.I("dve", "scalar_tensor_tensor", r=[S32, PCx, pS], w=[S32], out=S32[:], in0=S32[:], scalar=PCx[:, n:n + 1], in1=pS[0:64, 64:128],
                op0=ALU.mult, op1=ALU.add)
            p.I("dve", "tensor_copy", r=[S32], w=[Sb], out=Sb[:], in_=S32[:])
        p.dma(yd2[hs, :], yTh[:], r=[yTh], w=[dyd2])
        if stop == 3 + 2 * hd:
            p.finish()
            return p

    bavg = p.sb("bavg", [128, 128], F32)
    p.I("dve", "tensor_scalar", r=[cs], w=[bavg], out=bavg[:], in0=cs[:, 0:128], scalar1=1.0 / 64, scalar2=None, op0=ALU.mult)
    for blk in range(8):
        sl = slice(blk * 512, (blk + 1) * 512)
        pm, pv = ps[(2 * blk) % 8], ps[(2 * blk + 1) % 8]
        yc, y2, o, yl = tf[0], tf[1], tf[2], tf[3 + blk % 2]
        p.dma(yl[:], yd2[:, sl], r=[dyd2], w=[yl])
        p.I("pool", "tensor_copy", r=[yl], w=[tb[0]], out=tb[0][:], in_=yl[:])
        p.I("pe", "matmul", r=[bones, tb[0]], w=[pm], out=pm[:], lhsT=bones[:], rhs=tb[0][:], start=True, stop=True)
        p.I("dve", "scalar_tensor_tensor", r=[yl, pm], w=[yc], out=yc[:], in0=pm[:], scalar=-1.0 / 64, in1=yl[:], op0=ALU.mult, op1=ALU.add)
        p.I("pool", "tensor_tensor", r=[yc], w=[tb[1]], out=tb[1][:], in0=yc[:], in1=yc[:], op=ALU.mult)
        p.I("pe", "matmul", r=[bones, tb[1]], w=[pv], out=pv[:], lhsT=bones[:], rhs=tb[1][:], start=True, stop=True)
        p.I("act", "activation", r=[pv, epsb], w=[y2], out=y2[:], in_=pv[:], func=AF.Sqrt, bias=epsb[:, 0:1], scale=1.0 / 64)
        p.I("dve", "reciprocal", r=[y2], w=[y2], out=y2[:], in_=y2[:])
        p.I("dve", "tensor_tensor", r=[yc, y2], w=[yc], out=yc[:], in0=yc[:], in1=y2[:], op=ALU.mult)
        p.I("dve", "tensor_scalar", r=[yc, ch], w=[yc], out=yc[:], in0=yc[:], scalar1=col(GNG), scalar2=col(GNB), op0=ALU.mult, op1=ALU.add)
        p.I("dve", "tensor_tensor", r=[yc, bonus], w=[yc], out=yc[:], in0=yc[:], in1=bonus[:, sl], op=ALU.add)
        p.I("dve", "tensor_tensor", r=[yc, gT], w=[o], out=o[:], in0=yc[:], in1=gT[:, sl], op=ALU.mult)
        p.dma(ybT[:, sl], o[:], r=[o])
    p.finish()
    return p


def cst_B():
    c = np.zeros((128, 1152), np.float32)
    c[0:64, 0:64] = 1.0
    c[64:128, 64:128] = 1.0
    c[:, 128:256] = np.eye(128)
    rm = np.ones(512, np.float32)
    rm[0::64] = 0.0
    c[:, 256:768] = rm[None, :]
    i = np.arange(64)[:, None]
    t = np.arange(64)[None, :]
    strict = (i < t).astype(np.float32)
    incl = (i <= t).astype(np.float32)
    lower = (t < i).astype(np.float32)
    c[0:64, 768:832] = strict
    c[0:64, 832:896] = incl
    c[0:64, 896:960] = strict
    c[0:64, 960:1024] = incl
    c[0:64, 1024:1088] = lower
    c[0:64, 1088:1152] = np.eye(64)
    return c


S = 4096
NCH = 64


def build_C(layer):
    p = Prog()
    qfT = p.dram("qfT", [2, 128, S], F32, "ExternalInput")
    ig = p.dram("ig", [2, S, 128], F32, "ExternalInput")
    lbl = p.dram("lbl", [128, 2], F32, "ExternalInput")
    ng = p.dram("ng", [64, 128], F32, "ExternalInput")
    cst = p.dram("cst", [128, 768], F32, "ExternalInput")
    yc = p.dram("yc", [S, 128], F32, "ExternalOutput")
    cs = p.sb("cs", [128, 768], F32)
    lb = p.sb("lb", [128, 4], F32)
    ngt = p.sb("ngt", [64, 128], F32)
    epsb = p.sb("epsb", [128, 1], F32)
    p.I("dve", "memset", w=[epsb], ap=epsb[:], constant=1e-6)
    p.dma(cs[:], cst, w=[cs])
    p.dma(lb[:, 0:2], lbl, w=[lb])
    p.dma(ngt[:], ng, w=[ngt])
    rmask = cs[:, 0:512]
    ident = cs[:, 512:640]
    incl = cs[0:64, 640:704]
    if layer == 0:
        p.I("dve", "memset", w=[lb], ap=lb[:, 2:3], constant=0.0)
    else:
        p.I("dve", "tensor_tensor", r=[lb], w=[lb], out=lb[:, 2:3], in0=lb[:, 1:2], in1=lb[:, 0:1], op=ALU.subtract)
        p.I("act", "activation", r=[lb], w=[lb], out=lb[:, 2:3], in_=lb[:, 2:3], func=AF.Sigmoid)
    p.I("dve", "tensor_scalar", r=[lb], w=[lb], out=lb[:, 3:4], in0=lb[:, 2:3], scalar1=-1.0, scalar2=1.0, op0=ALU.mult, op1=ALU.add)

    Qt = p.sb("Qt", [128, S], BF16)
    Kt = p.sb("Kt", [128, S], BF16)
    Qb = p.sb("Qb", [128, S], BF16)
    Kbh = p.sb("Kbh", [64, NCH, 128], BF16)
    Ih = p.sb("Ih", [64, NCH, 128], BF16)
    Sall = p.sb("Sall", [128, NCH, 128], BF16)
    dec = p.sb("dec", [128, NCH], F32)
    tf = [p.sb("tf%d" % i, [128, 512], F32) for i in range(8)]
    xin = [p.sb("xin%d" % i, [128, 512], F32) for i in range(2)]
    ist = [p.sb("ist%d" % i, [64, 8, 128], F32) for i in range(2)]
    ps = [p.ps("ps%d" % i, [128, 512], F32) for i in range(8)]
    v3 = lambda t: t[:].rearrange("p (c t) -> p c t", t=64)
    igv = ig.rearrange("w (n s) v -> w s n v", s=64)
    for blk in range(8):
        sl = slice(blk * 512, (blk + 1) * 512)
        p.dma(xin[0][:], qfT[0][:, sl], w=[xin[0]])
        p.dma(xin[1][:], qfT[1][:, sl], w=[xin[1]], q="act")
        it = ist[blk % 2]
        p.dma(it[:], igv[0][:, blk * 8:(blk + 1) * 8, :], w=[it])
        p.I("pool", "tensor_copy", r=[it], w=[Ih], out=Ih[:, blk * 8:(blk + 1) * 8, :], in_=it[:])
        qf, fg, lf, kf, b, t1, t2, t3 = tf
        p.I("act", "activation", r=[xin[0]], w=[qf], out=qf[:], in_=xin[0][:], func=AF.Silu)
        p.I("act", "activation", r=[xin[1]], w=[fg], out=fg[:], in_=xin[1][:], func=AF.Sigmoid)
        p.I("dve", "tensor_scalar", r=[fg, lb], w=[fg], out=fg[:], in0=fg[:], scalar1=lb[:, 3:4], scalar2=lb[:, 2:3], op0=ALU.mult, op1=ALU.add)
        p.I("act", "activation", r=[fg], w=[lf], out=lf[:], in_=fg[:], func=AF.Ln)
        p.I("dve", "tensor_scalar", r=[fg], w=[kf], out=kf[:], in0=fg[:], scalar1=-1.0, scalar2=1.0, op0=ALU.mult, op1=ALU.add)
        p.I("dve", "tensor_tensor_scan", r=[cs, lf], w=[b], out=b[:], data0=rmask, data1=lf[:], initial=0.0, op0=ALU.mult, op1=ALU.add)
        bv = v3(b)
        p.I("dve", "tensor_tensor", r=[b], w=[t1], out=v3(t1), in0=bv, in1=bv[:, :, 31:32].to_broadcast([128, 8, 64]), op=ALU.subtract)
        p.I("act", "activation", r=[t1], w=[t2], out=t2[:], in_=t1[:], func=AF.Exp)
        p.I("dve", "tensor_tensor", r=[qf, t2], w=[Qt], out=Qt[:, sl], in0=qf[:], in1=t2[:], op=ALU.mult)
        p.I("act", "activation", r=[t1], w=[t2], out=t2[:], in_=t1[:], func=AF.Exp, scale=-1.0)
        p.I("dve", "tensor_tensor", r=[kf, t2], w=[Kt], out=Kt[:, sl], in0=kf[:], in1=t2[:], op=ALU.mult)
        p.I("act", "activation", r=[b], w=[t2], out=t2[:], in_=b[:], func=AF.Exp)
        p.I("pool", "tensor_tensor", r=[qf, t2], w=[Qb], out=Qb[:, sl], in0=qf[:], in1=t2[:], op=ALU.mult)
        p.I("pool", "tensor_copy", r=[t2], w=[dec], out=dec[:, blk * 8:(blk + 1) * 8], in_=v3(t2)[:, :, 63])
        p.I("dve", "tensor_tensor", r=[b], w=[t1], out=v3(t1), in0=bv, in1=bv[:, :, 63:64].to_broadcast([128, 8, 64]), op=ALU.subtract)
        p.I("act", "activation", r=[t1], w=[t3], out=t3[:], in_=t1[:], func=AF.Exp, scale=-1.0)
        p.I("dve", "tensor_tensor", r=[kf, t3], w=[t3], out=t3[:], in0=kf[:], in1=t3[:], op=ALU.mult)
        for half in range(2):
            pt = ps[half]
            for c4 in range(4):
                c = half * 4 + c4
                p.I("pe", "transpose", r=[t3, cs], w=[pt], out=pt[0:64, c4 * 128:(c4 + 1) * 128], in_=t3[:, c * 64:(c + 1) * 64], identity=ident)
            p.I("act", "activation", r=[pt], w=[Kbh], out=Kbh[:, blk * 8 + half * 4:blk * 8 + half * 4 + 4, :],
                in_=pt[0:64, :].rearrange("p (c k) -> p c k", c=4), func=AF.Copy)
    St = p.sb("St", [128, 128], F32)
    p.I("dve", "memset", w=[St], ap=St[:], constant=0.0)
    p.I("pool", "memset", w=[Sall.reg(0)], ap=Sall[:, 0, :], constant=0.0)
    for n in range(NCH - 1):
        pk = ps[2 + n % 3]
        p.I("pe", "matmul", r=[Kbh, Ih], w=[pk], out=pk[:, 0:128], lhsT=Kbh[:, n, :], rhs=Ih[:, n, :], start=True, stop=True)
        p.I("dve", "scalar_tensor_tensor", r=[St, dec, pk], w=[St], out=St[:], in0=St[:], scalar=dec[:, n:n + 1], in1=pk[:, 0:128],
            op0=ALU.mult, op1=ALU.add)
        p.I("pool", "tensor_copy", r=[St], w=[Sall.reg(n + 1)], out=Sall[:, n + 1, :], in_=St[:])
    at = [p.sb("at%d" % i, [64, 64], BF16) for i in range(2)]
    o1 = [p.sb("o1_%d" % i, [64, 128], F32) for i in range(2)]
    ot = [p.sb("ot%d" % i, [64, 8, 128], F32) for i in range(2)]
    gs = [p.sb("gs%d" % i, [64, 8, 128], F32) for i in range(2)]
    sm = [p.sb("sm%d" % i, [64, 2], F32) for i in range(2)]
    junk = p.sb("junk", [64, 128], F32)
    for n in range(NCH):
        g8 = n // 8
        if n % 8 == 0:
            gt = gs[g8 % 2]
            p.dma(gt[:], igv[1][:, n:n + 8, :], w=[gt])
            p.I("act", "activation", r=[gt], w=[gt], out=gt[:], in_=gt[:], func=AF.Silu)
            p.I("dve", "tensor_tensor", r=[gt, ngt], w=[gt], out=gt[:], in0=gt[:],
                in1=ngt[:].rearrange("p (o v) -> p o v", o=1).to_broadcast([64, 8, 128]), op=ALU.mult)
        gt = gs[g8 % 2]
        o8 = ot[g8 % 2]
        csl = slice(n * 64, (n + 1) * 64)
        pa, po = ps[5 + n % 2], ps[7 if n % 2 else 0]
        p.I("pe", "matmul", r=[Kt, Qt], w=[pa], out=pa[0:64, 0:64], lhsT=Kt[:, csl], rhs=Qt[:, csl], start=True, stop=True)
        a = at[n % 2]
        p.I("dve", "tensor_tensor", r=[pa, cs], w=[a], out=a[:], in0=pa[0:64, 0:64], in1=incl, op=ALU.mult)
        p.I("pe", "matmul", r=[a, Ih], w=[po], out=po[0:64, 0:128], lhsT=a[:], rhs=Ih[:, n, :], start=True, stop=True)
        p.I("pe", "matmul", r=[Qb, Sall.reg(n)], w=[po], out=po[0:64, 128:256], lhsT=Qb[:, csl], rhs=Sall[:, n, :], start=True, stop=True)
        oo = o1[n % 2]
        p.I("act", "activation", r=[po], w=[oo], out=oo[:], in_=po[0:64, 0:128], func=AF.Copy)
        p.I("dve", "tensor_tensor", r=[oo, po], w=[oo], out=oo[:], in0=oo[:], in1=po[0:64, 128:256], op=ALU.add)
        r = sm[n % 2]
        p.I("act", "activation", r=[oo], w=[junk, r], out=junk[:], in_=oo[:], func=AF.Square, accum_out=r[:, 0:1])
        p.I("act", "activation", r=[r, epsb], w=[r], out=r[:, 1:2], in_=r[:, 0:1], func=AF.Sqrt, scale=1.0 / 128, bias=epsb[0:64, 0:1])
        p.I("dve", "reciprocal", r=[r], w=[r], out=r[:, 1:2], in_=r[:, 1:2])
        p.I("dve", "scalar_tensor_tensor", r=[oo, r, gt], w=[o8], out=o8[:, n % 8, :], in0=oo[:], scalar=r[:, 1:2], in1=gt[:, n % 8, :],
            op0=ALU.mult, op1=ALU.mult)
        if n % 8 == 7:
            p.dma(yc.rearrange("(n s) v -> s n v", s=64)[:, n - 7:n + 1, :], o8[:], r=[o8])
    p.finish()
    return p


def cst_C():
    c = np.zeros((128, 768), np.float32)
    rm = np.ones(512, np.float32)
    rm[0::64] = 0.0
    c[:, 0:512] = rm[None, :]
    c[:, 512:640] = np.eye(128)
    i = np.arange(64)[:, None]
    t = np.arange(64)[None, :]
    c[0:64, 640:704] = (i <= t)
    return c


S = 4096
NEG = -30000.0


def build_D():
    p = Prog()
    qT = p.dram("qT", [64, 8192], F32, "ExternalInput")
    iqT = p.dram("iqT", [64, 8192], F32, "ExternalInput")
    iw = p.dram("iw", [128, 64], F32, "ExternalInput")
    kT = p.dram("kT", [64, S], F32, "ExternalInput")
    ikT = p.dram("ikT", [64, S], F32, "ExternalInput")
    v = p.dram("v", [S, 64], F32, "ExternalInput")
    vmask = p.dram("vmask", [128, 512], F32, "ExternalInput")
    id4 = p.dram("id4", [128, 512], BF16, "ExternalInput")
    yd = p.dram("yd", [1024, 512], F32, "ExternalOutput")
    qb = p.sb("qb", [64, 8192], BF16)
    iqb = p.sb("iqb", [64, 8192], BF16)
    kb = p.sb("kb", [64, S], BF16)
    ikb = p.sb("ikb", [64, S], BF16)
    vb = p.sb("vb", [128, 32, 65], BF16)
    iwt = p.sb("iwt", [128, 64], F32)
    vm = p.sb("vm", [128, 512], F32)
    i4 = p.sb("i4", [128, 512], BF16)
    st = [p.sb("st%d" % i, [128, 2048], F32) for i in range(2)]
    k = 0
    for (src, dst, n) in ((qT, qb, 8192), (iqT, iqb, 8192), (kT, kb, S), (ikT, ikb, S)):
        for c0 in range(0, n, 2048):
            s = st[k % 2]
            k += 1
            p.dma(s[0:64, :], src[:, c0:c0 + 2048], w=[s])
            p.I(("dve", "pool")[k % 2], "tensor_copy", r=[s], w=[dst], out=dst[:, c0:c0 + 2048], in_=s[0:64, :])
    s = st[k % 2]
    k += 1
    p.dma(s[:].rearrange("p (t e) -> p t e", e=64), v.rearrange("(t p) e -> p t e", p=128), w=[s])
    p.I("pool", "memset", w=[vb], ap=vb[:], constant=1.0)
    p.I("dve", "tensor_copy", r=[s], w=[vb], out=vb[:, :, 0:64], in_=s[:].rearrange("p (t e) -> p t e", e=64))
    p.dma(iwt[:], iw, w=[iwt])
    p.dma(vm[:], vmask, w=[vm])
    p.dma(i4[:], id4, w=[i4])
    p.I("dve", "tensor_scalar", r=[iwt], w=[iwt], out=iwt[:], in0=iwt[:], scalar1=(8 ** -0.5) * (64 ** -0.5), scalar2=None, op0=ALU.mult)

    sc = p.sb("sc", [128, S], F32)
    wk = p.sb("wk", [128, S], F32)
    bias = p.sb("bias", [128, S], BF16)
    pts = p.sb("pts", [128, 32, 512], BF16)
    rl = [p.sb("rl%d" % i, [128, 512], F32) for i in range(2)]
    mx = p.sb("mx", [128, 8], F32)
    yo = [p.sb("yo%d" % i, [128, 8, 64], F32) for i in range(2)]
    sm = [p.sb("sm%d" % i, [128, 1], F32) for i in range(3)]
    pss = [p.ps("pss%d" % i, [128, 512], F32) for i in range(4)]
    psa = [p.ps("psa%d" % i, [128, 512], F32) for i in range(3)]
    ks = 0
    ka = 0
    kr = 0
    for m in range(8):
        N = (m + 1) * 512
        nkt = 4 * m + 4
        qsl = slice(m * 1024, (m + 1) * 1024)
        for g in range(m + 1):
            gs = slice(g * 512, (g + 1) * 512)
            for h in range(8):
                ps = pss[ks % 4]
                ks += 1
                p.I("pe", "matmul", r=[iqb, ikb], w=[ps], out=ps[:], lhsT=iqb[:, m * 1024 + h * 128:m * 1024 + (h + 1) * 128],
                    rhs=ikb[:, gs], start=True, stop=True)
                r = rl[kr % 2]
                kr += 1
                p.I("act", "activation", r=[ps], w=[r], out=r[:], in_=ps[:], func=AF.Relu)
                ws = iwt[:, m * 8 + h:m * 8 + h + 1]
                if h == 0:
                    p.I("dve", "tensor_scalar", r=[r, iwt], w=[sc], out=sc[:, gs], in0=r[:], scalar1=ws, scalar2=None, op0=ALU.mult)
                else:
                    p.I("dve", "scalar_tensor_tensor", r=[r, iwt, sc], w=[sc], out=sc[:, gs], in0=r[:], scalar=ws, in1=sc[:, gs],
                        op0=ALU.mult, op1=ALU.add)
            if g == m:
                p.I("dve", "tensor_tensor", r=[sc, vm], w=[sc], out=sc[:, gs], in0=sc[:, gs], in1=vm[:], op=ALU.add)
        p.I("pool", "tensor_copy", r=[sc], w=[wk], out=wk[:, 0:N], in_=sc[:, 0:N])
        for rnd in range(32):
            p.I("dve", "max", r=[wk], w=[mx], out=mx[:], in_=wk[:, 0:N])
            if rnd < 31:
                p.I("dve", "match_replace", r=[wk, mx], w=[wk], out=wk[:, 0:N], in_to_replace=mx[:], in_values=wk[:, 0:N], imm_value=-1e30)
        p.I("dve", "tensor_scalar", r=[sc, mx], w=[bias], out=bias[:, 0:N], in0=sc[:, 0:N], scalar1=mx[:, 7:8], scalar2=NEG,
            op0=ALU.is_lt, op1=ALU.mult)
        p.I("dve", "tensor_tensor", r=[bias, vm], w=[bias], out=bias[:, m * 512:N], in0=bias[:, m * 512:N], in1=vm[:], op=ALU.add)
        o = yo[m % 2]
        for hg in range(2):
            for kt in range(nkt):
                ps = pss[ks % 4]
                ks += 1
                p.I("pe", "matmul", r=[kb, qb], w=[ps], out=ps[:], lhsT=kb[:, kt * 128:(kt + 1) * 128],
                    rhs=qb[:, m * 1024 + hg * 512:m * 1024 + (hg + 1) * 512], start=True, stop=False)
                p.I("pe", "matmul", r=[bias, i4], w=[ps], out=ps[:], lhsT=bias[:, kt * 128:(kt + 1) * 128], rhs=i4[:],
                    start=False, stop=True)
                p.I("act", "activation", r=[ps], w=[pts.reg(kt)], out=pts[:, kt, :], in_=ps[:], func=AF.Exp, scale=0.125)
            for h4 in range(4):
                pa = psa[ka % 3]
                r = sm[ka % 3]
                ka += 1
                for kt in range(nkt):
                    p.I("pe", "matmul", r=[pts.reg(kt), vb], w=[pa], out=pa[:, 0:65], lhsT=pts[:, kt, h4 * 128:(h4 + 1) * 128],
                        rhs=vb[:, kt, :], start=(kt == 0), stop=(kt == nkt - 1))
                p.I("dve", "reciprocal", r=[pa], w=[r], out=r[:], in_=pa[:, 64:65])
                p.I("dve", "tensor_scalar", r=[pa, r], w=[o], out=o[:, hg * 4 + h4, :], in0=pa[:, 0:64], scalar1=r[:, 0:1],
                    scalar2=None, op0=ALU.mult)
        p.dma(yd[m * 128:(m + 1) * 128, :], o[:].rearrange("p h d -> p (h d)"), r=[o])
    p.finish()
    return p


def vmask_const(j):
    pp = 2 * j + np.arange(128)[:, None] // 64
    ff = np.arange(512)[None, :] // 64
    return np.where(ff <= pp, 0.0, NEG).astype(np.float32)


def id4_const():
    return np.ascontiguousarray(np.tile(np.eye(128, dtype=np.float32), (1, 4))).astype(BF)


_PROGS = {}


def _prog(key, fn):
    if key not in _PROGS:
        _PROGS[key] = fn()
    return _PROGS[key]


def _run(p, ims):
    n = len(ims)
    return run_bass_kernel_spmd(p.nc, ims, core_ids=list(range(n))).results


def _cast_weights(arrs):
    sizes = [a.size for a in arrs]
    tot = sum(sizes)
    per = -(-tot // (8 * 128 * 4096)) * 4096
    flat = np.zeros(8 * 128 * per, np.float32)
    o = 0
    for a in arrs:
        flat[o:o + a.size] = a.reshape(-1)
        o += a.size
    flat = flat.reshape(8, 128, per)
    p = _prog(("L0", per), lambda: build_L0(per))
    res = _run(p, [{"src": flat[c]} for c in range(8)])
    out = np.concatenate([r["dst"].reshape(-1) for r in res])
    outs = []
    o = 0
    for a in arrs:
        outs.append(out[o:o + a.size].reshape(a.shape))
        o += a.size
    return outs


def _layer(l, x, positions, inp, W):
    f32 = np.float32
    g0 = np.ascontiguousarray(inp["norm_g"][l, 0].reshape(16, 128).T)
    cf = cf_const()
    ims = []
    for c in range(8):
        sl = slice(c * 1024, (c + 1) * 1024)
        ims.append({"xT": np.ascontiguousarray(x[sl].T), "g": g0, "wb": W["wb"],
                    "pos": np.ascontiguousarray(positions[sl].reshape(8, 128).T.astype(np.int32)), "cf": cf})
    res = _run(_prog("L1", build_L1), ims)
    proj = np.concatenate([r["proj"] for r in res], 0)
    y = np.zeros((4, 8192, 512), f32)
    lam_v = inp["diff_lambda"][l]
    sgv = inp["diff_subln_g"][l]
    ims = []
    for c in range(8):
        b, h = c // 4, c % 4
        pb = proj[b * 4096:(b + 1) * 4096]
        q = pb[:, h * 128:(h + 1) * 128].reshape(4096, 2, 64)
        k = pb[:, 512 + h * 128:512 + (h + 1) * 128].reshape(4096, 2, 64)
        ims.append({"qT": np.ascontiguousarray(q.transpose(1, 2, 0)), "kT": np.ascontiguousarray(k.transpose(1, 2, 0)),
                    "v": np.ascontiguousarray(pb[:, 1024 + h * 128:1024 + (h + 1) * 128]),
                    "lam4": np.ascontiguousarray(np.broadcast_to(lam_v.reshape(1, 256), (128, 256))),
                    "sg": np.ascontiguousarray(np.broadcast_to(sgv[None, :], (128, 128))), "masks": mask_const()})
    res = _run(_prog(("A", l), lambda: build_A(l)), ims)
    for c in range(8):
        b, h = c // 4, c % 4
        y[0, b * 4096:(b + 1) * 4096, h * 128:(h + 1) * 128] = res[c]["ya"]
    mu = inp["rwkv_mu"][l]
    ims = []
    cstb = cst_B()
    for c in range(8):
        b, hp = c // 4, c % 4
        pb = proj[b * 4096:(b + 1) * 4096, 1536:3328]
        cols = slice(hp * 128, (hp + 1) * 128)
        rkv = np.stack([pb[:, i * 512 + hp * 128:i * 512 + (hp + 1) * 128].T for i in range(3)])
        chp = np.zeros((128, 16), f32)
        for i in range(3):
            chp[:, i] = mu[i * 512 + hp * 128:i * 512 + (hp + 1) * 128]
        chp[:, 3] = mu[1536:1664]
        chp[:, 4] = mu[1664:1792]
        chp[:, 5] = inp["rwkv_w0"][l][cols]
        chp[:, 6] = inp["rwkv_a0"][l][cols]
        chp[:, 7] = inp["rwkv_k_k"][l][cols]
        chp[:, 8] = inp["rwkv_k_a"][l][cols]
        chp[:, 9] = inp["rwkv_r_k"][l].reshape(512)[cols]
        chp[:, 10] = inp["rwkv_gn_g"][l][cols]
        chp[:, 11] = inp["rwkv_gn_b"][l][cols]
        wup = np.concatenate([inp["rwkv_w_up"][l][:, cols], inp["rwkv_a_up"][l][:, cols]], 0)
        ims.append({"rkvT": np.ascontiguousarray(rkv), "lowT": np.ascontiguousarray(pb[:, 1536:1792].T), "chp": chp,
                    "wup": np.ascontiguousarray(wup), "gup": np.ascontiguousarray(inp["rwkv_g_up"][l][:, cols]), "cst": cstb})
    res = _run(_prog("B", build_B), ims)
    for c in range(8):
        b, hp = c // 4, c % 4
        y[1, b * 4096:(b + 1) * 4096, hp * 128:(hp + 1) * 128] = res[c]["ybT"].T
    ims = []
    cstc = cst_C()
    for c in range(8):
        b, h = c // 4, c % 4
        pc = proj[b * 4096:(b + 1) * 4096, 3328:5376]
        hc = slice(h * 128, (h + 1) * 128)
        qf = np.stack([pc[:, 0:512][:, hc].T, pc[:, 512:1024][:, hc].T])
        ig = np.stack([pc[:, 1024:1536][:, hc], pc[:, 1536:2048][:, hc]])
        ims.append({"qfT": np.ascontiguousarray(qf), "ig": np.ascontiguousarray(ig),
                    "lbl": np.ascontiguousarray(inp["hgrn_lb_logits"][:, hc].T),
                    "ng": np.ascontiguousarray(np.broadcast_to(inp["hgrn_norm_g"][l][hc][None, :], (64, 128))), "cst": cstc})
    res = _run(_prog(("C", l), lambda: build_C(l)), ims)
    for c in range(8):
        b, h = c // 4, c % 4
        y[2, b * 4096:(b + 1) * 4096, h * 128:(h + 1) * 128] = res[c]["yc"]
    O = 5376
    ims = []
    rows_all = []
    for c in range(8):
        b, j = c // 4, c % 4
        pb = proj[b * 4096:(b + 1) * 4096]
        rows = np.concatenate([np.arange(i * 128, (i + 1) * 128) for i in [4 * m + j for m in range(8)]])
        rows_all.append(rows)
        q = pb[rows, O:O + 512].reshape(8, 128, 8, 64)
        iq = pb[rows, O + 640:O + 1152].reshape(8, 128, 8, 64)
        iw = pb[rows, O + 1216:O + 1224].reshape(8, 128, 8)
        ims.append({"qT": np.ascontiguousarray(q.transpose(3, 0, 2, 1).reshape(64, 8192)),
                    "iqT": np.ascontiguousarray(iq.transpose(3, 0, 2, 1).reshape(64, 8192)),
                    "iw": np.ascontiguousarray(iw.transpose(1, 0, 2).reshape(128, 64)),
                    "kT": np.ascontiguousarray(pb[:, O + 512:O + 576].T), "ikT": np.ascontiguousarray(pb[:, O + 1152:O + 1216].T),
                    "v": np.ascontiguousarray(pb[:, O + 576:O + 640]), "vmask": vmask_const(j), "id4": id4_const()})
    res = _run(_prog("D", build_D), ims)
    for c in range(8):
        b = c // 4
        y[3, b * 4096 + rows_all[c]] = res[c]["yd"]
    del proj
    g3 = np.ascontiguousarray(np.concatenate([inp["norm_g"][l, i].reshape(16, 128).T for i in range(4)], axis=1))
    ims = []
    for c in range(8):
        sl = slice(c * 1024, (c + 1) * 1024)
        ims.append({"xT": np.ascontiguousarray(x[sl].T), "yT": np.ascontiguousarray(y[:, sl].transpose(0, 2, 1)), "wg": W["wg"], "g3": g3,
                    "wbr": W["wbr"], "wout": W["wout"], "w1": W["w1"], "w2": W["w2"]})
    res = _run(_prog("L3", lambda: build_L3(1024)), ims)
    return np.ascontiguousarray(np.concatenate([r["xo"].T for r in res], 0))


def kernel(**inputs):
    inp = {k: np.asarray(v) for k, v in inputs.items()}
    x = np.ascontiguousarray(inp["x"].reshape(8192, D)).astype(np.float32, copy=False)
    positions = inp["positions"].reshape(8192)
    tiled = []
    for l in range(2):
        w_in = inp["w_in"][l]
        tiled.append(prep_w_in(w_in))
        tiled.append(prep_wg(np.ascontiguousarray(w_in[:, NMIX:])))
        tiled.extend(prep_L3_weights(inp["w_branch"][l], inp["w_out"][l], inp["mlp_w1"][l], inp["mlp_w2"][l]))
    cast = _cast_weights(tiled)
    del tiled
    for l in range(2):
        W = dict(zip(("wb", "wg", "wbr", "wout", "w1", "w2"), cast[l * 6:(l + 1) * 6]))
        x = _layer(l, x, positions, inp, W)
    return x.reshape(2, 4096, D).astype(np.float32)
```

```python
import math
import numpy as np
import ml_dtypes
from contextlib import ExitStack
import concourse.bass as bass
import concourse.mybir as mybir
from concourse.bass_utils import run_bass_kernel_spmd

BF = ml_dtypes.bfloat16


F32 = mybir.dt.float32
BF16 = mybir.dt.bfloat16
I32 = mybir.dt.int32
AF = mybir.ActivationFunctionType
ALU = mybir.AluOpType
AX = mybir.AxisListType

SAME_ENG_SYNC = True
NDMA = 24


class Dep:
    __slots__ = ("w", "r")

    def __init__(self):
        self.w = None
        self.r = []


class Buf:
    def __init__(self, t, nreg=1):
        self.t = t
        self.d = Dep()
        self.regs = {}

    def reg(self, key):
        if key not in self.regs:
            self.regs[key] = Dep()
        return self.regs[key]

    def __getitem__(self, idx):
        return self.t[idx]


class Prog:
    def __init__(self):
        self.nc = bass.Bass("TRN2", target_bir_lowering=False)
        nc = self.nc
        self.es = ExitStack()
        self.eng = {"pe": nc.tensor, "act": nc.scalar, "dve": nc.vector, "pool": nc.gpsimd, "sp": nc.sync}
        self.sem = {}
        self.cnt = {}
        self.clock = {}
        self.hist = {}
        for e in self.eng:
            self.sem[e] = self.es.enter_context(nc.semaphore("s_" + e))
            self.cnt[e] = 0
            self.clock[e] = {}
            self.hist[e] = {}
        for j in range(NDMA):
            e = "d%d" % j
            self.sem[e] = self.es.enter_context(nc.semaphore("s_" + e))
            self.cnt[e] = 0
            self.hist[e] = {}
        self.dma_k = 0
        self.nwaits = 0
        self.ninst = 0
        self.q = {e: [] for e in self.eng}

    def dram(self, name, shape, dtype, kind):
        return self.nc.dram_tensor(name, list(shape), dtype, kind=kind).ap()

    def sb(self, name, shape, dtype=F32):
        return Buf(self.es.enter_context(self.nc.sbuf_tensor(name, list(shape), dtype)))

    def ps(self, name, shape, dtype=F32):
        return Buf(self.es.enter_context(self.nc.psum_tensor(name, list(shape), dtype)))

    def _semval(self, e, n):
        return n * 16 if (e[0] == "d" and e[1:].isdigit()) else n

    def _wait(self, e, deps):
        need = {}
        for (e2, n) in deps:
            if e2 == e and (e == "pe" or not SAME_ENG_SYNC):
                continue
            if self.clock[e].get(e2, 0) >= n:
                continue
            if need.get(e2, 0) < n:
                need[e2] = n
        for e2, n in need.items():
            if self.clock[e].get(e2, 0) >= n:
                continue
            self.q[e].append(("w", self.sem[e2], self._semval(e2, n)))
            self.nwaits += 1
            h = self.hist[e2].get(n)
            if h:
                for k, v in h.items():
                    if self.clock[e].get(k, 0) < v:
                        self.clock[e][k] = v
            self.clock[e][e2] = max(self.clock[e].get(e2, 0), n)

    def _deps(self, r, w):
        deps = []
        for d in r:
            d = d.d if isinstance(d, Buf) else d
            if d.w is not None:
                deps.append(d.w)
        for d in w:
            d = d.d if isinstance(d, Buf) else d
            if d.w is not None:
                deps.append(d.w)
            deps.extend(d.r)
        return deps

    def _commit(self, tag, r, w):
        for d in r:
            d = d.d if isinstance(d, Buf) else d
            d.r.append(tag)
            if len(d.r) > 64:
                best = {}
                for (e2, n) in d.r:
                    if best.get(e2, 0) < n:
                        best[e2] = n
                d.r = list(best.items())
        for d in w:
            d = d.d if isinstance(d, Buf) else d
            d.w = tag
            d.r = []

    def I(self, e, method, r=(), w=(), **kw):
        self._wait(e, self._deps(r, w))
        self.cnt[e] += 1
        n = self.cnt[e]
        self.q[e].append(("i", (method, kw), self.sem[e], 1))
        self.hist[e][n] = dict(self.clock[e])
        if e == "pe" or not SAME_ENG_SYNC:
            self.clock[e][e] = n
        self._commit((e, n), r, w)
        self.ninst += 1

    def dma(self, out, in_, r=(), w=(), q="sp", **kw):
        j = self.dma_k % NDMA
        self.dma_k += 1
        de = "d%d" % j
        prev = self.cnt[de]
        deps = self._deps(r, w)
        if prev > 0:
            deps.append((de, prev))
        self._wait(q, deps)
        self.q[q].append(("d", (out, in_, kw), self.sem[de], 16))
        self.cnt[de] = prev + 1
        self.hist[de][prev + 1] = dict(self.clock[q])
        self._commit((de, prev + 1), r, w)
        self.ninst += 1

    def coll(self, kind, ins, outs, r=(), w=()):
        q = "pool"
        j = self.dma_k % NDMA
        self.dma_k += 1
        de = "d%d" % j
        prev = self.cnt[de]
        deps = self._deps(r, w)
        if prev > 0:
            deps.append((de, prev))
        self._wait(q, deps)
        self.q[q].append(("c", (kind, ins, outs), self.sem[de], 16))
        self.cnt[de] = prev + 1
        self.hist[de][prev + 1] = dict(self.clock[q])
        self._commit((de, prev + 1), r, w)
        self.ninst += 1

    def finish(self, q="sp"):
        deps = []
        for j in range(NDMA):
            de = "d%d" % j
            if self.cnt[de] > 0:
                deps.append((de, self.cnt[de]))
        self._wait(q, deps)

        nc = self.nc
        prog = self
        with nc.Block() as block:
            def mk(e):
                def body(engh):
                    for it in prog.q[e]:
                        if it[0] == "w":
                            engh.wait_ge(it[1], it[2])
                        elif it[0] == "i":
                            getattr(engh, it[1][0])(**it[1][1]).then_inc(it[2], it[3])
                        elif it[0] == "c":
                            kind, ins, outs = it[1]
                            engh.collective_compute(kind, ALU.bypass, [[0,1,2,3],[4,5,6,7]], ins, outs).then_inc(it[2], it[3])
                        else:
                            o, i, kw = it[1]
                            engh.dma_start(out=o, in_=i, **kw).then_inc(it[2], it[3])
                return body
            block.tensor(mk("pe"))
            block.scalar(mk("act"))
            block.vector(mk("dve"))
            block.gpsimd(mk("pool"))
            block.sync(mk("sp"))
        self.es.close()


D = 2048
DIN = 14792
NCB = 13
NMIX = 6600
EPS = 1e-6
ROPE = {0: [(0, 8)], 1: [(0, 8)], 10: [(256, 4)], 11: [(0, 5), (384, 2)], 12: [(0, 7)]}


def build_L0(n):
    p = Prog()
    src = p.dram("src", [128, n], F32, "ExternalInput")
    dst = p.dram("dst", [128, n], BF16, "ExternalOutput")
    CH = 4096
    st = [p.sb("st%d" % i, [128, CH], F32) for i in range(3)]
    ob = [p.sb("ob%d" % i, [128, CH], BF16) for i in range(3)]
    k = 0
    for c0 in range(0, n, CH):
        c1 = min(n, c0 + CH)
        s, o = st[k % 3], ob[k % 3]
        p.dma(s[:, 0:c1 - c0], src[:, c0:c1], w=[s], q="sp")
        e = ("dve", "pool")[k % 2]
        p.I(e, "tensor_copy", r=[s], w=[o], out=o[:, 0:c1 - c0], in_=s[:, 0:c1 - c0])
        p.dma(dst[:, c0:c1], o[:, 0:c1 - c0], r=[o], q="act")
        k += 1
    p.finish()
    return p


def fm_rmsnorm(p, src, gt, gcol, dst, KC, T, ones, psl, sq, rstd, dim):
    nh = T // 512
    for kc in range(KC):
        s = sq[kc % 2]
        p.I("act", "activation", r=[src], w=[s], out=s[:, 0:T], in_=src[:, kc, :], func=AF.Square)
        for h in range(nh):
            p.I("pe", "matmul", r=[s, ones], w=[psl[h]], out=psl[h][:], lhsT=ones[:], rhs=s[:, h * 512:(h + 1) * 512],
                start=(kc == 0), stop=(kc == KC - 1))
    for h in range(nh):
        p.I("act", "activation", r=[psl[h], EPSB[0]], w=[rstd], out=rstd[:, h * 512:(h + 1) * 512], in_=psl[h][:],
            func=AF.Sqrt, scale=1.0 / dim, bias=EPSB[0][:, 0:1])
    p.I("dve", "reciprocal", r=[rstd], w=[rstd], out=rstd[:, 0:T], in_=rstd[:, 0:T])
    for kc in range(KC):
        p.I("dve", "scalar_tensor_tensor", r=[src, gt, rstd], w=[dst], out=dst[:, kc, :], in0=src[:, kc, :],
            scalar=gt[:, gcol + kc:gcol + kc + 1], in1=rstd[:, 0:T], op0=ALU.mult, op1=ALU.mult)


EPSB = [None]


def consts(p):
    ones = p.sb("ones", [128, 128], BF16)
    p.I("dve", "memset", w=[ones], ap=ones[:], constant=1.0)
    eb = p.sb("epsb", [128, 1], F32)
    p.I("dve", "memset", w=[eb], ap=eb[:], constant=EPS)
    EPSB[0] = eb
    return ones


def build_L1():
    p = Prog()
    T = 1024
    xT = p.dram("xT", [D, T], F32, "ExternalInput")
    g = p.dram("g", [128, 16], F32, "ExternalInput")
    wb = p.dram("wb", [NCB, 128, 8192], BF16, "ExternalInput")
    pos = p.dram("pos", [128, 8], I32, "ExternalInput")
    cf = p.dram("cf", [128, 32], F32, "ExternalInput")
    proj = p.dram("proj", [T, NCB * 512], F32, "ExternalOutput")
    ones = consts(p)
    xs = p.sb("xs", [128, 16, T], F32)
    hT = p.sb("hT", [128, 16, T], BF16)
    gt = p.sb("gt", [128, 16], F32)
    sq = [p.sb("sq%d" % i, [128, T], BF16) for i in range(2)]
    rstd = p.sb("rstd", [128, T], F32)
    psn = [p.ps("psn%d" % i, [128, 512], F32) for i in range(2)]
    pst = [p.ps("ps%d" % i, [128, 512], F32) for i in range(4)]
    xv = xT.rearrange("(kc p) t -> p kc t", p=128)
    for kc in range(0, 16, 4):
        p.dma(xs[:, kc:kc + 4, :], xv[:, kc:kc + 4, :], w=[xs], q=("sp", "act")[(kc // 4) % 2])
    p.dma(gt[:], g, w=[gt])
    posi = p.sb("posi", [128, 8], I32)
    posf = p.sb("posf", [128, 8], F32)
    cft = p.sb("cft", [128, 32], F32)
    p.dma(posi[:], pos, w=[posi])
    p.dma(cft[:], cf, w=[cft])
    p.I("dve", "tensor_copy", r=[posi], w=[posf], out=posf[:], in_=posi[:])
    qq = p.sb("qq", [128, 2, 8, 32], F32)
    qi = p.sb("qi", [128, 2, 8, 32], I32)
    qf = p.sb("qf", [128, 2, 8, 32], F32)
    msk = p.sb("msk", [128, 2, 8, 32], F32)
    sc = p.sb("sc", [128, 2, 8, 32], F32)
    for tt in range(8):
        p.I("dve", "tensor_scalar", r=[cft, posf], w=[qq], out=qq[:, 0, tt, :], in0=cft[:], scalar1=posf[:, tt:tt + 1],
            scalar2=None, op0=ALU.mult)
    p.I("dve", "tensor_scalar", r=[qq], w=[qq], out=qq[:, 1, :, :], in0=qq[:, 0, :, :], scalar1=0.25, scalar2=None, op0=ALU.add)
    p.I("dve", "tensor_copy", r=[qq], w=[qi], out=qi[:], in_=qq[:])
    p.I("dve", "tensor_copy", r=[qi], w=[qf], out=qf[:], in_=qi[:])
    p.I("dve", "tensor_tensor", r=[qq, qf], w=[qq], out=qq[:], in0=qq[:], in1=qf[:], op=ALU.subtract)
    p.I("dve", "tensor_scalar", r=[qq], w=[msk], out=msk[:], in0=qq[:], scalar1=0.5, scalar2=None, op0=ALU.is_gt)
    p.I("dve", "tensor_tensor", r=[qq, msk], w=[qq], out=qq[:], in0=qq[:], in1=msk[:], op=ALU.subtract)
    p.I("dve", "tensor_scalar", r=[qq], w=[msk], out=msk[:], in0=qq[:], scalar1=-0.5, scalar2=None, op0=ALU.is_lt)
    p.I("dve", "tensor_tensor", r=[qq, msk], w=[qq], out=qq[:], in0=qq[:], in1=msk[:], op=ALU.add)
    p.I("act", "activation", r=[qq], w=[sc], out=sc[:], in_=qq[:], func=AF.Sin, scale=6.28318)
    fm_rmsnorm(p, xs, gt, 0, hT, 16, T, ones, psn, sq, rstd, D)
    wt = [p.sb("w%d" % i, [128, 8192], BF16) for i in range(3)]
    ot = [p.sb("o%d" % i, [128, 512], F32) for i in range(4)]
    tmp = [p.sb("rt%d" % i, [128, 8, 32], F32) for i in range(4)]
    k = 0
    for cb in range(NCB):
        w = wt[cb % 3]
        p.dma(w[:, 0:4096], wb[cb, :, 0:4096], w=[w], q="sp")
        p.dma(w[:, 4096:8192], wb[cb, :, 4096:8192], w=[w], q="act")
        for tt in range(8):
            ps = pst[k % 4]
            o = ot[k % 4]
            k += 1
            for kc in range(16):
                p.I("pe", "matmul", r=[hT, w], w=[ps], out=ps[:], lhsT=hT[:, kc, tt * 128:(tt + 1) * 128],
                    rhs=w[:, kc * 512:(kc + 1) * 512], start=(kc == 0), stop=(kc == 15))
            p.I("act", "activation", r=[ps], w=[o], out=o[:], in_=ps[:], func=AF.Copy)
            for (s0, nh) in ROPE.get(cb, []):
                ov = o[:, s0:s0 + nh * 64].rearrange("p (h two d) -> p h two d", two=2, d=32)
                x1, x2 = ov[:, :, 0, :], ov[:, :, 1, :]
                sn = sc[:, 0, tt:tt + 1, :].to_broadcast([128, nh, 32])
                cs = sc[:, 1, tt:tt + 1, :].to_broadcast([128, nh, 32])
                t1, t2, t3, t4 = [t[:, 0:nh, :] for t in tmp]
                p.I("dve", "tensor_tensor", r=[o, sc], w=[tmp[0]], out=t1, in0=x1, in1=cs, op=ALU.mult)
                p.I("dve", "tensor_tensor", r=[o, sc], w=[tmp[1]], out=t2, in0=x2, in1=sn, op=ALU.mult)
                p.I("dve", "tensor_tensor", r=[o, sc], w=[tmp[2]], out=t3, in0=x2, in1=cs, op=ALU.mult)
                p.I("dve", "tensor_tensor", r=[o, sc], w=[tmp[3]], out=t4, in0=x1, in1=sn, op=ALU.mult)
                p.I("dve", "tensor_tensor", r=[tmp[0], tmp[1]], w=[o], out=x1, in0=t1, in1=t2, op=ALU.subtract)
                p.I("dve", "tensor_tensor", r=[tmp[2], tmp[3]], w=[o], out=x2, in0=t3, in1=t4, op=ALU.add)
            p.dma(proj[tt * 128:(tt + 1) * 128, cb * 512:(cb + 1) * 512], o[:], r=[o], q="sp")
    p.finish()
    return p


def prep_w_in(wbf):
    w = np.zeros((D, NCB * 512), dtype=wbf.dtype)
    w[:, :NMIX] = wbf[:, :NMIX]
    w = w.reshape(16, 128, NCB, 512).transpose(2, 1, 0, 3).reshape(NCB, 128, 8192)
    return np.ascontiguousarray(w)


def cf_const():
    inv = 10000.0 ** (-np.arange(0, 64, 2, dtype=np.float64) / 64.0)
    c = (inv / (2 * math.pi)).astype(np.float32)
    return np.ascontiguousarray(np.broadcast_to(c[None, :], (128, 32)))


def build_L3(T=2048):
    p = Prog()
    H = 512
    xT = p.dram("xT", [D, T], F32, "ExternalInput")
    yT = p.dram("yT", [4, 512, T], F32, "ExternalInput")
    wg = p.dram("wg", [64, 128, 2048], BF16, "ExternalInput")
    g3 = p.dram("g3", [128, 64], F32, "ExternalInput")
    wbr = p.dram("wbr", [16, 128, 2048], BF16, "ExternalInput")
    wout = p.dram("wout", [16, 128, 2048], BF16, "ExternalInput")
    w1 = p.dram("w1", [64, 128, 2048], BF16, "ExternalInput")
    w2 = p.dram("w2", [16, 4, 128, 2048], BF16, "ExternalInput")
    xo = p.dram("xo", [D, T], F32, "ExternalOutput")
    ones = consts(p)
    xh = p.sb("xh", [128, 16, H], F32)
    zT = p.sb("zT", [128, 16, H], F32)
    mb = p.sb("mb", [128, 16, H], BF16)
    uT = p.sb("uT", [128, 64, H], BF16)
    gt = p.sb("gt", [128, 64], F32)
    hT = p.sb("hT", [128, 16, H], BF16)
    sq = [p.sb("sq%d" % i, [128, H], BF16) for i in range(2)]
    rstd = p.sb("rstd", [128, H], F32)
    psn = [p.ps("psn0", [128, 512], F32)]
    pst = [p.ps("ps%d" % i, [128, 512], F32) for i in range(4)]
    wt = [p.sb("wt%d" % i, [128, 2048], BF16) for i in range(4)]
    psg = [p.ps("psg%d" % i, [128, 512], F32) for i in range(2)]
    sg = [p.sb("sg%d" % i, [128, H], F32) for i in range(2)]
    tm = [p.sb("tm%d" % i, [128, H], F32) for i in range(2)]
    ys = [p.sb("ys%d" % i, [128, 4, H], F32) for i in range(1)]
    wgp = [p.sb("wgp%d" % i, [128, 2048], BF16) for i in range(2)]
    gk = 0
    p.dma(gt[:], g3, w=[gt])
    xv = xT.rearrange("(kc p) t -> p kc t", p=128)
    xov = xo.rearrange("(kc p) t -> p kc t", p=128)
    yv = yT.rearrange("n (kc p) t -> n p kc t", p=128)
    wk = 0
    pk = 0
    for hf in range(T // H):
        tsl = slice(hf * H, (hf + 1) * H)
        for kc in range(0, 16, 8):
            p.dma(xh[:, kc:kc + 8, :], xv[:, kc:kc + 8, tsl], w=[xh], q="act")
        fm_rmsnorm(p, xh, gt, 0, hT, 16, H, ones, psn, sq, rstd, D)
        for n in range(4):
            y = ys[0]
            p.dma(y[:], yv[n, :, :, tsl], w=[y], q="act")
            p.I("dve", "tensor_copy", r=[y], w=[uT], out=uT[:, n * 4:(n + 1) * 4, :], in_=y[:])
        for oc in range(16):
            w = wt[wk % 4]
            wk += 1
            p.dma(w[:], wbr[oc], w=[w], q="sp")
            for n in range(4):
                wgt = wgp[gk % 2]
                gk += 1
                p.dma(wgt[:], wg[oc * 4 + n], w=[wgt], q="act")
                pg = psg[n % 2]
                for kc in range(16):
                    p.I("pe", "matmul", r=[hT, wgt], w=[pg], out=pg[:], lhsT=wgt[:, kc * 128:(kc + 1) * 128], rhs=hT[:, kc, :],
                        start=(kc == 0), stop=(kc == 15))
                ps = pst[pk % 4]
                pk += 1
                for kc in range(4):
                    p.I("pe", "matmul", r=[uT, w], w=[ps], out=ps[:], lhsT=w[:, (n * 4 + kc) * 128:(n * 4 + kc + 1) * 128],
                        rhs=uT[:, n * 4 + kc, :], start=(kc == 0), stop=(kc == 3))
                s = sg[n % 2]
                p.I("act", "activation", r=[pg], w=[s], out=s[:], in_=pg[:], func=AF.Sigmoid)
                if n == 0:
                    p.I("dve", "tensor_tensor", r=[ps, s], w=[zT], out=zT[:, oc, :], in0=ps[:], in1=s[:], op=ALU.mult)
                else:
                    t = tm[n % 2]
                    p.I("dve", "tensor_tensor", r=[ps, s], w=[t], out=t[:], in0=ps[:], in1=s[:], op=ALU.mult)
                    p.I("pool", "tensor_tensor", r=[t, zT], w=[zT], out=zT[:, oc, :], in0=zT[:, oc, :], in1=t[:], op=ALU.add)
            p.I("pool", "tensor_copy", r=[zT], w=[mb], out=mb[:, oc, :], in_=zT[:, oc, :])
        for oc in range(16):
            w = wt[wk % 4]
            wk += 1
            p.dma(w[:], wout[oc], w=[w], q="sp")
            ps = pst[pk % 4]
            pk += 1
            for kc in range(16):
                p.I("pe", "matmul", r=[mb, w], w=[ps], out=ps[:], lhsT=w[:, kc * 128:(kc + 1) * 128], rhs=mb[:, kc, :],
                    start=(kc == 0), stop=(kc == 15))
            p.I("act", "activation", r=[ps], w=[zT], out=zT[:, oc, :], in_=ps[:], func=AF.Copy)
        fm_rmsnorm(p, zT, gt, 16, zT, 16, H, ones, psn, sq, rstd, D)
        for kc in range(0, 16, 4):
            p.I("pool", "tensor_tensor", r=[xh, zT], w=[xh], out=xh[:, kc:kc + 4, :], in0=xh[:, kc:kc + 4, :], in1=zT[:, kc:kc + 4, :], op=ALU.add)
        fm_rmsnorm(p, xh, gt, 32, mb, 16, H, ones, psn, sq, rstd, D)
        for oc in range(64):
            w = wt[wk % 4]
            wk += 1
            p.dma(w[:], w1[oc], w=[w], q=("sp", "act")[oc % 2])
            ps = pst[pk % 4]
            pk += 1
            for kc in range(16):
                p.I("pe", "matmul", r=[mb, w], w=[ps], out=ps[:], lhsT=w[:, kc * 128:(kc + 1) * 128], rhs=mb[:, kc, :],
                    start=(kc == 0), stop=(kc == 15))
            t = tm[oc % 2]
            p.I("act", "activation", r=[ps], w=[t], out=t[:], in_=ps[:], func=AF.Relu)
            p.I(("dve", "pool")[oc % 2], "tensor_tensor", r=[t], w=[uT], out=uT[:, oc, :], in0=t[:], in1=t[:], op=ALU.mult)
        for oc in range(16):
            ps = pst[pk % 4]
            pk += 1
            for q in range(4):
                w = wt[wk % 4]
                wk += 1
                p.dma(w[:], w2[oc, q], w=[w], q=("sp", "act")[q % 2])
                for kc in range(16):
                    p.I("pe", "matmul", r=[uT, w], w=[ps], out=ps[:], lhsT=w[:, kc * 128:(kc + 1) * 128], rhs=uT[:, q * 16 + kc, :],
                        start=(q == 0 and kc == 0), stop=(q == 3 and kc == 15))
            p.I("act", "activation", r=[ps], w=[zT], out=zT[:, oc, :], in_=ps[:], func=AF.Copy)
        fm_rmsnorm(p, zT, gt, 48, zT, 16, H, ones, psn, sq, rstd, D)
        for kc in range(0, 16, 4):
            p.I("pool", "tensor_tensor", r=[xh, zT], w=[xh], out=xh[:, kc:kc + 4, :], in0=xh[:, kc:kc + 4, :], in1=zT[:, kc:kc + 4, :], op=ALU.add)
        for kc in range(0, 16, 8):
            p.dma(xov[:, kc:kc + 8, tsl], xh[:, kc:kc + 8, :], r=[xh], q="sp")
    p.finish()
    return p


def prep_wg(wgate):
    g = wgate.reshape(16, 128, 4, 16, 128).transpose(3, 2, 1, 0, 4).reshape(64, 128, 2048)
    return np.ascontiguousarray(g)


def prep_L3_weights(wbr, wout, w1, w2):
    a = wbr.reshape(4, 4, 128, 16, 128).transpose(3, 2, 0, 1, 4).reshape(16, 128, 2048)
    b = wout.reshape(16, 128, 16, 128).transpose(2, 1, 0, 3).reshape(16, 128, 2048)
    c = w1.reshape(16, 128, 64, 128).transpose(2, 1, 0, 3).reshape(64, 128, 2048)
    d = w2.reshape(4, 16, 128, 16, 128).transpose(3, 0, 2, 1, 4).reshape(16, 4, 128, 2048)
    return [np.ascontiguousarray(t) for t in (a, b, c, d)]


S = 4096


def build_A(layer):
    p = Prog()
    lam_init = 0.8 - 0.6 * math.exp(-0.3 * layer)
    qT = p.dram("qT", [2, 64, S], F32, "ExternalInput")
    kT = p.dram("kT", [2, 64, S], F32, "ExternalInput")
    v = p.dram("v", [S, 128], F32, "ExternalInput")
    lam4 = p.dram("lam4", [128, 256], F32, "ExternalInput")
    sg = p.dram("sg", [128, 128], F32, "ExternalInput")
    masks = p.dram("masks", [4, 128, 512], BF16, "ExternalInput")
    ya = p.dram("ya", [S, 128], F32, "ExternalOutput")
    qb = [p.sb("qb%d" % m, [64, S], BF16) for m in range(2)]
    kb = [p.sb("kb%d" % m, [64, S], BF16) for m in range(2)]
    vb = p.sb("vb", [128, 32, 129], BF16)
    st = [p.sb("st%d" % i, [128, 4096], F32) for i in range(2)]
    mk_ = p.sb("mk", [128, 4, 512], BF16)
    lt = p.sb("lt", [128, 256], F32)
    sgt = p.sb("sgt", [128, 128], F32)
    epsb = p.sb("epsb", [128, 1], F32)
    p.I("dve", "memset", w=[epsb], ap=epsb[:], constant=1e-6)
    k = 0
    for m in range(2):
        for (src, dst) in ((qT, qb[m]), (kT, kb[m])):
            s = st[k % 2]
            k += 1
            p.dma(s[0:64, :], src[m], w=[s])
            p.I(("dve", "pool")[k % 2], "tensor_copy", r=[s], w=[dst], out=dst[:], in_=s[0:64, :])
    s = st[k % 2]
    k += 1
    p.dma(s[:].rearrange("p (t e) -> p t e", e=128), v.rearrange("(t p) e -> p t e", p=128), w=[s])
    p.I("pool", "memset", w=[vb], ap=vb[:], constant=1.0)
    p.I("dve", "tensor_copy", r=[s], w=[vb], out=vb[:, :, 0:128], in_=s[:].rearrange("p (t e) -> p t e", e=128))
    p.dma(mk_[:], masks.rearrange("j p f -> p j f"), w=[mk_])
    p.dma(lt[:], lam4, w=[lt])
    p.dma(sgt[:], sg, w=[sgt])
    pr = p.sb("pr", [128, 2, 64], F32)
    s12 = p.sb("s12", [128, 2], F32)
    e12 = p.sb("e12", [128, 2], F32)
    nlam = p.sb("nlam", [128, 1], F32)
    ltv = lt[:].rearrange("p (a d) -> p a d", d=64)
    p.I("dve", "tensor_tensor", r=[lt], w=[pr], out=pr[:, 0, :], in0=ltv[:, 0, :], in1=ltv[:, 1, :], op=ALU.mult)
    p.I("dve", "tensor_tensor", r=[lt], w=[pr], out=pr[:, 1, :], in0=ltv[:, 2, :], in1=ltv[:, 3, :], op=ALU.mult)
    p.I("dve", "tensor_reduce", r=[pr], w=[s12], out=s12[:], in_=pr[:], axis=AX.X, op=ALU.add)
    p.I("act", "activation", r=[s12], w=[e12], out=e12[:], in_=s12[:], func=AF.Exp)
    p.I("dve", "tensor_tensor", r=[e12], w=[nlam], out=nlam[:], in0=e12[:, 1:2], in1=e12[:, 0:1], op=ALU.subtract)
    p.I("dve", "tensor_scalar", r=[nlam], w=[nlam], out=nlam[:], in0=nlam[:], scalar1=-lam_init, scalar2=None, op0=ALU.add)
    p.I("dve", "tensor_scalar", r=[sgt], w=[sgt], out=sgt[:], in0=sgt[:], scalar1=1.0 - lam_init, scalar2=None, op0=ALU.mult)
    pss = [p.ps("pss%d" % i, [128, 512], F32) for i in range(3)]
    psa = [p.ps("psa%d" % i, [128, 512], F32) for i in range(3)]
    pts = p.sb("pts", [128, 32, 512], BF16)
    ob = [p.sb("ob%d" % i, [128, 4, 128], F32) for i in range(2)]
    sm = [p.sb("sm%d" % i, [128, 4], F32) for i in range(3)]
    junk = p.sb("junk", [128, 128], F32)
    ks = 0
    ka = 0
    for qblk in range(8):
        Q0 = qblk * 512
        nkt = 4 * qblk + 4
        o = ob[qblk % 2]
        for m in range(2):
            for kt in range(nkt):
                ps = pss[ks % 3]
                ks += 1
                p.I("pe", "matmul", r=[kb[m], qb[m]], w=[ps], out=ps[:], lhsT=kb[m][:, kt * 128:(kt + 1) * 128],
                    rhs=qb[m][:, Q0:Q0 + 512], start=True, stop=True)
                dpt = pts.reg(kt)
                p.I("act", "activation", r=[ps], w=[dpt], out=pts[:, kt, :], in_=ps[:], func=AF.Exp, scale=0.125)
                if kt >= 4 * qblk:
                    p.I("pool", "tensor_tensor", r=[dpt, mk_], w=[dpt], out=pts[:, kt, :], in0=pts[:, kt, :],
                        in1=mk_[:, kt - 4 * qblk, :], op=ALU.mult)
            for j in range(4):
                pa = psa[ka % 3]
                ka += 1
                for kt in range(nkt):
                    p.I("pe", "matmul", r=[pts.reg(kt), vb], w=[pa], out=pa[:, 0:129], lhsT=pts[:, kt, j * 128:(j + 1) * 128],
                        rhs=vb[:, kt, :], start=(kt == 0), stop=(kt == nkt - 1))
                r = sm[ka % 3]
                p.I("dve", "reciprocal", r=[pa], w=[r], out=r[:, 0:1], in_=pa[:, 128:129])
                if m == 0:
                    p.I("dve", "tensor_scalar", r=[pa, r], w=[o], out=o[:, j, :], in0=pa[:, 0:128], scalar1=r[:, 0:1],
                        scalar2=None, op0=ALU.mult)
                else:
                    p.I("dve", "tensor_tensor", r=[r, nlam], w=[r], out=r[:, 1:2], in0=r[:, 0:1], in1=nlam[:], op=ALU.mult)
                    p.I("dve", "scalar_tensor_tensor", r=[pa, r, o], w=[o], out=o[:, j, :], in0=pa[:, 0:128],
                        scalar=r[:, 1:2], in1=o[:, j, :], op0=ALU.mult, op1=ALU.add)
                    p.I("act", "activation", r=[o], w=[junk, r], out=junk[:], in_=o[:, j, :], func=AF.Square,
                        accum_out=r[:, 2:3])
                    p.I("act", "activation", r=[r, epsb], w=[r], out=r[:, 3:4], in_=r[:, 2:3], func=AF.Sqrt, scale=1.0 / 128,
                        bias=epsb[:, 0:1])
                    p.I("dve", "reciprocal", r=[r], w=[r], out=r[:, 3:4], in_=r[:, 3:4])
                    p.I("dve", "scalar_tensor_tensor", r=[o, r, sgt], w=[o], out=o[:, j, :], in0=o[:, j, :],
                        scalar=r[:, 3:4], in1=sgt[:], op0=ALU.mult, op1=ALU.mult)
        p.dma(ya[Q0:Q0 + 512, :].rearrange("(j p) e -> p j e", p=128), o[:], r=[o])
    p.finish()
    return p


def mask_const():
    m = np.zeros((4, 128, 512), np.float32)
    pp = np.arange(128)[:, None] // 64
    ff = np.arange(512)[None, :] // 64
    for j in range(4):
        m[j] = ((2 * j + pp) <= ff)
    return m.astype(BF)


S = 4096
NCH = 64
EM05 = math.exp(-0.5)
GN_EPS = 64e-5


def build_B(stop=99):
    p = Prog()
    rkvT = p.dram("rkvT", [3, 128, S], F32, "ExternalInput")
    lowT = p.dram("lowT", [256, S], F32, "ExternalInput")
    chp = p.dram("chp", [128, 16], F32, "ExternalInput")
    wup = p.dram("wup", [128, 128], F32, "ExternalInput")
    gup = p.dram("gup", [128, 128], F32, "ExternalInput")
    cst = p.dram("cst", [128, 1152], F32, "ExternalInput")
    ybT = p.dram("ybT", [128, S], F32, "ExternalOutput")

    ch = p.sb("ch", [128, 16], F32)
    cs = p.sb("cs", [128, 1152], F32)
    wupf = p.sb("wupf", [128, 128], F32)
    gupf = p.sb("gupf", [128, 128], F32)
    wupb = p.sb("wupb", [128, 128], BF16)
    gupb = p.sb("gupb", [128, 128], BF16)
    bones = p.sb("bones", [128, 128], BF16)
    p.dma(ch[:], chp, w=[ch])
    p.dma(cs[:], cst, w=[cs])
    p.dma(wupf[:], wup, w=[wupf])
    p.dma(gupf[:], gup, w=[gupf])
    p.I("dve", "tensor_copy", r=[wupf], w=[wupb], out=wupb[:], in_=wupf[:])
    p.I("dve", "tensor_copy", r=[gupf], w=[gupb], out=gupb[:], in_=gupf[:])
    p.I("dve", "tensor_copy", r=[cs], w=[bones], out=bones[:], in_=cs[:, 0:128])
    ident = cs[:, 128:256]
    rmask = cs[:, 256:768]
    epsb = p.sb("epsb", [128, 2], F32)
    p.I("dve", "memset", w=[epsb], ap=epsb[:, 0:1], constant=GN_EPS)
    p.I("dve", "memset", w=[epsb], ap=epsb[:, 1:2], constant=0.0)

    ARd = p.nc.dram_tensor("ARd", [128, NCH * 128], BF16, kind="Internal").ap()
    BKd = p.nc.dram_tensor("BKd", [128, NCH * 128], BF16, kind="Internal").ap()
    PCd = p.nc.dram_tensor("PCd", [128, NCH], F32, kind="Internal").ap()
    yd2 = p.nc.dram_tensor("yd2", [128, S], F32, kind="Internal").ap()
    dARd, dBKd, dPCd, dyd2 = Dep(), Dep(), Dep(), Dep()
    ARs = [p.sb("ARs%d" % i, [128, 8, 2, 64], BF16) for i in range(2)]
    BKs = [p.sb("BKs%d" % i, [128, 8, 2, 64], BF16) for i in range(2)]
    Bh = p.sb("Bh", [64, NCH, 2, 64], BF16)
    Kh = p.sb("Kh", [64, NCH, 2, 64], BF16)
    Vh = p.sb("Vh", [64, NCH, 2, 64], BF16)
    PC = p.sb("PC", [128, NCH], F32)
    bonus = p.sb("bonus", [128, S], BF16)
    gT = p.sb("gT", [128, S], BF16)

    NT = 12
    tf = [p.sb("tf%d" % i, [128, 512], F32) for i in range(NT)]
    tb = [p.sb("tb%d" % i, [128, 512], BF16) for i in range(4)]
    xin = [p.sb("xin%d" % i, [128, 513], F32) for i in range(5)]
    ps = [p.ps("ps%d" % i, [128, 512], F32) for i in range(8)]

    MU_R, MU_K, MU_V, MU_WA, MU_G, W0, A0, KKG, KAG, RK, GNG, GNB = range(12)

    def col(i):
        return ch[:, i:i + 1]

    for blk in range(8):
        t0 = blk * 512
        srcs = [rkvT[0], rkvT[1], rkvT[2], lowT[0:128], lowT[128:256]]
        for i in range(5):
            if blk == 0:
                p.I("pool", "memset", w=[xin[i]], ap=xin[i][:, 0:1], constant=0.0)
                p.dma(xin[i][:, 1:513], srcs[i][:, 0:512], w=[xin[i]], q=("sp", "act")[i % 2])
            else:
                p.dma(xin[i][:], srcs[i][:, t0 - 1:t0 + 512], w=[xin[i]], q=("sp", "act")[i % 2])
        for i in range(5):
            d = tf[5]
            p.I("dve", "tensor_tensor", r=[xin[i]], w=[d], out=d[:], in0=xin[i][:, 0:512], in1=xin[i][:, 1:513], op=ALU.subtract)
            p.I("dve", "scalar_tensor_tensor", r=[d, ch, xin[i]], w=[tf[i]], out=tf[i][:], in0=d[:], scalar=col(MU_R + i),
                in1=xin[i][:, 1:513], op0=ALU.mult, op1=ALU.add)
        r_, k_, v_, wa_, gd_ = tf[0], tf[1], tf[2], tf[3], tf[4]
        p.I("act", "activation", r=[wa_], w=[tb[0]], out=tb[0][0:64, :], in_=wa_[0:64, :], func=AF.Tanh)
        p.I("act", "activation", r=[wa_], w=[tb[0]], out=tb[0][64:128, :], in_=wa_[64:128, :], func=AF.Copy)
        p.I("act", "activation", r=[gd_], w=[tb[1]], out=tb[1][:], in_=gd_[:], func=AF.Sigmoid)
        p.I("pe", "matmul", r=[wupb, tb[0]], w=[ps[0]], out=ps[0][:], lhsT=wupb[0:64, :], rhs=tb[0][0:64, :], start=True, stop=True)
        p.I("pe", "matmul", r=[wupb, tb[0]], w=[ps[1]], out=ps[1][:], lhsT=wupb[64:128, :], rhs=tb[0][64:128, :], start=True, stop=True)
        p.I("pe", "matmul", r=[gupb, tb[1]], w=[ps[2]], out=ps[2][:], lhsT=gupb[:], rhs=tb[1][:], start=True, stop=True)
        dl, a_ = tf[5], tf[6]
        p.I("act", "activation", r=[ps[0], ch], w=[dl], out=dl[:], in_=ps[0][:], func=AF.Sigmoid, bias=col(W0))
        p.I("dve", "tensor_scalar", r=[dl], w=[dl], out=dl[:], in0=dl[:], scalar1=-EM05, scalar2=None, op0=ALU.mult)
        p.I("act", "activation", r=[ps[1], ch], w=[a_], out=a_[:], in_=ps[1][:], func=AF.Sigmoid, bias=col(A0))
        p.I("act", "activation", r=[ps[2]], w=[gT], out=gT[:, t0:t0 + 512], in_=ps[2][:], func=AF.Copy)
        kk, kap = tf[7], tf[8]
        p.I("dve", "tensor_scalar", r=[k_, ch], w=[kk], out=kk[:], in0=k_[:], scalar1=col(KKG), scalar2=None, op0=ALU.mult)
        p.I("pool", "tensor_tensor", r=[kk], w=[tb[2]], out=tb[2][:], in0=kk[:], in1=kk[:], op=ALU.mult)
        p.I("pe", "matmul", r=[bones, tb[2]], w=[ps[3]], out=ps[3][:], lhsT=bones[:], rhs=tb[2][:], start=True, stop=True)
        rn = tf[9]
        p.I("act", "activation", r=[ps[3], epsb], w=[rn], out=rn[:], in_=ps[3][:], func=AF.Sqrt, bias=epsb[:, 1:2])
        p.I("dve", "tensor_scalar", r=[rn], w=[rn], out=rn[:], in0=rn[:], scalar1=1e-12, scalar2=None, op0=ALU.max)
        p.I("dve", "reciprocal", r=[rn], w=[rn], out=rn[:], in_=rn[:])
        p.I("dve", "tensor_tensor", r=[kk, rn], w=[kap], out=kap[:], in0=kk[:], in1=rn[:], op=ALU.mult)
        km = tf[7]
        p.I("dve", "tensor_scalar", r=[a_, ch], w=[tf[9]], out=tf[9][:], in0=a_[:], scalar1=-1.0, scalar2=col(KAG), op0=ALU.add, op1=ALU.mult)
        p.I("dve", "scalar_tensor_tensor", r=[tf[9], k_], w=[km], out=km[:], in0=tf[9][:], scalar=1.0, in1=k_[:], op0=ALU.add, op1=ALU.mult)
        p.I("dve", "scalar_tensor_tensor", r=[r_, ch, km], w=[tb[3]], out=tb[3][:], in0=r_[:], scalar=col(RK), in1=km[:], op0=ALU.mult, op1=ALU.mult)
        p.I("pe", "matmul", r=[bones, tb[3]], w=[ps[4]], out=ps[4][:], lhsT=bones[:], rhs=tb[3][:], start=True, stop=True)
        p.I("dve", "tensor_tensor", r=[ps[4], v_], w=[bonus], out=bonus[:, t0:t0 + 512], in0=ps[4][:], in1=v_[:], op=ALU.mult)
        L = tf[9]
        p.I("dve", "tensor_tensor_scan", r=[cs, dl], w=[L], out=L[:], data0=rmask, data1=dl[:], initial=0.0, op0=ALU.mult, op1=ALU.add)
        P_, Pp, Pi, E_ = tf[10], tf[11], tf[1], tf[4]
        p.I("act", "activation", r=[L], w=[P_], out=P_[:], in_=L[:], func=AF.Exp)
        p.I("dve", "tensor_tensor", r=[L, dl], w=[Pp], out=Pp[:], in0=L[:], in1=dl[:], op=ALU.subtract)
        p.I("act", "activation", r=[Pp], w=[Pp], out=Pp[:], in_=Pp[:], func=AF.Exp)
        p.I("act", "activation", r=[L], w=[Pi], out=Pi[:], in_=L[:], func=AF.Exp, scale=-1.0)
        Lv = L[:].rearrange("p (c t) -> p c t", t=64)
        p.I("dve", "tensor_tensor", r=[L], w=[E_], out=E_[:].rearrange("p (c t) -> p c t", t=64), in0=Lv,
            in1=Lv[:, :, 63:64].to_broadcast([128, 8, 64]), op=ALU.subtract)
        p.I("act", "activation", r=[E_], w=[E_], out=E_[:], in_=E_[:], func=AF.Exp, scale=-1.0)
        p.I("pool", "tensor_copy", r=[P_], w=[PC], out=PC[:, blk * 8:(blk + 1) * 8],
            in_=P_[:].rearrange("p (c t) -> p c t", t=64)[:, :, 63])
        csl = slice(0, 8)
        AR, BK = ARs[blk % 2], BKs[blk % 2]
        v3 = lambda t: t[:].rearrange("p (c t) -> p c t", t=64)
        p.I("dve", "scalar_tensor_tensor", r=[kap, Pp], w=[AR], out=AR[:, csl, 0, :], in0=v3(kap), scalar=-1.0, in1=v3(Pp), op0=ALU.mult, op1=ALU.mult)
        p.I("pool", "tensor_tensor", r=[r_, P_], w=[AR], out=AR[:, csl, 1, :], in0=v3(r_), in1=v3(P_), op=ALU.mult)
        ka = tf[5]
        p.I("dve", "tensor_tensor", r=[kap, a_], w=[ka], out=ka[:], in0=kap[:], in1=a_[:], op=ALU.mult)
        p.I("dve", "tensor_tensor", r=[ka, Pi], w=[BK], out=BK[:, csl, 0, :], in0=v3(ka), in1=v3(Pi), op=ALU.mult)
        p.I("pool", "tensor_tensor", r=[km, Pi], w=[BK], out=BK[:, csl, 1, :], in0=v3(km), in1=v3(Pi), op=ALU.mult)
        p.dma(ARd[:, blk * 1024:(blk + 1) * 1024], AR[:].rearrange("p c a k -> p (c a k)"), r=[AR], w=[dARd])
        p.dma(BKd[:, blk * 1024:(blk + 1) * 1024], BK[:].rearrange("p c a k -> p (c a k)"), r=[BK], w=[dBKd], q="act")
        Bf, Kf = tf[6], tf[8]
        p.I("dve", "tensor_tensor", r=[ka, E_], w=[Bf], out=Bf[:], in0=ka[:], in1=E_[:], op=ALU.mult)
        p.I("pool", "tensor_tensor", r=[km, E_], w=[Kf], out=Kf[:], in0=km[:], in1=E_[:], op=ALU.mult)
        for (src, dst, pi) in ((Bf, Bh, 5), (Kf, Kh, 6), (v_, Vh, 7)):
            for half in range(2):
                pt = ps[pi] if half == 0 else ps[(pi + 3) % 8 if pi != 7 else 0]
                for c4 in range(4):
                    c = half * 4 + c4
                    p.I("pe", "transpose", r=[src, cs], w=[pt], out=pt[0:64, c4 * 128:(c4 + 1) * 128], in_=src[:, c * 64:(c + 1) * 64], identity=ident)
                p.I("act", "activation", r=[pt], w=[dst], out=dst[:, blk * 8 + half * 4:blk * 8 + half * 4 + 4, :, :],
                    in_=pt[0:64, :].rearrange("p (c h k) -> p c h k", c=4, h=2), func=AF.Copy)

    p.dma(PCd, PC[:], r=[PC], w=[dPCd])
    if stop == 1:
        p.finish()
        return p
    m5 = cs[0:64, 768:1088]
    eye = cs[0:64, 1088:1152]
    mstrict = cs[0:64, 768:832]
    mlower = cs[0:64, 1024:1088]
    m4 = cs[0:64, 768:1024]
    ARx = p.sb("ARx", [64, NCH, 2, 64], BF16)
    BKx = p.sb("BKx", [64, NCH, 2, 64], BF16)
    PCx = p.sb("PCx", [64, NCH], F32)
    TT = p.sb("TT", [64, NCH, 64], BF16)
    yTh = p.sb("yTh", [64, S], F32)
    LM = [[p.sb("LM%d_%d" % (s_, i), [64, 2, 64], F32) for i in range(2)] for s_ in range(8)]
    XX = [[p.sb("XX%d_%d" % (s_, i), [64, 64], F32) for i in range(2)] for s_ in range(8)]
    S32 = p.sb("S32", [64, 64], F32)
    Sb = p.sb("Sb", [64, 64], BF16)
    Am = [p.sb("Am%d" % i, [64, 4, 64], BF16) for i in range(2)]
    Zb = [p.sb("Zb%d" % i, [64, 64], BF16) for i in range(2)]
    Ub = [p.sb("Ub%d" % i, [64, 64], BF16) for i in range(2)]
    for hd in range(2):
        hs = slice(hd * 64, (hd + 1) * 64)
        p.dma(ARx[:].rearrange("p n a k -> p (n a k)"), ARd[hs, :], r=[dARd], w=[ARx])
        p.dma(BKx[:].rearrange("p n a k -> p (n a k)"), BKd[hs, :], r=[dBKd], w=[BKx], q="act")
        p.dma(PCx[:], PCd[hs, :], r=[dPCd], w=[PCx])
        for n in range(NCH):
            s_ = n % 4
            pa, pb = ps[2 * s_], ps[2 * s_ + 1]
            lm0, x0 = LM[s_][0], XX[s_][0]
            p.I("pe", "matmul", r=[BKx, ARx], w=[pa], out=pa[0:64, 64:128], lhsT=BKx[:, n, 0, :], rhs=ARx[:, n, 0, :], start=True, stop=True)
            p.I("pe", "matmul", r=[BKx, ARx], w=[pa], out=pa[0:64, 0:64], lhsT=ARx[:, n, 0, :], rhs=BKx[:, n, 0, :], start=True, stop=True)
            p.I("dve", "tensor_tensor", r=[pa, cs], w=[lm0], out=lm0[:, 0, :], in0=pa[0:64, 0:64], in1=mlower, op=ALU.mult)
            p.I("dve", "tensor_tensor", r=[pa, cs], w=[lm0], out=lm0[:, 1, :], in0=pa[0:64, 64:128], in1=mstrict, op=ALU.mult)
            p.I("dve", "tensor_tensor", r=[lm0, cs], w=[x0], out=x0[:], in0=lm0[:, 1, :], in1=eye, op=ALU.add)
            cur = 0
            for j in range(1, 6):
                lmp, lmn = LM[s_][cur], LM[s_][1 - cur]
                xp, xn = XX[s_][cur], XX[s_][1 - cur]
                p.I("pe", "matmul", r=[lmp], w=[pa], out=pa[0:64, 0:64], lhsT=lmp[:, 1, :], rhs=lmp[:, 0, :], start=True, stop=True)
                if j < 5:
                    p.I("pe", "matmul", r=[lmp], w=[pa], out=pa[0:64, 64:128], lhsT=lmp[:, 0, :], rhs=lmp[:, 1, :], start=True, stop=True)
                p.I("act", "activation", r=[pa], w=[lmn], out=lmn[:].rearrange("p two k -> p (two k)"), in_=pa[0:64, 0:128], func=AF.Copy)
                p.I("pe", "matmul", r=[lmn, xp], w=[pb], out=pb[0:64, 0:64], lhsT=lmn[:, 0, :], rhs=xp[:], start=True, stop=True)
                p.I("dve", "tensor_tensor", r=[pb, xp], w=[xn], out=xn[:], in0=pb[0:64, 0:64], in1=xp[:], op=ALU.add)
                cur = 1 - cur
            p.I("pool", "tensor_copy", r=[XX[s_][cur]], w=[TT.reg(n)], out=TT[:, n, :], in_=XX[s_][cur][:])
        if stop == 2 + 2 * hd:
            p.finish()
            return p
        p.I("dve", "memset", w=[S32], ap=S32[:], constant=0.0)
        p.I("pool", "memset", w=[Sb], ap=Sb[:], constant=0.0)
        for n in range(NCH):
            am, zb, ub = Am[n % 2], Zb[n % 2], Ub[n % 2]
            o4 = 5 * (n % 2)
            pg, pz, pu, py, pS = ps[o4], ps[o4 + 1], ps[o4 + 2], ps[3], ps[4]
            p.I("pe", "matmul", r=[BKx, ARx], w=[pg], out=pg[0:64, 0:128], lhsT=BKx[:, n, 0, :], rhs=ARx[:, n, :, :], start=True, stop=True)
            p.I("pe", "matmul", r=[BKx, ARx], w=[pg], out=pg[0:64, 128:256], lhsT=BKx[:, n, 1, :], rhs=ARx[:, n, :, :], start=True, stop=True)
            p.I("dve", "tensor_tensor", r=[pg, cs], w=[am], out=am[:].rearrange("p q t -> p (q t)"), in0=pg[0:64, 0:256], in1=m4, op=ALU.mult)
            p.I("pe", "matmul", r=[am, Vh], w=[pz], out=pz[0:64, 0:64], lhsT=am[:, 2, :], rhs=Vh[:, n, hd, :], start=True, stop=False)
            p.I("pe", "matmul", r=[ARx, Sb], w=[pz], out=pz[0:64, 0:64], lhsT=ARx[:, n, 0, :], rhs=Sb[:], start=False, stop=True)
            p.I("act", "activation", r=[pz], w=[zb], out=zb[:], in_=pz[0:64, 0:64], func=AF.Copy)
            p.I("pe", "matmul", r=[TT.reg(n), zb], w=[pu], out=pu[0:64, 0:64], lhsT=TT[:, n, :], rhs=zb[:], start=True, stop=True)
            p.I("act", "activation", r=[pu], w=[ub], out=ub[:], in_=pu[0:64, 0:64], func=AF.Copy)
            p.I("pe", "matmul", r=[Sb, ARx], w=[py], out=py[0:64, 0:64], lhsT=Sb[:], rhs=ARx[:, n, 1, :], start=True, stop=False)
            p.I("pe", "matmul", r=[ub, am], w=[py], out=py[0:64, 0:64], lhsT=ub[:], rhs=am[:, 1, :], start=False, stop=False)
            p.I("pe", "matmul", r=[Vh, am], w=[py], out=py[0:64, 0:64], lhsT=Vh[:, n, hd, :], rhs=am[:, 3, :], start=False, stop=True)
            p.I("pe", "matmul", r=[Bh, ub], w=[pS], out=pS[0:64, 64:128], lhsT=Bh[:, n, hd, :], rhs=ub[:], start=True, stop=False)
            p.I("pe", "matmul", r=[Kh, Vh], w=[pS], out=pS[0:64, 64:128], lhsT=Kh[:, n, hd, :], rhs=Vh[:, n, hd, :], start=False, stop=True)
            p.I("act", "activation", r=[py], w=[yTh], out=yTh[:, n * 64:(n + 1) * 64], in_=py[0:64, 0:64], func=AF.Copy)
            p.I("dve", "scalar_tensor_tensor", r=[S32, PCx, pS], w=[S32], out=S32[:], in0=S32[:], scalar=PCx[:, n:n + 1], in1=pS[0:64, 64:128],
                op0=ALU.mult, op1=ALU.add)
            p.I("dve", "tensor_copy", r=[S32], w=[Sb], out=Sb[:], in_=S32[:])
        p.dma(yd2[hs, :], yTh[:], r=[yTh], w=[dyd2])
        if stop == 3 + 2 * hd:
            p.finish()
            return p

    bavg = p.sb("bavg", [128, 128], F32)
    p.I("dve", "tensor_scalar", r=[cs], w=[bavg], out=bavg[:], in0=cs[:, 0:128], scalar1=1.0 / 64, scalar2=None, op0=ALU.mult)
    for blk in range(8):
        sl = slice(blk * 512, (blk + 1) * 512)
        pm, pv = ps[(2 * blk) % 8], ps[(2 * blk + 1) % 8]
        yc, y2, o, yl = tf[0], tf[1], tf[2], tf[3 + blk % 2]
        p.dma(yl[:], yd2[:, sl], r=[dyd2], w=[yl])
        p.I("pool", "tensor_copy", r=[yl], w=[tb[0]], out=tb[0][:], in_=yl[:])
        p.I("pe", "matmul", r=[bones, tb[0]], w=[pm], out=pm[:], lhsT=bones[:], rhs=tb[0][:], start=True, stop=True)
        p.I("dve", "scalar_tensor_tensor", r=[yl, pm], w=[yc], out=yc[:], in0=pm[:], scalar=-1.0 / 64, in1=yl[:], op0=ALU.mult, op1=ALU.add)
        p.I("pool", "tensor_tensor", r=[yc], w=[tb[1]], out=tb[1][:], in0=yc[:], in1=yc[:], op=ALU.mult)
        p.I("pe", "matmul", r=[bones, tb[1]], w=[pv], out=pv[:], lhsT=bones[:], rhs=tb[1][:], start=True, stop=True)
        p.I("act", "activation", r=[pv, epsb], w=[y2], out=y2[:], in_=pv[:], func=AF.Sqrt, bias=epsb[:, 0:1], scale=1.0 / 64)
        p.I("dve", "reciprocal", r=[y2], w=[y2], out=y2[:], in_=y2[:])
        p.I("dve", "tensor_tensor", r=[yc, y2], w=[yc], out=yc[:], in0=yc[:], in1=y2[:], op=ALU.mult)
        p.I("dve", "tensor_scalar", r=[yc, ch], w=[yc], out=yc[:], in0=yc[:], scalar1=col(GNG), scalar2=col(GNB), op0=ALU.mult, op1=ALU.add)
        p.I("dve", "tensor_tensor", r=[yc, bonus], w=[yc], out=yc[:], in0=yc[:], in1=bonus[:, sl], op=ALU.add)
        p.I("dve", "tensor_tensor", r=[yc, gT], w=[o], out=o[:], in0=yc[:], in1=gT[:, sl], op=ALU.mult)
        p.dma(ybT[:, sl], o[:], r=[o])
    p.finish()
    return p


def cst_B():
    c = np.zeros((128, 1152), np.float32)
    c[0:64, 0:64] = 1.0
    c[64:128, 64:128] = 1.0
    c[:, 128:256] = np.eye(128)
    rm = np.ones(512, np.float32)
    rm[0::64] = 0.0
    c[:, 256:768] = rm[None, :]
    i = np.arange(64)[:, None]
    t = np.arange(64)[None, :]
    strict = (i < t).astype(np.float32)
    incl = (i <= t).astype(np.float32)
    lower = (t < i).astype(np.float32)
    c[0:64, 768:832] = strict
    c[0:64, 832:896] = incl
    c[0:64, 896:960] = strict
    c[0:64, 960:1024] = incl
    c[0:64, 1024:1088] = lower
    c[0:64, 1088:1152] = np.eye(64)
    return c


S = 4096
NCH = 64


def build_C(layer):
    p = Prog()
    qfT = p.dram("qfT", [2, 128, S], F32, "ExternalInput")
    ig = p.dram("ig", [2, S, 128], F32, "ExternalInput")
    lbl = p.dram("lbl", [128, 2], F32, "ExternalInput")
    ng = p.dram("ng", [64, 128], F32, "ExternalInput")
    cst = p.dram("cst", [128, 768], F32, "ExternalInput")
    yc = p.dram("yc", [S, 128], F32, "ExternalOutput")
    cs = p.sb("cs", [128, 768], F32)
    lb = p.sb("lb", [128, 4], F32)
    ngt = p.sb("ngt", [64, 128], F32)
    epsb = p.sb("epsb", [128, 1], F32)
    p.I("dve", "memset", w=[epsb], ap=epsb[:], constant=1e-6)
    p.dma(cs[:], cst, w=[cs])
    p.dma(lb[:, 0:2], lbl, w=[lb])
    p.dma(ngt[:], ng, w=[ngt])
    rmask = cs[:, 0:512]
    ident = cs[:, 512:640]
    incl = cs[0:64, 640:704]
    if layer == 0:
        p.I("dve", "memset", w=[lb], ap=lb[:, 2:3], constant=0.0)
    else:
        p.I("dve", "tensor_tensor", r=[lb], w=[lb], out=lb[:, 2:3], in0=lb[:, 1:2], in1=lb[:, 0:1], op=ALU.subtract)
        p.I("act", "activation", r=[lb], w=[lb], out=lb[:, 2:3], in_=lb[:, 2:3], func=AF.Sigmoid)
    p.I("dve", "tensor_scalar", r=[lb], w=[lb], out=lb[:, 3:4], in0=lb[:, 2:3], scalar1=-1.0, scalar2=1.0, op0=ALU.mult, op1=ALU.add)

    Qt = p.sb("Qt", [128, S], BF16)
    Kt = p.sb("Kt", [128, S], BF16)
    Qb = p.sb("Qb", [128, S], BF16)
    Kbh = p.sb("Kbh", [64, NCH, 128], BF16)
    Ih = p.sb("Ih", [64, NCH, 128], BF16)
    Sall = p.sb("Sall", [128, NCH, 128], BF16)
    dec = p.sb("dec", [128, NCH], F32)
    tf = [p.sb("tf%d" % i, [128, 512], F32) for i in range(8)]
    xin = [p.sb("xin%d" % i, [128, 512], F32) for i in range(2)]
    ist = [p.sb("ist%d" % i, [64, 8, 128], F32) for i in range(2)]
    ps = [p.ps("ps%d" % i, [128, 512], F32) for i in range(8)]
    v3 = lambda t: t[:].rearrange("p (c t) -> p c t", t=64)
    igv = ig.rearrange("w (n s) v -> w s n v", s=64)
    for blk in range(8):
        sl = slice(blk * 512, (blk + 1) * 512)
        p.dma(xin[0][:], qfT[0][:, sl], w=[xin[0]])
        p.dma(xin[1][:], qfT[1][:, sl], w=[xin[1]], q="act")
        it = ist[blk % 2]
        p.dma(it[:], igv[0][:, blk * 8:(blk + 1) * 8, :], w=[it])
        p.I("pool", "tensor_copy", r=[it], w=[Ih], out=Ih[:, blk * 8:(blk + 1) * 8, :], in_=it[:])
        qf, fg, lf, kf, b, t1, t2, t3 = tf
        p.I("act", "activation", r=[xin[0]], w=[qf], out=qf[:], in_=xin[0][:], func=AF.Silu)
        p.I("act", "activation", r=[xin[1]], w=[fg], out=fg[:], in_=xin[1][:], func=AF.Sigmoid)
        p.I("dve", "tensor_scalar", r=[fg, lb], w=[fg], out=fg[:], in0=fg[:], scalar1=lb[:, 3:4], scalar2=lb[:, 2:3], op0=ALU.mult, op1=ALU.add)
        p.I("act", "activation", r=[fg], w=[lf], out=lf[:], in_=fg[:], func=AF.Ln)
        p.I("dve", "tensor_scalar", r=[fg], w=[kf], out=kf[:], in0=fg[:], scalar1=-1.0, scalar2=1.0, op0=ALU.mult, op1=ALU.add)
        p.I("dve", "tensor_tensor_scan", r=[cs, lf], w=[b], out=b[:], data0=rmask, data1=lf[:], initial=0.0, op0=ALU.mult, op1=ALU.add)
        bv = v3(b)
        p.I("dve", "tensor_tensor", r=[b], w=[t1], out=v3(t1), in0=bv, in1=bv[:, :, 31:32].to_broadcast([128, 8, 64]), op=ALU.subtract)
        p.I("act", "activation", r=[t1], w=[t2], out=t2[:], in_=t1[:], func=AF.Exp)
        p.I("dve", "tensor_tensor", r=[qf, t2], w=[Qt], out=Qt[:, sl], in0=qf[:], in1=t2[:], op=ALU.mult)
        p.I("act", "activation", r=[t1], w=[t2], out=t2[:], in_=t1[:], func=AF.Exp, scale=-1.0)
        p.I("dve", "tensor_tensor", r=[kf, t2], w=[Kt], out=Kt[:, sl], in0=kf[:], in1=t2[:], op=ALU.mult)
        p.I("act", "activation", r=[b], w=[t2], out=t2[:], in_=b[:], func=AF.Exp)
        p.I("pool", "tensor_tensor", r=[qf, t2], w=[Qb], out=Qb[:, sl], in0=qf[:], in1=t2[:], op=ALU.mult)
        p.I("pool", "tensor_copy", r=[t2], w=[dec], out=dec[:, blk * 8:(blk + 1) * 8], in_=v3(t2)[:, :, 63])
        p.I("dve", "tensor_tensor", r=[b], w=[t1], out=v3(t1), in0=bv, in1=bv[:, :, 63:64].to_broadcast([128, 8, 64]), op=ALU.subtract)
        p.I("act", "activation", r=[t1], w=[t3], out=t3[:], in_=t1[:], func=AF.Exp, scale=-1.0)
        p.I("dve", "tensor_tensor", r=[kf, t3], w=[t3], out=t3[:], in0=kf[:], in1=t3[:], op=ALU.mult)
        for half in range(2):
            pt = ps[half]
            for c4 in range(4):
                c = half * 4 + c4
                p.I("pe", "transpose", r=[t3, cs], w=[pt], out=pt[0:64, c4 * 128:(c4 + 1) * 128], in_=t3[:, c * 64:(c + 1) * 64], identity=ident)
            p.I("act", "activation", r=[pt], w=[Kbh], out=Kbh[:, blk * 8 + half * 4:blk * 8 + half * 4 + 4, :],
                in_=pt[0:64, :].rearrange("p (c k) -> p c k", c=4), func=AF.Copy)
    St = p.sb("St", [128, 128], F32)
    p.I("dve", "memset", w=[St], ap=St[:], constant=0.0)
    p.I("pool", "memset", w=[Sall.reg(0)], ap=Sall[:, 0, :], constant=0.0)
    for n in range(NCH - 1):
        pk = ps[2 + n % 3]
        p.I("pe", "matmul", r=[Kbh, Ih], w=[pk], out=pk[:, 0:128], lhsT=Kbh[:, n, :], rhs=Ih[:, n, :], start=True, stop=True)
        p.I("dve", "scalar_tensor_tensor", r=[St, dec, pk], w=[St], out=St[:], in0=St[:], scalar=dec[:, n:n + 1], in1=pk[:, 0:128],
            op0=ALU.mult, op1=ALU.add)
        p.I("pool", "tensor_copy", r=[St], w=[Sall.reg(n + 1)], out=Sall[:, n + 1, :], in_=St[:])
    at = [p.sb("at%d" % i, [64, 64], BF16) for i in range(2)]
    o1 = [p.sb("o1_%d" % i, [64, 128], F32) for i in range(2)]
    ot = [p.sb("ot%d" % i, [64, 8, 128], F32) for i in range(2)]
    gs = [p.sb("gs%d" % i, [64, 8, 128], F32) for i in range(2)]
    sm = [p.sb("sm%d" % i, [64, 2], F32) for i in range(2)]
    junk = p.sb("junk", [64, 128], F32)
    for n in range(NCH):
        g8 = n // 8
        if n % 8 == 0:
            gt = gs[g8 % 2]
            p.dma(gt[:], igv[1][:, n:n + 8, :], w=[gt])
            p.I("act", "activation", r=[gt], w=[gt], out=gt[:], in_=gt[:], func=AF.Silu)
            p.I("dve", "tensor_tensor", r=[gt, ngt], w=[gt], out=gt[:], in0=gt[:],
                in1=ngt[:].rearrange("p (o v) -> p o v", o=1).to_broadcast([64, 8, 128]), op=ALU.mult)
        gt = gs[g8 % 2]
        o8 = ot[g8 % 2]
        csl = slice(n * 64, (n + 1) * 64)
        pa, po = ps[5 + n % 2], ps[7 if n % 2 else 0]
        p.I("pe", "matmul", r=[Kt, Qt], w=[pa], out=pa[0:64, 0:64], lhsT=Kt[:, csl], rhs=Qt[:, csl], start=True, stop=True)
        a = at[n % 2]
        p.I("dve", "tensor_tensor", r=[pa, cs], w=[a], out=a[:], in0=pa[0:64, 0:64], in1=incl, op=ALU.mult)
        p.I("pe", "matmul", r=[a, Ih], w=[po], out=po[0:64, 0:128], lhsT=a[:], rhs=Ih[:, n, :], start=True, stop=True)
        p.I("pe", "matmul", r=[Qb, Sall.reg(n)], w=[po], out=po[0:64, 128:256], lhsT=Qb[:, csl], rhs=Sall[:, n, :], start=True, stop=True)
        oo = o1[n % 2]
        p.I("act", "activation", r=[po], w=[oo], out=oo[:], in_=po[0:64, 0:128], func=AF.Copy)
        p.I("dve", "tensor_tensor", r=[oo, po], w=[oo], out=oo[:], in0=oo[:], in1=po[0:64, 128:256], op=ALU.add)
        r = sm[n % 2]
        p.I("act", "activation", r=[oo], w=[junk, r], out=junk[:], in_=oo[:], func=AF.Square, accum_out=r[:, 0:1])
        p.I("act", "activation", r=[r, epsb], w=[r], out=r[:, 1:2], in_=r[:, 0:1], func=AF.Sqrt, scale=1.0 / 128, bias=epsb[0:64, 0:1])
        p.I("dve", "reciprocal", r=[r], w=[r], out=r[:, 1:2], in_=r[:, 1:2])
        p.I("dve", "scalar_tensor_tensor", r=[oo, r, gt], w=[o8], out=o8[:, n % 8, :], in0=oo[:], scalar=r[:, 1:2], in1=gt[:, n % 8, :],
            op0=ALU.mult, op1=ALU.mult)
        if n % 8 == 7:
            p.dma(yc.rearrange("(n s) v -> s n v", s=64)[:, n - 7:n + 1, :], o8[:], r=[o8])
    p.finish()
    return p


def cst_C():
    c = np.zeros((128, 768), np.float32)
    rm = np.ones(512, np.float32)
    rm[0::64] = 0.0
    c[:, 0:512] = rm[None, :]
    c[:, 512:640] = np.eye(128)
    i = np.arange(64)[:, None]
    t = np.arange(64)[None, :]
    c[0:64, 640:704] = (i <= t)
    return c


S = 4096
NEG = -30000.0


def build_D():
    p = Prog()
    qT = p.dram("qT", [64, 8192], F32, "ExternalInput")
    iqT = p.dram("iqT", [64, 8192], F32, "ExternalInput")
    iw = p.dram("iw", [128, 64], F32, "ExternalInput")
    kT = p.dram("kT", [64, S], F32, "ExternalInput")
    ikT = p.dram("ikT", [64, S], F32, "ExternalInput")
    v = p.dram("v", [S, 64], F32, "ExternalInput")
    vmask = p.dram("vmask", [128, 512], F32, "ExternalInput")
    id4 = p.dram("id4", [128, 512], BF16, "ExternalInput")
    yd = p.dram("yd", [1024, 512], F32, "ExternalOutput")
    qb = p.sb("qb", [64, 8192], BF16)
    iqb = p.sb("iqb", [64, 8192], BF16)
    kb = p.sb("kb", [64, S], BF16)
    ikb = p.sb("ikb", [64, S], BF16)
    vb = p.sb("vb", [128, 32, 65], BF16)
    iwt = p.sb("iwt", [128, 64], F32)
    vm = p.sb("vm", [128, 512], F32)
    i4 = p.sb("i4", [128, 512], BF16)
    st = [p.sb("st%d" % i, [128, 2048], F32) for i in range(2)]
    k = 0
    for (src, dst, n) in ((qT, qb, 8192), (iqT, iqb, 8192), (kT, kb, S), (ikT, ikb, S)):
        for c0 in range(0, n, 2048):
            s = st[k % 2]
            k += 1
            p.dma(s[0:64, :], src[:, c0:c0 + 2048], w=[s])
            p.I(("dve", "pool")[k % 2], "tensor_copy", r=[s], w=[dst], out=dst[:, c0:c0 + 2048], in_=s[0:64, :])
    s = st[k % 2]
    k += 1
    p.dma(s[:].rearrange("p (t e) -> p t e", e=64), v.rearrange("(t p) e -> p t e", p=128), w=[s])
    p.I("pool", "memset", w=[vb], ap=vb[:], constant=1.0)
    p.I("dve", "tensor_copy", r=[s], w=[vb], out=vb[:, :, 0:64], in_=s[:].rearrange("p (t e) -> p t e", e=64))
    p.dma(iwt[:], iw, w=[iwt])
    p.dma(vm[:], vmask, w=[vm])
    p.dma(i4[:], id4, w=[i4])
    p.I("dve", "tensor_scalar", r=[iwt], w=[iwt], out=iwt[:], in0=iwt[:], scalar1=(8 ** -0.5) * (64 ** -0.5), scalar2=None, op0=ALU.mult)

    sc = p.sb("sc", [128, S], F32)
    wk = p.sb("wk", [128, S], F32)
    biasb = [p.sb("bias%d" % i, [128, S], BF16) for i in range(2)]
    pts = p.sb("pts", [128, 32, 512], BF16)
    rl = [p.sb("rl%d" % i, [128, 512], F32) for i in range(2)]
    mx = p.sb("mx", [128, 8], F32)
    yo = [p.sb("yo%d" % i, [128, 8, 64], F32) for i in range(2)]
    sm = [p.sb("sm%d" % i, [128, 2], F32) for i in range(3)]
    pss = [p.ps("pss%d" % i, [128, 512], F32) for i in range(4)]
    psa = [p.ps("psa%d" % i, [128, 512], F32) for i in range(3)]
    cnt = {"ks": 0, "ka": 0, "kr": 0}

    def idx(m):
        for g in range(m + 1):
            gs = slice(g * 512, (g + 1) * 512)
            for h in range(8):
                ps = pss[cnt["ks"] % 4]
                cnt["ks"] += 1
                p.I("pe", "matmul", r=[iqb, ikb], w=[ps], out=ps[:], lhsT=iqb[:, m * 1024 + h * 128:m * 1024 + (h + 1) * 128],
                    rhs=ikb[:, gs], start=True, stop=True)
                r = rl[cnt["kr"] % 2]
                cnt["kr"] += 1
                p.I("act", "activation", r=[ps], w=[r], out=r[:], in_=ps[:], func=AF.Relu)
                ws = iwt[:, m * 8 + h:m * 8 + h + 1]
                if h == 0:
                    p.I("dve", "tensor_scalar", r=[r, iwt], w=[sc], out=sc[:, gs], in0=r[:], scalar1=ws, scalar2=None, op0=ALU.mult)
                else:
                    p.I("dve", "scalar_tensor_tensor", r=[r, iwt, sc], w=[sc], out=sc[:, gs], in0=r[:], scalar=ws, in1=sc[:, gs],
                        op0=ALU.mult, op1=ALU.add)
            if g == m:
                p.I("dve", "tensor_tensor", r=[sc, vm], w=[sc], out=sc[:, gs], in0=sc[:, gs], in1=vm[:], op=ALU.add)

    def topk_bias(m):
        N = (m + 1) * 512
        bias = biasb[m % 2]
        p.I("pool", "tensor_copy", r=[sc], w=[wk], out=wk[:, 0:N], in_=sc[:, 0:N])
        for rnd in range(32):
            p.I("dve", "max", r=[wk], w=[mx], out=mx[:], in_=wk[:, 0:N])
            if rnd < 31:
                p.I("dve", "match_replace", r=[wk, mx], w=[wk], out=wk[:, 0:N], in_to_replace=mx[:], in_values=wk[:, 0:N], imm_value=-1e30)
        p.I("dve", "tensor_scalar", r=[sc, mx], w=[bias], out=bias[:, 0:N], in0=sc[:, 0:N], scalar1=mx[:, 7:8], scalar2=NEG,
            op0=ALU.is_lt, op1=ALU.mult)
        p.I("dve", "tensor_tensor", r=[bias, vm], w=[bias], out=bias[:, m * 512:N], in0=bias[:, m * 512:N], in1=vm[:], op=ALU.add)

    def attn(m):
        nkt = 4 * m + 4
        bias = biasb[m % 2]
        o = yo[m % 2]
        for hg in range(2):
            for kt in range(nkt):
                ps = pss[cnt["ks"] % 4]
                cnt["ks"] += 1
                p.I("pe", "matmul", r=[kb, qb], w=[ps], out=ps[:], lhsT=kb[:, kt * 128:(kt + 1) * 128],
                    rhs=qb[:, m * 1024 + hg * 512:m * 1024 + (hg + 1) * 512], start=True, stop=False)
                p.I("pe", "matmul", r=[bias, i4], w=[ps], out=ps[:], lhsT=bias[:, kt * 128:(kt + 1) * 128], rhs=i4[:],
                    start=False, stop=True)
                p.I("act", "activation", r=[ps], w=[pts.reg(kt)], out=pts[:, kt, :], in_=ps[:], func=AF.Exp, scale=0.125)
            for h4 in range(4):
                pa = psa[cnt["ka"] % 3]
                r = sm[cnt["ka"] % 3]
                cnt["ka"] += 1
                for kt in range(nkt):
                    p.I("pe", "matmul", r=[pts.reg(kt), vb], w=[pa], out=pa[:, 0:65], lhsT=pts[:, kt, h4 * 128:(h4 + 1) * 128],
                        rhs=vb[:, kt, :], start=(kt == 0), stop=(kt == nkt - 1))
                p.I("act", "activation", r=[pa], w=[r], out=r[:, 0:1], in_=pa[:, 64:65], func=AF.Ln)
                p.I("act", "activation", r=[r], w=[r], out=r[:, 1:2], in_=r[:, 0:1], func=AF.Exp, scale=-1.0)
                p.I("act", "activation", r=[pa, r], w=[o], out=o[:, hg * 4 + h4, :], in_=pa[:, 0:64], func=AF.Copy, scale=r[:, 1:2])
        p.dma(yd[m * 128:(m + 1) * 128, :], o[:].rearrange("p h d -> p (h d)"), r=[o])

    idx(0)
    topk_bias(0)
    for m in range(8):
        if m + 1 < 8:
            idx(m + 1)
            topk_bias(m + 1)
        attn(m)
    p.finish()
    return p


def vmask_const(j):
    pp = 2 * j + np.arange(128)[:, None] // 64
    ff = np.arange(512)[None, :] // 64
    return np.where(ff <= pp, 0.0, NEG).astype(np.float32)


def id4_const():
    return np.ascontiguousarray(np.tile(np.eye(128, dtype=np.float32), (1, 4))).astype(BF)


_PROGS = {}


def _prog(key, fn):
    if key not in _PROGS:
        _PROGS[key] = fn()
    return _PROGS[key]


def _run(p, ims):
    n = len(ims)
    return run_bass_kernel_spmd(p.nc, ims, core_ids=list(range(n))).results


def _cast_weights(arrs):
    sizes = [a.size for a in arrs]
    tot = sum(sizes)
    per = -(-tot // (8 * 128 * 4096)) * 4096
    flat = np.zeros(8 * 128 * per, np.float32)
    o = 0
    for a in arrs:
        flat[o:o + a.size] = a.reshape(-1)
        o += a.size
    flat = flat.reshape(8, 128, per)
    p = _prog(("L0", per), lambda: build_L0(per))
    res = _run(p, [{"src": flat[c]} for c in range(8)])
    out = np.concatenate([r["dst"].reshape(-1) for r in res])
    outs = []
    o = 0
    for a in arrs:
        outs.append(out[o:o + a.size].reshape(a.shape))
        o += a.size
    return outs


def _layer(l, x, positions, inp, W):
    f32 = np.float32
    g0 = np.ascontiguousarray(inp["norm_g"][l, 0].reshape(16, 128).T)
    cf = cf_const()
    ims = []
    for c in range(8):
        sl = slice(c * 1024, (c + 1) * 1024)
        ims.append({"xT": np.ascontiguousarray(x[sl].T), "g": g0, "wb": W["wb"],
                    "pos": np.ascontiguousarray(positions[sl].reshape(8, 128).T.astype(np.int32)), "cf": cf})
    res = _run(_prog("L1", build_L1), ims)
    proj = np.concatenate([r["proj"] for r in res], 0)
    y = np.zeros((4, 8192, 512), f32)
    lam_v = inp["diff_lambda"][l]
    sgv = inp["diff_subln_g"][l]
    ims = []
    for c in range(8):
        b, h = c // 4, c % 4
        pb = proj[b * 4096:(b + 1) * 4096]
        q = pb[:, h * 128:(h + 1) * 128].reshape(4096, 2, 64)
        k = pb[:, 512 + h * 128:512 + (h + 1) * 128].reshape(4096, 2, 64)
        ims.append({"qT": np.ascontiguousarray(q.transpose(1, 2, 0)), "kT": np.ascontiguousarray(k.transpose(1, 2, 0)),
                    "v": np.ascontiguousarray(pb[:, 1024 + h * 128:1024 + (h + 1) * 128]),
                    "lam4": np.ascontiguousarray(np.broadcast_to(lam_v.reshape(1, 256), (128, 256))),
                    "sg": np.ascontiguousarray(np.broadcast_to(sgv[None, :], (128, 128))), "masks": mask_const()})
    res = _run(_prog(("A", l), lambda: build_A(l)), ims)
    for c in range(8):
        b, h = c // 4, c % 4
        y[0, b * 4096:(b + 1) * 4096, h * 128:(h + 1) * 128] = res[c]["ya"]
    mu = inp["rwkv_mu"][l]
    ims = []
    cstb = cst_B()
    for c in range(8):
        b, hp = c // 4, c % 4
        pb = proj[b * 4096:(b + 1) * 4096, 1536:3328]
        cols = slice(hp * 128, (hp + 1) * 128)
        rkv = np.stack([pb[:, i * 512 + hp * 128:i * 512 + (hp + 1) * 128].T for i in range(3)])
        chp = np.zeros((128, 16), f32)
        for i in range(3):
            chp[:, i] = mu[i * 512 + hp * 128:i * 512 + (hp + 1) * 128]
        chp[:, 3] = mu[1536:1664]
        chp[:, 4] = mu[1664:1792]
        chp[:, 5] = inp["rwkv_w0"][l][cols]
        chp[:, 6] = inp["rwkv_a0"][l][cols]
        chp[:, 7] = inp["rwkv_k_k"][l][cols]
        chp[:, 8] = inp["rwkv_k_a"][l][cols]
        chp[:, 9] = inp["rwkv_r_k"][l].reshape(512)[cols]
        chp[:, 10] = inp["rwkv_gn_g"][l][cols]
        chp[:, 11] = inp["rwkv_gn_b"][l][cols]
        wup = np.concatenate([inp["rwkv_w_up"][l][:, cols], inp["rwkv_a_up"][l][:, cols]], 0)
        ims.append({"rkvT": np.ascontiguousarray(rkv), "lowT": np.ascontiguousarray(pb[:, 1536:1792].T), "chp": chp,
                    "wup": np.ascontiguousarray(wup), "gup": np.ascontiguousarray(inp["rwkv_g_up"][l][:, cols]), "cst": cstb})
    res = _run(_prog("B", build_B), ims)
    for c in range(8):
        b, hp = c // 4, c % 4
        y[1, b * 4096:(b + 1) * 4096, hp * 128:(hp + 1) * 128] = res[c]["ybT"].T
    ims = []
    cstc = cst_C()
    for c in range(8):
        b, h = c // 4, c % 4
        pc = proj[b * 4096:(b + 1) * 4096, 3328:5376]
        hc = slice(h * 128, (h + 1) * 128)
        qf = np.stack([pc[:, 0:512][:, hc].T, pc[:, 512:1024][:, hc].T])
        ig = np.stack([pc[:, 1024:1536][:, hc], pc[:, 1536:2048][:, hc]])
        ims.append({"qfT": np.ascontiguousarray(qf), "ig": np.ascontiguousarray(ig),
                    "lbl": np.ascontiguousarray(inp["hgrn_lb_logits"][:, hc].T),
                    "ng": np.ascontiguousarray(np.broadcast_to(inp["hgrn_norm_g"][l][hc][None, :], (64, 128))), "cst": cstc})
    res = _run(_prog(("C", l), lambda: build_C(l)), ims)
    for c in range(8):
        b, h = c // 4, c % 4
        y[2, b * 4096:(b + 1) * 4096, h * 128:(h + 1) * 128] = res[c]["yc"]
    O = 5376
    ims = []
    rows_all = []
    for c in range(8):
        b, j = c // 4, c % 4
        pb = proj[b * 4096:(b + 1) * 4096]
        rows = np.concatenate([np.arange(i * 128, (i + 1) * 128) for i in [4 * m + j for m in range(8)]])
        rows_all.append(rows)
        q = pb[rows, O:O + 512].reshape(8, 128, 8, 64)
        iq = pb[rows, O + 640:O + 1152].reshape(8, 128, 8, 64)
        iw = pb[rows, O + 1216:O + 1224].reshape(8, 128, 8)
        ims.append({"qT": np.ascontiguousarray(q.transpose(3, 0, 2, 1).reshape(64, 8192)),
                    "iqT": np.ascontiguousarray(iq.transpose(3, 0, 2, 1).reshape(64, 8192)),
                    "iw": np.ascontiguousarray(iw.transpose(1, 0, 2).reshape(128, 64)),
                    "kT": np.ascontiguousarray(pb[:, O + 512:O + 576].T), "ikT": np.ascontiguousarray(pb[:, O + 1152:O + 1216].T),
                    "v": np.ascontiguousarray(pb[:, O + 576:O + 640]), "vmask": vmask_const(j), "id4": id4_const()})
    res = _run(_prog("D", build_D), ims)
    for c in range(8):
        b = c // 4
        y[3, b * 4096 + rows_all[c]] = res[c]["yd"]
    del proj
    g3 = np.ascontiguousarray(np.concatenate([inp["norm_g"][l, i].reshape(16, 128).T for i in range(4)], axis=1))
    ims = []
    for c in range(8):
        sl = slice(c * 1024, (c + 1) * 1024)
        ims.append({"xT": np.ascontiguousarray(x[sl].T), "yT": np.ascontiguousarray(y[:, sl].transpose(0, 2, 1)), "wg": W["wg"], "g3": g3,
                    "wbr": W["wbr"], "wout": W["wout"], "w1": W["w1"], "w2": W["w2"]})
    res = _run(_prog("L3", lambda: build_L3(1024)), ims)
    return np.ascontiguousarray(np.concatenate([r["xo"].T for r in res], 0))


def kernel(**inputs):
    inp = {k: np.asarray(v) for k, v in inputs.items()}
    x = np.ascontiguousarray(inp["x"].reshape(8192, D)).astype(np.float32, copy=False)
    positions = inp["positions"].reshape(8192)
    tiled = []
    for l in range(2):
        w_in = inp["w_in"][l]
        tiled.append(prep_w_in(w_in))
        tiled.append(prep_wg(np.ascontiguousarray(w_in[:, NMIX:])))
        tiled.extend(prep_L3_weights(inp["w_branch"][l], inp["w_out"][l], inp["mlp_w1"][l], inp["mlp_w2"][l]))
    cast = _cast_weights(tiled)
    del tiled
    for l in range(2):
        W = dict(zip(("wb", "wg", "wbr", "wout", "w1", "w2"), cast[l * 6:(l + 1) * 6]))
        x = _layer(l, x, positions, inp, W)
    return x.reshape(2, 4096, D).astype(np.float32)
```

```python
import math
import numpy as np
import ml_dtypes
from contextlib import ExitStack
import concourse.bass as bass
import concourse.mybir as mybir
from concourse.bass_utils import run_bass_kernel_spmd

BF = ml_dtypes.bfloat16


F32 = mybir.dt.float32
BF16 = mybir.dt.bfloat16
I32 = mybir.dt.int32
AF = mybir.ActivationFunctionType
ALU = mybir.AluOpType
AX = mybir.AxisListType

SAME_ENG_SYNC = True
NDMA = 24


class Dep:
    __slots__ = ("w", "r")

    def __init__(self):
        self.w = None
        self.r = []


class Buf:
    def __init__(self, t, nreg=1):
        self.t = t
        self.d = Dep()
        self.regs = {}

    def reg(self, key):
        if key not in self.regs:
            self.regs[key] = Dep()
        return self.regs[key]

    def __getitem__(self, idx):
        return self.t[idx]


class Prog:
    def __init__(self):
        self.nc = bass.Bass("TRN2", target_bir_lowering=False)
        nc = self.nc
        self.es = ExitStack()
        self.eng = {"pe": nc.tensor, "act": nc.scalar, "dve": nc.vector, "pool": nc.gpsimd, "sp": nc.sync}
        self.sem = {}
        self.cnt = {}
        self.clock = {}
        self.hist = {}
        for e in self.eng:
            self.sem[e] = self.es.enter_context(nc.semaphore("s_" + e))
            self.cnt[e] = 0
            self.clock[e] = {}
            self.hist[e] = {}
        for j in range(NDMA):
            e = "d%d" % j
            self.sem[e] = self.es.enter_context(nc.semaphore("s_" + e))
            self.cnt[e] = 0
            self.hist[e] = {}
        self.dma_k = 0
        self.nwaits = 0
        self.ninst = 0
        self.q = {e: [] for e in self.eng}

    def dram(self, name, shape, dtype, kind):
        return self.nc.dram_tensor(name, list(shape), dtype, kind=kind).ap()

    def sb(self, name, shape, dtype=F32):
        return Buf(self.es.enter_context(self.nc.sbuf_tensor(name, list(shape), dtype)))

    def ps(self, name, shape, dtype=F32):
        return Buf(self.es.enter_context(self.nc.psum_tensor(name, list(shape), dtype)))

    def _semval(self, e, n):
        return n * 16 if (e[0] == "d" and e[1:].isdigit()) else n

    def _wait(self, e, deps):
        need = {}
        for (e2, n) in deps:
            if e2 == e and (e == "pe" or not SAME_ENG_SYNC):
                continue
            if self.clock[e].get(e2, 0) >= n:
                continue
            if need.get(e2, 0) < n:
                need[e2] = n
        for e2, n in need.items():
            if self.clock[e].get(e2, 0) >= n:
                continue
            self.q[e].append(("w", self.sem[e2], self._semval(e2, n)))
            self.nwaits += 1
            h = self.hist[e2].get(n)
            if h:
                for k, v in h.items():
                    if self.clock[e].get(k, 0) < v:
                        self.clock[e][k] = v
            self.clock[e][e2] = max(self.clock[e].get(e2, 0), n)

    def _deps(self, r, w):
        deps = []
        for d in r:
            d = d.d if isinstance(d, Buf) else d
            if d.w is not None:
                deps.append(d.w)
        for d in w:
            d = d.d if isinstance(d, Buf) else d
            if d.w is not None:
                deps.append(d.w)
            deps.extend(d.r)
        return deps

    def _commit(self, tag, r, w):
        for d in r:
            d = d.d if isinstance(d, Buf) else d
            d.r.append(tag)
            if len(d.r) > 64:
                best = {}
                for (e2, n) in d.r:
                    if best.get(e2, 0) < n:
                        best[e2] = n
                d.r = list(best.items())
        for d in w:
            d = d.d if isinstance(d, Buf) else d
            d.w = tag
            d.r = []

    def I(self, e, method, r=(), w=(), **kw):
        self._wait(e, self._deps(r, w))
        self.cnt[e] += 1
        n = self.cnt[e]
        self.q[e].append(("i", (method, kw), self.sem[e], 1))
        self.hist[e][n] = dict(self.clock[e])
        if e == "pe" or not SAME_ENG_SYNC:
            self.clock[e][e] = n
        self._commit((e, n), r, w)
        self.ninst += 1

    def dma(self, out, in_, r=(), w=(), q="sp", **kw):
        j = self.dma_k % NDMA
        self.dma_k += 1
        de = "d%d" % j
        prev = self.cnt[de]
        deps = self._deps(r, w)
        if prev > 0:
            deps.append((de, prev))
        self._wait(q, deps)
        self.q[q].append(("d", (out, in_, kw), self.sem[de], 16))
        self.cnt[de] = prev + 1
        self.hist[de][prev + 1] = dict(self.clock[q])
        self._commit((de, prev + 1), r, w)
        self.ninst += 1

    def coll(self, kind, ins, outs, r=(), w=()):
        q = "pool"
        j = self.dma_k % NDMA
        self.dma_k += 1
        de = "d%d" % j
        prev = self.cnt[de]
        deps = self._deps(r, w)
        if prev > 0:
            deps.append((de, prev))
        self._wait(q, deps)
        self.q[q].append(("c", (kind, ins, outs), self.sem[de], 16))
        self.cnt[de] = prev + 1
        self.hist[de][prev + 1] = dict(self.clock[q])
        self._commit((de, prev + 1), r, w)
        self.ninst += 1

    def finish(self, q="sp"):
        deps = []
        for j in range(NDMA):
            de = "d%d" % j
            if self.cnt[de] > 0:
                deps.append((de, self.cnt[de]))
        self._wait(q, deps)

        nc = self.nc
        prog = self
        with nc.Block() as block:
            def mk(e):
                def body(engh):
                    for it in prog.q[e]:
                        if it[0] == "w":
                            engh.wait_ge(it[1], it[2])
                        elif it[0] == "i":
                            getattr(engh, it[1][0])(**it[1][1]).then_inc(it[2], it[3])
                        elif it[0] == "c":
                            kind, ins, outs = it[1]
                            engh.collective_compute(kind, ALU.bypass, [[0,1,2,3],[4,5,6,7]], ins, outs).then_inc(it[2], it[3])
                        else:
                            o, i, kw = it[1]
                            engh.dma_start(out=o, in_=i, **kw).then_inc(it[2], it[3])
                return body
            block.tensor(mk("pe"))
            block.scalar(mk("act"))
            block.vector(mk("dve"))
            block.gpsimd(mk("pool"))
            block.sync(mk("sp"))
        self.es.close()


D = 2048
DIN = 14792
NCB = 13
NMIX = 6600
EPS = 1e-6
ROPE = {0: [(0, 8)], 1: [(0, 8)], 10: [(256, 4)], 11: [(0, 5), (384, 2)], 12: [(0, 7)]}


def build_L0(n):
    p = Prog()
    src = p.dram("src", [128, n], F32, "ExternalInput")
    dst = p.dram("dst", [128, n], BF16, "ExternalOutput")
    CH = 4096
    st = [p.sb("st%d" % i, [128, CH], F32) for i in range(3)]
    ob = [p.sb("ob%d" % i, [128, CH], BF16) for i in range(3)]
    k = 0
    for c0 in range(0, n, CH):
        c1 = min(n, c0 + CH)
        s, o = st[k % 3], ob[k % 3]
        p.dma(s[:, 0:c1 - c0], src[:, c0:c1], w=[s], q="sp")
        e = ("dve", "pool")[k % 2]
        p.I(e, "tensor_copy", r=[s], w=[o], out=o[:, 0:c1 - c0], in_=s[:, 0:c1 - c0])
        p.dma(dst[:, c0:c1], o[:, 0:c1 - c0], r=[o], q="act")
        k += 1
    p.finish()
    return p


def fm_rmsnorm(p, src, gt, gcol, dst, KC, T, ones, psl, sq, rstd, dim):
    nh = T // 512
    for kc in range(KC):
        s = sq[kc % 2]
        p.I("act", "activation", r=[src], w=[s], out=s[:, 0:T], in_=src[:, kc, :], func=AF.Square)
        for h in range(nh):
            p.I("pe", "matmul", r=[s, ones], w=[psl[h]], out=psl[h][:], lhsT=ones[:], rhs=s[:, h * 512:(h + 1) * 512],
                start=(kc == 0), stop=(kc == KC - 1))
    for h in range(nh):
        p.I("act", "activation", r=[psl[h], EPSB[0]], w=[rstd], out=rstd[:, h * 512:(h + 1) * 512], in_=psl[h][:],
            func=AF.Sqrt, scale=1.0 / dim, bias=EPSB[0][:, 0:1])
    p.I("dve", "reciprocal", r=[rstd], w=[rstd], out=rstd[:, 0:T], in_=rstd[:, 0:T])
    for kc in range(KC):
        p.I("dve", "scalar_tensor_tensor", r=[src, gt, rstd], w=[dst], out=dst[:, kc, :], in0=src[:, kc, :],
            scalar=gt[:, gcol + kc:gcol + kc + 1], in1=rstd[:, 0:T], op0=ALU.mult, op1=ALU.mult)


EPSB = [None]


def consts(p):
    ones = p.sb("ones", [128, 128], BF16)
    p.I("dve", "memset", w=[ones], ap=ones[:], constant=1.0)
    eb = p.sb("epsb", [128, 1], F32)
    p.I("dve", "memset", w=[eb], ap=eb[:], constant=EPS)
    EPSB[0] = eb
    return ones


def build_L1():
    p = Prog()
    T = 1024
    xT = p.dram("xT", [D, T], F32, "ExternalInput")
    g = p.dram("g", [128, 16], F32, "ExternalInput")
    wb = p.dram("wb", [NCB, 128, 8192], BF16, "ExternalInput")
    pos = p.dram("pos", [128, 8], I32, "ExternalInput")
    cf = p.dram("cf", [128, 32], F32, "ExternalInput")
    proj = p.dram("proj", [T, NCB * 512], F32, "ExternalOutput")
    ones = consts(p)
    xs = p.sb("xs", [128, 16, T], F32)
    hT = p.sb("hT", [128, 16, T], BF16)
    gt = p.sb("gt", [128, 16], F32)
    sq = [p.sb("sq%d" % i, [128, T], BF16) for i in range(2)]
    rstd = p.sb("rstd", [128, T], F32)
    psn = [p.ps("psn%d" % i, [128, 512], F32) for i in range(2)]
    pst = [p.ps("ps%d" % i, [128, 512], F32) for i in range(4)]
    xv = xT.rearrange("(kc p) t -> p kc t", p=128)
    for kc in range(0, 16, 4):
        p.dma(xs[:, kc:kc + 4, :], xv[:, kc:kc + 4, :], w=[xs], q=("sp", "act")[(kc // 4) % 2])
    p.dma(gt[:], g, w=[gt])
    posi = p.sb("posi", [128, 8], I32)
    posf = p.sb("posf", [128, 8], F32)
    cft = p.sb("cft", [128, 32], F32)
    p.dma(posi[:], pos, w=[posi])
    p.dma(cft[:], cf, w=[cft])
    p.I("dve", "tensor_copy", r=[posi], w=[posf], out=posf[:], in_=posi[:])
    qq = p.sb("qq", [128, 2, 8, 32], F32)
    qi = p.sb("qi", [128, 2, 8, 32], I32)
    qf = p.sb("qf", [128, 2, 8, 32], F32)
    msk = p.sb("msk", [128, 2, 8, 32], F32)
    sc = p.sb("sc", [128, 2, 8, 32], F32)
    for tt in range(8):
        p.I("dve", "tensor_scalar", r=[cft, posf], w=[qq], out=qq[:, 0, tt, :], in0=cft[:], scalar1=posf[:, tt:tt + 1],
            scalar2=None, op0=ALU.mult)
    p.I("dve", "tensor_scalar", r=[qq], w=[qq], out=qq[:, 1, :, :], in0=qq[:, 0, :, :], scalar1=0.25, scalar2=None, op0=ALU.add)
    p.I("dve", "tensor_copy", r=[qq], w=[qi], out=qi[:], in_=qq[:])
    p.I("dve", "tensor_copy", r=[qi], w=[qf], out=qf[:], in_=qi[:])
    p.I("dve", "tensor_tensor", r=[qq, qf], w=[qq], out=qq[:], in0=qq[:], in1=qf[:], op=ALU.subtract)
    p.I("dve", "tensor_scalar", r=[qq], w=[msk], out=msk[:], in0=qq[:], scalar1=0.5, scalar2=None, op0=ALU.is_gt)
    p.I("dve", "tensor_tensor", r=[qq, msk], w=[qq], out=qq[:], in0=qq[:], in1=msk[:], op=ALU.subtract)
    p.I("dve", "tensor_scalar", r=[qq], w=[msk], out=msk[:], in0=qq[:], scalar1=-0.5, scalar2=None, op0=ALU.is_lt)
    p.I("dve", "tensor_tensor", r=[qq, msk], w=[qq], out=qq[:], in0=qq[:], in1=msk[:], op=ALU.add)
    p.I("act", "activation", r=[qq], w=[sc], out=sc[:], in_=qq[:], func=AF.Sin, scale=6.28318)
    fm_rmsnorm(p, xs, gt, 0, hT, 16, T, ones, psn, sq, rstd, D)
    wt = [p.sb("w%d" % i, [128, 8192], BF16) for i in range(3)]
    ot = [p.sb("o%d" % i, [128, 512], F32) for i in range(4)]
    tmp = [p.sb("rt%d" % i, [128, 8, 32], F32) for i in range(4)]
    k = 0
    for cb in range(NCB):
        w = wt[cb % 3]
        p.dma(w[:, 0:4096], wb[cb, :, 0:4096], w=[w], q="sp")
        p.dma(w[:, 4096:8192], wb[cb, :, 4096:8192], w=[w], q="act")
        for tt in range(8):
            ps = pst[k % 4]
            o = ot[k % 4]
            k += 1
            for kc in range(16):
                p.I("pe", "matmul", r=[hT, w], w=[ps], out=ps[:], lhsT=hT[:, kc, tt * 128:(tt + 1) * 128],
                    rhs=w[:, kc * 512:(kc + 1) * 512], start=(kc == 0), stop=(kc == 15))
            p.I("act", "activation", r=[ps], w=[o], out=o[:], in_=ps[:], func=AF.Copy)
            for (s0, nh) in ROPE.get(cb, []):
                ov = o[:, s0:s0 + nh * 64].rearrange("p (h two d) -> p h two d", two=2, d=32)
                x1, x2 = ov[:, :, 0, :], ov[:, :, 1, :]
                sn = sc[:, 0, tt:tt + 1, :].to_broadcast([128, nh, 32])
                cs = sc[:, 1, tt:tt + 1, :].to_broadcast([128, nh, 32])
                t1, t2, t3, t4 = [t[:, 0:nh, :] for t in tmp]
                p.I("dve", "tensor_tensor", r=[o, sc], w=[tmp[0]], out=t1, in0=x1, in1=cs, op=ALU.mult)
                p.I("dve", "tensor_tensor", r=[o, sc], w=[tmp[1]], out=t2, in0=x2, in1=sn, op=ALU.mult)
                p.I("dve", "tensor_tensor", r=[o, sc], w=[tmp[2]], out=t3, in0=x2, in1=cs, op=ALU.mult)
                p.I("dve", "tensor_tensor", r=[o, sc], w=[tmp[3]], out=t4, in0=x1, in1=sn, op=ALU.mult)
                p.I("dve", "tensor_tensor", r=[tmp[0], tmp[1]], w=[o], out=x1, in0=t1, in1=t2, op=ALU.subtract)
                p.I("dve", "tensor_tensor", r=[tmp[2], tmp[3]], w=[o], out=x2, in0=t3, in1=t4, op=ALU.add)
            p.dma(proj[tt * 128:(tt + 1) * 128, cb * 512:(cb + 1) * 512], o[:], r=[o], q="sp")
    p.finish()
    return p


def prep_w_in(wbf):
    w = np.zeros((D, NCB * 512), dtype=wbf.dtype)
    w[:, :NMIX] = wbf[:, :NMIX]
    w = w.reshape(16, 128, NCB, 512).transpose(2, 1, 0, 3).reshape(NCB, 128, 8192)
    return np.ascontiguousarray(w)


def cf_const():
    inv = 10000.0 ** (-np.arange(0, 64, 2, dtype=np.float64) / 64.0)
    c = (inv / (2 * math.pi)).astype(np.float32)
    return np.ascontiguousarray(np.broadcast_to(c[None, :], (128, 32)))


def build_L3(T=2048):
    p = Prog()
    H = 512
    xT = p.dram("xT", [D, T], F32, "ExternalInput")
    yT = p.dram("yT", [4, 512, T], F32, "ExternalInput")
    wg = p.dram("wg", [64, 128, 2048], BF16, "ExternalInput")
    g3 = p.dram("g3", [128, 64], F32, "ExternalInput")
    wbr = p.dram("wbr", [16, 128, 2048], BF16, "ExternalInput")
    wout = p.dram("wout", [16, 128, 2048], BF16, "ExternalInput")
    w1 = p.dram("w1", [64, 128, 2048], BF16, "ExternalInput")
    w2 = p.dram("w2", [16, 4, 128, 2048], BF16, "ExternalInput")
    xo = p.dram("xo", [D, T], F32, "ExternalOutput")
    ones = consts(p)
    xh = p.sb("xh", [128, 16, H], F32)
    zT = p.sb("zT", [128, 16, H], F32)
    mb = p.sb("mb", [128, 16, H], BF16)
    uT = p.sb("uT", [128, 64, H], BF16)
    gt = p.sb("gt", [128, 64], F32)
    hT = p.sb("hT", [128, 16, H], BF16)
    sq = [p.sb("sq%d" % i, [128, H], BF16) for i in range(2)]
    rstd = p.sb("rstd", [128, H], F32)
    psn = [p.ps("psn0", [128, 512], F32)]
    pst = [p.ps("ps%d" % i, [128, 512], F32) for i in range(4)]
    wt = [p.sb("wt%d" % i, [128, 2048], BF16) for i in range(4)]
    psg = [p.ps("psg%d" % i, [128, 512], F32) for i in range(2)]
    sg = [p.sb("sg%d" % i, [128, H], F32) for i in range(2)]
    tm = [p.sb("tm%d" % i, [128, H], F32) for i in range(2)]
    ys = [p.sb("ys%d" % i, [128, 4, H], F32) for i in range(1)]
    wgp = [p.sb("wgp%d" % i, [128, 2048], BF16) for i in range(2)]
    gk = 0
    p.dma(gt[:], g3, w=[gt])
    xv = xT.rearrange("(kc p) t -> p kc t", p=128)
    xov = xo.rearrange("(kc p) t -> p kc t", p=128)
    yv = yT.rearrange("n (kc p) t -> n p kc t", p=128)
    wk = 0
    pk = 0
    for hf in range(T // H):
        tsl = slice(hf * H, (hf + 1) * H)
        for kc in range(0, 16, 8):
            p.dma(xh[:, kc:kc + 8, :], xv[:, kc:kc + 8, tsl], w=[xh], q="act")
        fm_rmsnorm(p, xh, gt, 0, hT, 16, H, ones, psn, sq, rstd, D)
        for n in range(4):
            y = ys[0]
            p.dma(y[:], yv[n, :, :, tsl], w=[y], q="act")
            p.I("dve", "tensor_copy", r=[y], w=[uT], out=uT[:, n * 4:(n + 1) * 4, :], in_=y[:])
        for oc in range(16):
            w = wt[wk % 4]
            wk += 1
            p.dma(w[:], wbr[oc], w=[w], q="sp")
            for n in range(4):
                wgt = wgp[gk % 2]
                gk += 1
                p.dma(wgt[:], wg[oc * 4 + n], w=[wgt], q="act")
                pg = psg[n % 2]
                for kc in range(16):
                    p.I("pe", "matmul", r=[hT, wgt], w=[pg], out=pg[:], lhsT=wgt[:, kc * 128:(kc + 1) * 128], rhs=hT[:, kc, :],
                        start=(kc == 0), stop=(kc == 15))
                ps = pst[pk % 4]
                pk += 1
                for kc in range(4):
                    p.I("pe", "matmul", r=[uT, w], w=[ps], out=ps[:], lhsT=w[:, (n * 4 + kc) * 128:(n * 4 + kc + 1) * 128],
                        rhs=uT[:, n * 4 + kc, :], start=(kc == 0), stop=(kc == 3))
                s = sg[n % 2]
                p.I("act", "activation", r=[pg], w=[s], out=s[:], in_=pg[:], func=AF.Sigmoid)
                if n == 0:
                    p.I("dve", "tensor_tensor", r=[ps, s], w=[zT], out=zT[:, oc, :], in0=ps[:], in1=s[:], op=ALU.mult)
                else:
                    t = tm[n % 2]
                    p.I("dve", "tensor_tensor", r=[ps, s], w=[t], out=t[:], in0=ps[:], in1=s[:], op=ALU.mult)
                    p.I("pool", "tensor_tensor", r=[t, zT], w=[zT], out=zT[:, oc, :], in0=zT[:, oc, :], in1=t[:], op=ALU.add)
            p.I("pool", "tensor_copy", r=[zT], w=[mb], out=mb[:, oc, :], in_=zT[:, oc, :])
        for oc in range(16):
            w = wt[wk % 4]
            wk += 1
            p.dma(w[:], wout[oc], w=[w], q="sp")
            ps = pst[pk % 4]
            pk += 1
            for kc in range(16):
                p.I("pe", "matmul", r=[mb, w], w=[ps], out=ps[:], lhsT=w[:, kc * 128:(kc + 1) * 128], rhs=mb[:, kc, :],
                    start=(kc == 0), stop=(kc == 15))
            p.I("act", "activation", r=[ps], w=[zT], out=zT[:, oc, :], in_=ps[:], func=AF.Copy)
        fm_rmsnorm(p, zT, gt, 16, zT, 16, H, ones, psn, sq, rstd, D)
        for kc in range(0, 16, 4):
            p.I("pool", "tensor_tensor", r=[xh, zT], w=[xh], out=xh[:, kc:kc + 4, :], in0=xh[:, kc:kc + 4, :], in1=zT[:, kc:kc + 4, :], op=ALU.add)
        fm_rmsnorm(p, xh, gt, 32, mb, 16, H, ones, psn, sq, rstd, D)
        for oc in range(64):
            w = wt[wk % 4]
            wk += 1
            p.dma(w[:], w1[oc], w=[w], q=("sp", "act")[oc % 2])
            ps = pst[pk % 4]
            pk += 1
            for kc in range(16):
                p.I("pe", "matmul", r=[mb, w], w=[ps], out=ps[:], lhsT=w[:, kc * 128:(kc + 1) * 128], rhs=mb[:, kc, :],
                    start=(kc == 0), stop=(kc == 15))
            t = tm[oc % 2]
            p.I("act", "activation", r=[ps], w=[t], out=t[:], in_=ps[:], func=AF.Relu)
            p.I(("dve", "pool")[oc % 2], "tensor_tensor", r=[t], w=[uT], out=uT[:, oc, :], in0=t[:], in1=t[:], op=ALU.mult)
        for oc in range(16):
            ps = pst[pk % 4]
            pk += 1
            for q in range(4):
                w = wt[wk % 4]
                wk += 1
                p.dma(w[:], w2[oc, q], w=[w], q=("sp", "act")[q % 2])
                for kc in range(16):
                    p.I("pe", "matmul", r=[uT, w], w=[ps], out=ps[:], lhsT=w[:, kc * 128:(kc + 1) * 128], rhs=uT[:, q * 16 + kc, :],
                        start=(q == 0 and kc == 0), stop=(q == 3 and kc == 15))
            p.I("act", "activation", r=[ps], w=[zT], out=zT[:, oc, :], in_=ps[:], func=AF.Copy)
        fm_rmsnorm(p, zT, gt, 48, zT, 16, H, ones, psn, sq, rstd, D)
        for kc in range(0, 16, 4):
            p.I("pool", "tensor_tensor", r=[xh, zT], w=[xh], out=xh[:, kc:kc + 4, :], in0=xh[:, kc:kc + 4, :], in1=zT[:, kc:kc + 4, :], op=ALU.add)
        for kc in range(0, 16, 8):
            p.dma(xov[:, kc:kc + 8, tsl], xh[:, kc:kc + 8, :], r=[xh], q="sp")
    p.finish()
    return p


def prep_wg(wgate):
    g = wgate.reshape(16, 128, 4, 16, 128).transpose(3, 2, 1, 0, 4).reshape(64, 128, 2048)
    return np.ascontiguousarray(g)


def prep_L3_weights(wbr, wout, w1, w2):
    a = wbr.reshape(4, 4, 128, 16, 128).transpose(3, 2, 0, 1, 4).reshape(16, 128, 2048)
    b = wout.reshape(16, 128, 16, 128).transpose(2, 1, 0, 3).reshape(16, 128, 2048)
    c = w1.reshape(16, 128, 64, 128).transpose(2, 1, 0, 3).reshape(64, 128, 2048)
    d = w2.reshape(4, 16, 128, 16, 128).transpose(3, 0, 2, 1, 4).reshape(16, 4, 128, 2048)
    return [np.ascontiguousarray(t) for t in (a, b, c, d)]


S = 4096


def build_A(layer):
    p = Prog()
    lam_init = 0.8 - 0.6 * math.exp(-0.3 * layer)
    qT = p.dram("qT", [2, 64, S], F32, "ExternalInput")
    kT = p.dram("kT", [2, 64, S], F32, "ExternalInput")
    v = p.dram("v", [S, 128], F32, "ExternalInput")
    lam4 = p.dram("lam4", [128, 256], F32, "ExternalInput")
    sg = p.dram("sg", [128, 128], F32, "ExternalInput")
    masks = p.dram("masks", [4, 128, 512], BF16, "ExternalInput")
    ya = p.dram("ya", [S, 128], F32, "ExternalOutput")
    qb = [p.sb("qb%d" % m, [64, S], BF16) for m in range(2)]
    kb = [p.sb("kb%d" % m, [64, S], BF16) for m in range(2)]
    vb = p.sb("vb", [128, 32, 129], BF16)
    st = [p.sb("st%d" % i, [128, 4096], F32) for i in range(2)]
    mk_ = p.sb("mk", [128, 4, 512], BF16)
    lt = p.sb("lt", [128, 256], F32)
    sgt = p.sb("sgt", [128, 128], F32)
    epsb = p.sb("epsb", [128, 1], F32)
    p.I("dve", "memset", w=[epsb], ap=epsb[:], constant=1e-6)
    k = 0
    for m in range(2):
        for (src, dst) in ((qT, qb[m]), (kT, kb[m])):
            s = st[k % 2]
            k += 1
            p.dma(s[0:64, :], src[m], w=[s])
            p.I(("dve", "pool")[k % 2], "tensor_copy", r=[s], w=[dst], out=dst[:], in_=s[0:64, :])
    s = st[k % 2]
    k += 1
    p.dma(s[:].rearrange("p (t e) -> p t e", e=128), v.rearrange("(t p) e -> p t e", p=128), w=[s])
    p.I("pool", "memset", w=[vb], ap=vb[:], constant=1.0)
    p.I("dve", "tensor_copy", r=[s], w=[vb], out=vb[:, :, 0:128], in_=s[:].rearrange("p (t e) -> p t e", e=128))
    p.dma(mk_[:], masks.rearrange("j p f -> p j f"), w=[mk_])
    p.dma(lt[:], lam4, w=[lt])
    p.dma(sgt[:], sg, w=[sgt])
    pr = p.sb("pr", [128, 2, 64], F32)
    s12 = p.sb("s12", [128, 2], F32)
    e12 = p.sb("e12", [128, 2], F32)
    nlam = p.sb("nlam", [128, 1], F32)
    ltv = lt[:].rearrange("p (a d) -> p a d", d=64)
    p.I("dve", "tensor_tensor", r=[lt], w=[pr], out=pr[:, 0, :], in0=ltv[:, 0, :], in1=ltv[:, 1, :], op=ALU.mult)
    p.I("dve", "tensor_tensor", r=[lt], w=[pr], out=pr[:, 1, :], in0=ltv[:, 2, :], in1=ltv[:, 3, :], op=ALU.mult)
    p.I("dve", "tensor_reduce", r=[pr], w=[s12], out=s12[:], in_=pr[:], axis=AX.X, op=ALU.add)
    p.I("act", "activation", r=[s12], w=[e12], out=e12[:], in_=s12[:], func=AF.Exp)
    p.I("dve", "tensor_tensor", r=[e12], w=[nlam], out=nlam[:], in0=e12[:, 1:2], in1=e12[:, 0:1], op=ALU.subtract)
    p.I("dve", "tensor_scalar", r=[nlam], w=[nlam], out=nlam[:], in0=nlam[:], scalar1=-lam_init, scalar2=None, op0=ALU.add)
    p.I("dve", "tensor_scalar", r=[sgt], w=[sgt], out=sgt[:], in0=sgt[:], scalar1=1.0 - lam_init, scalar2=None, op0=ALU.mult)
    pss = [p.ps("pss%d" % i, [128, 512], F32) for i in range(3)]
    psa = [p.ps("psa%d" % i, [128, 512], F32) for i in range(3)]
    pts = p.sb("pts", [128, 32, 512], BF16)
    ob = [p.sb("ob%d" % i, [128, 4, 128], F32) for i in range(2)]
    sm = [p.sb("sm%d" % i, [128, 4], F32) for i in range(3)]
    junk = p.sb("junk", [128, 128], F32)
    ks = 0
    ka = 0
    for qblk in range(8):
        Q0 = qblk * 512
        nkt = 4 * qblk + 4
        o = ob[qblk % 2]
        for m in range(2):
            for kt in range(nkt):
                ps = pss[ks % 3]
                ks += 1
                p.I("pe", "matmul", r=[kb[m], qb[m]], w=[ps], out=ps[:], lhsT=kb[m][:, kt * 128:(kt + 1) * 128],
                    rhs=qb[m][:, Q0:Q0 + 512], start=True, stop=True)
                dpt = pts.reg(kt)
                p.I("act", "activation", r=[ps], w=[dpt], out=pts[:, kt, :], in_=ps[:], func=AF.Exp, scale=0.125)
                if kt >= 4 * qblk:
                    p.I("pool", "tensor_tensor", r=[dpt, mk_], w=[dpt], out=pts[:, kt, :], in0=pts[:, kt, :],
                        in1=mk_[:, kt - 4 * qblk, :], op=ALU.mult)
            for j in range(4):
                pa = psa[ka % 3]
                ka += 1
                for kt in range(nkt):
                    p.I("pe", "matmul", r=[pts.reg(kt), vb], w=[pa], out=pa[:, 0:129], lhsT=pts[:, kt, j * 128:(j + 1) * 128],
                        rhs=vb[:, kt, :], start=(kt == 0), stop=(kt == nkt - 1))
                r = sm[ka % 3]
                p.I("dve", "reciprocal", r=[pa], w=[r], out=r[:, 0:1], in_=pa[:, 128:129])
                if m == 0:
                    p.I("dve", "tensor_scalar", r=[pa, r], w=[o], out=o[:, j, :], in0=pa[:, 0:128], scalar1=r[:, 0:1],
                        scalar2=None, op0=ALU.mult)
                else:
                    p.I("dve", "tensor_tensor", r=[r, nlam], w=[r], out=r[:, 1:2], in0=r[:, 0:1], in1=nlam[:], op=ALU.mult)
                    p.I("dve", "scalar_tensor_tensor", r=[pa, r, o], w=[o], out=o[:, j, :], in0=pa[:, 0:128],
                        scalar=r[:, 1:2], in1=o[:, j, :], op0=ALU.mult, op1=ALU.add)
                    p.I("act", "activation", r=[o], w=[junk, r], out=junk[:], in_=o[:, j, :], func=AF.Square,
                        accum_out=r[:, 2:3])
                    p.I("act", "activation", r=[r, epsb], w=[r], out=r[:, 3:4], in_=r[:, 2:3], func=AF.Sqrt, scale=1.0 / 128,
                        bias=epsb[:, 0:1])
                    p.I("dve", "reciprocal", r=[r], w=[r], out=r[:, 3:4], in_=r[:, 3:4])
                    p.I("dve", "scalar_tensor_tensor", r=[o, r, sgt], w=[o], out=o[:, j, :], in0=o[:, j, :],
                        scalar=r[:, 3:4], in1=sgt[:], op0=ALU.mult, op1=ALU.mult)
        p.dma(ya[Q0:Q0 + 512, :].rearrange("(j p) e -> p j e", p=128), o[:], r=[o])
    p.finish()
    return p


def mask_const():
    m = np.zeros((4, 128, 512), np.float32)
    pp = np.arange(128)[:, None] // 64
    ff = np.arange(512)[None, :] // 64
    for j in range(4):
        m[j] = ((2 * j + pp) <= ff)
    return m.astype(BF)


S = 4096
NCH = 64
EM05 = math.exp(-0.5)
GN_EPS = 64e-5


def build_B(stop=99):
    p = Prog()
    rkvT = p.dram("rkvT", [3, 128, S], F32, "ExternalInput")
    lowT = p.dram("lowT", [256, S], F32, "ExternalInput")
    chp = p.dram("chp", [128, 16], F32, "ExternalInput")
    wup = p.dram("wup", [128, 128], F32, "ExternalInput")
    gup = p.dram("gup", [128, 128], F32, "ExternalInput")
    cst = p.dram("cst", [128, 1152], F32, "ExternalInput")
    ybT = p.dram("ybT", [128, S], F32, "ExternalOutput")

    ch = p.sb("ch", [128, 16], F32)
    cs = p.sb("cs", [128, 1152], F32)
    wupf = p.sb("wupf", [128, 128], F32)
    gupf = p.sb("gupf", [128, 128], F32)
    wupb = p.sb("wupb", [128, 128], BF16)
    gupb = p.sb("gupb", [128, 128], BF16)
    bones = p.sb("bones", [128, 128], BF16)
    p.dma(ch[:], chp, w=[ch])
    p.dma(cs[:], cst, w=[cs])
    p.dma(wupf[:], wup, w=[wupf])
    p.dma(gupf[:], gup, w=[gupf])
    p.I("dve", "tensor_copy", r=[wupf], w=[wupb], out=wupb[:], in_=wupf[:])
    p.I("dve", "tensor_copy", r=[gupf], w=[gupb], out=gupb[:], in_=gupf[:])
    p.I("dve", "tensor_copy", r=[cs], w=[bones], out=bones[:], in_=cs[:, 0:128])
    ident = cs[:, 128:256]
    rmask = cs[:, 256:768]
    epsb = p.sb("epsb", [128, 2], F32)
    p.I("dve", "memset", w=[epsb], ap=epsb[:, 0:1], constant=GN_EPS)
    p.I("dve", "memset", w=[epsb], ap=epsb[:, 1:2], constant=0.0)

    ARd = p.nc.dram_tensor("ARd", [128, NCH * 128], BF16, kind="Internal").ap()
    BKd = p.nc.dram_tensor("BKd", [128, NCH * 128], BF16, kind="Internal").ap()
    PCd = p.nc.dram_tensor("PCd", [128, NCH], F32, kind="Internal").ap()
    yd2 = p.nc.dram_tensor("yd2", [128, S], F32, kind="Internal").ap()
    dARd, dBKd, dPCd, dyd2 = Dep(), Dep(), Dep(), Dep()
    ARs = [p.sb("ARs%d" % i, [128, 8, 2, 64], BF16) for i in range(2)]
    BKs = [p.sb("BKs%d" % i, [128, 8, 2, 64], BF16) for i in range(2)]
    Bh = p.sb("Bh", [64, NCH, 2, 64], BF16)
    Kh = p.sb("Kh", [64, NCH, 2, 64], BF16)
    Vh = p.sb("Vh", [64, NCH, 2, 64], BF16)
    PC = p.sb("PC", [128, NCH], F32)
    bonus = p.sb("bonus", [128, S], BF16)
    gT = p.sb("gT", [128, S], BF16)

    NT = 12
    tf = [p.sb("tf%d" % i, [128, 512], F32) for i in range(NT)]
    tb = [p.sb("tb%d" % i, [128, 512], BF16) for i in range(4)]
    xin = [p.sb("xin%d" % i, [128, 513], F32) for i in range(5)]
    ps = [p.ps("ps%d" % i, [128, 512], F32) for i in range(8)]

    MU_R, MU_K, MU_V, MU_WA, MU_G, W0, A0, KKG, KAG, RK, GNG, GNB = range(12)

    def col(i):
        return ch[:, i:i + 1]

    for blk in range(8):
        t0 = blk * 512
        srcs = [rkvT[0], rkvT[1], rkvT[2], lowT[0:128], lowT[128:256]]
        for i in range(5):
            if blk == 0:
                p.I("pool", "memset", w=[xin[i]], ap=xin[i][:, 0:1], constant=0.0)
                p.dma(xin[i][:, 1:513], srcs[i][:, 0:512], w=[xin[i]], q=("sp", "act")[i % 2])
            else:
                p.dma(xin[i][:], srcs[i][:, t0 - 1:t0 + 512], w=[xin[i]], q=("sp", "act")[i % 2])
        for i in range(5):
            d = tf[5]
            p.I("dve", "tensor_tensor", r=[xin[i]], w=[d], out=d[:], in0=xin[i][:, 0:512], in1=xin[i][:, 1:513], op=ALU.subtract)
            p.I("dve", "scalar_tensor_tensor", r=[d, ch, xin[i]], w=[tf[i]], out=tf[i][:], in0=d[:], scalar=col(MU_R + i),
                in1=xin[i][:, 1:513], op0=ALU.mult, op1=ALU.add)
        r_, k_, v_, wa_, gd_ = tf[0], tf[1], tf[2], tf[3], tf[4]
        p.I("act", "activation", r=[wa_], w=[tb[0]], out=tb[0][0:64, :], in_=wa_[0:64, :], func=AF.Tanh)
        p.I("act", "activation", r=[wa_], w=[tb[0]], out=tb[0][64:128, :], in_=wa_[64:128, :], func=AF.Copy)
        p.I("act", "activation", r=[gd_], w=[tb[1]], out=tb[1][:], in_=gd_[:], func=AF.Sigmoid)
        p.I("pe", "matmul", r=[wupb, tb[0]], w=[ps[0]], out=ps[0][:], lhsT=wupb[0:64, :], rhs=tb[0][0:64, :], start=True, stop=True)
        p.I("pe", "matmul", r=[wupb, tb[0]], w=[ps[1]], out=ps[1][:], lhsT=wupb[64:128, :], rhs=tb[0][64:128, :], start=True, stop=True)
        p.I("pe", "matmul", r=[gupb, tb[1]], w=[ps[2]], out=ps[2][:], lhsT=gupb[:], rhs=tb[1][:], start=True, stop=True)
        dl, a_ = tf[5], tf[6]
        p.I("act", "activation", r=[ps[0], ch], w=[dl], out=dl[:], in_=ps[0][:], func=AF.Sigmoid, bias=col(W0))
        p.I("dve", "tensor_scalar", r=[dl], w=[dl], out=dl[:], in0=dl[:], scalar1=-EM05, scalar2=None, op0=ALU.mult)
        p.I("act", "activation", r=[ps[1], ch], w=[a_], out=a_[:], in_=ps[1][:], func=AF.Sigmoid, bias=col(A0))
        p.I("act", "activation", r=[ps[2]], w=[gT], out=gT[:, t0:t0 + 512], in_=ps[2][:], func=AF.Copy)
        kk, kap = tf[7], tf[8]
        p.I("dve", "tensor_scalar", r=[k_, ch], w=[kk], out=kk[:], in0=k_[:], scalar1=col(KKG), scalar2=None, op0=ALU.mult)
        p.I("pool", "tensor_tensor", r=[kk], w=[tb[2]], out=tb[2][:], in0=kk[:], in1=kk[:], op=ALU.mult)
        p.I("pe", "matmul", r=[bones, tb[2]], w=[ps[3]], out=ps[3][:], lhsT=bones[:], rhs=tb[2][:], start=True, stop=True)
        rn = tf[9]
        p.I("act", "activation", r=[ps[3], epsb], w=[rn], out=rn[:], in_=ps[3][:], func=AF.Sqrt, bias=epsb[:, 1:2])
        p.I("dve", "tensor_scalar", r=[rn], w=[rn], out=rn[:], in0=rn[:], scalar1=1e-12, scalar2=None, op0=ALU.max)
        p.I("dve", "reciprocal", r=[rn], w=[rn], out=rn[:], in_=rn[:])
        p.I("dve", "tensor_tensor", r=[kk, rn], w=[kap], out=kap[:], in0=kk[:], in1=rn[:], op=ALU.mult)
        km = tf[7]
        p.I("dve", "tensor_scalar", r=[a_, ch], w=[tf[9]], out=tf[9][:], in0=a_[:], scalar1=-1.0, scalar2=col(KAG), op0=ALU.add, op1=ALU.mult)
        p.I("dve", "scalar_tensor_tensor", r=[tf[9], k_], w=[km], out=km[:], in0=tf[9][:], scalar=1.0, in1=k_[:], op0=ALU.add, op1=ALU.mult)
        p.I("dve", "scalar_tensor_tensor", r=[r_, ch, km], w=[tb[3]], out=tb[3][:], in0=r_[:], scalar=col(RK), in1=km[:], op0=ALU.mult, op1=ALU.mult)
        p.I("pe", "matmul", r=[bones, tb[3]], w=[ps[4]], out=ps[4][:], lhsT=bones[:], rhs=tb[3][:], start=True, stop=True)
        p.I("dve", "tensor_tensor", r=[ps[4], v_], w=[bonus], out=bonus[:, t0:t0 + 512], in0=ps[4][:], in1=v_[:], op=ALU.mult)
        L = tf[9]
        p.I("dve", "tensor_tensor_scan", r=[cs, dl], w=[L], out=L[:], data0=rmask, data1=dl[:], initial=0.0, op0=ALU.mult, op1=ALU.add)
        P_, Pp, Pi, E_ = tf[10], tf[11], tf[1], tf[4]
        p.I("act", "activation", r=[L], w=[P_], out=P_[:], in_=L[:], func=AF.Exp)
        p.I("dve", "tensor_tensor", r=[L, dl], w=[Pp], out=Pp[:], in0=L[:], in1=dl[:], op=ALU.subtract)
        p.I("act", "activation", r=[Pp], w=[Pp], out=Pp[:], in_=Pp[:], func=AF.Exp)
        p.I("act", "activation", r=[L], w=[Pi], out=Pi[:], in_=L[:], func=AF.Exp, scale=-1.0)
        Lv = L[:].rearrange("p (c t) -> p c t", t=64)
        p.I("dve", "tensor_tensor", r=[L], w=[E_], out=E_[:].rearrange("p (c t) -> p c t", t=64), in0=Lv,
            in1=Lv[:, :, 63:64].to_broadcast([128, 8, 64]), op=ALU.subtract)
        p.I("act", "activation", r=[E_], w=[E_], out=E_[:], in_=E_[:], func=AF.Exp, scale=-1.0)
        p.I("pool", "tensor_copy", r=[P_], w=[PC], out=PC[:, blk * 8:(blk + 1) * 8],
            in_=P_[:].rearrange("p (c t) -> p c t", t=64)[:, :, 63])
        csl = slice(0, 8)
        AR, BK = ARs[blk % 2], BKs[blk % 2]
        v3 = lambda t: t[:].rearrange("p (c t) -> p c t", t=64)
        p.I("dve", "scalar_tensor_tensor", r=[kap, Pp], w=[AR], out=AR[:, csl, 0, :], in0=v3(kap), scalar=-1.0, in1=v3(Pp), op0=ALU.mult, op1=ALU.mult)
        p.I("pool", "tensor_tensor", r=[r_, P_], w=[AR], out=AR[:, csl, 1, :], in0=v3(r_), in1=v3(P_), op=ALU.mult)
        ka = tf[5]
        p.I("dve", "tensor_tensor", r=[kap, a_], w=[ka], out=ka[:], in0=kap[:], in1=a_[:], op=ALU.mult)
        p.I("dve", "tensor_tensor", r=[ka, Pi], w=[BK], out=BK[:, csl, 0, :], in0=v3(ka), in1=v3(Pi), op=ALU.mult)
        p.I("pool", "tensor_tensor", r=[km, Pi], w=[BK], out=BK[:, csl, 1, :], in0=v3(km), in1=v3(Pi), op=ALU.mult)
        p.dma(ARd[:, blk * 1024:(blk + 1) * 1024], AR[:].rearrange("p c a k -> p (c a k)"), r=[AR], w=[dARd])
        p.dma(BKd[:, blk * 1024:(blk + 1) * 1024], BK[:].rearrange("p c a k -> p (c a k)"), r=[BK], w=[dBKd], q="act")
        Bf, Kf = tf[6], tf[8]
        p.I("dve", "tensor_tensor", r=[ka, E_], w=[Bf], out=Bf[:], in0=ka[:], in1=E_[:], op=ALU.mult)
        p.I("pool", "tensor_tensor", r=[km, E_], w=[Kf], out=Kf[:], in0=km[:], in1=E_[:], op=ALU.mult)
        for (src, dst, pi) in ((Bf, Bh, 5), (Kf, Kh, 6), (v_, Vh, 7)):
            for half in range(2):
                pt = ps[pi] if half == 0 else ps[(pi + 3) % 8 if pi != 7 else 0]
                for c4 in range(4):
                    c = half * 4 + c4
                    p.I("pe", "transpose", r=[src, cs], w=[pt], out=pt[0:64, c4 * 128:(c4 + 1) * 128], in_=src[:, c * 64:(c + 1) * 64], identity=ident)
                p.I("act", "activation", r=[pt], w=[dst], out=dst[:, blk * 8 + half * 4:blk * 8 + half * 4 + 4, :, :],
                    in_=pt[0:64, :].rearrange("p (c h k) -> p c h k", c=4, h=2), func=AF.Copy)

    p.dma(PCd, PC[:], r=[PC], w=[dPCd])
    if stop == 1:
        p.finish()
        return p
    m5 = cs[0:64, 768:1088]
    eye = cs[0:64, 1088:1152]
    mstrict = cs[0:64, 768:832]
    mlower = cs[0:64, 1024:1088]
    m4 = cs[0:64, 768:1024]
    ARx = p.sb("ARx", [64, NCH, 2, 64], BF16)
    BKx = p.sb("BKx", [64, NCH, 2, 64], BF16)
    PCx = p.sb("PCx", [64, NCH], F32)
    TT = p.sb("TT", [64, NCH, 64], BF16)
    yTh = p.sb("yTh", [64, S], F32)
    LM = [[p.sb("LM%d_%d" % (s_, i), [64, 2, 64], F32) for i in range(2)] for s_ in range(8)]
    XX = [[p.sb("XX%d_%d" % (s_, i), [64, 64], F32) for i in range(2)] for s_ in range(8)]
    S32 = p.sb("S32", [64, 64], F32)
    Sb = p.sb("Sb", [64, 64], BF16)
    Am = [p.sb("Am%d" % i, [64, 4, 64], BF16) for i in range(2)]
    Zb = [p.sb("Zb%d" % i, [64, 64], BF16) for i in range(2)]
    Ub = [p.sb("Ub%d" % i, [64, 64], BF16) for i in range(2)]
    for hd in range(2):
        hs = slice(hd * 64, (hd + 1) * 64)
        p.dma(ARx[:].rearrange("p n a k -> p (n a k)"), ARd[hs, :], r=[dARd], w=[ARx])
        p.dma(BKx[:].rearrange("p n a k -> p (n a k)"), BKd[hs, :], r=[dBKd], w=[BKx], q="act")
        p.dma(PCx[:], PCd[hs, :], r=[dPCd], w=[PCx])
        for n in range(NCH):
            s_ = n % 4
            pa, pb = ps[2 * s_], ps[2 * s_ + 1]
            lm0, x0 = LM[s_][0], XX[s_][0]
            p.I("pe", "matmul", r=[BKx, ARx], w=[pa], out=pa[0:64, 64:128], lhsT=BKx[:, n, 0, :], rhs=ARx[:, n, 0, :], start=True, stop=True)
            p.I("pe", "matmul", r=[BKx, ARx], w=[pa], out=pa[0:64, 0:64], lhsT=ARx[:, n, 0, :], rhs=BKx[:, n, 0, :], start=True, stop=True)
            p.I("dve", "tensor_tensor", r=[pa, cs], w=[lm0], out=lm0[:, 0, :], in0=pa[0:64, 0:64], in1=mlower, op=ALU.mult)
            p.I("dve", "tensor_tensor", r=[pa, cs], w=[lm0], out=lm0[:, 1, :], in0=pa[0:64, 64:128], in1=mstrict, op=ALU.mult)
            p.I("dve", "tensor_tensor", r=[lm0, cs], w=[x0], out=x0[:], in0=lm0[:, 1, :], in1=eye, op=ALU.add)
            cur = 0
            for j in range(1, 6):
                lmp, lmn = LM[s_][cur], LM[s_][1 - cur]
                xp, xn = XX[s_][cur], XX[s_][1 - cur]
                p.I("pe", "matmul", r=[lmp], w=[pa], out=pa[0:64, 0:64], lhsT=lmp[:, 1, :], rhs=lmp[:, 0, :], start=True, stop=True)
                if j < 5:
                    p.I("pe", "matmul", r=[lmp], w=[pa], out=pa[0:64, 64:128], lhsT=lmp[:, 0, :], rhs=lmp[:, 1, :], start=True, stop=True)
                p.I("act", "activation", r=[pa], w=[lmn], out=lmn[:].rearrange("p two k -> p (two k)"), in_=pa[0:64, 0:128], func=AF.Copy)
                p.I("pe", "matmul", r=[lmn, xp], w=[pb], out=pb[0:64, 0:64], lhsT=lmn[:, 0, :], rhs=xp[:], start=True, stop=True)
                p.I("dve", "tensor_tensor", r=[pb, xp], w=[xn], out=xn[:], in0=pb[0:64, 0:64], in1=xp[:], op=ALU.add)
                cur = 1 - cur
            p.I("pool", "tensor_copy", r=[XX[s_][cur]], w=[TT.reg(n)], out=TT[:, n, :], in_=XX[s_][cur][:])
        if stop == 2 + 2 * hd:
            p.finish()
            return p
        p.I("dve", "memset", w=[S32], ap=S32[:], constant=0.0)
        p.I("pool", "memset", w=[Sb], ap=Sb[:], constant=0.0)
        def gmat(n):
            am = Am[n % 2]
            pg = ps[5 * (n % 2)]
            p.I("pe", "matmul", r=[BKx, ARx], w=[pg], out=pg[0:64, 0:128], lhsT=BKx[:, n, 0, :], rhs=ARx[:, n, :, :], start=True, stop=True)
            p.I("pe", "matmul", r=[BKx, ARx], w=[pg], out=pg[0:64, 128:256], lhsT=BKx[:, n, 1, :], rhs=ARx[:, n, :, :], start=True, stop=True)
            p.I("dve", "tensor_tensor", r=[pg, cs], w=[am], out=am[:].rearrange("p q t -> p (q t)"), in0=pg[0:64, 0:256], in1=m4, op=ALU.mult)

        gmat(0)
        for n in range(NCH):
            am, zb, ub = Am[n % 2], Zb[n % 2], Ub[n % 2]
            o4 = 5 * (n % 2)
            pz, pu, py, pS = ps[o4 + 1], ps[o4 + 2], ps[3], ps[4]
            p.I("pe", "matmul", r=[am, Vh], w=[pz], out=pz[0:64, 0:64], lhsT=am[:, 2, :], rhs=Vh[:, n, hd, :], start=True, stop=False)
            p.I("pe", "matmul", r=[ARx, Sb], w=[pz], out=pz[0:64, 0:64], lhsT=ARx[:, n, 0, :], rhs=Sb[:], start=False, stop=True)
            p.I("act", "activation", r=[pz], w=[zb], out=zb[:], in_=pz[0:64, 0:64], func=AF.Copy)
            if n + 1 < NCH:
                gmat(n + 1)
            p.I("pe", "matmul", r=[TT.reg(n), zb], w=[pu], out=pu[0:64, 0:64], lhsT=TT[:, n, :], rhs=zb[:], start=True, stop=True)
            p.I("act", "activation", r=[pu], w=[ub], out=ub[:], in_=pu[0:64, 0:64], func=AF.Copy)
            p.I("pe", "matmul", r=[Bh, ub], w=[pS], out=pS[0:64, 64:128], lhsT=Bh[:, n, hd, :], rhs=ub[:], start=True, stop=False)
            p.I("pe", "matmul", r=[Kh, Vh], w=[pS], out=pS[0:64, 64:128], lhsT=Kh[:, n, hd, :], rhs=Vh[:, n, hd, :], start=False, stop=True)
            p.I("pe", "matmul", r=[Sb, ARx], w=[py], out=py[0:64, 0:64], lhsT=Sb[:], rhs=ARx[:, n, 1, :], start=True, stop=False)
            p.I("pe", "matmul", r=[ub, am], w=[py], out=py[0:64, 0:64], lhsT=ub[:], rhs=am[:, 1, :], start=False, stop=False)
            p.I("pe", "matmul", r=[Vh, am], w=[py], out=py[0:64, 0:64], lhsT=Vh[:, n, hd, :], rhs=am[:, 3, :], start=False, stop=True)
            p.I("dve", "scalar_tensor_tensor", r=[S32, PCx, pS], w=[Sb], out=Sb[:], in0=S32[:], scalar=PCx[:, n:n + 1], in1=pS[0:64, 64:128],
                op0=ALU.mult, op1=ALU.add)
            p.I("dve", "scalar_tensor_tensor", r=[S32, PCx, pS], w=[S32], out=S32[:], in0=S32[:], scalar=PCx[:, n:n + 1], in1=pS[0:64, 64:128],
                op0=ALU.mult, op1=ALU.add)
            p.I("act", "activation", r=[py], w=[yTh], out=yTh[:, n * 64:(n + 1) * 64], in_=py[0:64, 0:64], func=AF.Copy)
        p.dma(yd2[hs, :], yTh[:], r=[yTh], w=[dyd2])
        if stop == 3 + 2 * hd:
            p.finish()
            return p

    bavg = p.sb("bavg", [128, 128], F32)
    p.I("dve", "tensor_scalar", r=[cs], w=[bavg], out=bavg[:], in0=cs[:, 0:128], scalar1=1.0 / 64, scalar2=None, op0=ALU.mult)
    for blk in range(8):
        sl = slice(blk * 512, (blk + 1) * 512)
        pm, pv = ps[(2 * blk) % 8], ps[(2 * blk + 1) % 8]
        yc, y2, o, yl = tf[0], tf[1], tf[2], tf[3 + blk % 2]
        p.dma(yl[:], yd2[:, sl], r=[dyd2], w=[yl])
        p.I("pool", "tensor_copy", r=[yl], w=[tb[0]], out=tb[0][:], in_=yl[:])
        p.I("pe", "matmul", r=[bones, tb[0]], w=[pm], out=pm[:], lhsT=bones[:], rhs=tb[0][:], start=True, stop=True)
        p.I("dve", "scalar_tensor_tensor", r=[yl, pm], w=[yc], out=yc[:], in0=pm[:], scalar=-1.0 / 64, in1=yl[:], op0=ALU.mult, op1=ALU.add)
        p.I("pool", "tensor_tensor", r=[yc], w=[tb[1]], out=tb[1][:], in0=yc[:], in1=yc[:], op=ALU.mult)
        p.I("pe", "matmul", r=[bones, tb[1]], w=[pv], out=pv[:], lhsT=bones[:], rhs=tb[1][:], start=True, stop=True)
        p.I("act", "activation", r=[pv, epsb], w=[y2], out=y2[:], in_=pv[:], func=AF.Sqrt, bias=epsb[:, 0:1], scale=1.0 / 64)
        p.I("dve", "reciprocal", r=[y2], w=[y2], out=y2[:], in_=y2[:])
        p.I("dve", "tensor_tensor", r=[yc, y2], w=[yc], out=yc[:], in0=yc[:], in1=y2[:], op=ALU.mult)
        p.I("dve", "tensor_scalar", r=[yc, ch], w=[yc], out=yc[:], in0=yc[:], scalar1=col(GNG), scalar2=col(GNB), op0=ALU.mult, op1=ALU.add)
        p.I("dve", "tensor_tensor", r=[yc, bonus], w=[yc], out=yc[:], in0=yc[:], in1=bonus[:, sl], op=ALU.add)
        p.I("dve", "tensor_tensor", r=[yc, gT], w=[o], out=o[:], in0=yc[:], in1=gT[:, sl], op=ALU.mult)
        p.dma(ybT[:, sl], o[:], r=[o])
    p.finish()
    return p


def cst_B():
    c = np.zeros((128, 1152), np.float32)
    c[0:64, 0:64] = 1.0
    c[64:128, 64:128] = 1.0
    c[:, 128:256] = np.eye(128)
    rm = np.ones(512, np.float32)
    rm[0::64] = 0.0
    c[:, 256:768] = rm[None, :]
    i = np.arange(64)[:, None]
    t = np.arange(64)[None, :]
    strict = (i < t).astype(np.float32)
    incl = (i <= t).astype(np.float32)
    lower = (t < i).astype(np.float32)
    c[0:64, 768:832] = strict
    c[0:64, 832:896] = incl
    c[0:64, 896:960] = strict
    c[0:64, 960:1024] = incl
    c[0:64, 1024:1088] = lower
    c[0:64, 1088:1152] = np.eye(64)
    return c


S = 4096
NCH = 64


def build_C(layer):
    p = Prog()
    qfT = p.dram("qfT", [2, 128, S], F32, "ExternalInput")
    ig = p.dram("ig", [2, S, 128], F32, "ExternalInput")
    lbl = p.dram("lbl", [128, 2], F32, "ExternalInput")
    ng = p.dram("ng", [64, 128], F32, "ExternalInput")
    cst = p.dram("cst", [128, 768], F32, "ExternalInput")
    yc = p.dram("yc", [S, 128], F32, "ExternalOutput")
    cs = p.sb("cs", [128, 768], F32)
    lb = p.sb("lb", [128, 4], F32)
    ngt = p.sb("ngt", [64, 128], F32)
    epsb = p.sb("epsb", [128, 1], F32)
    p.I("dve", "memset", w=[epsb], ap=epsb[:], constant=1e-6)
    p.dma(cs[:], cst, w=[cs])
    p.dma(lb[:, 0:2], lbl, w=[lb])
    p.dma(ngt[:], ng, w=[ngt])
    rmask = cs[:, 0:512]
    ident = cs[:, 512:640]
    incl = cs[0:64, 640:704]
    if layer == 0:
        p.I("dve", "memset", w=[lb], ap=lb[:, 2:3], constant=0.0)
    else:
        p.I("dve", "tensor_tensor", r=[lb], w=[lb], out=lb[:, 2:3], in0=lb[:, 1:2], in1=lb[:, 0:1], op=ALU.subtract)
        p.I("act", "activation", r=[lb], w=[lb], out=lb[:, 2:3], in_=lb[:, 2:3], func=AF.Sigmoid)
    p.I("dve", "tensor_scalar", r=[lb], w=[lb], out=lb[:, 3:4], in0=lb[:, 2:3], scalar1=-1.0, scalar2=1.0, op0=ALU.mult, op1=ALU.add)

    Qt = p.sb("Qt", [128, S], BF16)
    Kt = p.sb("Kt", [128, S], BF16)
    Qb = p.sb("Qb", [128, S], BF16)
    Kbh = p.sb("Kbh", [64, NCH, 128], BF16)
    Ih = p.sb("Ih", [64, NCH, 128], BF16)
    Sall = p.sb("Sall", [128, NCH, 128], BF16)
    dec = p.sb("dec", [128, NCH], F32)
    tf = [p.sb("tf%d" % i, [128, 512], F32) for i in range(8)]
    xin = [p.sb("xin%d" % i, [128, 512], F32) for i in range(2)]
    ist = [p.sb("ist%d" % i, [64, 8, 128], F32) for i in range(2)]
    ps = [p.ps("ps%d" % i, [128, 512], F32) for i in range(8)]
    v3 = lambda t: t[:].rearrange("p (c t) -> p c t", t=64)
    igv = ig.rearrange("w (n s) v -> w s n v", s=64)
    for blk in range(8):
        sl = slice(blk * 512, (blk + 1) * 512)
        p.dma(xin[0][:], qfT[0][:, sl], w=[xin[0]])
        p.dma(xin[1][:], qfT[1][:, sl], w=[xin[1]], q="act")
        it = ist[blk % 2]
        p.dma(it[:], igv[0][:, blk * 8:(blk + 1) * 8, :], w=[it])
        p.I("pool", "tensor_copy", r=[it], w=[Ih], out=Ih[:, blk * 8:(blk + 1) * 8, :], in_=it[:])
        qf, fg, lf, kf, b, t1, t2, t3 = tf
        p.I("act", "activation", r=[xin[0]], w=[qf], out=qf[:], in_=xin[0][:], func=AF.Silu)
        p.I("act", "activation", r=[xin[1]], w=[fg], out=fg[:], in_=xin[1][:], func=AF.Sigmoid)
        p.I("dve", "tensor_scalar", r=[fg, lb], w=[fg], out=fg[:], in0=fg[:], scalar1=lb[:, 3:4], scalar2=lb[:, 2:3], op0=ALU.mult, op1=ALU.add)
        p.I("act", "activation", r=[fg], w=[lf], out=lf[:], in_=fg[:], func=AF.Ln)
        p.I("dve", "tensor_scalar", r=[fg], w=[kf], out=kf[:], in0=fg[:], scalar1=-1.0, scalar2=1.0, op0=ALU.mult, op1=ALU.add)
        p.I("dve", "tensor_tensor_scan", r=[cs, lf], w=[b], out=b[:], data0=rmask, data1=lf[:], initial=0.0, op0=ALU.mult, op1=ALU.add)
        bv = v3(b)
        p.I("dve", "tensor_tensor", r=[b], w=[t1], out=v3(t1), in0=bv, in1=bv[:, :, 31:32].to_broadcast([128, 8, 64]), op=ALU.subtract)
        p.I("act", "activation", r=[t1], w=[t2], out=t2[:], in_=t1[:], func=AF.Exp)
        p.I("dve", "tensor_tensor", r=[qf, t2], w=[Qt], out=Qt[:, sl], in0=qf[:], in1=t2[:], op=ALU.mult)
        p.I("act", "activation", r=[t1], w=[t2], out=t2[:], in_=t1[:], func=AF.Exp, scale=-1.0)
        p.I("dve", "tensor_tensor", r=[kf, t2], w=[Kt], out=Kt[:, sl], in0=kf[:], in1=t2[:], op=ALU.mult)
        p.I("act", "activation", r=[b], w=[t2], out=t2[:], in_=b[:], func=AF.Exp)
        p.I("pool", "tensor_tensor", r=[qf, t2], w=[Qb], out=Qb[:, sl], in0=qf[:], in1=t2[:], op=ALU.mult)
        p.I("pool", "tensor_copy", r=[t2], w=[dec], out=dec[:, blk * 8:(blk + 1) * 8], in_=v3(t2)[:, :, 63])
        p.I("dve", "tensor_tensor", r=[b], w=[t1], out=v3(t1), in0=bv, in1=bv[:, :, 63:64].to_broadcast([128, 8, 64]), op=ALU.subtract)
        p.I("act", "activation", r=[t1], w=[t3], out=t3[:], in_=t1[:], func=AF.Exp, scale=-1.0)
        p.I("dve", "tensor_tensor", r=[kf, t3], w=[t3], out=t3[:], in0=kf[:], in1=t3[:], op=ALU.mult)
        for half in range(2):
            pt = ps[half]
            for c4 in range(4):
                c = half * 4 + c4
                p.I("pe", "transpose", r=[t3, cs], w=[pt], out=pt[0:64, c4 * 128:(c4 + 1) * 128], in_=t3[:, c * 64:(c + 1) * 64], identity=ident)
            p.I("act", "activation", r=[pt], w=[Kbh], out=Kbh[:, blk * 8 + half * 4:blk * 8 + half * 4 + 4, :],
                in_=pt[0:64, :].rearrange("p (c k) -> p c k", c=4), func=AF.Copy)
    St = p.sb("St", [128, 128], F32)
    p.I("dve", "memset", w=[St], ap=St[:], constant=0.0)
    p.I("pool", "memset", w=[Sall.reg(0)], ap=Sall[:, 0, :], constant=0.0)
    for n in range(NCH - 1):
        pk = ps[2 + n % 3]
        p.I("pe", "matmul", r=[Kbh, Ih], w=[pk], out=pk[:, 0:128], lhsT=Kbh[:, n, :], rhs=Ih[:, n, :], start=True, stop=True)
        p.I("dve", "scalar_tensor_tensor", r=[St, dec, pk], w=[St], out=St[:], in0=St[:], scalar=dec[:, n:n + 1], in1=pk[:, 0:128],
            op0=ALU.mult, op1=ALU.add)
        p.I("pool", "tensor_copy", r=[St], w=[Sall.reg(n + 1)], out=Sall[:, n + 1, :], in_=St[:])
    at = [p.sb("at%d" % i, [64, 64], BF16) for i in range(2)]
    o1 = [p.sb("o1_%d" % i, [64, 128], F32) for i in range(2)]
    ot = [p.sb("ot%d" % i, [64, 8, 128], F32) for i in range(2)]
    gs = [p.sb("gs%d" % i, [64, 8, 128], F32) for i in range(2)]
    sm = [p.sb("sm%d" % i, [64, 2], F32) for i in range(2)]
    junk = p.sb("junk", [64, 128], F32)
    for n in range(NCH):
        g8 = n // 8
        if n % 8 == 0:
            gt = gs[g8 % 2]
            p.dma(gt[:], igv[1][:, n:n + 8, :], w=[gt])
            p.I("act", "activation", r=[gt], w=[gt], out=gt[:], in_=gt[:], func=AF.Silu)
            p.I("dve", "tensor_tensor", r=[gt, ngt], w=[gt], out=gt[:], in0=gt[:],
                in1=ngt[:].rearrange("p (o v) -> p o v", o=1).to_broadcast([64, 8, 128]), op=ALU.mult)
        gt = gs[g8 % 2]
        o8 = ot[g8 % 2]
        csl = slice(n * 64, (n + 1) * 64)
        pa, po = ps[5 + n % 2], ps[7 if n % 2 else 0]
        p.I("pe", "matmul", r=[Kt, Qt], w=[pa], out=pa[0:64, 0:64], lhsT=Kt[:, csl], rhs=Qt[:, csl], start=True, stop=True)
        a = at[n % 2]
        p.I("dve", "tensor_tensor", r=[pa, cs], w=[a], out=a[:], in0=pa[0:64, 0:64], in1=incl, op=ALU.mult)
        p.I("pe", "matmul", r=[a, Ih], w=[po], out=po[0:64, 0:128], lhsT=a[:], rhs=Ih[:, n, :], start=True, stop=True)
        p.I("pe", "matmul", r=[Qb, Sall.reg(n)], w=[po], out=po[0:64, 128:256], lhsT=Qb[:, csl], rhs=Sall[:, n, :], start=True, stop=True)
        oo = o1[n % 2]
        p.I("act", "activation", r=[po], w=[oo], out=oo[:], in_=po[0:64, 0:128], func=AF.Copy)
        p.I("dve", "tensor_tensor", r=[oo, po], w=[oo], out=oo[:], in0=oo[:], in1=po[0:64, 128:256], op=ALU.add)
        r = sm[n % 2]
        p.I("act", "activation", r=[oo], w=[junk, r], out=junk[:], in_=oo[:], func=AF.Square, accum_out=r[:, 0:1])
        p.I("act", "activation", r=[r, epsb], w=[r], out=r[:, 1:2], in_=r[:, 0:1], func=AF.Sqrt, scale=1.0 / 128, bias=epsb[0:64, 0:1])
        p.I("dve", "reciprocal", r=[r], w=[r], out=r[:, 1:2], in_=r[:, 1:2])
        p.I("dve", "scalar_tensor_tensor", r=[oo, r, gt], w=[o8], out=o8[:, n % 8, :], in0=oo[:], scalar=r[:, 1:2], in1=gt[:, n % 8, :],
            op0=ALU.mult, op1=ALU.mult)
        if n % 8 == 7:
            p.dma(yc.rearrange("(n s) v -> s n v", s=64)[:, n - 7:n + 1, :], o8[:], r=[o8])
    p.finish()
    return p


def cst_C():
    c = np.zeros((128, 768), np.float32)
    rm = np.ones(512, np.float32)
    rm[0::64] = 0.0
    c[:, 0:512] = rm[None, :]
    c[:, 512:640] = np.eye(128)
    i = np.arange(64)[:, None]
    t = np.arange(64)[None, :]
    c[0:64, 640:704] = (i <= t)
    return c


S = 4096
NEG = -30000.0


def build_D():
    p = Prog()
    qT = p.dram("qT", [64, 8192], F32, "ExternalInput")
    iqT = p.dram("iqT", [64, 8192], F32, "ExternalInput")
    iw = p.dram("iw", [128, 64], F32, "ExternalInput")
    kT = p.dram("kT", [64, S], F32, "ExternalInput")
    ikT = p.dram("ikT", [64, S], F32, "ExternalInput")
    v = p.dram("v", [S, 64], F32, "ExternalInput")
    vmask = p.dram("vmask", [128, 512], F32, "ExternalInput")
    id4 = p.dram("id4", [128, 512], BF16, "ExternalInput")
    yd = p.dram("yd", [1024, 512], F32, "ExternalOutput")
    qb = p.sb("qb", [64, 8192], BF16)
    iqb = p.sb("iqb", [64, 8192], BF16)
    kb = p.sb("kb", [64, S], BF16)
    ikb = p.sb("ikb", [64, S], BF16)
    vb = p.sb("vb", [128, 32, 65], BF16)
    iwt = p.sb("iwt", [128, 64], F32)
    vm = p.sb("vm", [128, 512], F32)
    i4 = p.sb("i4", [128, 512], BF16)
    st = [p.sb("st%d" % i, [128, 2048], F32) for i in range(2)]
    k = 0
    for (src, dst, n) in ((qT, qb, 8192), (iqT, iqb, 8192), (kT, kb, S), (ikT, ikb, S)):
        for c0 in range(0, n, 2048):
            s = st[k % 2]
            k += 1
            p.dma(s[0:64, :], src[:, c0:c0 + 2048], w=[s])
            p.I(("dve", "pool")[k % 2], "tensor_copy", r=[s], w=[dst], out=dst[:, c0:c0 + 2048], in_=s[0:64, :])
    s = st[k % 2]
    k += 1
    p.dma(s[:].rearrange("p (t e) -> p t e", e=64), v.rearrange("(t p) e -> p t e", p=128), w=[s])
    p.I("pool", "memset", w=[vb], ap=vb[:], constant=1.0)
    p.I("dve", "tensor_copy", r=[s], w=[vb], out=vb[:, :, 0:64], in_=s[:].rearrange("p (t e) -> p t e", e=64))
    p.dma(iwt[:], iw, w=[iwt])
    p.dma(vm[:], vmask, w=[vm])
    p.dma(i4[:], id4, w=[i4])
    p.I("dve", "tensor_scalar", r=[iwt], w=[iwt], out=iwt[:], in0=iwt[:], scalar1=(8 ** -0.5) * (64 ** -0.5), scalar2=None, op0=ALU.mult)

    sc = p.sb("sc", [128, S], F32)
    wk = p.sb("wk", [128, S], F32)
    biasb = [p.sb("bias%d" % i, [128, S], BF16) for i in range(2)]
    pts = p.sb("pts", [128, 32, 512], BF16)
    rl = [p.sb("rl%d" % i, [128, 512], F32) for i in range(2)]
    mx = p.sb("mx", [128, 8], F32)
    yo = [p.sb("yo%d" % i, [128, 8, 64], F32) for i in range(2)]
    sm = [p.sb("sm%d" % i, [128, 2], F32) for i in range(3)]
    pss = [p.ps("pss%d" % i, [128, 512], F32) for i in range(4)]
    psa = [p.ps("psa%d" % i, [128, 512], F32) for i in range(3)]
    cnt = {"ks": 0, "ka": 0, "kr": 0}

    def idx(m):
        for g in range(m + 1):
            gs = slice(g * 512, (g + 1) * 512)
            for h in range(8):
                ps = pss[cnt["ks"] % 4]
                cnt["ks"] += 1
                p.I("pe", "matmul", r=[iqb, ikb], w=[ps], out=ps[:], lhsT=iqb[:, m * 1024 + h * 128:m * 1024 + (h + 1) * 128],
                    rhs=ikb[:, gs], start=True, stop=True)
                r = rl[cnt["kr"] % 2]
                cnt["kr"] += 1
                p.I("act", "activation", r=[ps], w=[r], out=r[:], in_=ps[:], func=AF.Relu)
                ws = iwt[:, m * 8 + h:m * 8 + h + 1]
                if h == 0:
                    p.I("dve", "tensor_scalar", r=[r, iwt], w=[sc], out=sc[:, gs], in0=r[:], scalar1=ws, scalar2=None, op0=ALU.mult)
                else:
                    p.I("dve", "scalar_tensor_tensor", r=[r, iwt, sc], w=[sc], out=sc[:, gs], in0=r[:], scalar=ws, in1=sc[:, gs],
                        op0=ALU.mult, op1=ALU.add)
            if g == m:
                p.I("dve", "tensor_tensor", r=[sc, vm], w=[sc], out=sc[:, gs], in0=sc[:, gs], in1=vm[:], op=ALU.add)

    def topk_bias(m):
        N = (m + 1) * 512
        bias = biasb[m % 2]
        p.I("pool", "tensor_copy", r=[sc], w=[wk], out=wk[:, 0:N], in_=sc[:, 0:N])
        for rnd in range(32):
            p.I("dve", "max", r=[wk], w=[mx], out=mx[:], in_=wk[:, 0:N])
            if rnd < 31:
                p.I("dve", "match_replace", r=[wk, mx], w=[wk], out=wk[:, 0:N], in_to_replace=mx[:], in_values=wk[:, 0:N], imm_value=-1e30)
        p.I("dve", "tensor_scalar", r=[sc, mx], w=[bias], out=bias[:, 0:N], in0=sc[:, 0:N], scalar1=mx[:, 7:8], scalar2=NEG,
            op0=ALU.is_lt, op1=ALU.mult)
        p.I("dve", "tensor_tensor", r=[bias, vm], w=[bias], out=bias[:, m * 512:N], in0=bias[:, m * 512:N], in1=vm[:], op=ALU.add)

    def attn(m):
        nkt = 4 * m + 4
        bias = biasb[m % 2]
        o = yo[m % 2]
        for hg in range(2):
            for kt in range(nkt):
                ps = pss[cnt["ks"] % 4]
                cnt["ks"] += 1
                p.I("pe", "matmul", r=[kb, qb], w=[ps], out=ps[:], lhsT=kb[:, kt * 128:(kt + 1) * 128],
                    rhs=qb[:, m * 1024 + hg * 512:m * 1024 + (hg + 1) * 512], start=True, stop=False)
                p.I("pe", "matmul", r=[bias, i4], w=[ps], out=ps[:], lhsT=bias[:, kt * 128:(kt + 1) * 128], rhs=i4[:],
                    start=False, stop=True)
                p.I("act", "activation", r=[ps], w=[pts.reg(kt)], out=pts[:, kt, :], in_=ps[:], func=AF.Exp, scale=0.125)
            for h4 in range(4):
                pa = psa[cnt["ka"] % 3]
                r = sm[cnt["ka"] % 3]
                cnt["ka"] += 1
                for kt in range(nkt):
                    p.I("pe", "matmul", r=[pts.reg(kt), vb], w=[pa], out=pa[:, 0:65], lhsT=pts[:, kt, h4 * 128:(h4 + 1) * 128],
                        rhs=vb[:, kt, :], start=(kt == 0), stop=(kt == nkt - 1))
                p.I("act", "activation", r=[pa], w=[r], out=r[:, 0:1], in_=pa[:, 64:65], func=AF.Ln)
                p.I("act", "activation", r=[r], w=[r], out=r[:, 1:2], in_=r[:, 0:1], func=AF.Exp, scale=-1.0)
                p.I("act", "activation", r=[pa, r], w=[o], out=o[:, hg * 4 + h4, :], in_=pa[:, 0:64], func=AF.Copy, scale=r[:, 1:2])
        p.dma(yd[m * 128:(m + 1) * 128, :], o[:].rearrange("p h d -> p (h d)"), r=[o])

    idx(7)
    topk_bias(7)
    for m in range(7, -1, -1):
        if m - 1 >= 0:
            idx(m - 1)
            topk_bias(m - 1)
        attn(m)
    p.finish()
    return p


def vmask_const(j):
    pp = 2 * j + np.arange(128)[:, None] // 64
    ff = np.arange(512)[None, :] // 64
    return np.where(ff <= pp, 0.0, NEG).astype(np.float32)


def id4_const():
    return np.ascontiguousarray(np.tile(np.eye(128, dtype=np.float32), (1, 4))).astype(BF)


_PROGS = {}


def _prog(key, fn):
    if key not in _PROGS:
        _PROGS[key] = fn()
    return _PROGS[key]


def _run(p, ims):
    n = len(ims)
    return run_bass_kernel_spmd(p.nc, ims, core_ids=list(range(n))).results


def _cast_weights(arrs):
    sizes = [a.size for a in arrs]
    tot = sum(sizes)
    per = -(-tot // (8 * 128 * 4096)) * 4096
    flat = np.zeros(8 * 128 * per, np.float32)
    o = 0
    for a in arrs:
        flat[o:o + a.size] = a.reshape(-1)
        o += a.size
    flat = flat.reshape(8, 128, per)
    p = _prog(("L0", per), lambda: build_L0(per))
    res = _run(p, [{"src": flat[c]} for c in range(8)])
    out = np.concatenate([r["dst"].reshape(-1) for r in res])
    outs = []
    o = 0
    for a in arrs:
        outs.append(out[o:o + a.size].reshape(a.shape))
        o += a.size
    return outs


def _layer(l, x, positions, inp, W):
    f32 = np.float32
    g0 = np.ascontiguousarray(inp["norm_g"][l, 0].reshape(16, 128).T)
    cf = cf_const()
    ims = []
    for c in range(8):
        sl = slice(c * 1024, (c + 1) * 1024)
        ims.append({"xT": np.ascontiguousarray(x[sl].T), "g": g0, "wb": W["wb"],
                    "pos": np.ascontiguousarray(positions[sl].reshape(8, 128).T.astype(np.int32)), "cf": cf})
    res = _run(_prog("L1", build_L1), ims)
    proj = np.concatenate([r["proj"] for r in res], 0)
    y = np.zeros((4, 8192, 512), f32)
    lam_v = inp["diff_lambda"][l]
    sgv = inp["diff_subln_g"][l]
    ims = []
    for c in range(8):
        b, h = c // 4, c % 4
        pb = proj[b * 4096:(b + 1) * 4096]
        q = pb[:, h * 128:(h + 1) * 128].reshape(4096, 2, 64)
        k = pb[:, 512 + h * 128:512 + (h + 1) * 128].reshape(4096, 2, 64)
        ims.append({"qT": np.ascontiguousarray(q.transpose(1, 2, 0)), "kT": np.ascontiguousarray(k.transpose(1, 2, 0)),
                    "v": np.ascontiguousarray(pb[:, 1024 + h * 128:1024 + (h + 1) * 128]),
                    "lam4": np.ascontiguousarray(np.broadcast_to(lam_v.reshape(1, 256), (128, 256))),
                    "sg": np.ascontiguousarray(np.broadcast_to(sgv[None, :], (128, 128))), "masks": mask_const()})
    res = _run(_prog(("A", l), lambda: build_A(l)), ims)
    for c in range(8):
        b, h = c // 4, c % 4
        y[0, b * 4096:(b + 1) * 4096, h * 128:(h + 1) * 128] = res[c]["ya"]
    mu = inp["rwkv_mu"][l]
    ims = []
    cstb = cst_B()
    for c in range(8):
        b, hp = c // 4, c % 4
        pb = proj[b * 4096:(b + 1) * 4096, 1536:3328]
        cols = slice(hp * 128, (hp + 1) * 128)
        rkv = np.stack([pb[:, i * 512 + hp * 128:i * 512 + (hp + 1) * 128].T for i in range(3)])
        chp = np.zeros((128, 16), f32)
        for i in range(3):
            chp[:, i] = mu[i * 512 + hp * 128:i * 512 + (hp + 1) * 128]
        chp[:, 3] = mu[1536:1664]
        chp[:, 4] = mu[1664:1792]
        chp[:, 5] = inp["rwkv_w0"][l][cols]
        chp[:, 6] = inp["rwkv_a0"][l][cols]
        chp[:, 7] = inp["rwkv_k_k"][l][cols]
        chp[:, 8] = inp["rwkv_k_a"][l][cols]
        chp[:, 9] = inp["rwkv_r_k"][l].reshape(512)[cols]
        chp[:, 10] = inp["rwkv_gn_g"][l][cols]
        chp[:, 11] = inp["rwkv_gn_b"][l][cols]
        wup = np.concatenate([inp["rwkv_w_up"][l][:, cols], inp["rwkv_a_up"][l][:, cols]], 0)
        ims.append({"rkvT": np.ascontiguousarray(rkv), "lowT": np.ascontiguousarray(pb[:, 1536:1792].T), "chp": chp,
                    "wup": np.ascontiguousarray(wup), "gup": np.ascontiguousarray(inp["rwkv_g_up"][l][:, cols]), "cst": cstb})
    res = _run(_prog("B", build_B), ims)
    for c in range(8):
        b, hp = c // 4, c % 4
        y[1, b * 4096:(b + 1) * 4096, hp * 128:(hp + 1) * 128] = res[c]["ybT"].T
    ims = []
    cstc = cst_C()
    for c in range(8):
        b, h = c // 4, c % 4
        pc = proj[b * 4096:(b + 1) * 4096, 3328:5376]
        hc = slice(h * 128, (h + 1) * 128)
        qf = np.stack([pc[:, 0:512][:, hc].T, pc[:, 512:1024][:, hc].T])
        ig = np.stack([pc[:, 1024:1536][:, hc], pc[:, 1536:2048][:, hc]])
        ims.append({"qfT": np.ascontiguousarray(qf), "ig": np.ascontiguousarray(ig),
                    "lbl": np.ascontiguousarray(inp["hgrn_lb_logits"][:, hc].T),
                    "ng": np.ascontiguousarray(np.broadcast_to(inp["hgrn_norm_g"][l][hc][None, :], (64, 128))), "cst": cstc})
    res = _run(_prog(("C", l), lambda: build_C(l)), ims)
    for c in range(8):
        b, h = c // 4, c % 4
        y[2, b * 4096:(b + 1) * 4096, h * 128:(h + 1) * 128] = res[c]["yc"]
    O = 5376
    ims = []
    rows_all = []
    for c in range(8):
        b, j = c // 4, c % 4
        pb = proj[b * 4096:(b + 1) * 4096]
        rows = np.concatenate([np.arange(i * 128, (i + 1) * 128) for i in [4 * m + j for m in range(8)]])
        rows_all.append(rows)
        q = pb[rows, O:O + 512].reshape(8, 128, 8, 64)
        iq = pb[rows, O + 640:O + 1152].reshape(8, 128, 8, 64)
        iw = pb[rows, O + 1216:O + 1224].reshape(8, 128, 8)
        ims.append({"qT": np.ascontiguousarray(q.transpose(3, 0, 2, 1).reshape(64, 8192)),
                    "iqT": np.ascontiguousarray(iq.transpose(3, 0, 2, 1).reshape(64, 8192)),
                    "iw": np.ascontiguousarray(iw.transpose(1, 0, 2).reshape(128, 64)),
                    "kT": np.ascontiguousarray(pb[:, O + 512:O + 576].T), "ikT": np.ascontiguousarray(pb[:, O + 1152:O + 1216].T),
                    "v": np.ascontiguousarray(pb[:, O + 576:O + 640]), "vmask": vmask_const(j), "id4": id4_const()})
    res = _run(_prog("D", build_D), ims)
    for c in range(8):
        b = c // 4
        y[3, b * 4096 + rows_all[c]] = res[c]["yd"]
    del proj
    g3 = np.ascontiguousarray(np.concatenate([inp["norm_g"][l, i].reshape(16, 128).T for i in range(4)], axis=1))
    ims = []
    for c in range(8):
        sl = slice(c * 1024, (c + 1) * 1024)
        ims.append({"xT": np.ascontiguousarray(x[sl].T), "yT": np.ascontiguousarray(y[:, sl].transpose(0, 2, 1)), "wg": W["wg"], "g3": g3,
                    "wbr": W["wbr"], "wout": W["wout"], "w1": W["w1"], "w2": W["w2"]})
    res = _run(_prog("L3", lambda: build_L3(1024)), ims)
    return np.ascontiguousarray(np.concatenate([r["xo"].T for r in res], 0))


def kernel(**inputs):
    inp = {k: np.asarray(v) for k, v in inputs.items()}
    x = np.ascontiguousarray(inp["x"].reshape(8192, D)).astype(np.float32, copy=False)
    positions = inp["positions"].reshape(8192)
    tiled = []
    for l in range(2):
        w_in = inp["w_in"][l]
        tiled.append(prep_w_in(w_in))
        tiled.append(prep_wg(np.ascontiguousarray(w_in[:, NMIX:])))
        tiled.extend(prep_L3_weights(inp["w_branch"][l], inp["w_out"][l], inp["mlp_w1"][l], inp["mlp_w2"][l]))
    cast = _cast_weights(tiled)
    del tiled
    for l in range(2):
        W = dict(zip(("wb", "wg", "wbr", "wout", "w1", "w2"), cast[l * 6:(l + 1) * 6]))
        x = _layer(l, x, positions, inp, W)
    return x.reshape(2, 4096, D).astype(np.float32)
```

```python
import math
import numpy as np
import ml_dtypes
from contextlib import ExitStack
import concourse.bass as bass
import concourse.mybir as mybir
from concourse.bass_utils import run_bass_kernel_spmd

BF = ml_dtypes.bfloat16


F32 = mybir.dt.float32
BF16 = mybir.dt.bfloat16
I32 = mybir.dt.int32
AF = mybir.ActivationFunctionType
ALU = mybir.AluOpType
AX = mybir.AxisListType

SAME_ENG_SYNC = True
NDMA = 24


class Dep:
    __slots__ = ("w", "r")

    def __init__(self):
        self.w = None
        self.r = []


class Buf:
    def __init__(self, t, nreg=1):
        self.t = t
        self.d = Dep()
        self.regs = {}

    def reg(self, key):
        if key not in self.regs:
            self.regs[key] = Dep()
        return self.regs[key]

    def __getitem__(self, idx):
        return self.t[idx]


class Prog:
    def __init__(self):
        self.nc = bass.Bass("TRN2", target_bir_lowering=False)
        nc = self.nc
        self.es = ExitStack()
        self.eng = {"pe": nc.tensor, "act": nc.scalar, "dve": nc.vector, "pool": nc.gpsimd, "sp": nc.sync}
        self.sem = {}
        self.cnt = {}
        self.clock = {}
        self.hist = {}
        for e in self.eng:
            self.sem[e] = self.es.enter_context(nc.semaphore("s_" + e))
            self.cnt[e] = 0
            self.clock[e] = {}
            self.hist[e] = {}
        for j in range(NDMA):
            e = "d%d" % j
            self.sem[e] = self.es.enter_context(nc.semaphore("s_" + e))
            self.cnt[e] = 0
            self.hist[e] = {}
        self.dma_k = 0
        self.nwaits = 0
        self.ninst = 0
        self.q = {e: [] for e in self.eng}

    def dram(self, name, shape, dtype, kind):
        return self.nc.dram_tensor(name, list(shape), dtype, kind=kind).ap()

    def sb(self, name, shape, dtype=F32):
        return Buf(self.es.enter_context(self.nc.sbuf_tensor(name, list(shape), dtype)))

    def ps(self, name, shape, dtype=F32):
        return Buf(self.es.enter_context(self.nc.psum_tensor(name, list(shape), dtype)))

    def _semval(self, e, n):
        return n * 16 if (e[0] == "d" and e[1:].isdigit()) else n

    def _wait(self, e, deps):
        need = {}
        for (e2, n) in deps:
            if e2 == e and (e == "pe" or not SAME_ENG_SYNC):
                continue
            if self.clock[e].get(e2, 0) >= n:
                continue
            if need.get(e2, 0) < n:
                need[e2] = n
        for e2, n in need.items():
            if self.clock[e].get(e2, 0) >= n:
                continue
            self.q[e].append(("w", self.sem[e2], self._semval(e2, n)))
            self.nwaits += 1
            h = self.hist[e2].get(n)
            if h:
                for k, v in h.items():
                    if self.clock[e].get(k, 0) < v:
                        self.clock[e][k] = v
            self.clock[e][e2] = max(self.clock[e].get(e2, 0), n)

    def _deps(self, r, w):
        deps = []
        for d in r:
            d = d.d if isinstance(d, Buf) else d
            if d.w is not None:
                deps.append(d.w)
        for d in w:
            d = d.d if isinstance(d, Buf) else d
            if d.w is not None:
                deps.append(d.w)
            deps.extend(d.r)
        return deps

    def _commit(self, tag, r, w):
        for d in r:
            d = d.d if isinstance(d, Buf) else d
            d.r.append(tag)
            if len(d.r) > 64:
                best = {}
                for (e2, n) in d.r:
                    if best.get(e2, 0) < n:
                        best[e2] = n
                d.r = list(best.items())
        for d in w:
            d = d.d if isinstance(d, Buf) else d
            d.w = tag
            d.r = []

    def I(self, e, method, r=(), w=(), **kw):
        self._wait(e, self._deps(r, w))
        self.cnt[e] += 1
        n = self.cnt[e]
        self.q[e].append(("i", (method, kw), self.sem[e], 1))
        self.hist[e][n] = dict(self.clock[e])
        if e == "pe" or not SAME_ENG_SYNC:
            self.clock[e][e] = n
        self._commit((e, n), r, w)
        self.ninst += 1

    def dma(self, out, in_, r=(), w=(), q="sp", **kw):
        j = self.dma_k % NDMA
        self.dma_k += 1
        de = "d%d" % j
        prev = self.cnt[de]
        deps = self._deps(r, w)
        if prev > 0:
            deps.append((de, prev))
        self._wait(q, deps)
        self.q[q].append(("d", (out, in_, kw), self.sem[de], 16))
        self.cnt[de] = prev + 1
        self.hist[de][prev + 1] = dict(self.clock[q])
        self._commit((de, prev + 1), r, w)
        self.ninst += 1

    def coll(self, kind, ins, outs, r=(), w=()):
        q = "pool"
        j = self.dma_k % NDMA
        self.dma_k += 1
        de = "d%d" % j
        prev = self.cnt[de]
        deps = self._deps(r, w)
        if prev > 0:
            deps.append((de, prev))
        self._wait(q, deps)
        self.q[q].append(("c", (kind, ins, outs), self.sem[de], 16))
        self.cnt[de] = prev + 1
        self.hist[de][prev + 1] = dict(self.clock[q])
        self._commit((de, prev + 1), r, w)
        self.ninst += 1

    def finish(self, q="sp"):
        deps = []
        for j in range(NDMA):
            de = "d%d" % j
            if self.cnt[de] > 0:
                deps.append((de, self.cnt[de]))
        self._wait(q, deps)

        nc = self.nc
        prog = self
        with nc.Block() as block:
            def mk(e):
                def body(engh):
                    for it in prog.q[e]:
                        if it[0] == "w":
                            engh.wait_ge(it[1], it[2])
                        elif it[0] == "i":
                            getattr(engh, it[1][0])(**it[1][1]).then_inc(it[2], it[3])
                        elif it[0] == "c":
                            kind, ins, outs = it[1]
                            engh.collective_compute(kind, ALU.bypass, [[0,1,2,3],[4,5,6,7]], ins, outs).then_inc(it[2], it[3])
                        else:
                            o, i, kw = it[1]
                            engh.dma_start(out=o, in_=i, **kw).then_inc(it[2], it[3])
                return body
            block.tensor(mk("pe"))
            block.scalar(mk("act"))
            block.vector(mk("dve"))
            block.gpsimd(mk("pool"))
            block.sync(mk("sp"))
        self.es.close()


D = 2048
DIN = 14792
NCB = 13
NMIX = 6600
EPS = 1e-6
ROPE = {0: [(0, 8)], 1: [(0, 8)], 10: [(256, 4)], 11: [(0, 5), (384, 2)], 12: [(0, 7)]}


def build_L0(n):
    p = Prog()
    src = p.dram("src", [128, n], F32, "ExternalInput")
    dst = p.dram("dst", [128, n], BF16, "ExternalOutput")
    CH = 4096
    st = [p.sb("st%d" % i, [128, CH], F32) for i in range(3)]
    ob = [p.sb("ob%d" % i, [128, CH], BF16) for i in range(3)]
    k = 0
    for c0 in range(0, n, CH):
        c1 = min(n, c0 + CH)
        s, o = st[k % 3], ob[k % 3]
        p.dma(s[:, 0:c1 - c0], src[:, c0:c1], w=[s], q="sp")
        e = ("dve", "pool")[k % 2]
        p.I(e, "tensor_copy", r=[s], w=[o], out=o[:, 0:c1 - c0], in_=s[:, 0:c1 - c0])
        p.dma(dst[:, c0:c1], o[:, 0:c1 - c0], r=[o], q="act")
        k += 1
    p.finish()
    return p


def fm_rmsnorm(p, src, gt, gcol, dst, KC, T, ones, psl, sq, rstd, dim):
    nh = T // 512
    for kc in range(KC):
        s = sq[kc % 2]
        p.I("act", "activation", r=[src], w=[s], out=s[:, 0:T], in_=src[:, kc, :], func=AF.Square)
        for h in range(nh):
            p.I("pe", "matmul", r=[s, ones], w=[psl[h]], out=psl[h][:], lhsT=ones[:], rhs=s[:, h * 512:(h + 1) * 512],
                start=(kc == 0), stop=(kc == KC - 1))
    for h in range(nh):
        p.I("act", "activation", r=[psl[h], EPSB[0]], w=[rstd], out=rstd[:, h * 512:(h + 1) * 512], in_=psl[h][:],
            func=AF.Sqrt, scale=1.0 / dim, bias=EPSB[0][:, 0:1])
    p.I("dve", "reciprocal", r=[rstd], w=[rstd], out=rstd[:, 0:T], in_=rstd[:, 0:T])
    for kc in range(KC):
        p.I("dve", "scalar_tensor_tensor", r=[src, gt, rstd], w=[dst], out=dst[:, kc, :], in0=src[:, kc, :],
            scalar=gt[:, gcol + kc:gcol + kc + 1], in1=rstd[:, 0:T], op0=ALU.mult, op1=ALU.mult)


EPSB = [None]


def consts(p):
    ones = p.sb("ones", [128, 128], BF16)
    p.I("dve", "memset", w=[ones], ap=ones[:], constant=1.0)
    eb = p.sb("epsb", [128, 1], F32)
    p.I("dve", "memset", w=[eb], ap=eb[:], constant=EPS)
    EPSB[0] = eb
    return ones


def build_L1():
    p = Prog()
    T = 1024
    xT = p.dram("xT", [D, T], F32, "ExternalInput")
    g = p.dram("g", [128, 16], F32, "ExternalInput")
    wb = p.dram("wb", [NCB, 128, 8192], BF16, "ExternalInput")
    pos = p.dram("pos", [128, 8], I32, "ExternalInput")
    cf = p.dram("cf", [128, 32], F32, "ExternalInput")
    proj = p.dram("proj", [T, NCB * 512], F32, "ExternalOutput")
    ones = consts(p)
    xs = p.sb("xs", [128, 16, T], F32)
    hT = p.sb("hT", [128, 16, T], BF16)
    gt = p.sb("gt", [128, 16], F32)
    sq = [p.sb("sq%d" % i, [128, T], BF16) for i in range(2)]
    rstd = p.sb("rstd", [128, T], F32)
    psn = [p.ps("psn%d" % i, [128, 512], F32) for i in range(2)]
    pst = [p.ps("ps%d" % i, [128, 512], F32) for i in range(4)]
    xv = xT.rearrange("(kc p) t -> p kc t", p=128)
    for kc in range(0, 16, 4):
        p.dma(xs[:, kc:kc + 4, :], xv[:, kc:kc + 4, :], w=[xs], q=("sp", "act")[(kc // 4) % 2])
    p.dma(gt[:], g, w=[gt])
    posi = p.sb("posi", [128, 8], I32)
    posf = p.sb("posf", [128, 8], F32)
    cft = p.sb("cft", [128, 32], F32)
    p.dma(posi[:], pos, w=[posi])
    p.dma(cft[:], cf, w=[cft])
    p.I("dve", "tensor_copy", r=[posi], w=[posf], out=posf[:], in_=posi[:])
    qq = p.sb("qq", [128, 2, 8, 32], F32)
    qi = p.sb("qi", [128, 2, 8, 32], I32)
    qf = p.sb("qf", [128, 2, 8, 32], F32)
    msk = p.sb("msk", [128, 2, 8, 32], F32)
    sc = p.sb("sc", [128, 2, 8, 32], F32)
    for tt in range(8):
        p.I("dve", "tensor_scalar", r=[cft, posf], w=[qq], out=qq[:, 0, tt, :], in0=cft[:], scalar1=posf[:, tt:tt + 1],
            scalar2=None, op0=ALU.mult)
    p.I("dve", "tensor_scalar", r=[qq], w=[qq], out=qq[:, 1, :, :], in0=qq[:, 0, :, :], scalar1=0.25, scalar2=None, op0=ALU.add)
    p.I("dve", "tensor_copy", r=[qq], w=[qi], out=qi[:], in_=qq[:])
    p.I("dve", "tensor_copy", r=[qi], w=[qf], out=qf[:], in_=qi[:])
    p.I("dve", "tensor_tensor", r=[qq, qf], w=[qq], out=qq[:], in0=qq[:], in1=qf[:], op=ALU.subtract)
    p.I("dve", "tensor_scalar", r=[qq], w=[msk], out=msk[:], in0=qq[:], scalar1=0.5, scalar2=None, op0=ALU.is_gt)
    p.I("dve", "tensor_tensor", r=[qq, msk], w=[qq], out=qq[:], in0=qq[:], in1=msk[:], op=ALU.subtract)
    p.I("dve", "tensor_scalar", r=[qq], w=[msk], out=msk[:], in0=qq[:], scalar1=-0.5, scalar2=None, op0=ALU.is_lt)
    p.I("dve", "tensor_tensor", r=[qq, msk], w=[qq], out=qq[:], in0=qq[:], in1=msk[:], op=ALU.add)
    p.I("act", "activation", r=[qq], w=[sc], out=sc[:], in_=qq[:], func=AF.Sin, scale=6.28318)
    fm_rmsnorm(p, xs, gt, 0, hT, 16, T, ones, psn, sq, rstd, D)
    wt = [p.sb("w%d" % i, [128, 8192], BF16) for i in range(3)]
    ot = [p.sb("o%d" % i, [128, 512], F32) for i in range(4)]
    tmp = [p.sb("rt%d" % i, [128, 8, 32], F32) for i in range(4)]
    k = 0
    def wload(cb):
        w_ = wt[cb % 3]
        p.dma(w_[:, 0:4096], wb[cb, :, 0:4096], w=[w_], q="sp")
        p.dma(w_[:, 4096:8192], wb[cb, :, 4096:8192], w=[w_], q="sp")

    wload(0)
    wload(1)
    for cb in range(NCB):
        w = wt[cb % 3]
        if cb + 2 < NCB:
            wload(cb + 2)
        for tt in range(8):
            ps = pst[k % 4]
            o = ot[k % 4]
            k += 1
            for kc in range(16):
                p.I("pe", "matmul", r=[hT, w], w=[ps], out=ps[:], lhsT=hT[:, kc, tt * 128:(tt + 1) * 128],
                    rhs=w[:, kc * 512:(kc + 1) * 512], start=(kc == 0), stop=(kc == 15))
            p.I("act", "activation", r=[ps], w=[o], out=o[:], in_=ps[:], func=AF.Copy)
            for (s0, nh) in ROPE.get(cb, []):
                ov = o[:, s0:s0 + nh * 64].rearrange("p (h two d) -> p h two d", two=2, d=32)
                x1, x2 = ov[:, :, 0, :], ov[:, :, 1, :]
                sn = sc[:, 0, tt:tt + 1, :].to_broadcast([128, nh, 32])
                cs = sc[:, 1, tt:tt + 1, :].to_broadcast([128, nh, 32])
                t1, t2, t3, t4 = [t[:, 0:nh, :] for t in tmp]
                p.I("dve", "tensor_tensor", r=[o, sc], w=[tmp[0]], out=t1, in0=x1, in1=cs, op=ALU.mult)
                p.I("dve", "tensor_tensor", r=[o, sc], w=[tmp[1]], out=t2, in0=x2, in1=sn, op=ALU.mult)
                p.I("dve", "tensor_tensor", r=[o, sc], w=[tmp[2]], out=t3, in0=x2, in1=cs, op=ALU.mult)
                p.I("dve", "tensor_tensor", r=[o, sc], w=[tmp[3]], out=t4, in0=x1, in1=sn, op=ALU.mult)
                p.I("dve", "tensor_tensor", r=[tmp[0], tmp[1]], w=[o], out=x1, in0=t1, in1=t2, op=ALU.subtract)
                p.I("dve", "tensor_tensor", r=[tmp[2], tmp[3]], w=[o], out=x2, in0=t3, in1=t4, op=ALU.add)
            p.dma(proj[tt * 128:(tt + 1) * 128, cb * 512:(cb + 1) * 512], o[:], r=[o], q="sp")
    p.finish()
    return p


def prep_w_in(wbf):
    w = np.zeros((D, NCB * 512), dtype=wbf.dtype)
    w[:, :NMIX] = wbf[:, :NMIX]
    w = w.reshape(16, 128, NCB, 512).transpose(2, 1, 0, 3).reshape(NCB, 128, 8192)
    return np.ascontiguousarray(w)


def cf_const():
    inv = 10000.0 ** (-np.arange(0, 64, 2, dtype=np.float64) / 64.0)
    c = (inv / (2 * math.pi)).astype(np.float32)
    return np.ascontiguousarray(np.broadcast_to(c[None, :], (128, 32)))


def build_L3(T=2048):
    p = Prog()
    H = 512
    xT = p.dram("xT", [D, T], F32, "ExternalInput")
    yT = p.dram("yT", [4, 512, T], F32, "ExternalInput")
    wg = p.dram("wg", [64, 128, 2048], BF16, "ExternalInput")
    g3 = p.dram("g3", [128, 64], F32, "ExternalInput")
    wbr = p.dram("wbr", [16, 128, 2048], BF16, "ExternalInput")
    wout = p.dram("wout", [16, 128, 2048], BF16, "ExternalInput")
    w1 = p.dram("w1", [64, 128, 2048], BF16, "ExternalInput")
    w2 = p.dram("w2", [16, 4, 128, 2048], BF16, "ExternalInput")
    xo = p.dram("xo", [D, T], F32, "ExternalOutput")
    ones = consts(p)
    xh = p.sb("xh", [128, 16, H], F32)
    zT = p.sb("zT", [128, 16, H], F32)
    mb = p.sb("mb", [128, 16, H], BF16)
    uT = p.sb("uT", [128, 64, H], BF16)
    gt = p.sb("gt", [128, 64], F32)
    hT = p.sb("hT", [128, 16, H], BF16)
    sq = [p.sb("sq%d" % i, [128, H], BF16) for i in range(2)]
    rstd = p.sb("rstd", [128, H], F32)
    psn = [p.ps("psn0", [128, 512], F32)]
    pst = [p.ps("ps%d" % i, [128, 512], F32) for i in range(4)]
    wt = [p.sb("wt%d" % i, [128, 2048], BF16) for i in range(4)]
    psg = [p.ps("psg%d" % i, [128, 512], F32) for i in range(2)]
    sg = [p.sb("sg%d" % i, [128, H], F32) for i in range(2)]
    tm = [p.sb("tm%d" % i, [128, H], F32) for i in range(2)]
    ys = [p.sb("ys%d" % i, [128, 4, H], F32) for i in range(1)]
    wgp = [p.sb("wgp%d" % i, [128, 2048], BF16) for i in range(2)]
    gk = 0
    p.dma(gt[:], g3, w=[gt])
    xv = xT.rearrange("(kc p) t -> p kc t", p=128)
    xov = xo.rearrange("(kc p) t -> p kc t", p=128)
    yv = yT.rearrange("n (kc p) t -> n p kc t", p=128)
    wk = 0
    pk = 0
    for hf in range(T // H):
        tsl = slice(hf * H, (hf + 1) * H)
        for kc in range(0, 16, 8):
            p.dma(xh[:, kc:kc + 8, :], xv[:, kc:kc + 8, tsl], w=[xh], q="sp")
        fm_rmsnorm(p, xh, gt, 0, hT, 16, H, ones, psn, sq, rstd, D)
        for n in range(4):
            y = ys[0]
            p.dma(y[:], yv[n, :, :, tsl], w=[y], q="sp")
            p.I("dve", "tensor_copy", r=[y], w=[uT], out=uT[:, n * 4:(n + 1) * 4, :], in_=y[:])
        for oc in range(16):
            w = wt[wk % 4]
            wk += 1
            p.dma(w[:], wbr[oc], w=[w], q="sp")
            for n in range(4):
                wgt = wgp[gk % 2]
                gk += 1
                p.dma(wgt[:], wg[oc * 4 + n], w=[wgt], q="sp")
                pg = psg[n % 2]
                for kc in range(16):
                    p.I("pe", "matmul", r=[hT, wgt], w=[pg], out=pg[:], lhsT=wgt[:, kc * 128:(kc + 1) * 128], rhs=hT[:, kc, :],
                        start=(kc == 0), stop=(kc == 15))
                ps = pst[pk % 4]
                pk += 1
                for kc in range(4):
                    p.I("pe", "matmul", r=[uT, w], w=[ps], out=ps[:], lhsT=w[:, (n * 4 + kc) * 128:(n * 4 + kc + 1) * 128],
                        rhs=uT[:, n * 4 + kc, :], start=(kc == 0), stop=(kc == 3))
                s = sg[n % 2]
                p.I("act", "activation", r=[pg], w=[s], out=s[:], in_=pg[:], func=AF.Sigmoid)
                if n == 0:
                    p.I("dve", "tensor_tensor", r=[ps, s], w=[zT], out=zT[:, oc, :], in0=ps[:], in1=s[:], op=ALU.mult)
                else:
                    t = tm[n % 2]
                    p.I("dve", "tensor_tensor", r=[ps, s], w=[t], out=t[:], in0=ps[:], in1=s[:], op=ALU.mult)
                    p.I("pool", "tensor_tensor", r=[t, zT], w=[zT], out=zT[:, oc, :], in0=zT[:, oc, :], in1=t[:], op=ALU.add)
            p.I("pool", "tensor_copy", r=[zT], w=[mb], out=mb[:, oc, :], in_=zT[:, oc, :])
        for oc in range(16):
            w = wt[wk % 4]
            wk += 1
            p.dma(w[:], wout[oc], w=[w], q="sp")
            ps = pst[pk % 4]
            pk += 1
            for kc in range(16):
                p.I("pe", "matmul", r=[mb, w], w=[ps], out=ps[:], lhsT=w[:, kc * 128:(kc + 1) * 128], rhs=mb[:, kc, :],
                    start=(kc == 0), stop=(kc == 15))
            p.I("act", "activation", r=[ps], w=[zT], out=zT[:, oc, :], in_=ps[:], func=AF.Copy)
        fm_rmsnorm(p, zT, gt, 16, zT, 16, H, ones, psn, sq, rstd, D)
        for kc in range(0, 16, 4):
            p.I("pool", "tensor_tensor", r=[xh, zT], w=[xh], out=xh[:, kc:kc + 4, :], in0=xh[:, kc:kc + 4, :], in1=zT[:, kc:kc + 4, :], op=ALU.add)
        fm_rmsnorm(p, xh, gt, 32, mb, 16, H, ones, psn, sq, rstd, D)
        for oc in range(64):
            w = wt[wk % 4]
            wk += 1
            p.dma(w[:], w1[oc], w=[w], q="sp")
            ps = pst[pk % 4]
            pk += 1
            for kc in range(16):
                p.I("pe", "matmul", r=[mb, w], w=[ps], out=ps[:], lhsT=w[:, kc * 128:(kc + 1) * 128], rhs=mb[:, kc, :],
                    start=(kc == 0), stop=(kc == 15))
            t = tm[oc % 2]
            p.I("act", "activation", r=[ps], w=[t], out=t[:], in_=ps[:], func=AF.Relu)
            p.I(("dve", "pool")[oc % 2], "tensor_tensor", r=[t], w=[uT], out=uT[:, oc, :], in0=t[:], in1=t[:], op=ALU.mult)
        for oc in range(16):
            ps = pst[pk % 4]
            pk += 1
            for q in range(4):
                w = wt[wk % 4]
                wk += 1
                p.dma(w[:], w2[oc, q], w=[w], q="sp")
                for kc in range(16):
                    p.I("pe", "matmul", r=[uT, w], w=[ps], out=ps[:], lhsT=w[:, kc * 128:(kc + 1) * 128], rhs=uT[:, q * 16 + kc, :],
                        start=(q == 0 and kc == 0), stop=(q == 3 and kc == 15))
            p.I("act", "activation", r=[ps], w=[zT], out=zT[:, oc, :], in_=ps[:], func=AF.Copy)
        fm_rmsnorm(p, zT, gt, 48, zT, 16, H, ones, psn, sq, rstd, D)
        for kc in range(0, 16, 4):
            p.I("pool", "tensor_tensor", r=[xh, zT], w=[xh], out=xh[:, kc:kc + 4, :], in0=xh[:, kc:kc + 4, :], in1=zT[:, kc:kc + 4, :], op=ALU.add)
        for kc in range(0, 16, 8):
            p.dma(xov[:, kc:kc + 8, tsl], xh[:, kc:kc + 8, :], r=[xh], q="sp")
    p.finish()
    return p


def prep_wg(wgate):
    g = wgate.reshape(16, 128, 4, 16, 128).transpose(3, 2, 1, 0, 4).reshape(64, 128, 2048)
    return np.ascontiguousarray(g)


def prep_L3_weights(wbr, wout, w1, w2):
    a = wbr.reshape(4, 4, 128, 16, 128).transpose(3, 2, 0, 1, 4).reshape(16, 128, 2048)
    b = wout.reshape(16, 128, 16, 128).transpose(2, 1, 0, 3).reshape(16, 128, 2048)
    c = w1.reshape(16, 128, 64, 128).transpose(2, 1, 0, 3).reshape(64, 128, 2048)
    d = w2.reshape(4, 16, 128, 16, 128).transpose(3, 0, 2, 1, 4).reshape(16, 4, 128, 2048)
    return [np.ascontiguousarray(t) for t in (a, b, c, d)]


S = 4096


def build_A(layer):
    p = Prog()
    lam_init = 0.8 - 0.6 * math.exp(-0.3 * layer)
    qT = p.dram("qT", [2, 64, S], F32, "ExternalInput")
    kT = p.dram("kT", [2, 64, S], F32, "ExternalInput")
    v = p.dram("v", [S, 128], F32, "ExternalInput")
    lam4 = p.dram("lam4", [128, 256], F32, "ExternalInput")
    sg = p.dram("sg", [128, 128], F32, "ExternalInput")
    masks = p.dram("masks", [4, 128, 512], BF16, "ExternalInput")
    ya = p.dram("ya", [S, 128], F32, "ExternalOutput")
    qb = [p.sb("qb%d" % m, [64, S], BF16) for m in range(2)]
    kb = [p.sb("kb%d" % m, [64, S], BF16) for m in range(2)]
    vb = p.sb("vb", [128, 32, 129], BF16)
    st = [p.sb("st%d" % i, [128, 4096], F32) for i in range(2)]
    mk_ = p.sb("mk", [128, 4, 512], BF16)
    lt = p.sb("lt", [128, 256], F32)
    sgt = p.sb("sgt", [128, 128], F32)
    epsb = p.sb("epsb", [128, 1], F32)
    p.I("dve", "memset", w=[epsb], ap=epsb[:], constant=1e-6)
    k = 0
    for m in range(2):
        for (src, dst) in ((qT, qb[m]), (kT, kb[m])):
            s = st[k % 2]
            k += 1
            p.dma(s[0:64, :], src[m], w=[s])
            p.I(("dve", "pool")[k % 2], "tensor_copy", r=[s], w=[dst], out=dst[:], in_=s[0:64, :])
    s = st[k % 2]
    k += 1
    p.dma(s[:].rearrange("p (t e) -> p t e", e=128), v.rearrange("(t p) e -> p t e", p=128), w=[s])
    p.I("pool", "memset", w=[vb], ap=vb[:], constant=1.0)
    p.I("dve", "tensor_copy", r=[s], w=[vb], out=vb[:, :, 0:128], in_=s[:].rearrange("p (t e) -> p t e", e=128))
    p.dma(mk_[:], masks.rearrange("j p f -> p j f"), w=[mk_])
    p.dma(lt[:], lam4, w=[lt])
    p.dma(sgt[:], sg, w=[sgt])
    pr = p.sb("pr", [128, 2, 64], F32)
    s12 = p.sb("s12", [128, 2], F32)
    e12 = p.sb("e12", [128, 2], F32)
    nlam = p.sb("nlam", [128, 1], F32)
    ltv = lt[:].rearrange("p (a d) -> p a d", d=64)
    p.I("dve", "tensor_tensor", r=[lt], w=[pr], out=pr[:, 0, :], in0=ltv[:, 0, :], in1=ltv[:, 1, :], op=ALU.mult)
    p.I("dve", "tensor_tensor", r=[lt], w=[pr], out=pr[:, 1, :], in0=ltv[:, 2, :], in1=ltv[:, 3, :], op=ALU.mult)
    p.I("dve", "tensor_reduce", r=[pr], w=[s12], out=s12[:], in_=pr[:], axis=AX.X, op=ALU.add)
    p.I("act", "activation", r=[s12], w=[e12], out=e12[:], in_=s12[:], func=AF.Exp)
    p.I("dve", "tensor_tensor", r=[e12], w=[nlam], out=nlam[:], in0=e12[:, 1:2], in1=e12[:, 0:1], op=ALU.subtract)
    p.I("dve", "tensor_scalar", r=[nlam], w=[nlam], out=nlam[:], in0=nlam[:], scalar1=-lam_init, scalar2=None, op0=ALU.add)
    p.I("dve", "tensor_scalar", r=[sgt], w=[sgt], out=sgt[:], in0=sgt[:], scalar1=1.0 - lam_init, scalar2=None, op0=ALU.mult)
    pss = [p.ps("pss%d" % i, [128, 512], F32) for i in range(3)]
    psa = [p.ps("psa%d" % i, [128, 512], F32) for i in range(3)]
    pts = p.sb("pts", [128, 32, 512], BF16)
    ob = [p.sb("ob%d" % i, [128, 4, 128], F32) for i in range(2)]
    sm = [p.sb("sm%d" % i, [128, 4], F32) for i in range(3)]
    junk = p.sb("junk", [128, 128], F32)
    ks = 0
    ka = 0
    for qblk in range(8):
        Q0 = qblk * 512
        nkt = 4 * qblk + 4
        o = ob[qblk % 2]
        for m in range(2):
            for kt in range(nkt):
                ps = pss[ks % 3]
                ks += 1
                p.I("pe", "matmul", r=[kb[m], qb[m]], w=[ps], out=ps[:], lhsT=kb[m][:, kt * 128:(kt + 1) * 128],
                    rhs=qb[m][:, Q0:Q0 + 512], start=True, stop=True)
                dpt = pts.reg(kt)
                p.I("act", "activation", r=[ps], w=[dpt], out=pts[:, kt, :], in_=ps[:], func=AF.Exp, scale=0.125)
                if kt >= 4 * qblk:
                    p.I("pool", "tensor_tensor", r=[dpt, mk_], w=[dpt], out=pts[:, kt, :], in0=pts[:, kt, :],
                        in1=mk_[:, kt - 4 * qblk, :], op=ALU.mult)
            for j in range(4):
                pa = psa[ka % 3]
                ka += 1
                for kt in range(nkt):
                    p.I("pe", "matmul", r=[pts.reg(kt), vb], w=[pa], out=pa[:, 0:129], lhsT=pts[:, kt, j * 128:(j + 1) * 128],
                        rhs=vb[:, kt, :], start=(kt == 0), stop=(kt == nkt - 1))
                r = sm[ka % 3]
                p.I("dve", "reciprocal", r=[pa], w=[r], out=r[:, 0:1], in_=pa[:, 128:129])
                if m == 0:
                    p.I("dve", "tensor_scalar", r=[pa, r], w=[o], out=o[:, j, :], in0=pa[:, 0:128], scalar1=r[:, 0:1],
                        scalar2=None, op0=ALU.mult)
                else:
                    p.I("dve", "tensor_tensor", r=[r, nlam], w=[r], out=r[:, 1:2], in0=r[:, 0:1], in1=nlam[:], op=ALU.mult)
                    p.I("dve", "scalar_tensor_tensor", r=[pa, r, o], w=[o], out=o[:, j, :], in0=pa[:, 0:128],
                        scalar=r[:, 1:2], in1=o[:, j, :], op0=ALU.mult, op1=ALU.add)
                    p.I("act", "activation", r=[o], w=[junk, r], out=junk[:], in_=o[:, j, :], func=AF.Square,
                        accum_out=r[:, 2:3])
                    p.I("act", "activation", r=[r, epsb], w=[r], out=r[:, 3:4], in_=r[:, 2:3], func=AF.Sqrt, scale=1.0 / 128,
                        bias=epsb[:, 0:1])
                    p.I("dve", "reciprocal", r=[r], w=[r], out=r[:, 3:4], in_=r[:, 3:4])
                    p.I("dve", "scalar_tensor_tensor", r=[o, r, sgt], w=[o], out=o[:, j, :], in0=o[:, j, :],
                        scalar=r[:, 3:4], in1=sgt[:], op0=ALU.mult, op1=ALU.mult)
        p.dma(ya[Q0:Q0 + 512, :].rearrange("(j p) e -> p j e", p=128), o[:], r=[o])
    p.finish()
    return p


def mask_const():
    m = np.zeros((4, 128, 512), np.float32)
    pp = np.arange(128)[:, None] // 64
    ff = np.arange(512)[None, :] // 64
    for j in range(4):
        m[j] = ((2 * j + pp) <= ff)
    return m.astype(BF)


S = 4096
NCH = 64
EM05 = math.exp(-0.5)
GN_EPS = 64e-5


def build_B(stop=99):
    p = Prog()
    rkvT = p.dram("rkvT", [3, 128, S], F32, "ExternalInput")
    lowT = p.dram("lowT", [256, S], F32, "ExternalInput")
    chp = p.dram("chp", [128, 16], F32, "ExternalInput")
    wup = p.dram("wup", [128, 128], F32, "ExternalInput")
    gup = p.dram("gup", [128, 128], F32, "ExternalInput")
    cst = p.dram("cst", [128, 1152], F32, "ExternalInput")
    ybT = p.dram("ybT", [128, S], F32, "ExternalOutput")

    ch = p.sb("ch", [128, 16], F32)
    cs = p.sb("cs", [128, 1152], F32)
    wupf = p.sb("wupf", [128, 128], F32)
    gupf = p.sb("gupf", [128, 128], F32)
    wupb = p.sb("wupb", [128, 128], BF16)
    gupb = p.sb("gupb", [128, 128], BF16)
    bones = p.sb("bones", [128, 128], BF16)
    p.dma(ch[:], chp, w=[ch])
    p.dma(cs[:], cst, w=[cs])
    p.dma(wupf[:], wup, w=[wupf])
    p.dma(gupf[:], gup, w=[gupf])
    p.I("dve", "tensor_copy", r=[wupf], w=[wupb], out=wupb[:], in_=wupf[:])
    p.I("dve", "tensor_copy", r=[gupf], w=[gupb], out=gupb[:], in_=gupf[:])
    p.I("dve", "tensor_copy", r=[cs], w=[bones], out=bones[:], in_=cs[:, 0:128])
    ident = cs[:, 128:256]
    rmask = cs[:, 256:768]
    epsb = p.sb("epsb", [128, 2], F32)
    p.I("dve", "memset", w=[epsb], ap=epsb[:, 0:1], constant=GN_EPS)
    p.I("dve", "memset", w=[epsb], ap=epsb[:, 1:2], constant=0.0)

    ARd = p.nc.dram_tensor("ARd", [128, NCH * 128], BF16, kind="Internal").ap()
    BKd = p.nc.dram_tensor("BKd", [128, NCH * 128], BF16, kind="Internal").ap()
    PCd = p.nc.dram_tensor("PCd", [128, NCH], F32, kind="Internal").ap()
    yd2 = p.nc.dram_tensor("yd2", [128, S], F32, kind="Internal").ap()
    dARd, dBKd, dPCd, dyd2 = Dep(), Dep(), Dep(), Dep()
    ARs = [p.sb("ARs%d" % i, [128, 8, 2, 64], BF16) for i in range(2)]
    BKs = [p.sb("BKs%d" % i, [128, 8, 2, 64], BF16) for i in range(2)]
    Bh = p.sb("Bh", [64, NCH, 2, 64], BF16)
    Kh = p.sb("Kh", [64, NCH, 2, 64], BF16)
    Vh = p.sb("Vh", [64, NCH, 2, 64], BF16)
    PC = p.sb("PC", [128, NCH], F32)
    bonus = p.sb("bonus", [128, S], BF16)
    gT = p.sb("gT", [128, S], BF16)

    NT = 12
    tf = [p.sb("tf%d" % i, [128, 512], F32) for i in range(NT)]
    tb = [p.sb("tb%d" % i, [128, 512], BF16) for i in range(4)]
    xin = [p.sb("xin%d" % i, [128, 513], F32) for i in range(5)]
    ps = [p.ps("ps%d" % i, [128, 512], F32) for i in range(8)]

    MU_R, MU_K, MU_V, MU_WA, MU_G, W0, A0, KKG, KAG, RK, GNG, GNB = range(12)

    def col(i):
        return ch[:, i:i + 1]

    for blk in range(8):
        t0 = blk * 512
        srcs = [rkvT[0], rkvT[1], rkvT[2], lowT[0:128], lowT[128:256]]
        for i in range(5):
            if blk == 0:
                p.I("pool", "memset", w=[xin[i]], ap=xin[i][:, 0:1], constant=0.0)
                p.dma(xin[i][:, 1:513], srcs[i][:, 0:512], w=[xin[i]], q="sp")
            else:
                p.dma(xin[i][:], srcs[i][:, t0 - 1:t0 + 512], w=[xin[i]], q="sp")
        for i in range(5):
            d = tf[5]
            p.I("dve", "tensor_tensor", r=[xin[i]], w=[d], out=d[:], in0=xin[i][:, 0:512], in1=xin[i][:, 1:513], op=ALU.subtract)
            p.I("dve", "scalar_tensor_tensor", r=[d, ch, xin[i]], w=[tf[i]], out=tf[i][:], in0=d[:], scalar=col(MU_R + i),
                in1=xin[i][:, 1:513], op0=ALU.mult, op1=ALU.add)
        r_, k_, v_, wa_, gd_ = tf[0], tf[1], tf[2], tf[3], tf[4]
        p.I("act", "activation", r=[wa_], w=[tb[0]], out=tb[0][0:64, :], in_=wa_[0:64, :], func=AF.Tanh)
        p.I("act", "activation", r=[wa_], w=[tb[0]], out=tb[0][64:128, :], in_=wa_[64:128, :], func=AF.Copy)
        p.I("act", "activation", r=[gd_], w=[tb[1]], out=tb[1][:], in_=gd_[:], func=AF.Sigmoid)
        p.I("pe", "matmul", r=[wupb, tb[0]], w=[ps[0]], out=ps[0][:], lhsT=wupb[0:64, :], rhs=tb[0][0:64, :], start=True, stop=True)
        p.I("pe", "matmul", r=[wupb, tb[0]], w=[ps[1]], out=ps[1][:], lhsT=wupb[64:128, :], rhs=tb[0][64:128, :], start=True, stop=True)
        p.I("pe", "matmul", r=[gupb, tb[1]], w=[ps[2]], out=ps[2][:], lhsT=gupb[:], rhs=tb[1][:], start=True, stop=True)
        dl, a_ = tf[5], tf[6]
        p.I("act", "activation", r=[ps[0], ch], w=[dl], out=dl[:], in_=ps[0][:], func=AF.Sigmoid, bias=col(W0))
        p.I("dve", "tensor_scalar", r=[dl], w=[dl], out=dl[:], in0=dl[:], scalar1=-EM05, scalar2=None, op0=ALU.mult)
        p.I("act", "activation", r=[ps[1], ch], w=[a_], out=a_[:], in_=ps[1][:], func=AF.Sigmoid, bias=col(A0))
        p.I("act", "activation", r=[ps[2]], w=[gT], out=gT[:, t0:t0 + 512], in_=ps[2][:], func=AF.Copy)
        kk, kap = tf[7], tf[8]
        p.I("dve", "tensor_scalar", r=[k_, ch], w=[kk], out=kk[:], in0=k_[:], scalar1=col(KKG), scalar2=None, op0=ALU.mult)
        p.I("pool", "tensor_tensor", r=[kk], w=[tb[2]], out=tb[2][:], in0=kk[:], in1=kk[:], op=ALU.mult)
        p.I("pe", "matmul", r=[bones, tb[2]], w=[ps[3]], out=ps[3][:], lhsT=bones[:], rhs=tb[2][:], start=True, stop=True)
        rn = tf[9]
        p.I("act", "activation", r=[ps[3], epsb], w=[rn], out=rn[:], in_=ps[3][:], func=AF.Sqrt, bias=epsb[:, 1:2])
        p.I("dve", "tensor_scalar", r=[rn], w=[rn], out=rn[:], in0=rn[:], scalar1=1e-12, scalar2=None, op0=ALU.max)
        p.I("dve", "reciprocal", r=[rn], w=[rn], out=rn[:], in_=rn[:])
        p.I("dve", "tensor_tensor", r=[kk, rn], w=[kap], out=kap[:], in0=kk[:], in1=rn[:], op=ALU.mult)
        km = tf[7]
        p.I("dve", "tensor_scalar", r=[a_, ch], w=[tf[9]], out=tf[9][:], in0=a_[:], scalar1=-1.0, scalar2=col(KAG), op0=ALU.add, op1=ALU.mult)
        p.I("dve", "scalar_tensor_tensor", r=[tf[9], k_], w=[km], out=km[:], in0=tf[9][:], scalar=1.0, in1=k_[:], op0=ALU.add, op1=ALU.mult)
        p.I("dve", "scalar_tensor_tensor", r=[r_, ch, km], w=[tb[3]], out=tb[3][:], in0=r_[:], scalar=col(RK), in1=km[:], op0=ALU.mult, op1=ALU.mult)
        p.I("pe", "matmul", r=[bones, tb[3]], w=[ps[4]], out=ps[4][:], lhsT=bones[:], rhs=tb[3][:], start=True, stop=True)
        p.I("dve", "tensor_tensor", r=[ps[4], v_], w=[bonus], out=bonus[:, t0:t0 + 512], in0=ps[4][:], in1=v_[:], op=ALU.mult)
        L = tf[9]
        p.I("dve", "tensor_tensor_scan", r=[cs, dl], w=[L], out=L[:], data0=rmask, data1=dl[:], initial=0.0, op0=ALU.mult, op1=ALU.add)
        P_, Pp, Pi, E_ = tf[10], tf[11], tf[1], tf[4]
        p.I("act", "activation", r=[L], w=[P_], out=P_[:], in_=L[:], func=AF.Exp)
        p.I("dve", "tensor_tensor", r=[L, dl], w=[Pp], out=Pp[:], in0=L[:], in1=dl[:], op=ALU.subtract)
        p.I("act", "activation", r=[Pp], w=[Pp], out=Pp[:], in_=Pp[:], func=AF.Exp)
        p.I("act", "activation", r=[L], w=[Pi], out=Pi[:], in_=L[:], func=AF.Exp, scale=-1.0)
        Lv = L[:].rearrange("p (c t) -> p c t", t=64)
        p.I("dve", "tensor_tensor", r=[L], w=[E_], out=E_[:].rearrange("p (c t) -> p c t", t=64), in0=Lv,
            in1=Lv[:, :, 63:64].to_broadcast([128, 8, 64]), op=ALU.subtract)
        p.I("act", "activation", r=[E_], w=[E_], out=E_[:], in_=E_[:], func=AF.Exp, scale=-1.0)
        p.I("pool", "tensor_copy", r=[P_], w=[PC], out=PC[:, blk * 8:(blk + 1) * 8],
            in_=P_[:].rearrange("p (c t) -> p c t", t=64)[:, :, 63])
        csl = slice(0, 8)
        AR, BK = ARs[blk % 2], BKs[blk % 2]
        v3 = lambda t: t[:].rearrange("p (c t) -> p c t", t=64)
        p.I("dve", "scalar_tensor_tensor", r=[kap, Pp], w=[AR], out=AR[:, csl, 0, :], in0=v3(kap), scalar=-1.0, in1=v3(Pp), op0=ALU.mult, op1=ALU.mult)
        p.I("pool", "tensor_tensor", r=[r_, P_], w=[AR], out=AR[:, csl, 1, :], in0=v3(r_), in1=v3(P_), op=ALU.mult)
        ka = tf[5]
        p.I("dve", "tensor_tensor", r=[kap, a_], w=[ka], out=ka[:], in0=kap[:], in1=a_[:], op=ALU.mult)
        p.I("dve", "tensor_tensor", r=[ka, Pi], w=[BK], out=BK[:, csl, 0, :], in0=v3(ka), in1=v3(Pi), op=ALU.mult)
        p.I("pool", "tensor_tensor", r=[km, Pi], w=[BK], out=BK[:, csl, 1, :], in0=v3(km), in1=v3(Pi), op=ALU.mult)
        p.dma(ARd[:, blk * 1024:(blk + 1) * 1024], AR[:].rearrange("p c a k -> p (c a k)"), r=[AR], w=[dARd])
        p.dma(BKd[:, blk * 1024:(blk + 1) * 1024], BK[:].rearrange("p c a k -> p (c a k)"), r=[BK], w=[dBKd], q="sp")
        Bf, Kf = tf[6], tf[8]
        p.I("dve", "tensor_tensor", r=[ka, E_], w=[Bf], out=Bf[:], in0=ka[:], in1=E_[:], op=ALU.mult)
        p.I("pool", "tensor_tensor", r=[km, E_], w=[Kf], out=Kf[:], in0=km[:], in1=E_[:], op=ALU.mult)
        for (src, dst, pi) in ((Bf, Bh, 5), (Kf, Kh, 6), (v_, Vh, 7)):
            for half in range(2):
                pt = ps[pi] if half == 0 else ps[(pi + 3) % 8 if pi != 7 else 0]
                for c4 in range(4):
                    c = half * 4 + c4
                    p.I("pe", "transpose", r=[src, cs], w=[pt], out=pt[0:64, c4 * 128:(c4 + 1) * 128], in_=src[:, c * 64:(c + 1) * 64], identity=ident)
                p.I("act", "activation", r=[pt], w=[dst], out=dst[:, blk * 8 + half * 4:blk * 8 + half * 4 + 4, :, :],
                    in_=pt[0:64, :].rearrange("p (c h k) -> p c h k", c=4, h=2), func=AF.Copy)

    p.dma(PCd, PC[:], r=[PC], w=[dPCd])
    if stop == 1:
        p.finish()
        return p
    m5 = cs[0:64, 768:1088]
    eye = cs[0:64, 1088:1152]
    mstrict = cs[0:64, 768:832]
    mlower = cs[0:64, 1024:1088]
    m4 = cs[0:64, 768:1024]
    ARx = p.sb("ARx", [64, NCH, 2, 64], BF16)
    BKx = p.sb("BKx", [64, NCH, 2, 64], BF16)
    PCx = p.sb("PCx", [64, NCH], F32)
    TT = p.sb("TT", [64, NCH, 64], BF16)
    yTh = p.sb("yTh", [64, S], F32)
    LM = [[p.sb("LM%d_%d" % (s_, i), [64, 2, 64], F32) for i in range(2)] for s_ in range(8)]
    XX = [[p.sb("XX%d_%d" % (s_, i), [64, 64], F32) for i in range(2)] for s_ in range(8)]
    S32 = p.sb("S32", [64, 64], F32)
    Sb = p.sb("Sb", [64, 64], BF16)
    Am = [p.sb("Am%d" % i, [64, 4, 64], BF16) for i in range(2)]
    Zb = [p.sb("Zb%d" % i, [64, 64], BF16) for i in range(2)]
    Ub = [p.sb("Ub%d" % i, [64, 64], BF16) for i in range(2)]
    for hd in range(2):
        hs = slice(hd * 64, (hd + 1) * 64)
        p.dma(ARx[:].rearrange("p n a k -> p (n a k)"), ARd[hs, :], r=[dARd], w=[ARx])
        p.dma(BKx[:].rearrange("p n a k -> p (n a k)"), BKd[hs, :], r=[dBKd], w=[BKx], q="sp")
        p.dma(PCx[:], PCd[hs, :], r=[dPCd], w=[PCx])
        for n in range(NCH):
            s_ = n % 4
            pa, pb = ps[2 * s_], ps[2 * s_ + 1]
            lm0, x0 = LM[s_][0], XX[s_][0]
            p.I("pe", "matmul", r=[BKx, ARx], w=[pa], out=pa[0:64, 64:128], lhsT=BKx[:, n, 0, :], rhs=ARx[:, n, 0, :], start=True, stop=True)
            p.I("pe", "matmul", r=[BKx, ARx], w=[pa], out=pa[0:64, 0:64], lhsT=ARx[:, n, 0, :], rhs=BKx[:, n, 0, :], start=True, stop=True)
            p.I("dve", "tensor_tensor", r=[pa, cs], w=[lm0], out=lm0[:, 0, :], in0=pa[0:64, 0:64], in1=mlower, op=ALU.mult)
            p.I("dve", "tensor_tensor", r=[pa, cs], w=[lm0], out=lm0[:, 1, :], in0=pa[0:64, 64:128], in1=mstrict, op=ALU.mult)
            p.I("dve", "tensor_tensor", r=[lm0, cs], w=[x0], out=x0[:], in0=lm0[:, 1, :], in1=eye, op=ALU.add)
            cur = 0
            for j in range(1, 6):
                lmp, lmn = LM[s_][cur], LM[s_][1 - cur]
                xp, xn = XX[s_][cur], XX[s_][1 - cur]
                p.I("pe", "matmul", r=[lmp], w=[pa], out=pa[0:64, 0:64], lhsT=lmp[:, 1, :], rhs=lmp[:, 0, :], start=True, stop=True)
                if j < 5:
                    p.I("pe", "matmul", r=[lmp], w=[pa], out=pa[0:64, 64:128], lhsT=lmp[:, 0, :], rhs=lmp[:, 1, :], start=True, stop=True)
                p.I("act", "activation", r=[pa], w=[lmn], out=lmn[:].rearrange("p two k -> p (two k)"), in_=pa[0:64, 0:128], func=AF.Copy)
                p.I("pe", "matmul", r=[lmn, xp], w=[pb], out=pb[0:64, 0:64], lhsT=lmn[:, 0, :], rhs=xp[:], start=True, stop=True)
                p.I("dve", "tensor_tensor", r=[pb, xp], w=[xn], out=xn[:], in0=pb[0:64, 0:64], in1=xp[:], op=ALU.add)
                cur = 1 - cur
            p.I("pool", "tensor_copy", r=[XX[s_][cur]], w=[TT.reg(n)], out=TT[:, n, :], in_=XX[s_][cur][:])
        if stop == 2 + 2 * hd:
            p.finish()
            return p
        p.I("dve", "memset", w=[S32], ap=S32[:], constant=0.0)
        p.I("pool", "memset", w=[Sb], ap=Sb[:], constant=0.0)
        def gmat(n):
            am = Am[n % 2]
            pg = ps[5 * (n % 2)]
            p.I("pe", "matmul", r=[BKx, ARx], w=[pg], out=pg[0:64, 0:128], lhsT=BKx[:, n, 0, :], rhs=ARx[:, n, :, :], start=True, stop=True)
            p.I("pe", "matmul", r=[BKx, ARx], w=[pg], out=pg[0:64, 128:256], lhsT=BKx[:, n, 1, :], rhs=ARx[:, n, :, :], start=True, stop=True)
            p.I("dve", "tensor_tensor", r=[pg, cs], w=[am], out=am[:].rearrange("p q t -> p (q t)"), in0=pg[0:64, 0:256], in1=m4, op=ALU.mult)

        gmat(0)
        for n in range(NCH):
            am, zb, ub = Am[n % 2], Zb[n % 2], Ub[n % 2]
            o4 = 5 * (n % 2)
            pz, pu, py, pS = ps[o4 + 1], ps[o4 + 2], ps[3], ps[4]
            p.I("pe", "matmul", r=[am, Vh], w=[pz], out=pz[0:64, 0:64], lhsT=am[:, 2, :], rhs=Vh[:, n, hd, :], start=True, stop=False)
            p.I("pe", "matmul", r=[ARx, Sb], w=[pz], out=pz[0:64, 0:64], lhsT=ARx[:, n, 0, :], rhs=Sb[:], start=False, stop=True)
            p.I("act", "activation", r=[pz], w=[zb], out=zb[:], in_=pz[0:64, 0:64], func=AF.Copy)
            if n + 1 < NCH:
                gmat(n + 1)
            p.I("pe", "matmul", r=[TT.reg(n), zb], w=[pu], out=pu[0:64, 0:64], lhsT=TT[:, n, :], rhs=zb[:], start=True, stop=True)
            p.I("act", "activation", r=[pu], w=[ub], out=ub[:], in_=pu[0:64, 0:64], func=AF.Copy)
            p.I("pe", "matmul", r=[Bh, ub], w=[pS], out=pS[0:64, 64:128], lhsT=Bh[:, n, hd, :], rhs=ub[:], start=True, stop=False)
            p.I("pe", "matmul", r=[Kh, Vh], w=[pS], out=pS[0:64, 64:128], lhsT=Kh[:, n, hd, :], rhs=Vh[:, n, hd, :], start=False, stop=True)
            p.I("pe", "matmul", r=[Sb, ARx], w=[py], out=py[0:64, 0:64], lhsT=Sb[:], rhs=ARx[:, n, 1, :], start=True, stop=False)
            p.I("pe", "matmul", r=[ub, am], w=[py], out=py[0:64, 0:64], lhsT=ub[:], rhs=am[:, 1, :], start=False, stop=False)
            p.I("pe", "matmul", r=[Vh, am], w=[py], out=py[0:64, 0:64], lhsT=Vh[:, n, hd, :], rhs=am[:, 3, :], start=False, stop=True)
            p.I("dve", "scalar_tensor_tensor", r=[S32, PCx, pS], w=[Sb], out=Sb[:], in0=S32[:], scalar=PCx[:, n:n + 1], in1=pS[0:64, 64:128],
                op0=ALU.mult, op1=ALU.add)
            p.I("dve", "scalar_tensor_tensor", r=[S32, PCx, pS], w=[S32], out=S32[:], in0=S32[:], scalar=PCx[:, n:n + 1], in1=pS[0:64, 64:128],
                op0=ALU.mult, op1=ALU.add)
            p.I("act", "activation", r=[py], w=[yTh], out=yTh[:, n * 64:(n + 1) * 64], in_=py[0:64, 0:64], func=AF.Copy)
        p.dma(yd2[hs, :], yTh[:], r=[yTh], w=[dyd2])
        if stop == 3 + 2 * hd:
            p.finish()
            return p

    bavg = p.sb("bavg", [128, 128], F32)
    p.I("dve", "tensor_scalar", r=[cs], w=[bavg], out=bavg[:], in0=cs[:, 0:128], scalar1=1.0 / 64, scalar2=None, op0=ALU.mult)
    for blk in range(8):
        sl = slice(blk * 512, (blk + 1) * 512)
        pm, pv = ps[(2 * blk) % 8], ps[(2 * blk + 1) % 8]
        yc, y2, o, yl = tf[0], tf[1], tf[2], tf[3 + blk % 2]
        p.dma(yl[:], yd2[:, sl], r=[dyd2], w=[yl])
        p.I("pool", "tensor_copy", r=[yl], w=[tb[0]], out=tb[0][:], in_=yl[:])
        p.I("pe", "matmul", r=[bones, tb[0]], w=[pm], out=pm[:], lhsT=bones[:], rhs=tb[0][:], start=True, stop=True)
        p.I("dve", "scalar_tensor_tensor", r=[yl, pm], w=[yc], out=yc[:], in0=pm[:], scalar=-1.0 / 64, in1=yl[:], op0=ALU.mult, op1=ALU.add)
        p.I("pool", "tensor_tensor", r=[yc], w=[tb[1]], out=tb[1][:], in0=yc[:], in1=yc[:], op=ALU.mult)
        p.I("pe", "matmul", r=[bones, tb[1]], w=[pv], out=pv[:], lhsT=bones[:], rhs=tb[1][:], start=True, stop=True)
        p.I("act", "activation", r=[pv, epsb], w=[y2], out=y2[:], in_=pv[:], func=AF.Sqrt, bias=epsb[:, 0:1], scale=1.0 / 64)
        p.I("dve", "reciprocal", r=[y2], w=[y2], out=y2[:], in_=y2[:])
        p.I("dve", "tensor_tensor", r=[yc, y2], w=[yc], out=yc[:], in0=yc[:], in1=y2[:], op=ALU.mult)
        p.I("dve", "tensor_scalar", r=[yc, ch], w=[yc], out=yc[:], in0=yc[:], scalar1=col(GNG), scalar2=col(GNB), op0=ALU.mult, op1=ALU.add)
        p.I("dve", "tensor_tensor", r=[yc, bonus], w=[yc], out=yc[:], in0=yc[:], in1=bonus[:, sl], op=ALU.add)
        p.I("dve", "tensor_tensor", r=[yc, gT], w=[o], out=o[:], in0=yc[:], in1=gT[:, sl], op=ALU.mult)
        p.dma(ybT[:, sl], o[:], r=[o])
    p.finish()
    return p


def cst_B():
    c = np.zeros((128, 1152), np.float32)
    c[0:64, 0:64] = 1.0
    c[64:128, 64:128] = 1.0
    c[:, 128:256] = np.eye(128)
    rm = np.ones(512, np.float32)
    rm[0::64] = 0.0
    c[:, 256:768] = rm[None, :]
    i = np.arange(64)[:, None]
    t = np.arange(64)[None, :]
    strict = (i < t).astype(np.float32)
    incl = (i <= t).astype(np.float32)
    lower = (t < i).astype(np.float32)
    c[0:64, 768:832] = strict
    c[0:64, 832:896] = incl
    c[0:64, 896:960] = strict
    c[0:64, 960:1024] = incl
    c[0:64, 1024:1088] = lower
    c[0:64, 1088:1152] = np.eye(64)
    return c


S = 4096
NCH = 64


def build_C(layer):
    p = Prog()
    qfT = p.dram("qfT", [2, 128, S], F32, "ExternalInput")
    ig = p.dram("ig", [2, S, 128], F32, "ExternalInput")
    lbl = p.dram("lbl", [128, 2], F32, "ExternalInput")
    ng = p.dram("ng", [64, 128], F32, "ExternalInput")
    cst = p.dram("cst", [128, 768], F32, "ExternalInput")
    yc = p.dram("yc", [S, 128], F32, "ExternalOutput")
    cs = p.sb("cs", [128, 768], F32)
    lb = p.sb("lb", [128, 4], F32)
    ngt = p.sb("ngt", [64, 128], F32)
    epsb = p.sb("epsb", [128, 1], F32)
    p.I("dve", "memset", w=[epsb], ap=epsb[:], constant=1e-6)
    p.dma(cs[:], cst, w=[cs])
    p.dma(lb[:, 0:2], lbl, w=[lb])
    p.dma(ngt[:], ng, w=[ngt])
    rmask = cs[:, 0:512]
    ident = cs[:, 512:640]
    incl = cs[0:64, 640:704]
    if layer == 0:
        p.I("dve", "memset", w=[lb], ap=lb[:, 2:3], constant=0.0)
    else:
        p.I("dve", "tensor_tensor", r=[lb], w=[lb], out=lb[:, 2:3], in0=lb[:, 1:2], in1=lb[:, 0:1], op=ALU.subtract)
        p.I("act", "activation", r=[lb], w=[lb], out=lb[:, 2:3], in_=lb[:, 2:3], func=AF.Sigmoid)
    p.I("dve", "tensor_scalar", r=[lb], w=[lb], out=lb[:, 3:4], in0=lb[:, 2:3], scalar1=-1.0, scalar2=1.0, op0=ALU.mult, op1=ALU.add)

    Qt = p.sb("Qt", [128, S], BF16)
    Kt = p.sb("Kt", [128, S], BF16)
    Qb = p.sb("Qb", [128, S], BF16)
    Kbh = p.sb("Kbh", [64, NCH, 128], BF16)
    Ih = p.sb("Ih", [64, NCH, 128], BF16)
    Sall = p.sb("Sall", [128, NCH, 128], BF16)
    dec = p.sb("dec", [128, NCH], F32)
    tf = [p.sb("tf%d" % i, [128, 512], F32) for i in range(8)]
    xin = [p.sb("xin%d" % i, [128, 512], F32) for i in range(2)]
    ist = [p.sb("ist%d" % i, [64, 8, 128], F32) for i in range(2)]
    ps = [p.ps("ps%d" % i, [128, 512], F32) for i in range(8)]
    v3 = lambda t: t[:].rearrange("p (c t) -> p c t", t=64)
    igv = ig.rearrange("w (n s) v -> w s n v", s=64)
    for blk in range(8):
        sl = slice(blk * 512, (blk + 1) * 512)
        p.dma(xin[0][:], qfT[0][:, sl], w=[xin[0]])
        p.dma(xin[1][:], qfT[1][:, sl], w=[xin[1]], q="sp")
        it = ist[blk % 2]
        p.dma(it[:], igv[0][:, blk * 8:(blk + 1) * 8, :], w=[it])
        p.I("pool", "tensor_copy", r=[it], w=[Ih], out=Ih[:, blk * 8:(blk + 1) * 8, :], in_=it[:])
        qf, fg, lf, kf, b, t1, t2, t3 = tf
        p.I("act", "activation", r=[xin[0]], w=[qf], out=qf[:], in_=xin[0][:], func=AF.Silu)
        p.I("act", "activation", r=[xin[1]], w=[fg], out=fg[:], in_=xin[1][:], func=AF.Sigmoid)
        p.I("dve", "tensor_scalar", r=[fg, lb], w=[fg], out=fg[:], in0=fg[:], scalar1=lb[:, 3:4], scalar2=lb[:, 2:3], op0=ALU.mult, op1=ALU.add)
        p.I("act", "activation", r=[fg], w=[lf], out=lf[:], in_=fg[:], func=AF.Ln)
        p.I("dve", "tensor_scalar", r=[fg], w=[kf], out=kf[:], in0=fg[:], scalar1=-1.0, scalar2=1.0, op0=ALU.mult, op1=ALU.add)
        p.I("dve", "tensor_tensor_scan", r=[cs, lf], w=[b], out=b[:], data0=rmask, data1=lf[:], initial=0.0, op0=ALU.mult, op1=ALU.add)
        bv = v3(b)
        p.I("dve", "tensor_tensor", r=[b], w=[t1], out=v3(t1), in0=bv, in1=bv[:, :, 31:32].to_broadcast([128, 8, 64]), op=ALU.subtract)
        p.I("act", "activation", r=[t1], w=[t2], out=t2[:], in_=t1[:], func=AF.Exp)
        p.I("dve", "tensor_tensor", r=[qf, t2], w=[Qt], out=Qt[:, sl], in0=qf[:], in1=t2[:], op=ALU.mult)
        p.I("act", "activation", r=[t1], w=[t2], out=t2[:], in_=t1[:], func=AF.Exp, scale=-1.0)
        p.I("dve", "tensor_tensor", r=[kf, t2], w=[Kt], out=Kt[:, sl], in0=kf[:], in1=t2[:], op=ALU.mult)
        p.I("act", "activation", r=[b], w=[t2], out=t2[:], in_=b[:], func=AF.Exp)
        p.I("pool", "tensor_tensor", r=[qf, t2], w=[Qb], out=Qb[:, sl], in0=qf[:], in1=t2[:], op=ALU.mult)
        p.I("pool", "tensor_copy", r=[t2], w=[dec], out=dec[:, blk * 8:(blk + 1) * 8], in_=v3(t2)[:, :, 63])
        p.I("dve", "tensor_tensor", r=[b], w=[t1], out=v3(t1), in0=bv, in1=bv[:, :, 63:64].to_broadcast([128, 8, 64]), op=ALU.subtract)
        p.I("act", "activation", r=[t1], w=[t3], out=t3[:], in_=t1[:], func=AF.Exp, scale=-1.0)
        p.I("dve", "tensor_tensor", r=[kf, t3], w=[t3], out=t3[:], in0=kf[:], in1=t3[:], op=ALU.mult)
        for half in range(2):
            pt = ps[half]
            for c4 in range(4):
                c = half * 4 + c4
                p.I("pe", "transpose", r=[t3, cs], w=[pt], out=pt[0:64, c4 * 128:(c4 + 1) * 128], in_=t3[:, c * 64:(c + 1) * 64], identity=ident)
            p.I("act", "activation", r=[pt], w=[Kbh], out=Kbh[:, blk * 8 + half * 4:blk * 8 + half * 4 + 4, :],
                in_=pt[0:64, :].rearrange("p (c k) -> p c k", c=4), func=AF.Copy)
    St = p.sb("St", [128, 128], F32)
    p.I("dve", "memset", w=[St], ap=St[:], constant=0.0)
    p.I("pool", "memset", w=[Sall.reg(0)], ap=Sall[:, 0, :], constant=0.0)
    for n in range(NCH - 1):
        pk = ps[2 + n % 3]
        p.I("pe", "matmul", r=[Kbh, Ih], w=[pk], out=pk[:, 0:128], lhsT=Kbh[:, n, :], rhs=Ih[:, n, :], start=True, stop=True)
        p.I("dve", "scalar_tensor_tensor", r=[St, dec, pk], w=[St], out=St[:], in0=St[:], scalar=dec[:, n:n + 1], in1=pk[:, 0:128],
            op0=ALU.mult, op1=ALU.add)
        p.I("pool", "tensor_copy", r=[St], w=[Sall.reg(n + 1)], out=Sall[:, n + 1, :], in_=St[:])
    at = [p.sb("at%d" % i, [64, 64], BF16) for i in range(2)]
    o1 = [p.sb("o1_%d" % i, [64, 128], F32) for i in range(2)]
    ot = [p.sb("ot%d" % i, [64, 8, 128], F32) for i in range(2)]
    gs = [p.sb("gs%d" % i, [64, 8, 128], F32) for i in range(2)]
    sm = [p.sb("sm%d" % i, [64, 2], F32) for i in range(2)]
    junk = p.sb("junk", [64, 128], F32)
    for n in range(NCH):
        g8 = n // 8
        if n % 8 == 0:
            gt = gs[g8 % 2]
            p.dma(gt[:], igv[1][:, n:n + 8, :], w=[gt])
            p.I("act", "activation", r=[gt], w=[gt], out=gt[:], in_=gt[:], func=AF.Silu)
            p.I("dve", "tensor_tensor", r=[gt, ngt], w=[gt], out=gt[:], in0=gt[:],
                in1=ngt[:].rearrange("p (o v) -> p o v", o=1).to_broadcast([64, 8, 128]), op=ALU.mult)
        gt = gs[g8 % 2]
        o8 = ot[g8 % 2]
        csl = slice(n * 64, (n + 1) * 64)
        pa, po = ps[5 + n % 2], ps[7 if n % 2 else 0]
        p.I("pe", "matmul", r=[Kt, Qt], w=[pa], out=pa[0:64, 0:64], lhsT=Kt[:, csl], rhs=Qt[:, csl], start=True, stop=True)
        a = at[n % 2]
        p.I("dve", "tensor_tensor", r=[pa, cs], w=[a], out=a[:], in0=pa[0:64, 0:64], in1=incl, op=ALU.mult)
        p.I("pe", "matmul", r=[a, Ih], w=[po], out=po[0:64, 0:128], lhsT=a[:], rhs=Ih[:, n, :], start=True, stop=True)
        p.I("pe", "matmul", r=[Qb, Sall.reg(n)], w=[po], out=po[0:64, 128:256], lhsT=Qb[:, csl], rhs=Sall[:, n, :], start=True, stop=True)
        oo = o1[n % 2]
        p.I("act", "activation", r=[po], w=[oo], out=oo[:], in_=po[0:64, 0:128], func=AF.Copy)
        p.I("dve", "tensor_tensor", r=[oo, po], w=[oo], out=oo[:], in0=oo[:], in1=po[0:64, 128:256], op=ALU.add)
        r = sm[n % 2]
        p.I("act", "activation", r=[oo], w=[junk, r], out=junk[:], in_=oo[:], func=AF.Square, accum_out=r[:, 0:1])
        p.I("act", "activation", r=[r, epsb], w=[r], out=r[:, 1:2], in_=r[:, 0:1], func=AF.Sqrt, scale=1.0 / 128, bias=epsb[0:64, 0:1])
        p.I("dve", "reciprocal", r=[r], w=[r], out=r[:, 1:2], in_=r[:, 1:2])
        p.I("dve", "scalar_tensor_tensor", r=[oo, r, gt], w=[o8], out=o8[:, n % 8, :], in0=oo[:], scalar=r[:, 1:2], in1=gt[:, n % 8, :],
            op0=ALU.mult, op1=ALU.mult)
        if n % 8 == 7:
            p.dma(yc.rearrange("(n s) v -> s n v", s=64)[:, n - 7:n + 1, :], o8[:], r=[o8])
    p.finish()
    return p


def cst_C():
    c = np.zeros((128, 768), np.float32)
    rm = np.ones(512, np.float32)
    rm[0::64] = 0.0
    c[:, 0:512] = rm[None, :]
    c[:, 512:640] = np.eye(128)
    i = np.arange(64)[:, None]
    t = np.arange(64)[None, :]
    c[0:64, 640:704] = (i <= t)
    return c


S = 4096
NEG = -30000.0


def build_D():
    p = Prog()
    qT = p.dram("qT", [64, 8192], F32, "ExternalInput")
    iqT = p.dram("iqT", [64, 8192], F32, "ExternalInput")
    iw = p.dram("iw", [128, 64], F32, "ExternalInput")
    kT = p.dram("kT", [64, S], F32, "ExternalInput")
    ikT = p.dram("ikT", [64, S], F32, "ExternalInput")
    v = p.dram("v", [S, 64], F32, "ExternalInput")
    vmask = p.dram("vmask", [128, 512], F32, "ExternalInput")
    id4 = p.dram("id4", [128, 512], BF16, "ExternalInput")
    yd = p.dram("yd", [1024, 512], F32, "ExternalOutput")
    qb = p.sb("qb", [64, 8192], BF16)
    iqb = p.sb("iqb", [64, 8192], BF16)
    kb = p.sb("kb", [64, S], BF16)
    ikb = p.sb("ikb", [64, S], BF16)
    vb = p.sb("vb", [128, 32, 65], BF16)
    iwt = p.sb("iwt", [128, 64], F32)
    vm = p.sb("vm", [128, 512], F32)
    i4 = p.sb("i4", [128, 512], BF16)
    st = [p.sb("st%d" % i, [128, 2048], F32) for i in range(2)]
    k = 0
    for (src, dst, n) in ((qT, qb, 8192), (iqT, iqb, 8192), (kT, kb, S), (ikT, ikb, S)):
        for c0 in range(0, n, 2048):
            s = st[k % 2]
            k += 1
            p.dma(s[0:64, :], src[:, c0:c0 + 2048], w=[s])
            p.I(("dve", "pool")[k % 2], "tensor_copy", r=[s], w=[dst], out=dst[:, c0:c0 + 2048], in_=s[0:64, :])
    s = st[k % 2]
    k += 1
    p.dma(s[:].rearrange("p (t e) -> p t e", e=64), v.rearrange("(t p) e -> p t e", p=128), w=[s])
    p.I("pool", "memset", w=[vb], ap=vb[:], constant=1.0)
    p.I("dve", "tensor_copy", r=[s], w=[vb], out=vb[:, :, 0:64], in_=s[:].rearrange("p (t e) -> p t e", e=64))
    p.dma(iwt[:], iw, w=[iwt])
    p.dma(vm[:], vmask, w=[vm])
    p.dma(i4[:], id4, w=[i4])
    p.I("dve", "tensor_scalar", r=[iwt], w=[iwt], out=iwt[:], in0=iwt[:], scalar1=(8 ** -0.5) * (64 ** -0.5), scalar2=None, op0=ALU.mult)

    sc = p.sb("sc", [128, S], F32)
    wk = p.sb("wk", [128, S], F32)
    biasb = [p.sb("bias%d" % i, [128, S], BF16) for i in range(2)]
    pts = p.sb("pts", [128, 32, 512], BF16)
    rl = [p.sb("rl%d" % i, [128, 512], F32) for i in range(2)]
    mx = p.sb("mx", [128, 8], F32)
    yo = [p.sb("yo%d" % i, [128, 8, 64], F32) for i in range(2)]
    sm = [p.sb("sm%d" % i, [128, 2], F32) for i in range(3)]
    pss = [p.ps("pss%d" % i, [128, 512], F32) for i in range(4)]
    psa = [p.ps("psa%d" % i, [128, 512], F32) for i in range(3)]
    cnt = {"ks": 0, "ka": 0, "kr": 0}

    def idx(m):
        for g in range(m + 1):
            gs = slice(g * 512, (g + 1) * 512)
            for h in range(8):
                ps = pss[cnt["ks"] % 4]
                cnt["ks"] += 1
                p.I("pe", "matmul", r=[iqb, ikb], w=[ps], out=ps[:], lhsT=iqb[:, m * 1024 + h * 128:m * 1024 + (h + 1) * 128],
                    rhs=ikb[:, gs], start=True, stop=True)
                r = rl[cnt["kr"] % 2]
                cnt["kr"] += 1
                p.I("act", "activation", r=[ps], w=[r], out=r[:], in_=ps[:], func=AF.Relu)
                ws = iwt[:, m * 8 + h:m * 8 + h + 1]
                if h == 0:
                    p.I("dve", "tensor_scalar", r=[r, iwt], w=[sc], out=sc[:, gs], in0=r[:], scalar1=ws, scalar2=None, op0=ALU.mult)
                else:
                    p.I("dve", "scalar_tensor_tensor", r=[r, iwt, sc], w=[sc], out=sc[:, gs], in0=r[:], scalar=ws, in1=sc[:, gs],
                        op0=ALU.mult, op1=ALU.add)
            if g == m:
                p.I("dve", "tensor_tensor", r=[sc, vm], w=[sc], out=sc[:, gs], in0=sc[:, gs], in1=vm[:], op=ALU.add)

    def topk_bias(m):
        N = (m + 1) * 512
        bias = biasb[m % 2]
        p.I("pool", "tensor_copy", r=[sc], w=[wk], out=wk[:, 0:N], in_=sc[:, 0:N])
        for rnd in range(32):
            p.I("dve", "max", r=[wk], w=[mx], out=mx[:], in_=wk[:, 0:N])
            if rnd < 31:
                p.I("dve", "match_replace", r=[wk, mx], w=[wk], out=wk[:, 0:N], in_to_replace=mx[:], in_values=wk[:, 0:N], imm_value=-1e30)
        p.I("dve", "tensor_scalar", r=[sc, mx], w=[bias], out=bias[:, 0:N], in0=sc[:, 0:N], scalar1=mx[:, 7:8], scalar2=NEG,
            op0=ALU.is_lt, op1=ALU.mult)
        p.I("dve", "tensor_tensor", r=[bias, vm], w=[bias], out=bias[:, m * 512:N], in0=bias[:, m * 512:N], in1=vm[:], op=ALU.add)

    def attn(m):
        nkt = 4 * m + 4
        bias = biasb[m % 2]
        o = yo[m % 2]
        for hg in range(2):
            for kt in range(nkt):
                ps = pss[cnt["ks"] % 4]
                cnt["ks"] += 1
                p.I("pe", "matmul", r=[kb, qb], w=[ps], out=ps[:], lhsT=kb[:, kt * 128:(kt + 1) * 128],
                    rhs=qb[:, m * 1024 + hg * 512:m * 1024 + (hg + 1) * 512], start=True, stop=False)
                p.I("pe", "matmul", r=[bias, i4], w=[ps], out=ps[:], lhsT=bias[:, kt * 128:(kt + 1) * 128], rhs=i4[:],
                    start=False, stop=True)
                p.I("act", "activation", r=[ps], w=[pts.reg(kt)], out=pts[:, kt, :], in_=ps[:], func=AF.Exp, scale=0.125)
            for h4 in range(4):
                pa = psa[cnt["ka"] % 3]
                r = sm[cnt["ka"] % 3]
                cnt["ka"] += 1
                for kt in range(nkt):
                    p.I("pe", "matmul", r=[pts.reg(kt), vb], w=[pa], out=pa[:, 0:65], lhsT=pts[:, kt, h4 * 128:(h4 + 1) * 128],
                        rhs=vb[:, kt, :], start=(kt == 0), stop=(kt == nkt - 1))
                p.I("act", "activation", r=[pa], w=[r], out=r[:, 0:1], in_=pa[:, 64:65], func=AF.Ln)
                p.I("act", "activation", r=[r], w=[r], out=r[:, 1:2], in_=r[:, 0:1], func=AF.Exp, scale=-1.0)
                p.I("act", "activation", r=[pa, r], w=[o], out=o[:, hg * 4 + h4, :], in_=pa[:, 0:64], func=AF.Copy, scale=r[:, 1:2])
        p.dma(yd[m * 128:(m + 1) * 128, :], o[:].rearrange("p h d -> p (h d)"), r=[o])

    idx(7)
    topk_bias(7)
    for m in range(7, -1, -1):
        if m - 1 >= 0:
            idx(m - 1)
            topk_bias(m - 1)
        attn(m)
    p.finish()
    return p


def vmask_const(j):
    pp = 2 * j + np.arange(128)[:, None] // 64
    ff = np.arange(512)[None, :] // 64
    return np.where(ff <= pp, 0.0, NEG).astype(np.float32)


def id4_const():
    return np.ascontiguousarray(np.tile(np.eye(128, dtype=np.float32), (1, 4))).astype(BF)


_PROGS = {}


def _prog(key, fn):
    if key not in _PROGS:
        _PROGS[key] = fn()
    return _PROGS[key]


def _run(p, ims):
    n = len(ims)
    return run_bass_kernel_spmd(p.nc, ims, core_ids=list(range(n))).results


def _cast_weights(arrs):
    sizes = [a.size for a in arrs]
    tot = sum(sizes)
    per = -(-tot // (8 * 128 * 4096)) * 4096
    flat = np.zeros(8 * 128 * per, np.float32)
    o = 0
    for a in arrs:
        flat[o:o + a.size] = a.reshape(-1)
        o += a.size
    flat = flat.reshape(8, 128, per)
    p = _prog(("L0", per), lambda: build_L0(per))
    res = _run(p, [{"src": flat[c]} for c in range(8)])
    out = np.concatenate([r["dst"].reshape(-1) for r in res])
    outs = []
    o = 0
    for a in arrs:
        outs.append(out[o:o + a.size].reshape(a.shape))
        o += a.size
    return outs


def _layer(l, x, positions, inp, W):
    f32 = np.float32
    g0 = np.ascontiguousarray(inp["norm_g"][l, 0].reshape(16, 128).T)
    cf = cf_const()
    ims = []
    for c in range(8):
        sl = slice(c * 1024, (c + 1) * 1024)
        ims.append({"xT": np.ascontiguousarray(x[sl].T), "g": g0, "wb": W["wb"],
                    "pos": np.ascontiguousarray(positions[sl].reshape(8, 128).T.astype(np.int32)), "cf": cf})
    res = _run(_prog("L1", build_L1), ims)
    proj = np.concatenate([r["proj"] for r in res], 0)
    y = np.zeros((4, 8192, 512), f32)
    lam_v = inp["diff_lambda"][l]
    sgv = inp["diff_subln_g"][l]
    ims = []
    for c in range(8):
        b, h = c // 4, c % 4
        pb = proj[b * 4096:(b + 1) * 4096]
        q = pb[:, h * 128:(h + 1) * 128].reshape(4096, 2, 64)
        k = pb[:, 512 + h * 128:512 + (h + 1) * 128].reshape(4096, 2, 64)
        ims.append({"qT": np.ascontiguousarray(q.transpose(1, 2, 0)), "kT": np.ascontiguousarray(k.transpose(1, 2, 0)),
                    "v": np.ascontiguousarray(pb[:, 1024 + h * 128:1024 + (h + 1) * 128]),
                    "lam4": np.ascontiguousarray(np.broadcast_to(lam_v.reshape(1, 256), (128, 256))),
                    "sg": np.ascontiguousarray(np.broadcast_to(sgv[None, :], (128, 128))), "masks": mask_const()})
    res = _run(_prog(("A", l), lambda: build_A(l)), ims)
    for c in range(8):
        b, h = c // 4, c % 4
        y[0, b * 4096:(b + 1) * 4096, h * 128:(h + 1) * 128] = res[c]["ya"]
    mu = inp["rwkv_mu"][l]
    ims = []
    cstb = cst_B()
    for c in range(8):
        b, hp = c // 4, c % 4
        pb = proj[b * 4096:(b + 1) * 4096, 1536:3328]
        cols = slice(hp * 128, (hp + 1) * 128)
        rkv = np.stack([pb[:, i * 512 + hp * 128:i * 512 + (hp + 1) * 128].T for i in range(3)])
        chp = np.zeros((128, 16), f32)
        for i in range(3):
            chp[:, i] = mu[i * 512 + hp * 128:i * 512 + (hp + 1) * 128]
        chp[:, 3] = mu[1536:1664]
        chp[:, 4] = mu[1664:1792]
        chp[:, 5] = inp["rwkv_w0"][l][cols]
        chp[:, 6] = inp["rwkv_a0"][l][cols]
        chp[:, 7] = inp["rwkv_k_k"][l][cols]
        chp[:, 8] = inp["rwkv_k_a"][l][cols]
        chp[:, 9] = inp["rwkv_r_k"][l].reshape(512)[cols]
        chp[:, 10] = inp["rwkv_gn_g"][l][cols]
        chp[:, 11] = inp["rwkv_gn_b"][l][cols]
        wup = np.concatenate([inp["rwkv_w_up"][l][:, cols], inp["rwkv_a_up"][l][:, cols]], 0)
        ims.append({"rkvT": np.ascontiguousarray(rkv), "lowT": np.ascontiguousarray(pb[:, 1536:1792].T), "chp": chp,
                    "wup": np.ascontiguousarray(wup), "gup": np.ascontiguousarray(inp["rwkv_g_up"][l][:, cols]), "cst": cstb})
    res = _run(_prog("B", build_B), ims)
    for c in range(8):
        b, hp = c // 4, c % 4
        y[1, b * 4096:(b + 1) * 4096, hp * 128:(hp + 1) * 128] = res[c]["ybT"].T
    ims = []
    cstc = cst_C()
    for c in range(8):
        b, h = c // 4, c % 4
        pc = proj[b * 4096:(b + 1) * 4096, 3328:5376]
        hc = slice(h * 128, (h + 1) * 128)
        qf = np.stack([pc[:, 0:512][:, hc].T, pc[:, 512:1024][:, hc].T])
        ig = np.stack([pc[:, 1024:1536][:, hc], pc[:, 1536:2048][:, hc]])
        ims.append({"qfT": np.ascontiguousarray(qf), "ig": np.ascontiguousarray(ig),
                    "lbl": np.ascontiguousarray(inp["hgrn_lb_logits"][:, hc].T),
                    "ng": np.ascontiguousarray(np.broadcast_to(inp["hgrn_norm_g"][l][hc][None, :], (64, 128))), "cst": cstc})
    res = _run(_prog(("C", l), lambda: build_C(l)), ims)
    for c in range(8):
        b, h = c // 4, c % 4
        y[2, b * 4096:(b + 1) * 4096, h * 128:(h + 1) * 128] = res[c]["yc"]
    O = 5376
    ims = []
    rows_all = []
    for c in range(8):
        b, j = c // 4, c % 4
        pb = proj[b * 4096:(b + 1) * 4096]
        rows = np.concatenate([np.arange(i * 128, (i + 1) * 128) for i in [4 * m + j for m in range(8)]])
        rows_all.append(rows)
        q = pb[rows, O:O + 512].reshape(8, 128, 8, 64)
        iq = pb[rows, O + 640:O + 1152].reshape(8, 128, 8, 64)
        iw = pb[rows, O + 1216:O + 1224].reshape(8, 128, 8)
        ims.append({"qT": np.ascontiguousarray(q.transpose(3, 0, 2, 1).reshape(64, 8192)),
                    "iqT": np.ascontiguousarray(iq.transpose(3, 0, 2, 1).reshape(64, 8192)),
                    "iw": np.ascontiguousarray(iw.transpose(1, 0, 2).reshape(128, 64)),
                    "kT": np.ascontiguousarray(pb[:, O + 512:O + 576].T), "ikT": np.ascontiguousarray(pb[:, O + 1152:O + 1216].T),
                    "v": np.ascontiguousarray(pb[:, O + 576:O + 640]), "vmask": vmask_const(j), "id4": id4_const()})
    res = _run(_prog("D", build_D), ims)
    for c in range(8):
        b = c // 4
        y[3, b * 4096 + rows_all[c]] = res[c]["yd"]
    del proj
    g3 = np.ascontiguousarray(np.concatenate([inp["norm_g"][l, i].reshape(16, 128).T for i in range(4)], axis=1))
    ims = []
    for c in range(8):
        sl = slice(c * 1024, (c + 1) * 1024)
        ims.append({"xT": np.ascontiguousarray(x[sl].T), "yT": np.ascontiguousarray(y[:, sl].transpose(0, 2, 1)), "wg": W["wg"], "g3": g3,
                    "wbr": W["wbr"], "wout": W["wout"], "w1": W["w1"], "w2": W["w2"]})
    res = _run(_prog("L3", lambda: build_L3(1024)), ims)
    return np.ascontiguousarray(np.concatenate([r["xo"].T for r in res], 0))


def kernel(**inputs):
    inp = {k: np.asarray(v) for k, v in inputs.items()}
    x = np.ascontiguousarray(inp["x"].reshape(8192, D)).astype(np.float32, copy=False)
    positions = inp["positions"].reshape(8192)
    tiled = []
    for l in range(2):
        w_in = inp["w_in"][l]
        tiled.append(prep_w_in(w_in))
        tiled.append(prep_wg(np.ascontiguousarray(w_in[:, NMIX:])))
        tiled.extend(prep_L3_weights(inp["w_branch"][l], inp["w_out"][l], inp["mlp_w1"][l], inp["mlp_w2"][l]))
    cast = _cast_weights(tiled)
    del tiled
    for l in range(2):
        W = dict(zip(("wb", "wg", "wbr", "wout", "w1", "w2"), cast[l * 6:(l + 1) * 6]))
        x = _layer(l, x, positions, inp, W)
    return x.reshape(2, 4096, D).astype(np.float32)
```
